# Optimizing a Trainium2 kernel written in Bass

```python
import jax, jax.numpy as jnp
from jax import lax
import numpy as np

D_MODEL = 1024
BATCH = 8
SEQ = 2048
DEPTH = 4

GDN_HEADS = 4
GDN_DK = 128
GDN_DV = 128
CONV_WIDTH = 4
GLA_HEADS = 4
GLA_DK = 128
GLA_DV = 256
GLA_GATE_RANK = 16
GLA_TAU = 16.0
CHUNK = 64
D_FF = 4 * D_MODEL
EPS = 1e-6

GDN_QK = GDN_HEADS * GDN_DK
GDN_V = GDN_HEADS * GDN_DV
GLA_QK = GLA_HEADS * GLA_DK
GLA_V = GLA_HEADS * GLA_DV
IN_SPLITS = (GDN_QK, GDN_QK, GDN_V, GDN_V, GDN_HEADS, GDN_HEADS,
             GLA_QK, GLA_QK, GLA_V, GLA_V, GLA_GATE_RANK,
             D_MODEL, D_MODEL)
IN_COLS = sum(IN_SPLITS)

kernel_name = 'hybrid_gdn_gla_sqrelu_sandwich'


def rms_norm(x, w):
    xf = x.astype(jnp.float32)
    y = xf * lax.rsqrt(jnp.mean(xf * xf, axis=-1, keepdims=True) + EPS)
    return (y * w.astype(jnp.float32)).astype(x.dtype)


def rms_norm_f32(x, w):
    return x * lax.rsqrt(jnp.mean(x * x, axis=-1, keepdims=True) + EPS) * w.astype(jnp.float32)


def l2_normalize(x):
    return x * lax.rsqrt(jnp.sum(x * x, axis=-1, keepdims=True) + EPS)


def causal_depthwise_conv(x, w):
    c = x.shape[-1]
    return lax.conv_general_dilated(
        x, w[:, None, :].astype(x.dtype), window_strides=(1,),
        padding=[(CONV_WIDTH - 1, 0)], dimension_numbers=('NWC', 'WIO', 'NWC'),
        feature_group_count=c)


def split_heads(t, nh, d):
    b, s, _ = t.shape
    return t.reshape(b, s, nh, d).transpose(0, 2, 1, 3).astype(jnp.float32)


def gated_delta_chunked(q, k, v, g, beta):
    bsz, nh, s, dk = q.shape
    dv = v.shape[-1]
    n = s // CHUNK
    q = q.reshape(bsz, nh, n, CHUNK, dk)
    k = k.reshape(bsz, nh, n, CHUNK, dk)
    v = v.reshape(bsz, nh, n, CHUNK, dv)
    beta = beta.reshape(bsz, nh, n, CHUNK)
    g = jnp.cumsum(g.reshape(bsz, nh, n, CHUNK), axis=-1)
    causal = jnp.tril(jnp.ones((CHUNK, CHUNK), dtype=bool))
    strict = jnp.tril(jnp.ones((CHUNK, CHUNK), dtype=bool), -1)
    decay = jnp.exp(jnp.where(causal, g[..., :, None] - g[..., None, :], -jnp.inf))
    k_beta = k * beta[..., None]
    a_mat = jnp.where(strict, jnp.einsum('bhnid,bhnjd->bhnij', k_beta, k) * decay, 0.0)
    t_mat = a_mat + jnp.eye(CHUNK, dtype=q.dtype)
    u = lax.linalg.triangular_solve(t_mat, v * beta[..., None], left_side=True,
                                    lower=True, unit_diagonal=True)
    w = lax.linalg.triangular_solve(t_mat, k_beta * jnp.exp(g)[..., None], left_side=True,
                                    lower=True, unit_diagonal=True)
    qk = jnp.where(causal, jnp.einsum('bhnid,bhnjd->bhnij', q, k) * decay, 0.0)
    q_dec = q * jnp.exp(g)[..., None]
    g_last = g[..., -1]
    k_dec = k * jnp.exp(g_last[..., None] - g)[..., None]
    chunk_decay = jnp.exp(g_last)

    def step(state, xs):
        u_c, w_c, qk_c, qd_c, kd_c, dec_c = xs
        v_new = u_c - jnp.einsum('bhcd,bhde->bhce', w_c, state)
        o_c = (jnp.einsum('bhcd,bhde->bhce', qd_c, state)
               + jnp.einsum('bhij,bhje->bhie', qk_c, v_new))
        state = state * dec_c[..., None, None] + jnp.einsum('bhcd,bhce->bhde', kd_c, v_new)
        return state, o_c

    xs = (jnp.moveaxis(u, 2, 0), jnp.moveaxis(w, 2, 0), jnp.moveaxis(qk, 2, 0),
          jnp.moveaxis(q_dec, 2, 0), jnp.moveaxis(k_dec, 2, 0), jnp.moveaxis(chunk_decay, 2, 0))
    state0 = jnp.zeros((bsz, nh, dk, dv), q.dtype)
    _, o = lax.scan(step, state0, xs)
    return jnp.moveaxis(o, 0, 2).reshape(bsz, nh, s, dv)


def gla_chunked(q, k, v, log_a):
    bsz, nh, s, dk = q.shape
    dv = v.shape[-1]
    n = s // CHUNK
    q = q.reshape(bsz, nh, n, CHUNK, dk)
    k = k.reshape(bsz, nh, n, CHUNK, dk)
    v = v.reshape(bsz, nh, n, CHUNK, dv)
    b = jnp.cumsum(log_a.reshape(bsz, nh, n, CHUNK, dk), axis=-2)
    b_last = b[..., -1:, :]
    b_ref = b[..., CHUNK // 2:CHUNK // 2 + 1, :]
    causal = jnp.tril(jnp.ones((CHUNK, CHUNK), dtype=bool))
    q_in = q * jnp.exp(b - b_ref)
    k_in = k * jnp.exp(b_ref - b)
    attn = jnp.where(causal, jnp.einsum('bhnid,bhnjd->bhnij', q_in, k_in), 0.0)
    o_intra = jnp.einsum('bhnij,bhnje->bhnie', attn, v)
    d_state = jnp.einsum('bhncd,bhnce->bhnde', k * jnp.exp(b_last - b), v)
    chunk_decay = jnp.exp(b_last[..., 0, :])

    def step(state, xs):
        ds_c, dec_c = xs
        return state * dec_c[..., None] + ds_c, state

    state0 = jnp.zeros((bsz, nh, dk, dv), q.dtype)
    _, s_prev = lax.scan(step, state0, (jnp.moveaxis(d_state, 2, 0), jnp.moveaxis(chunk_decay, 2, 0)))
    s_prev = jnp.moveaxis(s_prev, 0, 2)
    o_inter = jnp.einsum('bhncd,bhnde->bhnce', q * jnp.exp(b), s_prev)
    return (o_intra + o_inter).reshape(bsz, nh, s, dv)


def gdn_branch(q, k, v, z, b_logit, a_logit, conv_w, a_log, dt_bias, norm_w):
    bsz, s, _ = q.shape
    qkv = jax.nn.silu(causal_depthwise_conv(jnp.concatenate([q, k, v], axis=-1), conv_w))
    q, k, v = jnp.split(qkv, [GDN_QK, 2 * GDN_QK], axis=-1)
    q = l2_normalize(split_heads(q, GDN_HEADS, GDN_DK)) * (GDN_DK ** -0.5)
    k = l2_normalize(split_heads(k, GDN_HEADS, GDN_DK))
    v = split_heads(v, GDN_HEADS, GDN_DV)
    beta = jax.nn.sigmoid(b_logit.astype(jnp.float32)).transpose(0, 2, 1)
    g = -(jnp.exp(a_log.astype(jnp.float32))
          * jax.nn.softplus(a_logit.astype(jnp.float32) + dt_bias.astype(jnp.float32)))
    g = g.transpose(0, 2, 1)
    o = gated_delta_chunked(q, k, v, g, beta).transpose(0, 2, 1, 3)
    zg = jax.nn.silu(z.reshape(bsz, s, GDN_HEADS, GDN_DV).astype(jnp.float32))
    o = rms_norm_f32(o, norm_w) * zg
    return o.reshape(bsz, s, GDN_V).astype(q.dtype)


def gla_branch(q, k, v, r, gate_lr, gate_w2, gate_b, norm_w):
    bsz, s, _ = q.shape
    qh = split_heads(q, GLA_HEADS, GLA_DK) * (GLA_DK ** -0.5)
    kh = split_heads(k, GLA_HEADS, GLA_DK)
    vh = split_heads(v, GLA_HEADS, GLA_DV)
    gate_logit = (gate_lr @ gate_w2.astype(gate_lr.dtype)).astype(jnp.float32) + gate_b.astype(jnp.float32)
    log_a = jax.nn.log_sigmoid(gate_logit) / GLA_TAU
    log_a = split_heads(log_a, GLA_HEADS, GLA_DK)
    o = gla_chunked(qh, kh, vh, log_a).transpose(0, 2, 1, 3)
    rg = jax.nn.silu(r.reshape(bsz, s, GLA_HEADS, GLA_DV).astype(jnp.float32))
    o = rms_norm_f32(o, norm_w) * rg
    return o.reshape(bsz, s, GLA_V).astype(q.dtype)


def setup_inputs(seed: int = 0) -> dict:
    key = jax.random.key(seed)
    ks = jax.random.split(key, 20)
    f32 = jnp.float32
    L, D = DEPTH, D_MODEL

    def normal(k, shape, scale):
        return jax.random.normal(k, shape, f32) * scale

    def gain(k, shape):
        return 1.0 + 0.02 * jax.random.normal(k, shape, f32)

    x = jax.random.normal(ks[0], (BATCH, SEQ, D), f32)
    w_in = normal(ks[1], (L, D, IN_COLS), D ** -0.5)
    conv_w = normal(ks[2], (L, CONV_WIDTH, 2 * GDN_QK + GDN_V), CONV_WIDTH ** -0.5)
    a_log = jnp.log(jax.random.uniform(ks[3], (L, GDN_HEADS), f32, 1.0, 16.0))
    dt = jnp.exp(jax.random.uniform(ks[4], (L, GDN_HEADS), f32, np.log(1e-3), np.log(1e-1)))
    dt_bias = dt + jnp.log(-jnp.expm1(-dt))
    gdn_norm = gain(ks[5], (L, GDN_DV))
    gla_gate_w2 = normal(ks[6], (L, GLA_GATE_RANK, GLA_QK), GLA_GATE_RANK ** -0.5)
    gla_gate_b = normal(ks[7], (L, GLA_QK), 0.01)
    gla_norm = gain(ks[8], (L, GLA_DV))
    w_out_a = normal(ks[9], (L, GDN_V, D), GDN_V ** -0.5)
    w_out_b = normal(ks[10], (L, GLA_V, D), GLA_V ** -0.5)
    w_o = normal(ks[11], (L, D, D), D ** -0.5)
    norm_mix_pre = gain(ks[12], (L, D))
    norm_mix_post = gain(ks[13], (L, D))
    norm_mlp_pre = gain(ks[14], (L, D))
    norm_mlp_post = gain(ks[15], (L, D))
    w_mlp_up = normal(ks[16], (L, D, D_FF), D ** -0.5)
    w_mlp_down = normal(ks[17], (L, D_FF, D), D_FF ** -0.5)
    return {'x': x, 'w_in': w_in, 'conv_w': conv_w, 'a_log': a_log, 'dt_bias': dt_bias,
            'gdn_norm': gdn_norm, 'gla_gate_w2': gla_gate_w2, 'gla_gate_b': gla_gate_b,
            'gla_norm': gla_norm, 'w_out_a': w_out_a, 'w_out_b': w_out_b, 'w_o': w_o,
            'norm_mix_pre': norm_mix_pre, 'norm_mix_post': norm_mix_post,
            'norm_mlp_pre': norm_mlp_pre, 'norm_mlp_post': norm_mlp_post,
            'w_mlp_up': w_mlp_up, 'w_mlp_down': w_mlp_down}


def reference(x, w_in, conv_w, a_log, dt_bias, gdn_norm, gla_gate_w2, gla_gate_b, gla_norm,
              w_out_a, w_out_b, w_o, norm_mix_pre, norm_mix_post, norm_mlp_pre, norm_mlp_post,
              w_mlp_up, w_mlp_down):
    split_idx = list(np.cumsum(IN_SPLITS)[:-1])
    for l in range(DEPTH):
        h = rms_norm(x, norm_mix_pre[l])
        proj = h @ w_in[l]
        (a_q, a_k, a_v, a_z, a_b, a_a,
         b_q, b_k, b_v, b_r, b_glr,
         gate_a, gate_b) = jnp.split(proj, split_idx, axis=-1)
        y_a = gdn_branch(a_q, a_k, a_v, a_z, a_b, a_a, conv_w[l], a_log[l], dt_bias[l], gdn_norm[l])
        y_b = gla_branch(b_q, b_k, b_v, b_r, b_glr, gla_gate_w2[l], gla_gate_b[l], gla_norm[l])
        y_a = y_a @ w_out_a[l]
        y_b = y_b @ w_out_b[l]
        merged = jax.nn.sigmoid(gate_a) * y_a + jax.nn.sigmoid(gate_b) * y_b
        x = x + rms_norm(merged @ w_o[l], norm_mix_post[l])
        h = rms_norm(x, norm_mlp_pre[l])
        u = jnp.square(jax.nn.relu(h @ w_mlp_up[l]))
        x = x + rms_norm(u @ w_mlp_down[l], norm_mlp_post[l])
    return x
```

```python
import numpy as np
from contextlib import ExitStack
import concourse.bass as bass
import concourse.mybir as mybir
from concourse.bass_utils import run_bass_kernel_spmd

F32 = mybir.dt.float32
BF16 = mybir.dt.bfloat16
AF = mybir.ActivationFunctionType
ALU = mybir.AluOpType

D = 1024
T = 2048
L = 4
DFF = 4096
NCOL = 7192
NT = 4
TT = 512
KC = 8
EPS = 1e-6
O_AQ, O_AK, O_AV, O_AZ, O_AB, O_AA = 0, 512, 1024, 1536, 2048, 2052
O_BQ, O_BK, O_BV, O_BR, O_GLR, O_GA, O_GB = 2056, 2568, 3080, 4104, 5128, 5144, 6168
GDN_STOP = 0
GDN_REC = 0
NVEC = 87
C_ID, C_ONES, C_NEG, C_MN_INC_T, C_MN_STR_T, C_MN_STR, C_C01_T = 0, 1, 2, 3, 4, 5, 6
C_LV = 7
C_LVT = 14
NCB = 21


def make_consts():
    c = np.zeros((NCB, 128, 128), np.float32)
    i = np.arange(128)[:, None]
    j = np.arange(128)[None, :]
    c[C_ID] = (i == j)
    c[C_ONES] = 1.0
    c[C_NEG] = -1.0
    c[C_MN_INC_T] = np.where(j >= i, 0.0, -1e30)
    c[C_MN_STR_T] = np.where(j > i, 0.0, -1e30)
    c[C_MN_STR] = np.where(i > j, 0.0, -1e30)
    c[C_C01_T] = (j >= i)
    for li in range(7):
        m = 1 << li
        mm = ((i // (2 * m)) == (j // (2 * m))) & ((i % (2 * m)) >= m) & ((j % (2 * m)) < m)
        c[C_LV + li] = mm
        c[C_LVT + li] = mm.T
    return np.ascontiguousarray(c.transpose(1, 0, 2).reshape(128, NCB * 128))


class Res:
    __slots__ = ("w", "rs", "p")

    def __init__(self):
        self.w = None
        self.rs = {}
        self.p = set()


class KB:
    def __init__(self, nc, es):
        self.nc = nc
        self.es = es
        self.eng = {"pe": nc.tensor, "dve": nc.vector, "act": nc.scalar, "pool": nc.gpsimd, "sp": nc.sync}
        self.sems = {}
        self.cnt = {}
        self.seen = {e: {} for e in self.eng}
        self.pend = {e: [] for e in self.eng}
        for e in ("pe", "dve", "act", "pool"):
            self.sems[e] = es.enter_context(nc.semaphore("s_" + e))
            self.cnt[e] = 0
        self.nops = 0
        self.nwaits = 0

    def dsem(self, name):
        key = "d_" + name
        self.sems[key] = self.es.enter_context(self.nc.semaphore(key))
        self.cnt[key] = 0
        return key

    def flush(self, e):
        if not self.pend[e]:
            return
        self.cnt[e] += 1
        self.pend[e][-1][0].then_inc(self.sems[e], 1)
        ev = (e, self.cnt[e])
        for (_, r2, w2) in self.pend[e]:
            for r in list(r2) + list(w2):
                r.p.discard(e)
            self._reg(ev, r2, w2)
        self.pend[e] = []

    def barrier(self):
        for e in self.eng:
            self.flush(e)
        for e in self.eng:
            for k, v in self.cnt.items():
                if v > 0 and self.seen[e].get(k, 0) < v:
                    self.eng[e].wait_ge(self.sems[k], v)
                    self.seen[e][k] = v
                    self.nwaits += 1

    def _deps(self, e, R, W):
        for r in list(R) + list(W):
            for e2 in list(r.p):
                if e2 != e:
                    self.flush(e2)
        deps = {}

        def add(ev):
            k, v = ev
            if deps.get(k, 0) < v:
                deps[k] = v
        for r in R:
            if r.w is not None:
                add(r.w)
        for w in W:
            if w.w is not None:
                add(w.w)
            for k, v in w.rs.items():
                add((k, v))
        for k, v in deps.items():
            if k == e and e == "pe":
                continue
            if self.seen[e].get(k, 0) >= v:
                continue
            self.eng[e].wait_ge(self.sems[k], v)
            self.seen[e][k] = v
            self.nwaits += 1

    def _reg(self, ev, R, W):
        k, v = ev
        for w in W:
            w.w = ev
            w.rs = {}
        for r in R:
            if r.rs.get(k, 0) < v:
                r.rs[k] = v

    def op(self, e, fn, R=(), W=(), inc=True):
        self._deps(e, R, W)
        inst = fn()
        self.nops += 1
        if not inc:
            self.pend[e].append((inst, R, W))
            for r in list(R) + list(W):
                r.p.add(e)
            return
        self.cnt[e] += 1
        inst.then_inc(self.sems[e], 1)
        ev = (e, self.cnt[e])
        for (_, r2, w2) in self.pend[e]:
            for r in list(r2) + list(w2):
                r.p.discard(e)
            self._reg(ev, r2, w2)
        self.pend[e] = []
        self._reg(ev, R, W)

    def dma(self, e, pairs, key, R=(), W=()):
        self._deps(e, R, W)
        for (o, i) in pairs:
            inst = self.eng[e].dma_start(out=o, in_=i)
            inst.then_inc(self.sems[key], 16)
            self.cnt[key] += 16
            self.nops += 1
        ev = (key, self.cnt[key])
        self._reg(ev, R, W)


def build_program(n_layers, taps=(), phases=("gdn", "gla", "merge", "mlp")):
    nc = bass.Bass("TRN2", target_bir_lowering=False)
    NL = n_layers
    dt = lambda name, shape, kind="ExternalInput": nc.dram_tensor(name, shape, F32, kind=kind).ap()
    xT_d = dt("xT", [D, T])
    w_in_d = dt("w_in", [NL, D, NCOL])
    w_oa_d = dt("w_out_a", [NL, 512, D])
    w_ob_d = dt("w_out_b", [NL, 1024, D])
    w_o_d = dt("w_o", [NL, D, D])
    w_up_d = dt("w_mlp_up", [NL, D, DFF])
    w_dn_d = dt("w_mlp_down", [NL, DFF, D])
    w2_d = dt("gate_w2", [16, NL * 512])
    vecs_d = dt("vecs", [128, NL * NVEC])
    hv_d = dt("hv", [1, NL * 8])
    consts_d = dt("consts", [128, NCB * 128])
    outT_d = dt("outT", [D, T], kind="ExternalOutput")
    tap_out = {}

    with ExitStack() as es:
        kb = KB(nc, es)
        PE, DVE, ACT, POOL = nc.tensor, nc.vector, nc.scalar, nc.gpsimd

        uid = {"n": 0}

        def sb(name, shape, dtype=F32, stack=es):
            uid["n"] += 1
            return stack.enter_context(nc.sbuf_tensor(f"s{uid['n']}_{name}", shape, dtype))

        NPB = 5
        pbanks = [es.enter_context(nc.psum_tensor(f"pb{i}", [128, 512], F32)) for i in range(NPB)]
        pres = [Res() for _ in range(NPB)]
        pacc = es.enter_context(nc.psum_tensor("pacc", [128, 512], F32))
        pacc_r = Res()
        pbf = [es.enter_context(nc.psum_tensor(f"pbf{i}", [128, 1024], BF16)) for i in range(2)]
        pbf_res = [Res(), Res()]
        st = {"pi": 0, "bi": 0}

        def ps():
            i = st["pi"]
            st["pi"] = (i + 1) % NPB
            return pbanks[i], pres[i]

        def psb():
            i = st["bi"]
            st["bi"] = (i + 1) % 2
            return pbf[i][:, 0:512], pbf_res[i]

        xT = sb("xT", [128, KC, T])
        xr = [[Res() for _ in range(NT)] for _ in range(KC)]
        cb = sb("cb", [128, NCB * 128], BF16)
        cf = sb("cf", [128, 3 * 128])
        vecs = sb("vecs", [128, NL * NVEC])
        hv = sb("hv", [1, NL * 8])
        r_const = Res()

        def CB(i):
            return cb[:, i * 128:(i + 1) * 128]
        idf = cf[:, 0:128]
        onesf = cf[:, 128:256]
        negf = cf[:, 256:384]
        ident_b = CB(C_ID)
        ones_b = CB(C_ONES)

        k_in = kb.dsem("in")
        kb.dma("sp", [(xT[:, kc, :], xT_d[kc * 128:(kc + 1) * 128, :]) for kc in range(KC)], k_in,
               W=[xr[kc][tt] for kc in range(KC) for tt in range(NT)])
        k_c = kb.dsem("c")
        kb.dma("pool", [(cb[:], consts_d)], k_c, W=[r_const])
        k_c2 = kb.dsem("c2")
        kb.dma("sp", [(cf[:], consts_d[:, 0:384]), (vecs[:], vecs_d), (hv[:], hv_d)], k_c2, W=[r_const])

        def tap(name, ap, shape, R):
            if name not in taps:
                return
            d = nc.dram_tensor("tap_" + name, shape, F32, kind="ExternalOutput").ap()
            key = kb.dsem("t_" + name)
            tap_out[name] = key
            kb.dma("pool", [(d, ap)], key, R=R)

        def mm(out, lhsT, rhs, start, stop, R, W):
            kb.op("pe", lambda: PE.matmul(out, lhsT, rhs, start=start, stop=stop), R=R, W=W, inc=stop)

        def act(out, in_, func, R, W, bias=None, scale=None):
            kw = {}
            if bias is not None:
                kw["bias"] = bias
            if scale is not None:
                kw["scale"] = scale
            kb.op("act", lambda: ACT.activation(out=out, in_=in_, func=func, **kw), R=R, W=W)

        def amul(out, in_, c, R, W):
            kb.op("act", lambda: ACT.mul(out, in_, c), R=R, W=W)

        def tt_(e, out, in0, in1, op, R, W):
            eng = DVE if e == "dve" else POOL
            kb.op(e, lambda: eng.tensor_tensor(out, in0, in1, op), R=R, W=W)

        def ts_(e, out, in0, s1, s2, op0, op1, R, W):
            eng = DVE if e == "dve" else POOL
            if op1 is None:
                kb.op(e, lambda: eng.tensor_scalar(out, in0, s1, None, op0), R=R, W=W)
            else:
                kb.op(e, lambda: eng.tensor_scalar(out, in0, s1, s2, op0, op1), R=R, W=W)

        def stt_(e, out, in0, s, in1, op0, op1, R, W):
            eng = DVE if e == "dve" else POOL
            kb.op(e, lambda: eng.scalar_tensor_tensor(out=out, in0=in0, scalar=s, in1=in1, op0=op0, op1=op1), R=R, W=W)

        def b4(ap):
            return ap.unsqueeze(1).to_broadcast([ap.shape[0], 4, 128])

        def v4(ap):
            return ap.rearrange("p (c k) -> p c k", k=128)

        def rmsnorm_rstd(ps_ap, ps_res, n, rstd_ap, rstd_res):
            act(rstd_ap, ps_ap, AF.Ln, R=[ps_res], W=[rstd_res], bias=EPS, scale=1.0 / n)
            act(rstd_ap, rstd_ap, AF.Exp, R=[rstd_res], W=[rstd_res], scale=-0.5)

        def norm_tile(src_fn, src_res_fn, wcol_fn, dst_fn, dst_res_fn, sq, sq_res, rstd, rstd_res):
            pb, pr = ps()
            for kc in range(KC):
                s = sq[kc % 2]
                act(s[:], src_fn(kc), AF.Square, R=[src_res_fn(kc)], W=[sq_res[kc % 2]])
                mm(pb[:], ones_b, s[:], kc == 0, kc == KC - 1, R=[sq_res[kc % 2], r_const], W=[pr])
            rmsnorm_rstd(pb[:], pr, D, rstd[:], rstd_res)
            for kc in range(KC):
                stt_("dve", dst_fn(kc), src_fn(kc), wcol_fn(kc), rstd[:], ALU.mult, ALU.mult,
                     R=[src_res_fn(kc), rstd_res, r_const], W=[dst_res_fn(kc)])

        def silu_from(e_eng, out_ap, x_ap, tmp_ap, R, W, tmp_res):
            act(tmp_ap, x_ap, AF.Exp, R=R, W=[tmp_res], scale=-1.0)
            ts_("dve", tmp_ap, tmp_ap, 1.0, None, ALU.add, None, R=[tmp_res], W=[tmp_res])
            kb.op("dve", lambda: DVE.reciprocal(tmp_ap, tmp_ap), R=[tmp_res], W=[tmp_res])
            tt_(e_eng, out_ap, x_ap, tmp_ap, ALU.mult, R=list(R) + [tmp_res], W=W)

        wslot_key = [kb.dsem("ws0"), kb.dsem("ws1")]
        hw = {"n": 0, "slots": None, "res": None}

        def load_head_weights(l, kind, h):
            i = hw["n"] % 2
            hw["n"] += 1
            w = hw["slots"][i]
            if kind == "gdn":
                cols = [(O_AQ + h * 128, 128, 0), (O_AK + h * 128, 128, 128), (O_AV + h * 128, 128, 256), (O_AZ + h * 128, 128, 384)]
            else:
                cols = [(O_BQ + h * 128, 128, 0), (O_BK + h * 128, 128, 128), (O_BV + h * 256, 256, 256), (O_BR + h * 256, 256, 512)]
            pairs = [(w[:, :, o:o + n], w_in_d[l, :, c0:c0 + n].rearrange("(kc p) c -> p kc c", p=128)) for (c0, n, o) in cols]
            kb.dma("pool", pairs, wslot_key[i], W=[hw["res"][i]])
            return w, hw["res"][i]

        k_wsm = kb.dsem("wsm")
        wm_k = [kb.dsem(f"wm{i}") for i in range(2)]
        wo_k = [kb.dsem(f"wo{i}") for i in range(2)]
        wu_k = [kb.dsem(f"wu{i}") for i in range(3)]
        wd_k = [kb.dsem(f"wd{i}") for i in range(3)]

        for l in range(NL):
            V0 = l * NVEC

            def vcol(off, n=1, V0=V0):
                return vecs[:, V0 + off:V0 + off + n]
            with ExitStack() as ms:
                kb.barrier()
                hT = sb("hT", [128, KC, T], BF16, ms)
                hr = [[Res() for _ in range(NT)] for _ in range(KC)]
                oTa = sb("oTa", [128, 4, T], BF16, ms)
                oTa_r = [[Res() for _ in range(NT)] for _ in range(4)]
                oTb = sb("oTb", [128, 8, T], BF16, ms)
                oTb_r = [[Res() for _ in range(NT)] for _ in range(8)]
                wsm = sb("wsm", [128, KC, 24], BF16, ms)
                w2b = sb("w2b", [16, 512], BF16, ms)
                r_wsm = Res()
                with nc.allow_non_contiguous_dma(reason="small gate columns"):
                    kb.dma("pool", [(wsm[:, :, 0:8], w_in_d[l, :, O_AB:O_AB + 8].rearrange("(kc p) c -> p kc c", p=128)),
                                    (wsm[:, :, 8:24], w_in_d[l, :, O_GLR:O_GLR + 16].rearrange("(kc p) c -> p kc c", p=128)),
                                    (w2b[:], w2_d[:, l * 512:(l + 1) * 512])], k_wsm, W=[r_wsm])
                with ExitStack() as s1:
                    sq = [sb(f"sq{i}", [128, TT], BF16, s1) for i in range(2)]
                    sq_res = [Res(), Res()]
                    rstd = sb("rstd", [128, TT], F32, s1)
                    rstd_res = Res()
                    for tt in range(NT):
                        tsl = slice(tt * TT, (tt + 1) * TT)
                        norm_tile(lambda kc: xT[:, kc, tsl], lambda kc: xr[kc][tt], lambda kc: vcol(kc),
                                  lambda kc: hT[:, kc, tsl], lambda kc: hr[kc][tt], sq, sq_res, rstd, rstd_res)
                if l == 0:
                    tap("hT", hT[:, 0, :], [128, T], [hr[0][t_] for t_ in range(NT)])

                with ExitStack() as gs:
                    kb.barrier()
                    S = lambda n, shp, d=F32: sb(n, shp, d, gs)
                    hw["slots"] = [S(f"wsg{i}", [128, KC, 512], BF16) for i in range(2)]
                    hw["res"] = [Res(), Res()]
                    nxt = load_head_weights(l, "gdn", 0)
                    raw1 = S("raw1", [128, 3 + TT]); raw1_r = Res()
                    halo = S("halo", [128, 3, 3]); halo_r = [Res() for _ in range(3)]
                    cacc = S("cacc", [128, TT]); cacc_r = Res()
                    tmpf = S("tmpf", [128, TT]); tmpf_r = Res()
                    tmpg = S("tmpg", [128, TT]); tmpg_r = Res()
                    qT = S("qT", [128, TT], BF16); qT_r = Res()
                    kT = S("kT", [128, TT], BF16); kT_r = Res()
                    vT = S("vT", [128, TT], BF16); vT_r = Res()
                    zs = S("zs", [128, TT], BF16); zs_r = Res()
                    vb = S("vb", [128, TT], BF16); vb_r = Res()
                    kbg = S("kbg", [128, TT], BF16); kbg_r = Res()
                    kdc = S("kdc", [128, TT], BF16); kdc_r = Res()
                    Am = S("Am", [128, TT], BF16); Am_r = Res()
                    AT = S("AT", [128, TT], BF16); AT_r = Res()
                    Om = S("Om", [128, TT], BF16); Om_r = Res()
                    OT = S("OT", [128, TT], BF16); OT_r = Res()
                    Zp, Zp_r = OT, OT_r
                    Zt, Zt_r = Om, Om_r
                    Inv = S("Inv", [128, TT], BF16); Inv_r = Res()
                    Rm = S("Rm", [128, TT], BF16); Rm_r = Res()
                    qkm = S("qkm", [128, TT], BF16); qkm_r = Res()
                    qd = S("qd", [128, TT], BF16); qd_r = Res()
                    nwt, nwt_r = vT, vT_r
                    oraw = S("oraw", [128, TT]); oraw_r = Res()
                    sqg, sqg_r = Om, Om_r
                    vnew = [S(f"vnew{i}", [128, 128], BF16) for i in range(2)]
                    vnew_r = [Res(), Res()]
                    Sst = S("Sst", [128, 128]); Sst_r = Res()
                    Sbf = S("Sbf", [128, 128], BF16); Sbf_r = Res()
                    cols = S("cols", [128, 16]); cols_r = Res()
                    rowA = S("rowA", [1, TT]); rowA_r = Res()
                    rowB = S("rowB", [1, TT]); rowB_r = Res()
                    Grow = [S(f"Grow{i}", [1, 1 + TT]) for i in range(2)]
                    Grow_r = [Res(), Res()]
                    rGb = S("rGb", [1, TT]); rGb_r = Res()
                    rkb = S("rkb", [1, TT]); rkb_r = Res()
                    rcd = S("rcd", [1, 8]); rcd_r = Res()
                    nA = S("nA", [1, 4]); nA_r = Res()
                    act(nA[:], hv[0:1, l * 8:l * 8 + 4], AF.Exp, R=[r_const], W=[nA_r])
                    ts_("dve", nA[:], nA[:], -1.0, None, ALU.mult, None, R=[nA_r], W=[nA_r])

                    for h in (range(4) if "gdn" in phases else ()):
                        W_, W_r = nxt
                        nxt = load_head_weights(l, "gdn", h + 1) if h < 3 else None
                        kb.op("dve", lambda: DVE.memset(Sst[:], 0.0), W=[Sst_r])
                        kb.op("dve", lambda: DVE.memset(Sbf[:], 0.0), W=[Sbf_r])
                        for i in range(3):
                            kb.op("pool", lambda i=i: POOL.memset(halo[:, i, :], 0.0), W=[halo_r[i]])
                        for tt in range(NT):
                            tsl = slice(tt * TT, (tt + 1) * TT)
                            hres = [hr[kc][tt] for kc in range(KC)]
                            Gc_, Gc_r = Grow[tt % 2], Grow_r[tt % 2]
                            Gp_, Gp_r = Grow[(tt + 1) % 2], Grow_r[(tt + 1) % 2]
                            pb, pr = ps()
                            for kc in range(KC):
                                mm(pb[0:1, :], wsm[:, kc, h:h + 1], hT[:, kc, tsl], kc == 0, kc == KC - 1, R=[r_wsm, hres[kc]], W=[pr])
                            act(rowA[:], pb[0:1, :], AF.Exp, R=[pr], W=[rowA_r], scale=-1.0)
                            act(rowA[:], rowA[:], AF.Ln, R=[rowA_r], W=[rowA_r], bias=1.0)
                            pb, pr = ps()
                            for kc in range(KC):
                                mm(pb[0:1, :], wsm[:, kc, 4 + h:5 + h], hT[:, kc, tsl], kc == 0, kc == KC - 1, R=[r_wsm, hres[kc]], W=[pr])
                            act(rowB[:], pb[0:1, :], AF.Exp, R=[pr, r_const], W=[rowB_r], bias=hv[0:1, l * 8 + 4 + h:l * 8 + 5 + h])
                            act(rowB[:], rowB[:], AF.Ln, R=[rowB_r], W=[rowB_r], bias=1.0)
                            ts_("dve", rowB[:], rowB[:], nA[0:1, h:h + 1], None, ALU.mult, None, R=[rowB_r, nA_r], W=[rowB_r])
                            if tt == 0:
                                kb.op("dve", lambda: DVE.memset(Gc_[:, 0:1], 0.0), W=[Gc_r])
                            else:
                                kb.op("dve", lambda: DVE.tensor_copy(Gc_[:, 0:1], Gp_[:, TT:TT + 1]), R=[Gp_r], W=[Gc_r])
                            kb.op("dve", lambda: DVE.tensor_tensor_scan(Gc_[:, 1:1 + TT], onesf[0:1, 0:1].to_broadcast([1, TT]), rowB[:], Gc_[:, 0:1], ALU.mult, ALU.add),
                                  R=[r_const, rowB_r, Gc_r], W=[Gc_r])
                            Gv = Gc_[:, 1:1 + TT]
                            Gst4 = Gc_[:, 0:TT:128].unsqueeze(2).to_broadcast([1, 4, 128])
                            Gla4 = Gc_[:, 128:TT + 1:128].unsqueeze(2).to_broadcast([1, 4, 128])
                            tt_("dve", rGb[:], Gv, rowA[:], ALU.subtract, R=[Gc_r, rowA_r], W=[rGb_r])
                            tt_("dve", v4(rkb[:]), v4(rGb[:]), Gst4, ALU.subtract, R=[rGb_r, Gc_r], W=[rkb_r])
                            act(rkb[:], rkb[:], AF.Exp, R=[rkb_r], W=[rkb_r])
                            rkd, rkd_r = rowB, rowB_r
                            rbe, rbe_r = rowA, rowA_r
                            tt_("dve", v4(rkd[:]), v4(Gv), Gla4, ALU.subtract, R=[Gc_r], W=[rkd_r])
                            act(rkd[:], rkd[:], AF.Exp, R=[rkd_r], W=[rkd_r], scale=-1.0)
                            act(rbe[:], rowA[:], AF.Exp, R=[rowA_r], W=[rbe_r], scale=-1.0)
                            tt_("dve", rcd[:, 0:4], Gc_[:, 128:TT + 1:128], Gc_[:, 0:TT:128], ALU.subtract, R=[Gc_r], W=[rcd_r])
                            act(rcd[:, 0:4], rcd[:, 0:4], AF.Exp, R=[rcd_r], W=[rcd_r])
                            pcol, pcol_r = ps()
                            for cc in range(4):
                                csl = slice(cc * 128, (cc + 1) * 128)
                                for qi, (rw, rw_r) in enumerate(((rkb, rkb_r), (rkd, rkd_r), (rbe, rbe_r))):
                                    kb.op("pe", lambda rw=rw, qi=qi: PE.matmul(pcol[:, cc * 4 + qi:cc * 4 + qi + 1], rw[0:1, csl], onesf[0:1, 0:1], start=True, stop=True),
                                          R=[rw_r, r_const], W=[pcol_r], inc=False)
                                kb.op("pe", lambda: PE.matmul(pcol[:, cc * 4 + 3:cc * 4 + 4], onesf[0:1, 0:128], rcd[0:1, cc:cc + 1], start=True, stop=True),
                                      R=[rcd_r, r_const], W=[pcol_r], inc=(cc == 3))
                            kb.op("dve", lambda: DVE.tensor_copy(cols[:], pcol[:, 0:16]), R=[pcol_r], W=[cols_r])
                            colv = cols[:].rearrange("p (c q) -> p c q", q=4)

                            if GDN_STOP == 1:
                                continue
                            for xi, (dst, dst_r) in enumerate(((qT, qT_r), (kT, kT_r), (vT, vT_r))):
                                pb, pr = ps()
                                for kc in range(KC):
                                    mm(pb[:], W_[:, kc, xi * 128:(xi + 1) * 128], hT[:, kc, tsl], kc == 0, kc == KC - 1, R=[W_r, hres[kc]], W=[pr])
                                rw, rw_r = raw1, raw1_r
                                kb.op("pool", lambda xi=xi: POOL.tensor_copy(rw[:, 0:3], halo[:, xi, :]), R=[halo_r[xi]], W=[rw_r])
                                act(rw[:, 3:3 + TT], pb[:], AF.Copy, R=[pr], W=[rw_r])
                                cw = lambda tap_, xi=xi: vcol(32 + tap_ * 12 + xi * 4 + h)
                                ts_("pool", cacc[:], rw[:, 0:TT], cw(0), None, ALU.mult, None, R=[rw_r, r_const], W=[cacc_r])
                                for tp in (1, 2, 3):
                                    ts_("pool", oraw[:], rw[:, tp:tp + TT], cw(tp), None, ALU.mult, None, R=[rw_r, r_const], W=[oraw_r])
                                    tt_("pool", cacc[:], cacc[:], oraw[:], ALU.add, R=[cacc_r, oraw_r], W=[cacc_r])
                                kb.op("pool", lambda xi=xi: POOL.tensor_copy(halo[:, xi, :], rw[:, TT:TT + 3]), R=[rw_r], W=[halo_r[xi]])
                                if xi == 2:
                                    silu_from("dve", vT[:], cacc[:], tmpf[:], R=[cacc_r], W=[vT_r], tmp_res=tmpf_r)
                                else:
                                    silu_from("dve", tmpg[:], cacc[:], tmpf[:], R=[cacc_r], W=[tmpg_r], tmp_res=tmpf_r)
                                    act(sqg[:], tmpg[:], AF.Square, R=[tmpg_r], W=[sqg_r])
                                    pb2, pr2 = ps()
                                    mm(pb2[:], ones_b, sqg[:], True, True, R=[sqg_r, r_const], W=[pr2])
                                    act(tmpf[:], pb2[:], AF.Ln, R=[pr2], W=[tmpf_r], bias=EPS)
                                    act(tmpf[:], tmpf[:], AF.Exp, R=[tmpf_r], W=[tmpf_r], scale=-0.5)
                                    sc = (128.0 ** -0.5) if xi == 0 else 1.0
                                    stt_("dve", dst[:], tmpg[:], sc, tmpf[:], ALU.mult, ALU.mult, R=[tmpg_r, tmpf_r], W=[dst_r])
                            pb, pr = ps()
                            for kc in range(KC):
                                mm(pb[:], W_[:, kc, 384:512], hT[:, kc, tsl], kc == 0, kc == KC - 1, R=[W_r, hres[kc]], W=[pr])
                            silu_from("dve", zs[:], pb[:], tmpf[:], R=[pr], W=[zs_r], tmp_res=tmpf_r)

                            if GDN_STOP == 2:
                                continue
                            pt, ptr = psb()
                            for cc in range(4):
                                csl = slice(cc * 128, (cc + 1) * 128)
                                kb.op("pe", lambda: PE.transpose(pt[:, csl], vT[:, csl], ident_b), R=[vT_r, r_const], W=[ptr], inc=(cc == 3))
                            tt_("dve", v4(vb[:]), v4(pt), colv[:, :, 2:3].to_broadcast([128, 4, 128]), ALU.mult, R=[ptr, cols_r], W=[vb_r])
                            pt, ptr = psb()
                            for cc in range(4):
                                csl = slice(cc * 128, (cc + 1) * 128)
                                kb.op("pe", lambda: PE.transpose(pt[:, csl], kT[:, csl], ident_b), R=[kT_r, r_const], W=[ptr], inc=(cc == 3))
                            tt_("dve", v4(kbg[:]), v4(pt), colv[:, :, 0:1].to_broadcast([128, 4, 128]), ALU.mult, R=[ptr, cols_r], W=[kbg_r])
                            tt_("dve", v4(kdc[:]), v4(pt), colv[:, :, 1:2].to_broadcast([128, 4, 128]), ALU.mult, R=[ptr, cols_r], W=[kdc_r])

                            if GDN_STOP == 3:
                                continue
                            def expo(lrow, lrow_r, lneg, rrow, rrow_r, rneg, mask_idx):
                                pb_, pr_ = ps()
                                for cc in range(4):
                                    csl = slice(cc * 128, (cc + 1) * 128)
                                    kb.op("pe", lambda: PE.matmul(pb_[:, csl], (negf if rneg else onesf)[0:1, 0:128], rrow[0:1, csl], start=True, stop=False),
                                          R=[rrow_r, r_const], W=[pr_], inc=False)
                                    kb.op("pe", lambda: PE.matmul(pb_[:, csl], lrow[0:1, csl], (negf if lneg else onesf)[0:1, 0:128], start=False, stop=False),
                                          R=[lrow_r, r_const], W=[pr_], inc=False)
                                    kb.op("pe", lambda: PE.matmul(pb_[:, csl], ident_b, CB(mask_idx), start=False, stop=True),
                                          R=[r_const], W=[pr_], inc=(cc == 3))
                                return pb_, pr_
                            pkk, pkk_r = ps()
                            for cc in range(4):
                                csl = slice(cc * 128, (cc + 1) * 128)
                                mm(pkk[:, csl], kT[:, csl], kT[:, csl], True, True, R=[kT_r], W=[pkk_r])
                            pe1, pe1_r = expo(Gv, Gc_r, True, rGb, rGb_r, False, C_MN_STR_T)
                            act(tmpf[:], pe1[:], AF.Exp, R=[pe1_r], W=[tmpf_r])
                            tt_("dve", AT[:], pkk[:], tmpf[:], ALU.mult, R=[pkk_r, tmpf_r], W=[AT_r])
                            pe2, pe2_r = expo(rGb, rGb_r, False, Gv, Gc_r, True, C_MN_STR)
                            act(tmpg[:], pe2[:], AF.Exp, R=[pe2_r], W=[tmpg_r])
                            tt_("dve", Am[:], pkk[:], tmpg[:], ALU.mult, R=[pkk_r, tmpg_r], W=[Am_r])
                            pe3, pe3_r = expo(Gv, Gc_r, True, Gv, Gc_r, False, C_MN_INC_T)
                            act(tmpf[:], pe3[:], AF.Exp, R=[pe3_r], W=[tmpf_r])
                            pqk, pqk_r = ps()
                            for cc in range(4):
                                csl = slice(cc * 128, (cc + 1) * 128)
                                mm(pqk[:, csl], kT[:, csl], qT[:, csl], True, True, R=[kT_r, qT_r], W=[pqk_r])
                            tt_("dve", qkm[:], pqk[:], tmpf[:], ALU.mult, R=[pqk_r, tmpf_r], W=[qkm_r])
                            rGc, rGc_r = rkb, rkb_r
                            tt_("dve", v4(rGc[:]), v4(Gv), Gst4, ALU.subtract, R=[Gc_r], W=[rGc_r])
                            pg, pg_r = ps()
                            for cc in range(4):
                                csl = slice(cc * 128, (cc + 1) * 128)
                                kb.op("pe", lambda: PE.matmul(pg[:, csl], onesf[0:1, 0:128], rGc[0:1, csl], start=True, stop=True),
                                      R=[rGc_r, r_const], W=[pg_r], inc=(cc == 3))
                            act(tmpg[:], pg[:], AF.Exp, R=[pg_r], W=[tmpg_r])
                            tt_("dve", qd[:], qT[:], tmpg[:], ALU.mult, R=[qT_r, tmpg_r], W=[qd_r])

                            if GDN_STOP == 4:
                                continue
                            tt_("dve", v4(Om[:]), v4(Am[:]), b4(CB(C_LV + 0)), ALU.mult, R=[Am_r, r_const], W=[Om_r])
                            stt_("dve", v4(Inv[:]), v4(Om[:]), -1.0, b4(ident_b), ALU.mult, ALU.add, R=[Om_r, r_const], W=[Inv_r])
                            tt_("dve", v4(OT[:]), v4(AT[:]), b4(CB(C_LVT + 0)), ALU.mult, R=[AT_r, r_const], W=[OT_r])
                            stt_("dve", v4(Rm[:]), v4(OT[:]), -1.0, b4(ident_b), ALU.mult, ALU.add, R=[OT_r, r_const], W=[Rm_r])
                            for li in range(1, 7):
                                last = (li == 6)
                                pz, pz_r = ps()
                                for cc in range(4):
                                    csl = slice(cc * 128, (cc + 1) * 128)
                                    mm(pz[:, csl], Am[:, csl], Rm[:, csl], True, True, R=[Am_r, Rm_r], W=[pz_r])
                                if not last:
                                    pzp, pzp_r = ps()
                                    for cc in range(4):
                                        csl = slice(cc * 128, (cc + 1) * 128)
                                        mm(pzp[:, csl], AT[:, csl], Inv[:, csl], True, True, R=[AT_r, Inv_r], W=[pzp_r])
                                tt_("dve", v4(Zt[:]), v4(pz[:]), b4(CB(C_LVT + li)), ALU.mult, R=[pz_r, r_const], W=[Zt_r])
                                if not last:
                                    tt_("dve", v4(Zp[:]), v4(pzp[:]), b4(CB(C_LV + li)), ALU.mult, R=[pzp_r, r_const], W=[Zp_r])
                                pr2_, pr2_r = ps()
                                for cc in range(4):
                                    csl = slice(cc * 128, (cc + 1) * 128)
                                    mm(pr2_[:, csl], Inv[:, csl], Zt[:, csl], True, True, R=[Inv_r, Zt_r], W=[pr2_r])
                                if not last:
                                    pi2_, pi2_r = ps()
                                    for cc in range(4):
                                        csl = slice(cc * 128, (cc + 1) * 128)
                                        mm(pi2_[:, csl], Rm[:, csl], Zp[:, csl], True, True, R=[Rm_r, Zp_r], W=[pi2_r])
                                tt_("dve", Rm[:], Rm[:], pr2_[:], ALU.subtract, R=[Rm_r, pr2_r], W=[Rm_r])
                                if not last:
                                    tt_("dve", Inv[:], Inv[:], pi2_[:], ALU.subtract, R=[Inv_r, pi2_r], W=[Inv_r])
                            if GDN_STOP == 5:
                                continue
                            pw, pw_r = ps()
                            for cc in range(4):
                                csl = slice(cc * 128, (cc + 1) * 128)
                                mm(pw[:, csl], kbg[:, csl], Rm[:, csl], True, True, R=[kbg_r, Rm_r], W=[pw_r])
                            amul(nwt[:], pw[:], -1.0, R=[pw_r], W=[nwt_r])

                            if GDN_STOP == 6:
                                continue
                            for cc in range(4):
                                csl = slice(cc * 128, (cc + 1) * 128)
                                vn, vn_r = vnew[cc % 2], vnew_r[cc % 2]
                                pv, pv_r = ps()
                                mm(pv[:, 0:128], Rm[:, csl], vb[:, csl], True, False, R=[Rm_r, vb_r], W=[pv_r])
                                mm(pv[:, 0:128], nwt[:, csl], Sbf[:], False, True, R=[nwt_r, Sbf_r], W=[pv_r])
                                act(vn[:], pv[:, 0:128], AF.Copy, R=[pv_r], W=[vn_r])
                                if GDN_REC == 1:
                                    continue
                                po, po_r = ps()
                                mm(po[:, 0:128], Sbf[:], qd[:, csl], True, False, R=[Sbf_r, qd_r], W=[po_r])
                                mm(po[:, 0:128], vn[:], qkm[:, csl], False, True, R=[vn_r, qkm_r], W=[po_r])
                                pd, pd_r = ps()
                                mm(pd[:, 0:128], kdc[:, csl], vn[:], True, True, R=[kdc_r, vn_r], W=[pd_r])
                                act(oraw[:, csl], po[:, 0:128], AF.Copy, R=[po_r], W=[oraw_r])
                                if GDN_REC == 2:
                                    continue
                                ts_("pool", Sst[:], Sst[:], cols[:, cc * 4 + 3:cc * 4 + 4], None, ALU.mult, None, R=[Sst_r, cols_r], W=[Sst_r])
                                tt_("dve", Sst[:], pd[:, 0:128], Sst[:], ALU.add, R=[Sst_r, pd_r], W=[Sst_r])
                                if GDN_REC == 3:
                                    continue
                                act(Sbf[:], Sst[:], AF.Copy, R=[Sst_r], W=[Sbf_r])

                            if GDN_STOP == 7:
                                continue
                            act(sqg[:], oraw[:], AF.Square, R=[oraw_r], W=[sqg_r])
                            pb2, pr2 = ps()
                            mm(pb2[:], ones_b, sqg[:], True, True, R=[sqg_r, r_const], W=[pr2])
                            rmsnorm_rstd(pb2[:], pr2, 128, tmpf[:], tmpf_r)
                            stt_("dve", tmpg[:], oraw[:], vcol(80), tmpf[:], ALU.mult, ALU.mult, R=[oraw_r, tmpf_r, r_const], W=[tmpg_r])
                            tt_("pool", oTa[:, h, tsl], tmpg[:], zs[:], ALU.mult, R=[tmpg_r, zs_r], W=[oTa_r[h][tt]])
                    if "gdn_dbg" in taps and l == 0:
                        for nm, tl, rr in (("AT", AT, AT_r), ("Am", Am, Am_r), ("Rm", Rm, Rm_r), ("Inv", Inv, Inv_r), ("qkm", qkm, qkm_r), ("qd", qd, qd_r),
                                           ("nwt", nwt, nwt_r), ("vb", vb, vb_r), ("kbg", kbg, kbg_r), ("kdc", kdc, kdc_r), ("oraw", oraw, oraw_r),
                                           ("qT", qT, qT_r), ("kT", kT, kT_r), ("zs", zs, zs_r), ("tmpf", tmpf, tmpf_r), ("tmpg", tmpg, tmpg_r)):
                            taps = tuple(taps) + ("d_" + nm,)
                            tap("d_" + nm, tl[:], [128, TT], [rr])
                        taps = tuple(taps) + ("d_cols", "d_Sst", "d_G", "d_Gb")
                        tap("d_cols", cols[:], [128, 16], [cols_r])
                        tap("d_Sst", Sst[:], [128, 128], [Sst_r])
                        tap("d_G", Grow[1][:], [1, 1 + TT], [Grow_r[1]])
                        tap("d_Gb", rGb[:], [1, TT], [rGb_r])
                if l == 0:
                    for h in range(4):
                        tap(f"oTa{h}", oTa[:, h, :], [128, T], [oTa_r[h][t_] for t_ in range(NT)])
                with ExitStack() as gs:
                    kb.barrier()
                    S = lambda n, shp, d=F32: sb(n, shp, d, gs)
                    hw["slots"] = [S(f"wsl{i}", [128, KC, 768], BF16) for i in range(2)]
                    hw["res"] = [Res(), Res()]
                    nxt = load_head_weights(l, "gla", 0)
                    glrT = S("glrT", [16, TT], BF16); glr_r = Res()
                    qf = S("qf", [128, TT]); qf_r = Res()
                    kf = S("kf", [128, TT]); kf_r = Res()
                    tmpf = S("tmpf2", [128, TT]); tmpf_r = Res()
                    tmpg = S("tmpg2", [128, TT]); tmpg_r = Res()
                    tmph, tmph_r = tmpg, tmpg_r
                    Pp = [S(f"Pp{i}", [128, 1 + TT]) for i in range(2)]; Pp_r = [Res(), Res()]
                    lrow, lrow_r = tmpg, tmpg_r
                    qk2 = S("qk2", [128, 2, TT], BF16)
                    qin = qk2[:, 0, :]; qin_r = Res()
                    kin = qk2[:, 1, :]; kin_r = Res()
                    qdc = S("qdc", [128, TT], BF16); qdc_r = Res()
                    kdcT = S("kdcT", [128, TT], BF16); kdcT_r = Res()
                    kdt = S("kdt", [128, TT], BF16); kdt_r = Res()
                    vtok = S("vtok", [128, 4, 256], BF16); vtok_r = Res()
                    attn = S("attn", [128, TT], BF16); attn_r = Res()
                    rs = S("rs", [128, 2, TT], BF16); rs_r = Res()
                    oraw2 = S("oraw2", [128, 2, TT]); oraw2_r = Res()
                    S2 = S("S2", [128, 256]); S2_r = Res()
                    S2b = S("S2b", [128, 256], BF16); S2b_r = Res()
                    cdc = S("cdc", [128, 4]); cdc_r = Res()
                    ngb = S("ngb", [128, 4]); ngb_r = Res()
                    ts_("dve", ngb[:], vcol(83, 4), -1.0, None, ALU.mult, None, R=[r_const], W=[ngb_r])
                    for h in (range(4) if "gla" in phases else ()):
                        W_, W_r = nxt
                        nxt = load_head_weights(l, "gla", h + 1) if h < 3 else None
                        kb.op("dve", lambda: DVE.memset(S2[:], 0.0), W=[S2_r])
                        kb.op("dve", lambda: DVE.memset(S2b[:], 0.0), W=[S2b_r])
                        for tt in range(NT):
                            tsl = slice(tt * TT, (tt + 1) * TT)
                            hres = [hr[kc][tt] for kc in range(KC)]
                            Pc, Pc_r = Pp[tt % 2], Pp_r[tt % 2]
                            Pv_, Pv_r = Pp[(tt + 1) % 2], Pp_r[(tt + 1) % 2]
                            pb, pr = ps()
                            for kc in range(KC):
                                mm(pb[:], W_[:, kc, 0:128], hT[:, kc, tsl], kc == 0, kc == KC - 1, R=[W_r, hres[kc]], W=[pr])
                            amul(qf[:], pb[:], 128.0 ** -0.5, R=[pr], W=[qf_r])
                            pb, pr = ps()
                            for kc in range(KC):
                                mm(pb[:], W_[:, kc, 128:256], hT[:, kc, tsl], kc == 0, kc == KC - 1, R=[W_r, hres[kc]], W=[pr])
                            act(kf[:], pb[:], AF.Copy, R=[pr], W=[kf_r])
                            for half in range(2):
                                pb, pr = ps()
                                for c2 in range(2):
                                    cc = half * 2 + c2
                                    for kc in range(KC):
                                        mm(pb[:, c2 * 256:(c2 + 1) * 256], hT[:, kc, tt * TT + cc * 128: tt * TT + (cc + 1) * 128], W_[:, kc, 256:512],
                                           kc == 0, kc == KC - 1, R=[W_r, hres[kc]], W=[pr])
                                act(vtok[:, half * 2:half * 2 + 2, :], pb[:].rearrange("p (c e) -> p c e", e=256), AF.Copy, R=[pr], W=[vtok_r])
                            for et in range(2):
                                pb, pr = ps()
                                for kc in range(KC):
                                    mm(pb[:], W_[:, kc, 512 + et * 128:512 + (et + 1) * 128], hT[:, kc, tsl], kc == 0, kc == KC - 1, R=[W_r, hres[kc]], W=[pr])
                                silu_from("dve", rs[:, et, :], pb[:], tmpf[:], R=[pr], W=[rs_r], tmp_res=tmpf_r)
                            pb, pr = ps()
                            for kc in range(KC):
                                mm(pb[0:16, :], wsm[:, kc, 8:24], hT[:, kc, tsl], kc == 0, kc == KC - 1, R=[r_wsm, hres[kc]], W=[pr])
                            act(glrT[:], pb[0:16, :], AF.Copy, R=[pr], W=[glr_r])
                            pb, pr = ps()
                            mm(pb[:], w2b[0:16, h * 128:(h + 1) * 128], glrT[:], True, True, R=[r_wsm, glr_r], W=[pr])
                            act(lrow[:], pb[:], AF.Exp, R=[pr, ngb_r], W=[lrow_r], bias=ngb[:, h:h + 1], scale=-1.0)
                            act(lrow[:], lrow[:], AF.Ln, R=[lrow_r], W=[lrow_r], bias=1.0)
                            if tt == 0:
                                kb.op("dve", lambda: DVE.memset(Pc[:, 0:1], 0.0), W=[Pc_r])
                            else:
                                kb.op("dve", lambda: DVE.tensor_copy(Pc[:, 0:1], Pv_[:, TT:TT + 1]), R=[Pv_r], W=[Pc_r])
                            kb.op("dve", lambda: DVE.tensor_tensor_scan(Pc[:, 1:1 + TT], onesf[:, 0:1].to_broadcast([128, TT]), lrow[:], Pc[:, 0:1], ALU.mult, ALU.add),
                                  R=[r_const, lrow_r, Pc_r], W=[Pc_r])
                            Pvw = v4(Pc[:, 1:1 + TT])
                            Pst = Pc[:, 0:TT:128].unsqueeze(2).to_broadcast([128, 4, 128])
                            Pmid = Pc[:, 65:TT + 1:128].unsqueeze(2).to_broadcast([128, 4, 128])
                            Pla = Pc[:, 128:TT + 1:128].unsqueeze(2).to_broadcast([128, 4, 128])
                            isc = 1.0 / 16.0
                            tt_("dve", v4(tmpf[:]), Pvw, Pmid, ALU.subtract, R=[Pc_r], W=[tmpf_r])
                            act(tmpg[:], tmpf[:], AF.Exp, R=[tmpf_r], W=[tmpg_r], scale=-isc)
                            tt_("dve", qin, qf[:], tmpg[:], ALU.mult, R=[qf_r, tmpg_r], W=[qin_r])
                            act(tmph[:], tmpf[:], AF.Exp, R=[tmpf_r], W=[tmph_r], scale=isc)
                            tt_("dve", kin, kf[:], tmph[:], ALU.mult, R=[kf_r, tmph_r], W=[kin_r])
                            tt_("dve", v4(tmpf[:]), Pvw, Pst, ALU.subtract, R=[Pc_r], W=[tmpf_r])
                            act(tmpg[:], tmpf[:], AF.Exp, R=[tmpf_r], W=[tmpg_r], scale=-isc)
                            tt_("dve", qdc[:], qf[:], tmpg[:], ALU.mult, R=[qf_r, tmpg_r], W=[qdc_r])
                            tt_("dve", v4(tmpf[:]), Pvw, Pla, ALU.subtract, R=[Pc_r], W=[tmpf_r])
                            act(tmph[:], tmpf[:], AF.Exp, R=[tmpf_r], W=[tmph_r], scale=isc)
                            tt_("dve", kdcT[:], kf[:], tmph[:], ALU.mult, R=[kf_r, tmph_r], W=[kdcT_r])
                            tt_("dve", cdc[:], Pc[:, 128:TT + 1:128], Pc[:, 0:TT:128], ALU.subtract, R=[Pc_r], W=[cdc_r])
                            act(cdc[:], cdc[:], AF.Exp, R=[cdc_r], W=[cdc_r], scale=-isc)
                            pa, pa_r = ps()
                            for cc in range(4):
                                csl = slice(cc * 128, (cc + 1) * 128)
                                mm(pa[:, csl], kin[:, csl], qin[:, csl], True, True, R=[kin_r, qin_r], W=[pa_r])
                            tt_("dve", v4(attn[:]), v4(pa[:]), b4(CB(C_C01_T)), ALU.mult, R=[pa_r, r_const], W=[attn_r])
                            pt, ptr = psb()
                            for cc in range(4):
                                csl = slice(cc * 128, (cc + 1) * 128)
                                kb.op("pe", lambda: PE.transpose(pt[:, csl], kdcT[:, csl], ident_b), R=[kdcT_r, r_const], W=[ptr], inc=(cc == 3))
                            act(kdt[:], pt, AF.Copy, R=[ptr], W=[kdt_r])
                            for cc in range(4):
                                csl = slice(cc * 128, (cc + 1) * 128)
                                po, po_r = ps()
                                for et in range(2):
                                    esl = slice(et * 128, (et + 1) * 128)
                                    mm(po[:, esl], vtok[:, cc, esl], attn[:, csl], True, False, R=[vtok_r, attn_r], W=[po_r])
                                    mm(po[:, esl], S2b[:, esl], qdc[:, csl], False, True, R=[S2b_r, qdc_r], W=[po_r])
                                pd, pd_r = ps()
                                mm(pd[:, 0:256], kdt[:, csl], vtok[:, cc, :], True, True, R=[kdt_r, vtok_r], W=[pd_r])
                                act(oraw2[:, :, csl], po[:, 0:256].rearrange("p (e k) -> p e k", k=128), AF.Copy, R=[po_r], W=[oraw2_r])
                                ts_("pool", S2[:], S2[:], cdc[:, cc:cc + 1], None, ALU.mult, None, R=[S2_r, cdc_r], W=[S2_r])
                                tt_("dve", S2[:], pd[:, 0:256], S2[:], ALU.add, R=[S2_r, pd_r], W=[S2_r])
                                act(S2b[:], S2[:], AF.Copy, R=[S2_r], W=[S2b_r])
                            act(qk2[:], oraw2[:], AF.Square, R=[oraw2_r], W=[qin_r, kin_r])
                            pb2, pr2 = ps()
                            for et in range(2):
                                mm(pb2[:], ones_b, qk2[:, et, :], et == 0, et == 1, R=[qin_r, kin_r, r_const], W=[pr2])
                            rmsnorm_rstd(pb2[:], pr2, 256, tmpf[:], tmpf_r)
                            for et in range(2):
                                stt_("dve", tmpg[:], oraw2[:, et, :], vcol(81 + et), tmpf[:], ALU.mult, ALU.mult, R=[oraw2_r, tmpf_r, r_const], W=[tmpg_r])
                                tt_("pool", oTb[:, h * 2 + et, tsl], tmpg[:], rs[:, et, :], ALU.mult, R=[tmpg_r, rs_r], W=[oTb_r[h * 2 + et][tt]])

                if l == 0:
                    for h in range(8):
                        tap(f"oTb{h}", oTb[:, h, :], [128, T], [oTb_r[h][t_] for t_ in range(NT)])
                with ExitStack() as gs:
                    kb.barrier()
                    S = lambda n, shp, d=F32: sb(n, shp, d, gs)
                    NW = 2
                    wm = [S(f"wm{i}", [128, 28, 128], BF16) for i in range(NW)]
                    wm_r = [Res() for _ in range(NW)]
                    wo = [S(f"wo{i}", [128, KC, 128], BF16) for i in range(NW)]
                    wo_r = [Res() for _ in range(NW)]
                    stage = S("stage", [128, KC, TT], BF16); stage_r = [Res() for _ in range(KC)]
                    tmpo = S("tmpo", [128, KC, TT]); tmpo_r = [Res() for _ in range(KC)]
                    sa = S("sa", [128, TT]); sa_r = Res()
                    sb_ = S("sb_", [128, TT]); sb_r = Res()
                    m1 = S("m1", [128, TT]); m1_r = Res()
                    sq = [S(f"sqm{i}", [128, TT], BF16) for i in range(2)]; sq_res = [Res(), Res()]
                    rstd = S("rstdm", [128, TT]); rstd_res = Res()
                    t2 = S("t2", [128, TT]); t2_r = Res()
                    cnt = {"m": 0, "o": 0}

                    def load_wm(ct):
                        i = cnt["m"] % NW
                        cnt["m"] += 1
                        csl = slice(ct * 128, (ct + 1) * 128)
                        pairs = [(wm[i][:, 0:4, :], w_oa_d[l, :, csl].rearrange("(k p) c -> p k c", p=128)),
                                 (wm[i][:, 4:12, :], w_ob_d[l, :, csl].rearrange("(k p) c -> p k c", p=128)),
                                 (wm[i][:, 12:20, :], w_in_d[l, :, O_GA + ct * 128:O_GA + (ct + 1) * 128].rearrange("(k p) c -> p k c", p=128)),
                                 (wm[i][:, 20:28, :], w_in_d[l, :, O_GB + ct * 128:O_GB + (ct + 1) * 128].rearrange("(k p) c -> p k c", p=128))]
                        kb.dma("pool", pairs, wm_k[i], W=[wm_r[i]])
                        return wm[i], wm_r[i]

                    def load_wo(ct):
                        i = cnt["o"] % NW
                        cnt["o"] += 1
                        kb.dma("pool", [(wo[i][:], w_o_d[l, :, ct * 128:(ct + 1) * 128].rearrange("(k p) c -> p k c", p=128))], wo_k[i], W=[wo_r[i]])
                        return wo[i], wo_r[i]

                    for tt in (range(NT) if "merge" in phases else ()):
                        tsl = slice(tt * TT, (tt + 1) * TT)
                        nw = load_wm(0)
                        for ct in range(KC):
                            w_, w_r = nw
                            if ct < KC - 1:
                                nw = load_wm(ct + 1)
                            pya, pya_r = ps()
                            for k in range(4):
                                mm(pya[:], w_[:, k, :], oTa[:, k, tsl], k == 0, k == 3, R=[w_r, oTa_r[k][tt]], W=[pya_r])
                            pyb, pyb_r = ps()
                            for k in range(8):
                                mm(pyb[:], w_[:, 4 + k, :], oTb[:, k, tsl], k == 0, k == 7, R=[w_r, oTb_r[k][tt]], W=[pyb_r])
                            pga, pga_r = ps()
                            for k in range(8):
                                mm(pga[:], w_[:, 12 + k, :], hT[:, k, tsl], k == 0, k == 7, R=[w_r, hr[k][tt]], W=[pga_r])
                            pgb, pgb_r = ps()
                            for k in range(8):
                                mm(pgb[:], w_[:, 20 + k, :], hT[:, k, tsl], k == 0, k == 7, R=[w_r, hr[k][tt]], W=[pgb_r])
                            act(sa[:], pga[:], AF.Exp, R=[pga_r], W=[sa_r], scale=-1.0)
                            ts_("dve", sa[:], sa[:], 1.0, None, ALU.add, None, R=[sa_r], W=[sa_r])
                            kb.op("dve", lambda: DVE.reciprocal(sa[:], sa[:]), R=[sa_r], W=[sa_r])
                            act(sb_[:], pgb[:], AF.Exp, R=[pgb_r], W=[sb_r], scale=-1.0)
                            ts_("pool", sb_[:], sb_[:], 1.0, None, ALU.add, None, R=[sb_r], W=[sb_r])
                            kb.op("dve", lambda: DVE.reciprocal(sb_[:], sb_[:]), R=[sb_r], W=[sb_r])
                            tt_("dve", m1[:], pya[:], sa[:], ALU.mult, R=[pya_r, sa_r], W=[m1_r])
                            tt_("dve", sb_[:], pyb[:], sb_[:], ALU.mult, R=[pyb_r, sb_r], W=[sb_r])
                            tt_("pool", stage[:, ct, :], m1[:], sb_[:], ALU.add, R=[m1_r, sb_r], W=[stage_r[ct]])
                        pss, pss_r = pacc, pacc_r
                        nw = load_wo(0)
                        for ct in range(KC):
                            w_, w_r = nw
                            if ct < KC - 1:
                                nw = load_wo(ct + 1)
                            pb, pr = ps()
                            for k in range(KC):
                                mm(pb[:], w_[:, k, :], stage[:, k, :], k == 0, k == KC - 1, R=[w_r, stage_r[k]], W=[pr])
                            act(tmpo[:, ct, :], pb[:], AF.Copy, R=[pr], W=[tmpo_r[ct]])
                            s = sq[ct % 2]
                            act(s[:], pb[:], AF.Square, R=[pr], W=[sq_res[ct % 2]])
                            mm(pss[:], ones_b, s[:], ct == 0, ct == KC - 1, R=[sq_res[ct % 2], r_const], W=[pss_r])
                        rmsnorm_rstd(pss[:], pss_r, D, rstd[:], rstd_res)
                        for ct in range(KC):
                            stt_("dve", t2[:], tmpo[:, ct, :], vcol(8 + ct), rstd[:], ALU.mult, ALU.mult, R=[tmpo_r[ct], rstd_res, r_const], W=[t2_r])
                            tt_("pool", xT[:, ct, tsl], xT[:, ct, tsl], t2[:], ALU.add, R=[xr[ct][tt], t2_r], W=[xr[ct][tt]])

            if l == 0:
                for kc in range(KC):
                    tap(f"xmix{kc}", xT[:, kc, :], [128, T], [xr[kc][t_] for t_ in range(NT)])
            with ExitStack() as gs:
                kb.barrier()
                S = lambda n, shp, d=F32: sb(n, shp, d, gs)
                h2 = S("h2", [128, KC, TT], BF16); h2_r = [Res() for _ in range(KC)]
                uT = S("uT", [128, 32, TT], BF16); uT_r = [Res() for _ in range(32)]
                NW = 3
                wu = [S(f"wu{i}", [128, KC, 512], BF16) for i in range(NW)]; wu_r = [Res() for _ in range(NW)]
                wd = [S(f"wd{i}", [128, 32, 128], BF16) for i in range(NW)]; wd_r = [Res() for _ in range(NW)]
                tmpo = S("tmpo2", [128, KC, TT]); tmpo_r = [Res() for _ in range(KC)]
                sq = [S(f"sqn{i}", [128, TT], BF16) for i in range(2)]; sq_res = [Res(), Res()]
                rstd = S("rstdn", [128, TT]); rstd_res = Res()
                rl = S("rl", [128, TT]); rl_r = Res()
                t2 = S("t2n", [128, TT]); t2_r = Res()
                cnt = {"u": 0, "d": 0}

                def load_wu(fb):
                    i = cnt["u"] % NW
                    cnt["u"] += 1
                    kb.dma("pool", [(wu[i][:], w_up_d[l, :, fb * 512:(fb + 1) * 512].rearrange("(k p) c -> p k c", p=128))], wu_k[i], W=[wu_r[i]])
                    return wu[i], wu_r[i]

                def load_wd(ct):
                    i = cnt["d"] % NW
                    cnt["d"] += 1
                    kb.dma("pool", [(wd[i][:], w_dn_d[l, :, ct * 128:(ct + 1) * 128].rearrange("(k p) c -> p k c", p=128))], wd_k[i], W=[wd_r[i]])
                    return wd[i], wd_r[i]

                for tt in (range(NT) if "mlp" in phases else ()):
                    tsl = slice(tt * TT, (tt + 1) * TT)
                    q_u = [load_wu(0), load_wu(1)]
                    norm_tile(lambda kc: xT[:, kc, tsl], lambda kc: xr[kc][tt], lambda kc: vcol(16 + kc),
                              lambda kc: h2[:, kc, :], lambda kc: h2_r[kc], sq, sq_res, rstd, rstd_res)
                    for fb in range(8):
                        w_, w_r = q_u.pop(0)
                        if fb + 2 < 8:
                            q_u.append(load_wu(fb + 2))
                        for f4 in range(4):
                            ft = fb * 4 + f4
                            pb, pr = ps()
                            for k in range(KC):
                                mm(pb[:], w_[:, k, f4 * 128:(f4 + 1) * 128], h2[:, k, :], k == 0, k == KC - 1, R=[w_r, h2_r[k]], W=[pr])
                            act(rl[:], pb[:], AF.Relu, R=[pr], W=[rl_r])
                            tt_("pool" if ft % 2 else "dve", uT[:, ft, :], rl[:], rl[:], ALU.mult, R=[rl_r], W=[uT_r[ft]])
                    q_d = [load_wd(0), load_wd(1)]
                    pss, pss_r = pacc, pacc_r
                    for ct in range(KC):
                        w_, w_r = q_d.pop(0)
                        if ct + 2 < KC:
                            q_d.append(load_wd(ct + 2))
                        pb, pr = ps()
                        for k in range(32):
                            mm(pb[:], w_[:, k, :], uT[:, k, :], k == 0, k == 31, R=[w_r, uT_r[k]], W=[pr])
                        act(tmpo[:, ct, :], pb[:], AF.Copy, R=[pr], W=[tmpo_r[ct]])
                        s = sq[ct % 2]
                        act(s[:], pb[:], AF.Square, R=[pr], W=[sq_res[ct % 2]])
                        mm(pss[:], ones_b, s[:], ct == 0, ct == KC - 1, R=[sq_res[ct % 2], r_const], W=[pss_r])
                    rmsnorm_rstd(pss[:], pss_r, D, rstd[:], rstd_res)
                    for ct in range(KC):
                        stt_("dve", t2[:], tmpo[:, ct, :], vcol(24 + ct), rstd[:], ALU.mult, ALU.mult, R=[tmpo_r[ct], rstd_res, r_const], W=[t2_r])
                        tt_("pool", xT[:, ct, tsl], xT[:, ct, tsl], t2[:], ALU.add, R=[xr[ct][tt], t2_r], W=[xr[ct][tt]])

        k_out = kb.dsem("out")
        kb.dma("sp", [(outT_d[kc * 128:(kc + 1) * 128, :], xT[:, kc, :]) for kc in range(KC)], k_out,
               R=[xr[kc][tt] for kc in range(KC) for tt in range(NT)])
        nc.sync.wait_ge(kb.sems[k_out], kb.cnt[k_out])
        for name, key in tap_out.items():
            nc.sync.wait_ge(kb.sems[key], kb.cnt[key])
        print(f"[build] ops={kb.nops} waits={kb.nwaits} counts={ {k: v for k, v in kb.cnt.items() if not k.startswith('d_')} }", flush=True)
    return nc


def pack_small(inputs, l0, nl):
    vecs = np.zeros((128, nl * NVEC), np.float32)
    hv = np.zeros((1, nl * 8), np.float32)
    w2 = np.zeros((16, nl * 512), np.float32)
    for i in range(nl):
        l = l0 + i
        V0 = i * NVEC
        for j, name in enumerate(("norm_mix_pre", "norm_mix_post", "norm_mlp_pre", "norm_mlp_post")):
            vecs[:, V0 + j * 8:V0 + (j + 1) * 8] = np.asarray(inputs[name][l]).reshape(8, 128).T
        cw = np.asarray(inputs["conv_w"][l])
        for tp in range(4):
            vecs[:, V0 + 32 + tp * 12:V0 + 32 + (tp + 1) * 12] = cw[tp].reshape(12, 128).T
        vecs[:, V0 + 80] = np.asarray(inputs["gdn_norm"][l])
        vecs[:, V0 + 81:V0 + 83] = np.asarray(inputs["gla_norm"][l]).reshape(2, 128).T
        vecs[:, V0 + 83:V0 + 87] = np.asarray(inputs["gla_gate_b"][l]).reshape(4, 128).T
        hv[0, i * 8:i * 8 + 4] = np.asarray(inputs["a_log"][l])
        hv[0, i * 8 + 4:i * 8 + 8] = np.asarray(inputs["dt_bias"][l])
        w2[:, i * 512:(i + 1) * 512] = np.asarray(inputs["gla_gate_w2"][l])
    return vecs, hv, w2


_PROG = {}


def run_layers(xT_list, inputs, l0, nl):
    if nl not in _PROG:
        _PROG[nl] = build_program(nl)
    nc = _PROG[nl]
    vecs, hv, w2 = pack_small(inputs, l0, nl)
    consts = make_consts()
    sl = slice(l0, l0 + nl)
    shared = {
        "w_in": np.ascontiguousarray(inputs["w_in"][sl]), "w_out_a": np.ascontiguousarray(inputs["w_out_a"][sl]),
        "w_out_b": np.ascontiguousarray(inputs["w_out_b"][sl]), "w_o": np.ascontiguousarray(inputs["w_o"][sl]),
        "w_mlp_up": np.ascontiguousarray(inputs["w_mlp_up"][sl]), "w_mlp_down": np.ascontiguousarray(inputs["w_mlp_down"][sl]),
        "gate_w2": w2, "vecs": vecs, "hv": hv, "consts": consts,
    }
    in_maps = [dict(shared, xT=xT_list[c]) for c in range(len(xT_list))]
    res = run_bass_kernel_spmd(nc, in_maps, core_ids=list(range(len(xT_list))))
    return [np.asarray(r["outT"]) for r in res.results]


GDN_STOP = 0
N_FUSED = 1


def kernel(**inputs):
    inputs = {k: np.asarray(v) for k, v in inputs.items()}
    x = inputs["x"].astype(np.float32, copy=False)
    xT = [np.ascontiguousarray(x[b].T) for b in range(x.shape[0])]
    for l0 in range(0, L, N_FUSED):
        xT = run_layers(xT, inputs, l0, N_FUSED)
    out = np.stack([t.T for t in xT], axis=0)
    return np.ascontiguousarray(out.astype(np.float32))
```

```python
import numpy as np
from contextlib import ExitStack
import concourse.bass as bass
import concourse.mybir as mybir
from concourse.bass_utils import run_bass_kernel_spmd

F32 = mybir.dt.float32
BF16 = mybir.dt.bfloat16
AF = mybir.ActivationFunctionType
ALU = mybir.AluOpType

D = 1024
T = 2048
L = 4
DFF = 4096
NCOL = 7192
NT = 4
TT = 512
KC = 8
EPS = 1e-6
O_AQ, O_AK, O_AV, O_AZ, O_AB, O_AA = 0, 512, 1024, 1536, 2048, 2052
O_BQ, O_BK, O_BV, O_BR, O_GLR, O_GA, O_GB = 2056, 2568, 3080, 4104, 5128, 5144, 6168
GDN_STOP = 0
GDN_REC = 0
NVEC = 87
C_ID, C_ONES, C_NEG, C_MN_INC_T, C_MN_STR_T, C_MN_STR, C_C01_T = 0, 1, 2, 3, 4, 5, 6
C_LV = 7
C_LVT = 14
NCB = 21


def make_consts():
    c = np.zeros((NCB, 128, 128), np.float32)
    i = np.arange(128)[:, None]
    j = np.arange(128)[None, :]
    c[C_ID] = (i == j)
    c[C_ONES] = 1.0
    c[C_NEG] = -1.0
    c[C_MN_INC_T] = np.where(j >= i, 0.0, -1e30)
    c[C_MN_STR_T] = np.where(j > i, 0.0, -1e30)
    c[C_MN_STR] = np.where(i > j, 0.0, -1e30)
    c[C_C01_T] = (j >= i)
    for li in range(7):
        m = 1 << li
        mm = ((i // (2 * m)) == (j // (2 * m))) & ((i % (2 * m)) >= m) & ((j % (2 * m)) < m)
        c[C_LV + li] = mm
        c[C_LVT + li] = mm.T
    return np.ascontiguousarray(c.transpose(1, 0, 2).reshape(128, NCB * 128))


class Res:
    __slots__ = ("w", "rs", "p")

    def __init__(self):
        self.w = None
        self.rs = {}
        self.p = set()


class KB:
    def __init__(self, nc, es):
        self.nc = nc
        self.es = es
        self.eng = {"pe": nc.tensor, "dve": nc.vector, "act": nc.scalar, "pool": nc.gpsimd, "sp": nc.sync}
        self.sems = {}
        self.cnt = {}
        self.seen = {e: {} for e in self.eng}
        self.pend = {e: [] for e in self.eng}
        for e in ("pe", "dve", "act", "pool"):
            self.sems[e] = es.enter_context(nc.semaphore("s_" + e))
            self.cnt[e] = 0
        self.nops = 0
        self.nwaits = 0

    def dsem(self, name):
        key = "d_" + name
        self.sems[key] = self.es.enter_context(self.nc.semaphore(key))
        self.cnt[key] = 0
        return key

    def flush(self, e):
        if not self.pend[e]:
            return
        self.cnt[e] += 1
        self.pend[e][-1][0].then_inc(self.sems[e], 1)
        ev = (e, self.cnt[e])
        for (_, r2, w2) in self.pend[e]:
            for r in list(r2) + list(w2):
                r.p.discard(e)
            self._reg(ev, r2, w2)
        self.pend[e] = []

    def barrier(self):
        for e in self.eng:
            self.flush(e)
        for e in self.eng:
            for k, v in self.cnt.items():
                if v > 0 and self.seen[e].get(k, 0) < v:
                    self.eng[e].wait_ge(self.sems[k], v)
                    self.seen[e][k] = v
                    self.nwaits += 1

    def _deps(self, e, R, W):
        for r in list(R) + list(W):
            for e2 in list(r.p):
                if e2 != e:
                    self.flush(e2)
        deps = {}

        def add(ev):
            k, v = ev
            if deps.get(k, 0) < v:
                deps[k] = v
        for r in R:
            if r.w is not None:
                add(r.w)
        for w in W:
            if w.w is not None:
                add(w.w)
            for k, v in w.rs.items():
                add((k, v))
        for k, v in deps.items():
            if k == e and e == "pe":
                continue
            if self.seen[e].get(k, 0) >= v:
                continue
            self.eng[e].wait_ge(self.sems[k], v)
            self.seen[e][k] = v
            self.nwaits += 1

    def _reg(self, ev, R, W):
        k, v = ev
        for w in W:
            w.w = ev
            w.rs = {}
        for r in R:
            if r.rs.get(k, 0) < v:
                r.rs[k] = v

    def op(self, e, fn, R=(), W=(), inc=True):
        self._deps(e, R, W)
        inst = fn()
        self.nops += 1
        if not inc:
            self.pend[e].append((inst, R, W))
            for r in list(R) + list(W):
                r.p.add(e)
            return
        self.cnt[e] += 1
        inst.then_inc(self.sems[e], 1)
        ev = (e, self.cnt[e])
        for (_, r2, w2) in self.pend[e]:
            for r in list(r2) + list(w2):
                r.p.discard(e)
            self._reg(ev, r2, w2)
        self.pend[e] = []
        self._reg(ev, R, W)

    def dma(self, e, pairs, key, R=(), W=()):
        self._deps(e, R, W)
        for (o, i) in pairs:
            inst = self.eng[e].dma_start(out=o, in_=i)
            inst.then_inc(self.sems[key], 16)
            self.cnt[key] += 16
            self.nops += 1
        ev = (key, self.cnt[key])
        self._reg(ev, R, W)


def build_program(n_layers, taps=(), phases=("gdn", "gla", "merge", "mlp")):
    nc = bass.Bass("TRN2", target_bir_lowering=False)
    NL = n_layers
    dt = lambda name, shape, kind="ExternalInput": nc.dram_tensor(name, shape, F32, kind=kind).ap()
    xT_d = dt("xT", [D, T])
    w_in_d = dt("w_in", [NL, D, NCOL])
    w_oa_d = dt("w_out_a", [NL, 512, D])
    w_ob_d = dt("w_out_b", [NL, 1024, D])
    w_o_d = dt("w_o", [NL, D, D])
    w_up_d = dt("w_mlp_up", [NL, D, DFF])
    w_dn_d = dt("w_mlp_down", [NL, DFF, D])
    w2_d = dt("gate_w2", [16, NL * 512])
    vecs_d = dt("vecs", [128, NL * NVEC])
    hv_d = dt("hv", [1, NL * 8])
    consts_d = dt("consts", [128, NCB * 128])
    outT_d = dt("outT", [D, T], kind="ExternalOutput")
    tap_out = {}

    with ExitStack() as es:
        kb = KB(nc, es)
        PE, DVE, ACT, POOL = nc.tensor, nc.vector, nc.scalar, nc.gpsimd

        uid = {"n": 0}

        def sb(name, shape, dtype=F32, stack=es):
            uid["n"] += 1
            return stack.enter_context(nc.sbuf_tensor(f"s{uid['n']}_{name}", shape, dtype))

        NPB = 5
        pbanks = [es.enter_context(nc.psum_tensor(f"pb{i}", [128, 512], F32)) for i in range(NPB)]
        pres = [Res() for _ in range(NPB)]
        pacc = es.enter_context(nc.psum_tensor("pacc", [128, 512], F32))
        pacc_r = Res()
        pbf = [es.enter_context(nc.psum_tensor(f"pbf{i}", [128, 1024], BF16)) for i in range(2)]
        pbf_res = [Res(), Res()]
        st = {"pi": 0, "bi": 0}

        def ps():
            i = st["pi"]
            st["pi"] = (i + 1) % NPB
            return pbanks[i], pres[i]

        def psb():
            i = st["bi"]
            st["bi"] = (i + 1) % 2
            return pbf[i][:, 0:512], pbf_res[i]

        xT = sb("xT", [128, KC, T])
        xr = [[Res() for _ in range(NT)] for _ in range(KC)]
        cb = sb("cb", [128, NCB * 128], BF16)
        cf = sb("cf", [128, 2 * 128])
        hv = sb("hv", [1, NL * 8])
        r_const = Res()

        def CB(i):
            return cb[:, i * 128:(i + 1) * 128]
        onesf = cf[:, 0:128]
        negf = cf[:, 128:256]
        ident_b = CB(C_ID)
        ones_b = CB(C_ONES)

        k_in = kb.dsem("in")
        kb.dma("sp", [(xT[:, kc, :], xT_d[kc * 128:(kc + 1) * 128, :]) for kc in range(KC)], k_in,
               W=[xr[kc][tt] for kc in range(KC) for tt in range(NT)])
        k_c = kb.dsem("c")
        kb.dma("pool", [(cb[:], consts_d)], k_c, W=[r_const])
        k_c2 = kb.dsem("c2")
        kb.dma("sp", [(cf[:], consts_d[:, 128:384]), (hv[:], hv_d)], k_c2, W=[r_const])
        k_vec = kb.dsem("vec")

        def tap(name, ap, shape, R):
            if name not in taps:
                return
            d = nc.dram_tensor("tap_" + name, shape, F32, kind="ExternalOutput").ap()
            key = kb.dsem("t_" + name)
            tap_out[name] = key
            kb.dma("pool", [(d, ap)], key, R=R)

        def mm(out, lhsT, rhs, start, stop, R, W):
            kb.op("pe", lambda: PE.matmul(out, lhsT, rhs, start=start, stop=stop), R=R, W=W, inc=stop)

        def act(out, in_, func, R, W, bias=None, scale=None):
            kw = {}
            if bias is not None:
                kw["bias"] = bias
            if scale is not None:
                kw["scale"] = scale
            kb.op("act", lambda: ACT.activation(out=out, in_=in_, func=func, **kw), R=R, W=W)

        def amul(out, in_, c, R, W):
            kb.op("act", lambda: ACT.mul(out, in_, c), R=R, W=W)

        def tt_(e, out, in0, in1, op, R, W):
            eng = DVE if e == "dve" else POOL
            kb.op(e, lambda: eng.tensor_tensor(out, in0, in1, op), R=R, W=W)

        def ts_(e, out, in0, s1, s2, op0, op1, R, W):
            eng = DVE if e == "dve" else POOL
            if op1 is None and e == "pool" and op0 in (ALU.mult, ALU.add):
                o1, c2 = (ALU.add, 0.0) if op0 == ALU.mult else (ALU.mult, 1.0)
                kb.op(e, lambda: eng.tensor_scalar(out, in0, s1, c2, op0, o1), R=R, W=W)
            elif op1 is None:
                kb.op(e, lambda: eng.tensor_scalar(out, in0, s1, None, op0), R=R, W=W)
            else:
                kb.op(e, lambda: eng.tensor_scalar(out, in0, s1, s2, op0, op1), R=R, W=W)

        def stt_(e, out, in0, s, in1, op0, op1, R, W):
            eng = DVE if e == "dve" else POOL
            kb.op(e, lambda: eng.scalar_tensor_tensor(out=out, in0=in0, scalar=s, in1=in1, op0=op0, op1=op1), R=R, W=W)

        def b4(ap):
            return ap.unsqueeze(1).to_broadcast([ap.shape[0], 4, 128])

        def v4(ap):
            return ap.rearrange("p (c k) -> p c k", k=128)

        def rmsnorm_rstd(ps_ap, ps_res, n, rstd_ap, rstd_res):
            act(rstd_ap, ps_ap, AF.Ln, R=[ps_res], W=[rstd_res], bias=EPS, scale=1.0 / n)
            act(rstd_ap, rstd_ap, AF.Exp, R=[rstd_res], W=[rstd_res], scale=-0.5)

        def norm_tile(src_fn, src_res_fn, wcol_fn, dst_fn, dst_res_fn, sq, sq_res, rstd, rstd_res):
            pb, pr = ps()
            for kc in range(KC):
                s = sq[kc % 2]
                act(s[:], src_fn(kc), AF.Square, R=[src_res_fn(kc)], W=[sq_res[kc % 2]])
                mm(pb[:], ones_b, s[:], kc == 0, kc == KC - 1, R=[sq_res[kc % 2], r_const], W=[pr])
            rmsnorm_rstd(pb[:], pr, D, rstd[:], rstd_res)
            for kc in range(KC):
                stt_("dve", dst_fn(kc), src_fn(kc), wcol_fn(kc), rstd[:], ALU.mult, ALU.mult,
                     R=[src_res_fn(kc), rstd_res, r_const], W=[dst_res_fn(kc)])

        def silu_from(e_eng, out_ap, x_ap, tmp_ap, R, W, tmp_res):
            act(tmp_ap, x_ap, AF.Exp, R=R, W=[tmp_res], scale=-1.0)
            act(tmp_ap, tmp_ap, AF.Ln, R=[tmp_res], W=[tmp_res], bias=1.0)
            act(tmp_ap, tmp_ap, AF.Exp, R=[tmp_res], W=[tmp_res], scale=-1.0)
            tt_(e_eng, out_ap, x_ap, tmp_ap, ALU.mult, R=list(R) + [tmp_res], W=W)

        wslot_key = [kb.dsem("ws0"), kb.dsem("ws1")]
        hw = {"n": 0, "slots": None, "res": None}

        def load_head_weights(l, kind, h):
            i = hw["n"] % 2
            hw["n"] += 1
            w = hw["slots"][i]
            if kind == "gdn":
                cols = [(O_AQ + h * 128, 128, 0), (O_AK + h * 128, 128, 128), (O_AV + h * 128, 128, 256), (O_AZ + h * 128, 128, 384)]
            else:
                cols = [(O_BQ + h * 128, 128, 0), (O_BK + h * 128, 128, 128), (O_BV + h * 256, 256, 256), (O_BR + h * 256, 256, 512)]
            pairs = [(w[:, :, o:o + n], w_in_d[l, :, c0:c0 + n].rearrange("(kc p) c -> p kc c", p=128)) for (c0, n, o) in cols]
            kb.dma("pool", pairs, wslot_key[i], W=[hw["res"][i]])
            return w, hw["res"][i]

        k_wsm = kb.dsem("wsm")
        wm_k = [kb.dsem(f"wm{i}") for i in range(2)]
        wo_k = [kb.dsem(f"wo{i}") for i in range(2)]
        wu_k = [kb.dsem(f"wu{i}") for i in range(3)]
        wd_k = [kb.dsem(f"wd{i}") for i in range(3)]

        for l in range(NL):
            with ExitStack() as ls:
              vecs = sb("vecs", [128, NVEC], F32, ls)
              kb.barrier()
              kb.dma("sp", [(vecs[:], vecs_d[:, l * NVEC:(l + 1) * NVEC])], k_vec, W=[r_const])

              def vcol(off, n=1, vecs=vecs):
                  return vecs[:, off:off + n]
              with ExitStack() as ms:
                  hT = sb("hT", [128, KC, T], BF16, ms)
                  hr = [[Res() for _ in range(NT)] for _ in range(KC)]
                  oTa = sb("oTa", [128, 4, T], BF16, ms)
                  oTa_r = [[Res() for _ in range(NT)] for _ in range(4)]
                  oTb = sb("oTb", [128, 8, T], BF16, ms)
                  oTb_r = [[Res() for _ in range(NT)] for _ in range(8)]
                  wsm = sb("wsm", [128, KC, 24], BF16, ms)
                  w2b = sb("w2b", [16, 512], BF16, ms)
                  r_wsm = Res()
                  with nc.allow_non_contiguous_dma(reason="small gate columns"):
                      kb.dma("pool", [(wsm[:, :, 0:8], w_in_d[l, :, O_AB:O_AB + 8].rearrange("(kc p) c -> p kc c", p=128)),
                                      (wsm[:, :, 8:24], w_in_d[l, :, O_GLR:O_GLR + 16].rearrange("(kc p) c -> p kc c", p=128)),
                                      (w2b[:], w2_d[:, l * 512:(l + 1) * 512])], k_wsm, W=[r_wsm])
                  with ExitStack() as s1:
                      sq = [sb(f"sq{i}", [128, TT], BF16, s1) for i in range(2)]
                      sq_res = [Res(), Res()]
                      rstd = sb("rstd", [128, TT], F32, s1)
                      rstd_res = Res()
                      for tt in range(NT):
                          tsl = slice(tt * TT, (tt + 1) * TT)
                          norm_tile(lambda kc: xT[:, kc, tsl], lambda kc: xr[kc][tt], lambda kc: vcol(kc),
                                    lambda kc: hT[:, kc, tsl], lambda kc: hr[kc][tt], sq, sq_res, rstd, rstd_res)
                  if l == 0:
                      tap("hT", hT[:, 0, :], [128, T], [hr[0][t_] for t_ in range(NT)])

                  with ExitStack() as gs:
                      kb.barrier()
                      S = lambda n, shp, d=F32: sb(n, shp, d, gs)
                      hw["slots"] = [S(f"wsg{i}", [128, KC, 512], BF16) for i in range(2)]
                      hw["res"] = [Res(), Res()]
                      nxt = load_head_weights(l, "gdn", 0)
                      raw1 = S("raw1", [128, 3 + TT]); raw1_r = Res()
                      halo = S("halo", [128, 3, 3]); halo_r = [Res() for _ in range(3)]
                      cacc = S("cacc", [128, TT]); cacc_r = Res()
                      tmpf = S("tmpf", [128, TT]); tmpf_r = Res()
                      tmpg = S("tmpg", [128, TT]); tmpg_r = Res()
                      qT = S("qT", [128, TT], BF16); qT_r = Res()
                      kT = S("kT", [128, TT], BF16); kT_r = Res()
                      vT = S("vT", [128, TT], BF16); vT_r = Res()
                      zs = S("zs", [128, TT], BF16); zs_r = Res()
                      vb = S("vb", [128, TT], BF16); vb_r = Res()
                      kbg = S("kbg", [128, TT], BF16); kbg_r = Res()
                      kdc = S("kdc", [128, TT], BF16); kdc_r = Res()
                      Am = S("Am", [128, TT], BF16); Am_r = Res()
                      AT = S("AT", [128, TT], BF16); AT_r = Res()
                      Om = S("Om", [128, TT], BF16); Om_r = Res()
                      OT = S("OT", [128, TT], BF16); OT_r = Res()
                      Zp, Zp_r = OT, OT_r
                      Zt, Zt_r = Om, Om_r
                      Inv = S("Inv", [128, TT], BF16); Inv_r = Res()
                      Rm = S("Rm", [128, TT], BF16); Rm_r = Res()
                      qkm = S("qkm", [128, TT], BF16); qkm_r = Res()
                      qd = S("qd", [128, TT], BF16); qd_r = Res()
                      nwt, nwt_r = vT, vT_r
                      oraw = S("oraw", [128, TT]); oraw_r = Res()
                      sqg, sqg_r = Om, Om_r
                      vnew = [S(f"vnew{i}", [128, 128], BF16) for i in range(2)]
                      vnew_r = [Res(), Res()]
                      Sst = S("Sst", [128, 128]); Sst_r = Res()
                      Sbf = S("Sbf", [128, 128], BF16); Sbf_r = Res()
                      cols = S("cols", [128, 16]); cols_r = Res()
                      rowA = S("rowA", [1, TT]); rowA_r = Res()
                      rowB = S("rowB", [1, TT]); rowB_r = Res()
                      Grow = [S(f"Grow{i}", [1, 1 + TT]) for i in range(2)]
                      Grow_r = [Res(), Res()]
                      rGb = S("rGb", [1, TT]); rGb_r = Res()
                      rkb = S("rkb", [1, TT]); rkb_r = Res()
                      rcd = S("rcd", [1, 8]); rcd_r = Res()
                      nA = S("nA", [1, 4]); nA_r = Res()
                      act(nA[:], hv[0:1, l * 8:l * 8 + 4], AF.Exp, R=[r_const], W=[nA_r])
                      ts_("dve", nA[:], nA[:], -1.0, None, ALU.mult, None, R=[nA_r], W=[nA_r])

                      for h in (range(4) if "gdn" in phases else ()):
                          W_, W_r = nxt
                          nxt = load_head_weights(l, "gdn", h + 1) if h < 3 else None
                          kb.op("dve", lambda: DVE.memset(Sst[:], 0.0), W=[Sst_r])
                          kb.op("dve", lambda: DVE.memset(Sbf[:], 0.0), W=[Sbf_r])
                          for i in range(3):
                              kb.op("pool", lambda i=i: POOL.memset(halo[:, i, :], 0.0), W=[halo_r[i]])
                          for tt in range(NT):
                              tsl = slice(tt * TT, (tt + 1) * TT)
                              hres = [hr[kc][tt] for kc in range(KC)]
                              Gc_, Gc_r = Grow[tt % 2], Grow_r[tt % 2]
                              Gp_, Gp_r = Grow[(tt + 1) % 2], Grow_r[(tt + 1) % 2]
                              pb, pr = ps()
                              for kc in range(KC):
                                  mm(pb[0:1, :], wsm[:, kc, h:h + 1], hT[:, kc, tsl], kc == 0, kc == KC - 1, R=[r_wsm, hres[kc]], W=[pr])
                              act(rowA[:], pb[0:1, :], AF.Exp, R=[pr], W=[rowA_r], scale=-1.0)
                              act(rowA[:], rowA[:], AF.Ln, R=[rowA_r], W=[rowA_r], bias=1.0)
                              pb, pr = ps()
                              for kc in range(KC):
                                  mm(pb[0:1, :], wsm[:, kc, 4 + h:5 + h], hT[:, kc, tsl], kc == 0, kc == KC - 1, R=[r_wsm, hres[kc]], W=[pr])
                              act(rowB[:], pb[0:1, :], AF.Exp, R=[pr, r_const], W=[rowB_r], bias=hv[0:1, l * 8 + 4 + h:l * 8 + 5 + h])
                              act(rowB[:], rowB[:], AF.Ln, R=[rowB_r], W=[rowB_r], bias=1.0)
                              ts_("dve", rowB[:], rowB[:], nA[0:1, h:h + 1], None, ALU.mult, None, R=[rowB_r, nA_r], W=[rowB_r])
                              if tt == 0:
                                  kb.op("dve", lambda: DVE.memset(Gc_[:, 0:1], 0.0), W=[Gc_r])
                              else:
                                  kb.op("dve", lambda: DVE.tensor_copy(Gc_[:, 0:1], Gp_[:, TT:TT + 1]), R=[Gp_r], W=[Gc_r])
                              kb.op("dve", lambda: DVE.tensor_tensor_scan(Gc_[:, 1:1 + TT], onesf[0:1, 0:1].to_broadcast([1, TT]), rowB[:], Gc_[:, 0:1], ALU.mult, ALU.add),
                                    R=[r_const, rowB_r, Gc_r], W=[Gc_r])
                              Gv = Gc_[:, 1:1 + TT]
                              Gst4 = Gc_[:, 0:TT:128].unsqueeze(2).to_broadcast([1, 4, 128])
                              Gla4 = Gc_[:, 128:TT + 1:128].unsqueeze(2).to_broadcast([1, 4, 128])
                              tt_("dve", rGb[:], Gv, rowA[:], ALU.subtract, R=[Gc_r, rowA_r], W=[rGb_r])
                              tt_("dve", v4(rkb[:]), v4(rGb[:]), Gst4, ALU.subtract, R=[rGb_r, Gc_r], W=[rkb_r])
                              act(rkb[:], rkb[:], AF.Exp, R=[rkb_r], W=[rkb_r])
                              rkd, rkd_r = rowB, rowB_r
                              rbe, rbe_r = rowA, rowA_r
                              tt_("dve", v4(rkd[:]), v4(Gv), Gla4, ALU.subtract, R=[Gc_r], W=[rkd_r])
                              act(rkd[:], rkd[:], AF.Exp, R=[rkd_r], W=[rkd_r], scale=-1.0)
                              act(rbe[:], rowA[:], AF.Exp, R=[rowA_r], W=[rbe_r], scale=-1.0)
                              tt_("dve", rcd[:, 0:4], Gc_[:, 128:TT + 1:128], Gc_[:, 0:TT:128], ALU.subtract, R=[Gc_r], W=[rcd_r])
                              act(rcd[:, 0:4], rcd[:, 0:4], AF.Exp, R=[rcd_r], W=[rcd_r])
                              pcol, pcol_r = ps()
                              for cc in range(4):
                                  csl = slice(cc * 128, (cc + 1) * 128)
                                  for qi, (rw, rw_r) in enumerate(((rkb, rkb_r), (rkd, rkd_r), (rbe, rbe_r))):
                                      kb.op("pe", lambda rw=rw, qi=qi: PE.matmul(pcol[:, cc * 4 + qi:cc * 4 + qi + 1], rw[0:1, csl], onesf[0:1, 0:1], start=True, stop=True),
                                            R=[rw_r, r_const], W=[pcol_r], inc=False)
                                  kb.op("pe", lambda: PE.matmul(pcol[:, cc * 4 + 3:cc * 4 + 4], onesf[0:1, 0:128], rcd[0:1, cc:cc + 1], start=True, stop=True),
                                        R=[rcd_r, r_const], W=[pcol_r], inc=(cc == 3))
                              kb.op("dve", lambda: DVE.tensor_copy(cols[:], pcol[:, 0:16]), R=[pcol_r], W=[cols_r])
                              colv = cols[:].rearrange("p (c q) -> p c q", q=4)

                              if GDN_STOP == 1:
                                  continue
                              for xi, (dst, dst_r) in enumerate(((qT, qT_r), (kT, kT_r), (vT, vT_r))):
                                  pb, pr = ps()
                                  for kc in range(KC):
                                      mm(pb[:], W_[:, kc, xi * 128:(xi + 1) * 128], hT[:, kc, tsl], kc == 0, kc == KC - 1, R=[W_r, hres[kc]], W=[pr])
                                  rw, rw_r = raw1, raw1_r
                                  kb.op("pool", lambda xi=xi: POOL.tensor_copy(rw[:, 0:3], halo[:, xi, :]), R=[halo_r[xi]], W=[rw_r])
                                  act(rw[:, 3:3 + TT], pb[:], AF.Copy, R=[pr], W=[rw_r])
                                  cw = lambda tap_, xi=xi: vcol(32 + tap_ * 12 + xi * 4 + h)
                                  ts_("dve", cacc[:], rw[:, 0:TT], cw(0), None, ALU.mult, None, R=[rw_r, r_const], W=[cacc_r])
                                  for tp in (1, 2, 3):
                                      stt_("dve", cacc[:], rw[:, tp:tp + TT], cw(tp), cacc[:], ALU.mult, ALU.add, R=[rw_r, r_const, cacc_r], W=[cacc_r])
                                  kb.op("pool", lambda xi=xi: POOL.tensor_copy(halo[:, xi, :], rw[:, TT:TT + 3]), R=[rw_r], W=[halo_r[xi]])
                                  if xi == 2:
                                      silu_from("dve", vT[:], cacc[:], tmpf[:], R=[cacc_r], W=[vT_r], tmp_res=tmpf_r)
                                  else:
                                      silu_from("dve", tmpg[:], cacc[:], tmpf[:], R=[cacc_r], W=[tmpg_r], tmp_res=tmpf_r)
                                      act(sqg[:], tmpg[:], AF.Square, R=[tmpg_r], W=[sqg_r])
                                      pb2, pr2 = ps()
                                      mm(pb2[:], ones_b, sqg[:], True, True, R=[sqg_r, r_const], W=[pr2])
                                      act(tmpf[:], pb2[:], AF.Ln, R=[pr2], W=[tmpf_r], bias=EPS)
                                      act(tmpf[:], tmpf[:], AF.Exp, R=[tmpf_r], W=[tmpf_r], scale=-0.5)
                                      sc = (128.0 ** -0.5) if xi == 0 else 1.0
                                      stt_("dve", dst[:], tmpg[:], sc, tmpf[:], ALU.mult, ALU.mult, R=[tmpg_r, tmpf_r], W=[dst_r])
                              pb, pr = ps()
                              for kc in range(KC):
                                  mm(pb[:], W_[:, kc, 384:512], hT[:, kc, tsl], kc == 0, kc == KC - 1, R=[W_r, hres[kc]], W=[pr])
                              silu_from("dve", zs[:], pb[:], tmpf[:], R=[pr], W=[zs_r], tmp_res=tmpf_r)

                              if GDN_STOP == 2:
                                  continue
                              pt, ptr = psb()
                              for cc in range(4):
                                  csl = slice(cc * 128, (cc + 1) * 128)
                                  kb.op("pe", lambda: PE.transpose(pt[:, csl], vT[:, csl], ident_b), R=[vT_r, r_const], W=[ptr], inc=(cc == 3))
                              tt_("dve", v4(vb[:]), v4(pt), colv[:, :, 2:3].to_broadcast([128, 4, 128]), ALU.mult, R=[ptr, cols_r], W=[vb_r])
                              pt, ptr = psb()
                              for cc in range(4):
                                  csl = slice(cc * 128, (cc + 1) * 128)
                                  kb.op("pe", lambda: PE.transpose(pt[:, csl], kT[:, csl], ident_b), R=[kT_r, r_const], W=[ptr], inc=(cc == 3))
                              tt_("dve", v4(kbg[:]), v4(pt), colv[:, :, 0:1].to_broadcast([128, 4, 128]), ALU.mult, R=[ptr, cols_r], W=[kbg_r])
                              tt_("dve", v4(kdc[:]), v4(pt), colv[:, :, 1:2].to_broadcast([128, 4, 128]), ALU.mult, R=[ptr, cols_r], W=[kdc_r])

                              if GDN_STOP == 3:
                                  continue
                              def expo(lrow, lrow_r, lneg, rrow, rrow_r, rneg, mask_idx):
                                  pb_, pr_ = ps()
                                  for cc in range(4):
                                      csl = slice(cc * 128, (cc + 1) * 128)
                                      kb.op("pe", lambda: PE.matmul(pb_[:, csl], (negf if rneg else onesf)[0:1, 0:128], rrow[0:1, csl], start=True, stop=False),
                                            R=[rrow_r, r_const], W=[pr_], inc=False)
                                      kb.op("pe", lambda: PE.matmul(pb_[:, csl], lrow[0:1, csl], (negf if lneg else onesf)[0:1, 0:128], start=False, stop=False),
                                            R=[lrow_r, r_const], W=[pr_], inc=False)
                                      kb.op("pe", lambda: PE.matmul(pb_[:, csl], ident_b, CB(mask_idx), start=False, stop=True),
                                            R=[r_const], W=[pr_], inc=(cc == 3))
                                  return pb_, pr_
                              pkk, pkk_r = ps()
                              for cc in range(4):
                                  csl = slice(cc * 128, (cc + 1) * 128)
                                  mm(pkk[:, csl], kT[:, csl], kT[:, csl], True, True, R=[kT_r], W=[pkk_r])
                              pe1, pe1_r = expo(Gv, Gc_r, True, rGb, rGb_r, False, C_MN_STR_T)
                              act(tmpf[:], pe1[:], AF.Exp, R=[pe1_r], W=[tmpf_r])
                              tt_("dve", AT[:], pkk[:], tmpf[:], ALU.mult, R=[pkk_r, tmpf_r], W=[AT_r])
                              pe2, pe2_r = expo(rGb, rGb_r, False, Gv, Gc_r, True, C_MN_STR)
                              act(tmpg[:], pe2[:], AF.Exp, R=[pe2_r], W=[tmpg_r])
                              tt_("dve", Am[:], pkk[:], tmpg[:], ALU.mult, R=[pkk_r, tmpg_r], W=[Am_r])
                              pe3, pe3_r = expo(Gv, Gc_r, True, Gv, Gc_r, False, C_MN_INC_T)
                              act(tmpf[:], pe3[:], AF.Exp, R=[pe3_r], W=[tmpf_r])
                              pqk, pqk_r = ps()
                              for cc in range(4):
                                  csl = slice(cc * 128, (cc + 1) * 128)
                                  mm(pqk[:, csl], kT[:, csl], qT[:, csl], True, True, R=[kT_r, qT_r], W=[pqk_r])
                              tt_("dve", qkm[:], pqk[:], tmpf[:], ALU.mult, R=[pqk_r, tmpf_r], W=[qkm_r])
                              rGc, rGc_r = rkb, rkb_r
                              tt_("dve", v4(rGc[:]), v4(Gv), Gst4, ALU.subtract, R=[Gc_r], W=[rGc_r])
                              pg, pg_r = ps()
                              for cc in range(4):
                                  csl = slice(cc * 128, (cc + 1) * 128)
                                  kb.op("pe", lambda: PE.matmul(pg[:, csl], onesf[0:1, 0:128], rGc[0:1, csl], start=True, stop=True),
                                        R=[rGc_r, r_const], W=[pg_r], inc=(cc == 3))
                              act(tmpg[:], pg[:], AF.Exp, R=[pg_r], W=[tmpg_r])
                              tt_("dve", qd[:], qT[:], tmpg[:], ALU.mult, R=[qT_r, tmpg_r], W=[qd_r])

                              if GDN_STOP == 4:
                                  continue
                              tt_("dve", v4(Om[:]), v4(Am[:]), b4(CB(C_LV + 0)), ALU.mult, R=[Am_r, r_const], W=[Om_r])
                              stt_("dve", v4(Inv[:]), v4(Om[:]), -1.0, b4(ident_b), ALU.mult, ALU.add, R=[Om_r, r_const], W=[Inv_r])
                              tt_("dve", v4(OT[:]), v4(AT[:]), b4(CB(C_LVT + 0)), ALU.mult, R=[AT_r, r_const], W=[OT_r])
                              stt_("dve", v4(Rm[:]), v4(OT[:]), -1.0, b4(ident_b), ALU.mult, ALU.add, R=[OT_r, r_const], W=[Rm_r])
                              for li in range(1, 7):
                                  last = (li == 6)
                                  pz, pz_r = ps()
                                  for cc in range(4):
                                      csl = slice(cc * 128, (cc + 1) * 128)
                                      mm(pz[:, csl], Am[:, csl], Rm[:, csl], True, True, R=[Am_r, Rm_r], W=[pz_r])
                                  if not last:
                                      pzp, pzp_r = ps()
                                      for cc in range(4):
                                          csl = slice(cc * 128, (cc + 1) * 128)
                                          mm(pzp[:, csl], AT[:, csl], Inv[:, csl], True, True, R=[AT_r, Inv_r], W=[pzp_r])
                                  tt_("dve", v4(Zt[:]), v4(pz[:]), b4(CB(C_LVT + li)), ALU.mult, R=[pz_r, r_const], W=[Zt_r])
                                  if not last:
                                      tt_("dve", v4(Zp[:]), v4(pzp[:]), b4(CB(C_LV + li)), ALU.mult, R=[pzp_r, r_const], W=[Zp_r])
                                  pr2_, pr2_r = ps()
                                  for cc in range(4):
                                      csl = slice(cc * 128, (cc + 1) * 128)
                                      mm(pr2_[:, csl], Inv[:, csl], Zt[:, csl], True, True, R=[Inv_r, Zt_r], W=[pr2_r])
                                  if not last:
                                      pi2_, pi2_r = ps()
                                      for cc in range(4):
                                          csl = slice(cc * 128, (cc + 1) * 128)
                                          mm(pi2_[:, csl], Rm[:, csl], Zp[:, csl], True, True, R=[Rm_r, Zp_r], W=[pi2_r])
                                  tt_("dve", Rm[:], Rm[:], pr2_[:], ALU.subtract, R=[Rm_r, pr2_r], W=[Rm_r])
                                  if not last:
                                      tt_("dve", Inv[:], Inv[:], pi2_[:], ALU.subtract, R=[Inv_r, pi2_r], W=[Inv_r])
                              if GDN_STOP == 5:
                                  continue
                              pw, pw_r = ps()
                              for cc in range(4):
                                  csl = slice(cc * 128, (cc + 1) * 128)
                                  mm(pw[:, csl], kbg[:, csl], Rm[:, csl], True, True, R=[kbg_r, Rm_r], W=[pw_r])
                              amul(nwt[:], pw[:], -1.0, R=[pw_r], W=[nwt_r])

                              if GDN_STOP == 6:
                                  continue
                              for cc in range(4):
                                  csl = slice(cc * 128, (cc + 1) * 128)
                                  vn, vn_r = vnew[cc % 2], vnew_r[cc % 2]
                                  pv, pv_r = ps()
                                  mm(pv[:, 0:128], Rm[:, csl], vb[:, csl], True, False, R=[Rm_r, vb_r], W=[pv_r])
                                  mm(pv[:, 0:128], nwt[:, csl], Sbf[:], False, True, R=[nwt_r, Sbf_r], W=[pv_r])
                                  act(vn[:], pv[:, 0:128], AF.Copy, R=[pv_r], W=[vn_r])
                                  if GDN_REC == 1:
                                      continue
                                  po, po_r = ps()
                                  mm(po[:, 0:128], Sbf[:], qd[:, csl], True, False, R=[Sbf_r, qd_r], W=[po_r])
                                  mm(po[:, 0:128], vn[:], qkm[:, csl], False, True, R=[vn_r, qkm_r], W=[po_r])
                                  pd, pd_r = ps()
                                  mm(pd[:, 0:128], kdc[:, csl], vn[:], True, True, R=[kdc_r, vn_r], W=[pd_r])
                                  act(oraw[:, csl], po[:, 0:128], AF.Copy, R=[po_r], W=[oraw_r])
                                  if GDN_REC == 2:
                                      continue
                                  ts_("dve", Sst[:], Sst[:], cols[:, cc * 4 + 3:cc * 4 + 4], None, ALU.mult, None, R=[Sst_r, cols_r], W=[Sst_r])
                                  tt_("dve", Sst[:], pd[:, 0:128], Sst[:], ALU.add, R=[Sst_r, pd_r], W=[Sst_r])
                                  if GDN_REC == 3:
                                      continue
                                  act(Sbf[:], Sst[:], AF.Copy, R=[Sst_r], W=[Sbf_r])

                              if GDN_STOP == 7:
                                  continue
                              act(sqg[:], oraw[:], AF.Square, R=[oraw_r], W=[sqg_r])
                              pb2, pr2 = ps()
                              mm(pb2[:], ones_b, sqg[:], True, True, R=[sqg_r, r_const], W=[pr2])
                              rmsnorm_rstd(pb2[:], pr2, 128, tmpf[:], tmpf_r)
                              stt_("dve", tmpg[:], oraw[:], vcol(80), tmpf[:], ALU.mult, ALU.mult, R=[oraw_r, tmpf_r, r_const], W=[tmpg_r])
                              tt_("pool", oTa[:, h, tsl], tmpg[:], zs[:], ALU.mult, R=[tmpg_r, zs_r], W=[oTa_r[h][tt]])
                      if "gdn_dbg" in taps and l == 0:
                          for nm, tl, rr in (("AT", AT, AT_r), ("Am", Am, Am_r), ("Rm", Rm, Rm_r), ("Inv", Inv, Inv_r), ("qkm", qkm, qkm_r), ("qd", qd, qd_r),
                                             ("nwt", nwt, nwt_r), ("vb", vb, vb_r), ("kbg", kbg, kbg_r), ("kdc", kdc, kdc_r), ("oraw", oraw, oraw_r),
                                             ("qT", qT, qT_r), ("kT", kT, kT_r), ("zs", zs, zs_r), ("tmpf", tmpf, tmpf_r), ("tmpg", tmpg, tmpg_r)):
                              taps = tuple(taps) + ("d_" + nm,)
                              tap("d_" + nm, tl[:], [128, TT], [rr])
                          taps = tuple(taps) + ("d_cols", "d_Sst", "d_G", "d_Gb")
                          tap("d_cols", cols[:], [128, 16], [cols_r])
                          tap("d_Sst", Sst[:], [128, 128], [Sst_r])
                          tap("d_G", Grow[1][:], [1, 1 + TT], [Grow_r[1]])
                          tap("d_Gb", rGb[:], [1, TT], [rGb_r])
                  if l == 0:
                      for h in range(4):
                          tap(f"oTa{h}", oTa[:, h, :], [128, T], [oTa_r[h][t_] for t_ in range(NT)])
                  with ExitStack() as gs:
                      kb.barrier()
                      S = lambda n, shp, d=F32: sb(n, shp, d, gs)
                      hw["slots"] = [S(f"wsl{i}", [128, KC, 768], BF16) for i in range(2)]
                      hw["res"] = [Res(), Res()]
                      nxt = load_head_weights(l, "gla", 0)
                      glrT = S("glrT", [16, TT], BF16); glr_r = Res()
                      qf = S("qf", [128, TT]); qf_r = Res()
                      kf = S("kf", [128, TT]); kf_r = Res()
                      tmpf = S("tmpf2", [128, TT]); tmpf_r = Res()
                      tmpg = S("tmpg2", [128, TT]); tmpg_r = Res()
                      tmph, tmph_r = tmpg, tmpg_r
                      Pp = [S(f"Pp{i}", [128, 1 + TT]) for i in range(2)]; Pp_r = [Res(), Res()]
                      lrow, lrow_r = tmpg, tmpg_r
                      qk2 = S("qk2", [128, 2, TT], BF16)
                      qin = qk2[:, 0, :]; qin_r = Res()
                      kin = qk2[:, 1, :]; kin_r = Res()
                      qdc = S("qdc", [128, TT], BF16); qdc_r = Res()
                      kdcT = S("kdcT", [128, TT], BF16); kdcT_r = Res()
                      kdt = S("kdt", [128, TT], BF16); kdt_r = Res()
                      vtok = S("vtok", [128, 4, 256], BF16); vtok_r = Res()
                      attn = S("attn", [128, TT], BF16); attn_r = Res()
                      rs = S("rs", [128, 2, TT], BF16); rs_r = Res()
                      oraw2 = S("oraw2", [128, 2, TT]); oraw2_r = Res()
                      S2 = S("S2", [128, 256]); S2_r = Res()
                      S2b = S("S2b", [128, 256], BF16); S2b_r = Res()
                      cdc = S("cdc", [128, 4]); cdc_r = Res()
                      ngb = S("ngb", [128, 4]); ngb_r = Res()
                      ts_("dve", ngb[:], vcol(83, 4), -1.0, None, ALU.mult, None, R=[r_const], W=[ngb_r])
                      for h in (range(4) if "gla" in phases else ()):
                          W_, W_r = nxt
                          nxt = load_head_weights(l, "gla", h + 1) if h < 3 else None
                          kb.op("dve", lambda: DVE.memset(S2[:], 0.0), W=[S2_r])
                          kb.op("dve", lambda: DVE.memset(S2b[:], 0.0), W=[S2b_r])
                          for tt in range(NT):
                              tsl = slice(tt * TT, (tt + 1) * TT)
                              hres = [hr[kc][tt] for kc in range(KC)]
                              Pc, Pc_r = Pp[tt % 2], Pp_r[tt % 2]
                              Pv_, Pv_r = Pp[(tt + 1) % 2], Pp_r[(tt + 1) % 2]
                              pb, pr = ps()
                              for kc in range(KC):
                                  mm(pb[:], W_[:, kc, 0:128], hT[:, kc, tsl], kc == 0, kc == KC - 1, R=[W_r, hres[kc]], W=[pr])
                              amul(qf[:], pb[:], 128.0 ** -0.5, R=[pr], W=[qf_r])
                              pb, pr = ps()
                              for kc in range(KC):
                                  mm(pb[:], W_[:, kc, 128:256], hT[:, kc, tsl], kc == 0, kc == KC - 1, R=[W_r, hres[kc]], W=[pr])
                              act(kf[:], pb[:], AF.Copy, R=[pr], W=[kf_r])
                              for half in range(2):
                                  pb, pr = ps()
                                  for c2 in range(2):
                                      cc = half * 2 + c2
                                      for kc in range(KC):
                                          mm(pb[:, c2 * 256:(c2 + 1) * 256], hT[:, kc, tt * TT + cc * 128: tt * TT + (cc + 1) * 128], W_[:, kc, 256:512],
                                             kc == 0, kc == KC - 1, R=[W_r, hres[kc]], W=[pr])
                                  act(vtok[:, half * 2:half * 2 + 2, :], pb[:].rearrange("p (c e) -> p c e", e=256), AF.Copy, R=[pr], W=[vtok_r])
                              for et in range(2):
                                  pb, pr = ps()
                                  for kc in range(KC):
                                      mm(pb[:], W_[:, kc, 512 + et * 128:512 + (et + 1) * 128], hT[:, kc, tsl], kc == 0, kc == KC - 1, R=[W_r, hres[kc]], W=[pr])
                                  silu_from("dve", rs[:, et, :], pb[:], tmpf[:], R=[pr], W=[rs_r], tmp_res=tmpf_r)
                              pb, pr = ps()
                              for kc in range(KC):
                                  mm(pb[0:16, :], wsm[:, kc, 8:24], hT[:, kc, tsl], kc == 0, kc == KC - 1, R=[r_wsm, hres[kc]], W=[pr])
                              act(glrT[:], pb[0:16, :], AF.Copy, R=[pr], W=[glr_r])
                              pb, pr = ps()
                              mm(pb[:], w2b[0:16, h * 128:(h + 1) * 128], glrT[:], True, True, R=[r_wsm, glr_r], W=[pr])
                              act(lrow[:], pb[:], AF.Exp, R=[pr, ngb_r], W=[lrow_r], bias=ngb[:, h:h + 1], scale=-1.0)
                              act(lrow[:], lrow[:], AF.Ln, R=[lrow_r], W=[lrow_r], bias=1.0)
                              if tt == 0:
                                  kb.op("dve", lambda: DVE.memset(Pc[:, 0:1], 0.0), W=[Pc_r])
                              else:
                                  kb.op("dve", lambda: DVE.tensor_copy(Pc[:, 0:1], Pv_[:, TT:TT + 1]), R=[Pv_r], W=[Pc_r])
                              kb.op("dve", lambda: DVE.tensor_tensor_scan(Pc[:, 1:1 + TT], onesf[:, 0:1].to_broadcast([128, TT]), lrow[:], Pc[:, 0:1], ALU.mult, ALU.add),
                                    R=[r_const, lrow_r, Pc_r], W=[Pc_r])
                              Pvw = v4(Pc[:, 1:1 + TT])
                              Pst = Pc[:, 0:TT:128].unsqueeze(2).to_broadcast([128, 4, 128])
                              Pmid = Pc[:, 65:TT + 1:128].unsqueeze(2).to_broadcast([128, 4, 128])
                              Pla = Pc[:, 128:TT + 1:128].unsqueeze(2).to_broadcast([128, 4, 128])
                              isc = 1.0 / 16.0
                              tt_("dve", v4(tmpf[:]), Pvw, Pmid, ALU.subtract, R=[Pc_r], W=[tmpf_r])
                              act(tmpg[:], tmpf[:], AF.Exp, R=[tmpf_r], W=[tmpg_r], scale=-isc)
                              tt_("dve", qin, qf[:], tmpg[:], ALU.mult, R=[qf_r, tmpg_r], W=[qin_r])
                              act(tmph[:], tmpf[:], AF.Exp, R=[tmpf_r], W=[tmph_r], scale=isc)
                              tt_("dve", kin, kf[:], tmph[:], ALU.mult, R=[kf_r, tmph_r], W=[kin_r])
                              tt_("dve", v4(tmpf[:]), Pvw, Pst, ALU.subtract, R=[Pc_r], W=[tmpf_r])
                              act(tmpg[:], tmpf[:], AF.Exp, R=[tmpf_r], W=[tmpg_r], scale=-isc)
                              tt_("dve", qdc[:], qf[:], tmpg[:], ALU.mult, R=[qf_r, tmpg_r], W=[qdc_r])
                              tt_("dve", v4(tmpf[:]), Pvw, Pla, ALU.subtract, R=[Pc_r], W=[tmpf_r])
                              act(tmph[:], tmpf[:], AF.Exp, R=[tmpf_r], W=[tmph_r], scale=isc)
                              tt_("dve", kdcT[:], kf[:], tmph[:], ALU.mult, R=[kf_r, tmph_r], W=[kdcT_r])
                              tt_("dve", cdc[:], Pc[:, 128:TT + 1:128], Pc[:, 0:TT:128], ALU.subtract, R=[Pc_r], W=[cdc_r])
                              act(cdc[:], cdc[:], AF.Exp, R=[cdc_r], W=[cdc_r], scale=-isc)
                              pa, pa_r = ps()
                              for cc in range(4):
                                  csl = slice(cc * 128, (cc + 1) * 128)
                                  mm(pa[:, csl], kin[:, csl], qin[:, csl], True, True, R=[kin_r, qin_r], W=[pa_r])
                              tt_("dve", v4(attn[:]), v4(pa[:]), b4(CB(C_C01_T)), ALU.mult, R=[pa_r, r_const], W=[attn_r])
                              pt, ptr = psb()
                              for cc in range(4):
                                  csl = slice(cc * 128, (cc + 1) * 128)
                                  kb.op("pe", lambda: PE.transpose(pt[:, csl], kdcT[:, csl], ident_b), R=[kdcT_r, r_const], W=[ptr], inc=(cc == 3))
                              act(kdt[:], pt, AF.Copy, R=[ptr], W=[kdt_r])
                              for cc in range(4):
                                  csl = slice(cc * 128, (cc + 1) * 128)
                                  po, po_r = ps()
                                  for et in range(2):
                                      esl = slice(et * 128, (et + 1) * 128)
                                      mm(po[:, esl], vtok[:, cc, esl], attn[:, csl], True, False, R=[vtok_r, attn_r], W=[po_r])
                                      mm(po[:, esl], S2b[:, esl], qdc[:, csl], False, True, R=[S2b_r, qdc_r], W=[po_r])
                                  pd, pd_r = ps()
                                  mm(pd[:, 0:256], kdt[:, csl], vtok[:, cc, :], True, True, R=[kdt_r, vtok_r], W=[pd_r])
                                  act(oraw2[:, :, csl], po[:, 0:256].rearrange("p (e k) -> p e k", k=128), AF.Copy, R=[po_r], W=[oraw2_r])
                                  ts_("dve", S2[:], S2[:], cdc[:, cc:cc + 1], None, ALU.mult, None, R=[S2_r, cdc_r], W=[S2_r])
                                  tt_("dve", S2[:], pd[:, 0:256], S2[:], ALU.add, R=[S2_r, pd_r], W=[S2_r])
                                  act(S2b[:], S2[:], AF.Copy, R=[S2_r], W=[S2b_r])
                              act(qk2[:], oraw2[:], AF.Square, R=[oraw2_r], W=[qin_r, kin_r])
                              pb2, pr2 = ps()
                              for et in range(2):
                                  mm(pb2[:], ones_b, qk2[:, et, :], et == 0, et == 1, R=[qin_r, kin_r, r_const], W=[pr2])
                              rmsnorm_rstd(pb2[:], pr2, 256, tmpf[:], tmpf_r)
                              for et in range(2):
                                  stt_("dve", tmpg[:], oraw2[:, et, :], vcol(81 + et), tmpf[:], ALU.mult, ALU.mult, R=[oraw2_r, tmpf_r, r_const], W=[tmpg_r])
                                  tt_("pool", oTb[:, h * 2 + et, tsl], tmpg[:], rs[:, et, :], ALU.mult, R=[tmpg_r, rs_r], W=[oTb_r[h * 2 + et][tt]])

                  if l == 0:
                      for h in range(8):
                          tap(f"oTb{h}", oTb[:, h, :], [128, T], [oTb_r[h][t_] for t_ in range(NT)])
                  with ExitStack() as gs:
                      kb.barrier()
                      S = lambda n, shp, d=F32: sb(n, shp, d, gs)
                      NW = 2
                      wm = [S(f"wm{i}", [128, 28, 128], BF16) for i in range(NW)]
                      wm_r = [Res() for _ in range(NW)]
                      wo = [S(f"wo{i}", [128, KC, 128], BF16) for i in range(NW)]
                      wo_r = [Res() for _ in range(NW)]
                      stage = S("stage", [128, KC, TT], BF16); stage_r = [Res() for _ in range(KC)]
                      tmpo = S("tmpo", [128, KC, TT]); tmpo_r = [Res() for _ in range(KC)]
                      sa = S("sa", [128, TT]); sa_r = Res()
                      sb_ = S("sb_", [128, TT]); sb_r = Res()
                      m1 = S("m1", [128, TT]); m1_r = Res()
                      sq = [S(f"sqm{i}", [128, TT], BF16) for i in range(2)]; sq_res = [Res(), Res()]
                      rstd = S("rstdm", [128, TT]); rstd_res = Res()
                      t2 = S("t2", [128, TT]); t2_r = Res()
                      cnt = {"m": 0, "o": 0}

                      def load_wm(ct):
                          i = cnt["m"] % NW
                          cnt["m"] += 1
                          csl = slice(ct * 128, (ct + 1) * 128)
                          pairs = [(wm[i][:, 0:4, :], w_oa_d[l, :, csl].rearrange("(k p) c -> p k c", p=128)),
                                   (wm[i][:, 4:12, :], w_ob_d[l, :, csl].rearrange("(k p) c -> p k c", p=128)),
                                   (wm[i][:, 12:20, :], w_in_d[l, :, O_GA + ct * 128:O_GA + (ct + 1) * 128].rearrange("(k p) c -> p k c", p=128)),
                                   (wm[i][:, 20:28, :], w_in_d[l, :, O_GB + ct * 128:O_GB + (ct + 1) * 128].rearrange("(k p) c -> p k c", p=128))]
                          kb.dma("pool", pairs, wm_k[i], W=[wm_r[i]])
                          return wm[i], wm_r[i]

                      def load_wo(ct):
                          i = cnt["o"] % NW
                          cnt["o"] += 1
                          kb.dma("pool", [(wo[i][:], w_o_d[l, :, ct * 128:(ct + 1) * 128].rearrange("(k p) c -> p k c", p=128))], wo_k[i], W=[wo_r[i]])
                          return wo[i], wo_r[i]

                      for tt in (range(NT) if "merge" in phases else ()):
                          tsl = slice(tt * TT, (tt + 1) * TT)
                          nw = load_wm(0)
                          for ct in range(KC):
                              w_, w_r = nw
                              if ct < KC - 1:
                                  nw = load_wm(ct + 1)
                              pya, pya_r = ps()
                              for k in range(4):
                                  mm(pya[:], w_[:, k, :], oTa[:, k, tsl], k == 0, k == 3, R=[w_r, oTa_r[k][tt]], W=[pya_r])
                              pyb, pyb_r = ps()
                              for k in range(8):
                                  mm(pyb[:], w_[:, 4 + k, :], oTb[:, k, tsl], k == 0, k == 7, R=[w_r, oTb_r[k][tt]], W=[pyb_r])
                              pga, pga_r = ps()
                              for k in range(8):
                                  mm(pga[:], w_[:, 12 + k, :], hT[:, k, tsl], k == 0, k == 7, R=[w_r, hr[k][tt]], W=[pga_r])
                              pgb, pgb_r = ps()
                              for k in range(8):
                                  mm(pgb[:], w_[:, 20 + k, :], hT[:, k, tsl], k == 0, k == 7, R=[w_r, hr[k][tt]], W=[pgb_r])
                              act(sa[:], pga[:], AF.Exp, R=[pga_r], W=[sa_r], scale=-1.0)
                              act(sa[:], sa[:], AF.Ln, R=[sa_r], W=[sa_r], bias=1.0)
                              act(sa[:], sa[:], AF.Exp, R=[sa_r], W=[sa_r], scale=-1.0)
                              act(sb_[:], pgb[:], AF.Exp, R=[pgb_r], W=[sb_r], scale=-1.0)
                              act(sb_[:], sb_[:], AF.Ln, R=[sb_r], W=[sb_r], bias=1.0)
                              act(sb_[:], sb_[:], AF.Exp, R=[sb_r], W=[sb_r], scale=-1.0)
                              tt_("dve", m1[:], pya[:], sa[:], ALU.mult, R=[pya_r, sa_r], W=[m1_r])
                              tt_("dve", sb_[:], pyb[:], sb_[:], ALU.mult, R=[pyb_r, sb_r], W=[sb_r])
                              tt_("pool", stage[:, ct, :], m1[:], sb_[:], ALU.add, R=[m1_r, sb_r], W=[stage_r[ct]])
                          pss, pss_r = pacc, pacc_r
                          nw = load_wo(0)
                          for ct in range(KC):
                              w_, w_r = nw
                              if ct < KC - 1:
                                  nw = load_wo(ct + 1)
                              pb, pr = ps()
                              for k in range(KC):
                                  mm(pb[:], w_[:, k, :], stage[:, k, :], k == 0, k == KC - 1, R=[w_r, stage_r[k]], W=[pr])
                              act(tmpo[:, ct, :], pb[:], AF.Copy, R=[pr], W=[tmpo_r[ct]])
                              s = sq[ct % 2]
                              act(s[:], pb[:], AF.Square, R=[pr], W=[sq_res[ct % 2]])
                              mm(pss[:], ones_b, s[:], ct == 0, ct == KC - 1, R=[sq_res[ct % 2], r_const], W=[pss_r])
                          rmsnorm_rstd(pss[:], pss_r, D, rstd[:], rstd_res)
                          for ct in range(KC):
                              stt_("dve", t2[:], tmpo[:, ct, :], vcol(8 + ct), rstd[:], ALU.mult, ALU.mult, R=[tmpo_r[ct], rstd_res, r_const], W=[t2_r])
                              tt_("pool", xT[:, ct, tsl], xT[:, ct, tsl], t2[:], ALU.add, R=[xr[ct][tt], t2_r], W=[xr[ct][tt]])

              if l == 0:
                  for kc in range(KC):
                      tap(f"xmix{kc}", xT[:, kc, :], [128, T], [xr[kc][t_] for t_ in range(NT)])
              with ExitStack() as gs:
                  kb.barrier()
                  S = lambda n, shp, d=F32: sb(n, shp, d, gs)
                  h2 = S("h2", [128, KC, TT], BF16); h2_r = [Res() for _ in range(KC)]
                  uT = S("uT", [128, 32, TT], BF16); uT_r = [Res() for _ in range(32)]
                  NW = 3
                  wu = [S(f"wu{i}", [128, KC, 512], BF16) for i in range(NW)]; wu_r = [Res() for _ in range(NW)]
                  wd = [S(f"wd{i}", [128, 32, 128], BF16) for i in range(NW)]; wd_r = [Res() for _ in range(NW)]
                  tmpo = S("tmpo2", [128, KC, TT]); tmpo_r = [Res() for _ in range(KC)]
                  sq = [S(f"sqn{i}", [128, TT], BF16) for i in range(2)]; sq_res = [Res(), Res()]
                  rstd = S("rstdn", [128, TT]); rstd_res = Res()
                  rl = S("rl", [128, TT]); rl_r = Res()
                  t2 = S("t2n", [128, TT]); t2_r = Res()
                  cnt = {"u": 0, "d": 0}

                  def load_wu(fb):
                      i = cnt["u"] % NW
                      cnt["u"] += 1
                      kb.dma("pool", [(wu[i][:], w_up_d[l, :, fb * 512:(fb + 1) * 512].rearrange("(k p) c -> p k c", p=128))], wu_k[i], W=[wu_r[i]])
                      return wu[i], wu_r[i]

                  def load_wd(ct):
                      i = cnt["d"] % NW
                      cnt["d"] += 1
                      kb.dma("pool", [(wd[i][:], w_dn_d[l, :, ct * 128:(ct + 1) * 128].rearrange("(k p) c -> p k c", p=128))], wd_k[i], W=[wd_r[i]])
                      return wd[i], wd_r[i]

                  for tt in (range(NT) if "mlp" in phases else ()):
                      tsl = slice(tt * TT, (tt + 1) * TT)
                      q_u = [load_wu(0), load_wu(1)]
                      norm_tile(lambda kc: xT[:, kc, tsl], lambda kc: xr[kc][tt], lambda kc: vcol(16 + kc),
                                lambda kc: h2[:, kc, :], lambda kc: h2_r[kc], sq, sq_res, rstd, rstd_res)
                      for fb in range(8):
                          w_, w_r = q_u.pop(0)
                          if fb + 2 < 8:
                              q_u.append(load_wu(fb + 2))
                          for f4 in range(4):
                              ft = fb * 4 + f4
                              pb, pr = ps()
                              for k in range(KC):
                                  mm(pb[:], w_[:, k, f4 * 128:(f4 + 1) * 128], h2[:, k, :], k == 0, k == KC - 1, R=[w_r, h2_r[k]], W=[pr])
                              act(rl[:], pb[:], AF.Relu, R=[pr], W=[rl_r])
                              tt_("pool" if ft % 2 else "dve", uT[:, ft, :], rl[:], rl[:], ALU.mult, R=[rl_r], W=[uT_r[ft]])
                      q_d = [load_wd(0), load_wd(1)]
                      pss, pss_r = pacc, pacc_r
                      for ct in range(KC):
                          w_, w_r = q_d.pop(0)
                          if ct + 2 < KC:
                              q_d.append(load_wd(ct + 2))
                          pb, pr = ps()
                          for k in range(32):
                              mm(pb[:], w_[:, k, :], uT[:, k, :], k == 0, k == 31, R=[w_r, uT_r[k]], W=[pr])
                          act(tmpo[:, ct, :], pb[:], AF.Copy, R=[pr], W=[tmpo_r[ct]])
                          s = sq[ct % 2]
                          act(s[:], pb[:], AF.Square, R=[pr], W=[sq_res[ct % 2]])
                          mm(pss[:], ones_b, s[:], ct == 0, ct == KC - 1, R=[sq_res[ct % 2], r_const], W=[pss_r])
                      rmsnorm_rstd(pss[:], pss_r, D, rstd[:], rstd_res)
                      for ct in range(KC):
                          stt_("dve", t2[:], tmpo[:, ct, :], vcol(24 + ct), rstd[:], ALU.mult, ALU.mult, R=[tmpo_r[ct], rstd_res, r_const], W=[t2_r])
                          tt_("pool", xT[:, ct, tsl], xT[:, ct, tsl], t2[:], ALU.add, R=[xr[ct][tt], t2_r], W=[xr[ct][tt]])

        k_out = kb.dsem("out")
        kb.dma("sp", [(outT_d[kc * 128:(kc + 1) * 128, :], xT[:, kc, :]) for kc in range(KC)], k_out,
               R=[xr[kc][tt] for kc in range(KC) for tt in range(NT)])
        nc.sync.wait_ge(kb.sems[k_out], kb.cnt[k_out])
        for name, key in tap_out.items():
            nc.sync.wait_ge(kb.sems[key], kb.cnt[key])
        print(f"[build] ops={kb.nops} waits={kb.nwaits} counts={ {k: v for k, v in kb.cnt.items() if not k.startswith('d_')} }", flush=True)
    return nc


def pack_small(inputs, l0, nl):
    vecs = np.zeros((128, nl * NVEC), np.float32)
    hv = np.zeros((1, nl * 8), np.float32)
    w2 = np.zeros((16, nl * 512), np.float32)
    for i in range(nl):
        l = l0 + i
        V0 = i * NVEC
        for j, name in enumerate(("norm_mix_pre", "norm_mix_post", "norm_mlp_pre", "norm_mlp_post")):
            vecs[:, V0 + j * 8:V0 + (j + 1) * 8] = np.asarray(inputs[name][l]).reshape(8, 128).T
        cw = np.asarray(inputs["conv_w"][l])
        for tp in range(4):
            vecs[:, V0 + 32 + tp * 12:V0 + 32 + (tp + 1) * 12] = cw[tp].reshape(12, 128).T
        vecs[:, V0 + 80] = np.asarray(inputs["gdn_norm"][l])
        vecs[:, V0 + 81:V0 + 83] = np.asarray(inputs["gla_norm"][l]).reshape(2, 128).T
        vecs[:, V0 + 83:V0 + 87] = np.asarray(inputs["gla_gate_b"][l]).reshape(4, 128).T
        hv[0, i * 8:i * 8 + 4] = np.asarray(inputs["a_log"][l])
        hv[0, i * 8 + 4:i * 8 + 8] = np.asarray(inputs["dt_bias"][l])
        w2[:, i * 512:(i + 1) * 512] = np.asarray(inputs["gla_gate_w2"][l])
    return vecs, hv, w2


_PROG = {}


def run_layers(xT_list, inputs, l0, nl):
    if nl not in _PROG:
        _PROG[nl] = build_program(nl)
    nc = _PROG[nl]
    vecs, hv, w2 = pack_small(inputs, l0, nl)
    consts = make_consts()
    sl = slice(l0, l0 + nl)
    shared = {
        "w_in": np.ascontiguousarray(inputs["w_in"][sl]), "w_out_a": np.ascontiguousarray(inputs["w_out_a"][sl]),
        "w_out_b": np.ascontiguousarray(inputs["w_out_b"][sl]), "w_o": np.ascontiguousarray(inputs["w_o"][sl]),
        "w_mlp_up": np.ascontiguousarray(inputs["w_mlp_up"][sl]), "w_mlp_down": np.ascontiguousarray(inputs["w_mlp_down"][sl]),
        "gate_w2": w2, "vecs": vecs, "hv": hv, "consts": consts,
    }
    in_maps = [dict(shared, xT=xT_list[c]) for c in range(len(xT_list))]
    res = run_bass_kernel_spmd(nc, in_maps, core_ids=list(range(len(xT_list))))
    return [np.asarray(r["outT"]) for r in res.results]


GDN_STOP = 0
N_FUSED = 4


def kernel(**inputs):
    inputs = {k: np.asarray(v) for k, v in inputs.items()}
    x = inputs["x"].astype(np.float32, copy=False)
    xT = [np.ascontiguousarray(x[b].T) for b in range(x.shape[0])]
    for l0 in range(0, L, N_FUSED):
        xT = run_layers(xT, inputs, l0, N_FUSED)
    out = np.stack([t.T for t in xT], axis=0)
    return np.ascontiguousarray(out.astype(np.float32))
```

```python
import numpy as np
from contextlib import ExitStack
import concourse.bass as bass
import concourse.mybir as mybir
from concourse.bass_utils import run_bass_kernel_spmd

F32 = mybir.dt.float32
BF16 = mybir.dt.bfloat16
AF = mybir.ActivationFunctionType
ALU = mybir.AluOpType

D = 1024
T = 2048
L = 4
DFF = 4096
NCOL = 7192
NT = 4
TT = 512
KC = 8
EPS = 1e-6
O_AQ, O_AK, O_AV, O_AZ, O_AB, O_AA = 0, 512, 1024, 1536, 2048, 2052
O_BQ, O_BK, O_BV, O_BR, O_GLR, O_GA, O_GB = 2056, 2568, 3080, 4104, 5128, 5144, 6168
GDN_STOP = 0
GDN_REC = 0
NVEC = 87
C_ID, C_ONES, C_NEG, C_MN_INC_T, C_MN_STR_T, C_MN_STR, C_C01_T = 0, 1, 2, 3, 4, 5, 6
C_LV = 7
C_LVT = 14
NCB = 21


def make_consts():
    c = np.zeros((NCB, 128, 128), np.float32)
    i = np.arange(128)[:, None]
    j = np.arange(128)[None, :]
    c[C_ID] = (i == j)
    c[C_ONES] = 1.0
    c[C_NEG] = -1.0
    c[C_MN_INC_T] = np.where(j >= i, 0.0, -1e30)
    c[C_MN_STR_T] = np.where(j > i, 0.0, -1e30)
    c[C_MN_STR] = np.where(i > j, 0.0, -1e30)
    c[C_C01_T] = (j >= i)
    for li in range(7):
        m = 1 << li
        mm = ((i // (2 * m)) == (j // (2 * m))) & ((i % (2 * m)) >= m) & ((j % (2 * m)) < m)
        c[C_LV + li] = mm
        c[C_LVT + li] = mm.T
    return np.ascontiguousarray(c.transpose(1, 0, 2).reshape(128, NCB * 128))


class Res:
    __slots__ = ("w", "rs", "p")

    def __init__(self):
        self.w = None
        self.rs = {}
        self.p = set()


class KB:
    def __init__(self, nc, es):
        self.nc = nc
        self.es = es
        self.eng = {"pe": nc.tensor, "dve": nc.vector, "act": nc.scalar, "pool": nc.gpsimd, "sp": nc.sync}
        self.sems = {}
        self.cnt = {}
        self.seen = {e: {} for e in self.eng}
        self.pend = {e: [] for e in self.eng}
        for e in ("pe", "dve", "act", "pool"):
            self.sems[e] = es.enter_context(nc.semaphore("s_" + e))
            self.cnt[e] = 0
        self.nops = 0
        self.nwaits = 0

    def dsem(self, name):
        key = "d_" + name
        self.sems[key] = self.es.enter_context(self.nc.semaphore(key))
        self.cnt[key] = 0
        return key

    def flush(self, e):
        if not self.pend[e]:
            return
        self.cnt[e] += 1
        self.pend[e][-1][0].then_inc(self.sems[e], 1)
        ev = (e, self.cnt[e])
        for (_, r2, w2) in self.pend[e]:
            for r in list(r2) + list(w2):
                r.p.discard(e)
            self._reg(ev, r2, w2)
        self.pend[e] = []

    def barrier(self):
        for e in self.eng:
            self.flush(e)
        for e in self.eng:
            for k, v in self.cnt.items():
                if v > 0 and self.seen[e].get(k, 0) < v:
                    self.eng[e].wait_ge(self.sems[k], v)
                    self.seen[e][k] = v
                    self.nwaits += 1

    def _deps(self, e, R, W):
        for r in list(R) + list(W):
            for e2 in list(r.p):
                if e2 != e:
                    self.flush(e2)
        deps = {}

        def add(ev):
            k, v = ev
            if deps.get(k, 0) < v:
                deps[k] = v
        for r in R:
            if r.w is not None:
                add(r.w)
        for w in W:
            if w.w is not None:
                add(w.w)
            for k, v in w.rs.items():
                add((k, v))
        for k, v in deps.items():
            if k == e and e == "pe":
                continue
            if self.seen[e].get(k, 0) >= v:
                continue
            self.eng[e].wait_ge(self.sems[k], v)
            self.seen[e][k] = v
            self.nwaits += 1

    def _reg(self, ev, R, W):
        k, v = ev
        for w in W:
            w.w = ev
            w.rs = {}
        for r in R:
            if r.rs.get(k, 0) < v:
                r.rs[k] = v

    def op(self, e, fn, R=(), W=(), inc=True):
        self._deps(e, R, W)
        inst = fn()
        self.nops += 1
        if not inc:
            self.pend[e].append((inst, R, W))
            for r in list(R) + list(W):
                r.p.add(e)
            return
        self.cnt[e] += 1
        inst.then_inc(self.sems[e], 1)
        ev = (e, self.cnt[e])
        for (_, r2, w2) in self.pend[e]:
            for r in list(r2) + list(w2):
                r.p.discard(e)
            self._reg(ev, r2, w2)
        self.pend[e] = []
        self._reg(ev, R, W)

    def dma(self, e, pairs, key, R=(), W=()):
        self._deps(e, R, W)
        for (o, i) in pairs:
            inst = self.eng[e].dma_start(out=o, in_=i)
            inst.then_inc(self.sems[key], 16)
            self.cnt[key] += 16
            self.nops += 1
        ev = (key, self.cnt[key])
        self._reg(ev, R, W)


def build_program(n_layers, taps=(), phases=("gdn", "gla", "merge", "mlp")):
    nc = bass.Bass("TRN2", target_bir_lowering=False)
    NL = n_layers
    dt = lambda name, shape, kind="ExternalInput": nc.dram_tensor(name, shape, F32, kind=kind).ap()
    xT_d = dt("xT", [D, T])
    w_in_d = dt("w_in", [NL, D, NCOL])
    w_oa_d = dt("w_out_a", [NL, 512, D])
    w_ob_d = dt("w_out_b", [NL, 1024, D])
    w_o_d = dt("w_o", [NL, D, D])
    w_up_d = dt("w_mlp_up", [NL, D, DFF])
    w_dn_d = dt("w_mlp_down", [NL, DFF, D])
    w2_d = dt("gate_w2", [16, NL * 512])
    vecs_d = dt("vecs", [128, NL * NVEC])
    hv_d = dt("hv", [1, NL * 8])
    consts_d = dt("consts", [128, NCB * 128])
    outT_d = dt("outT", [D, T], kind="ExternalOutput")
    tap_out = {}

    with ExitStack() as es:
        kb = KB(nc, es)
        PE, DVE, ACT, POOL = nc.tensor, nc.vector, nc.scalar, nc.gpsimd

        uid = {"n": 0}

        def sb(name, shape, dtype=F32, stack=es):
            uid["n"] += 1
            return stack.enter_context(nc.sbuf_tensor(f"s{uid['n']}_{name}", shape, dtype))

        NPB = 5
        pbanks = [es.enter_context(nc.psum_tensor(f"pb{i}", [128, 512], F32)) for i in range(NPB)]
        pres = [Res() for _ in range(NPB)]
        pacc = es.enter_context(nc.psum_tensor("pacc", [128, 512], F32))
        pacc_r = Res()
        pbf = [es.enter_context(nc.psum_tensor(f"pbf{i}", [128, 1024], BF16)) for i in range(2)]
        pbf_res = [Res(), Res()]
        st = {"pi": 0, "bi": 0}

        def ps():
            i = st["pi"]
            st["pi"] = (i + 1) % NPB
            return pbanks[i], pres[i]

        def psb():
            i = st["bi"]
            st["bi"] = (i + 1) % 2
            return pbf[i][:, 0:512], pbf_res[i]

        xT = sb("xT", [128, KC, T])
        xr = [[Res() for _ in range(NT)] for _ in range(KC)]
        cb = sb("cb", [128, NCB * 128], BF16)
        cf = sb("cf", [128, 2 * 128])
        hv = sb("hv", [1, NL * 8])
        r_const = Res()

        def CB(i):
            return cb[:, i * 128:(i + 1) * 128]
        onesf = cf[:, 0:128]
        negf = cf[:, 128:256]
        ident_b = CB(C_ID)
        ones_b = CB(C_ONES)

        k_in = kb.dsem("in")
        kb.dma("sp", [(xT[:, kc, :], xT_d[kc * 128:(kc + 1) * 128, :]) for kc in range(KC)], k_in,
               W=[xr[kc][tt] for kc in range(KC) for tt in range(NT)])
        k_c = kb.dsem("c")
        kb.dma("pool", [(cb[:], consts_d)], k_c, W=[r_const])
        k_c2 = kb.dsem("c2")
        kb.dma("sp", [(cf[:], consts_d[:, 128:384]), (hv[:], hv_d)], k_c2, W=[r_const])
        k_vec = kb.dsem("vec")

        def tap(name, ap, shape, R):
            if name not in taps:
                return
            d = nc.dram_tensor("tap_" + name, shape, F32, kind="ExternalOutput").ap()
            key = kb.dsem("t_" + name)
            tap_out[name] = key
            kb.dma("pool", [(d, ap)], key, R=R)

        def mm(out, lhsT, rhs, start, stop, R, W):
            kb.op("pe", lambda: PE.matmul(out, lhsT, rhs, start=start, stop=stop), R=R, W=W, inc=stop)

        def act(out, in_, func, R, W, bias=None, scale=None):
            kw = {}
            if bias is not None:
                kw["bias"] = bias
            if scale is not None:
                kw["scale"] = scale
            kb.op("act", lambda: ACT.activation(out=out, in_=in_, func=func, **kw), R=R, W=W)

        def amul(out, in_, c, R, W):
            kb.op("act", lambda: ACT.mul(out, in_, c), R=R, W=W)

        def tt_(e, out, in0, in1, op, R, W):
            eng = DVE if e == "dve" else POOL
            kb.op(e, lambda: eng.tensor_tensor(out, in0, in1, op), R=R, W=W)

        def ts_(e, out, in0, s1, s2, op0, op1, R, W):
            eng = DVE if e == "dve" else POOL
            if op1 is None and e == "pool" and op0 in (ALU.mult, ALU.add):
                o1, c2 = (ALU.add, 0.0) if op0 == ALU.mult else (ALU.mult, 1.0)
                kb.op(e, lambda: eng.tensor_scalar(out, in0, s1, c2, op0, o1), R=R, W=W)
            elif op1 is None:
                kb.op(e, lambda: eng.tensor_scalar(out, in0, s1, None, op0), R=R, W=W)
            else:
                kb.op(e, lambda: eng.tensor_scalar(out, in0, s1, s2, op0, op1), R=R, W=W)

        def stt_(e, out, in0, s, in1, op0, op1, R, W):
            eng = DVE if e == "dve" else POOL
            kb.op(e, lambda: eng.scalar_tensor_tensor(out=out, in0=in0, scalar=s, in1=in1, op0=op0, op1=op1), R=R, W=W)

        def b4(ap):
            return ap.unsqueeze(1).to_broadcast([ap.shape[0], 4, 128])

        def v4(ap):
            return ap.rearrange("p (c k) -> p c k", k=128)

        def rmsnorm_rstd(ps_ap, ps_res, n, rstd_ap, rstd_res):
            act(rstd_ap, ps_ap, AF.Ln, R=[ps_res], W=[rstd_res], bias=EPS, scale=1.0 / n)
            act(rstd_ap, rstd_ap, AF.Exp, R=[rstd_res], W=[rstd_res], scale=-0.5)

        def norm_tile(src_fn, src_res_fn, wcol_fn, dst_fn, dst_res_fn, sq, sq_res, rstd, rstd_res):
            pb, pr = ps()
            for kc in range(KC):
                s = sq[kc % 2]
                act(s[:], src_fn(kc), AF.Square, R=[src_res_fn(kc)], W=[sq_res[kc % 2]])
                mm(pb[:], ones_b, s[:], kc == 0, kc == KC - 1, R=[sq_res[kc % 2], r_const], W=[pr])
            rmsnorm_rstd(pb[:], pr, D, rstd[:], rstd_res)
            for kc in range(KC):
                stt_("dve", dst_fn(kc), src_fn(kc), wcol_fn(kc), rstd[:], ALU.mult, ALU.mult,
                     R=[src_res_fn(kc), rstd_res, r_const], W=[dst_res_fn(kc)])

        def silu_from(e_eng, out_ap, x_ap, tmp_ap, R, W, tmp_res):
            act(tmp_ap, x_ap, AF.Exp, R=R, W=[tmp_res], scale=-1.0)
            act(tmp_ap, tmp_ap, AF.Ln, R=[tmp_res], W=[tmp_res], bias=1.0)
            act(tmp_ap, tmp_ap, AF.Exp, R=[tmp_res], W=[tmp_res], scale=-1.0)
            tt_(e_eng, out_ap, x_ap, tmp_ap, ALU.mult, R=list(R) + [tmp_res], W=W)

        wslot_key = [kb.dsem("ws0"), kb.dsem("ws1")]
        hw = {"n": 0, "slots": None, "res": None}

        def load_head_weights(l, kind, h):
            i = hw["n"] % 2
            hw["n"] += 1
            w = hw["slots"][i]
            if kind == "gdn":
                cols = [(O_AQ + h * 128, 128, 0), (O_AK + h * 128, 128, 128), (O_AV + h * 128, 128, 256), (O_AZ + h * 128, 128, 384)]
            else:
                cols = [(O_BQ + h * 128, 128, 0), (O_BK + h * 128, 128, 128), (O_BV + h * 256, 256, 256), (O_BR + h * 256, 256, 512)]
            pairs = [(w[:, :, o:o + n], w_in_d[l, :, c0:c0 + n].rearrange("(kc p) c -> p kc c", p=128)) for (c0, n, o) in cols]
            kb.dma("pool", pairs, wslot_key[i], W=[hw["res"][i]])
            return w, hw["res"][i]

        k_wsm = kb.dsem("wsm")
        k_w2b = kb.dsem("w2b")
        wm_k = [kb.dsem(f"wm{i}") for i in range(2)]
        wo_k = [kb.dsem(f"wo{i}") for i in range(2)]
        wu_k = [kb.dsem(f"wu{i}") for i in range(3)]
        wd_k = [kb.dsem(f"wd{i}") for i in range(3)]

        for l in range(NL):
            with ExitStack() as ls:
              vecs = sb("vecs", [128, NVEC], F32, ls)
              kb.barrier()
              kb.dma("sp", [(vecs[:], vecs_d[:, l * NVEC:(l + 1) * NVEC])], k_vec, W=[r_const])

              def vcol(off, n=1, vecs=vecs):
                  return vecs[:, off:off + n]
              with ExitStack() as ms:
                  hT = sb("hT", [128, KC, T], BF16, ms)
                  hr = [[Res() for _ in range(NT)] for _ in range(KC)]
                  oTa = sb("oTa", [128, 4, T], BF16, ms)
                  oTa_r = [[Res() for _ in range(NT)] for _ in range(4)]
                  wsm = sb("wsm", [128, KC, 24], BF16, ms)
                  r_wsm = Res()
                  with nc.allow_non_contiguous_dma(reason="small gate columns"):
                      kb.dma("pool", [(wsm[:, :, 0:8], w_in_d[l, :, O_AB:O_AB + 8].rearrange("(kc p) c -> p kc c", p=128)),
                                      (wsm[:, :, 8:24], w_in_d[l, :, O_GLR:O_GLR + 16].rearrange("(kc p) c -> p kc c", p=128)),
                                      ], k_wsm, W=[r_wsm])
                  with ExitStack() as s1:
                      sq = [sb(f"sq{i}", [128, TT], BF16, s1) for i in range(2)]
                      sq_res = [Res(), Res()]
                      rstd = sb("rstd", [128, TT], F32, s1)
                      rstd_res = Res()
                      for tt in range(NT):
                          tsl = slice(tt * TT, (tt + 1) * TT)
                          norm_tile(lambda kc: xT[:, kc, tsl], lambda kc: xr[kc][tt], lambda kc: vcol(kc),
                                    lambda kc: hT[:, kc, tsl], lambda kc: hr[kc][tt], sq, sq_res, rstd, rstd_res)
                  if l == 0:
                      tap("hT", hT[:, 0, :], [128, T], [hr[0][t_] for t_ in range(NT)])

                  with ExitStack() as gs:
                      kb.barrier()
                      S = lambda n, shp, d=F32: sb(n, shp, d, gs)
                      hw["slots"] = [S(f"wsg{i}", [128, KC, 512], BF16) for i in range(2)]
                      hw["res"] = [Res(), Res()]
                      raw1 = S("raw1", [128, 3 + TT]); raw1_r = Res()
                      cacc = S("cacc", [128, TT]); cacc_r = Res()
                      nA = S("nA", [1, 4]); nA_r = Res()
                      act(nA[:], hv[0:1, l * 8:l * 8 + 4], AF.Exp, R=[r_const], W=[nA_r])
                      ts_("dve", nA[:], nA[:], -1.0, None, ALU.mult, None, R=[nA_r], W=[nA_r])

                      def make_head(i):
                          halo = S("halo", [128, 3, 3]); halo_r = [Res() for _ in range(3)]
                          tmpf = S("tmpf", [128, TT]); tmpf_r = Res()
                          tmpg = S("tmpg", [128, TT]); tmpg_r = Res()
                          qT = S("qT", [128, TT], BF16); qT_r = Res()
                          kT = S("kT", [128, TT], BF16); kT_r = Res()
                          vT = S("vT", [128, TT], BF16); vT_r = Res()
                          zs = S("zs", [128, TT], BF16); zs_r = Res()
                          vb = S("vb", [128, TT], BF16); vb_r = Res()
                          kbg = S("kbg", [128, TT], BF16); kbg_r = Res()
                          kdc = S("kdc", [128, TT], BF16); kdc_r = Res()
                          Am = S("Am", [128, TT], BF16); Am_r = Res()
                          AT = S("AT", [128, TT], BF16); AT_r = Res()
                          Om = S("Om", [128, TT], BF16); Om_r = Res()
                          OT = S("OT", [128, TT], BF16); OT_r = Res()
                          Zp, Zp_r = OT, OT_r
                          Zt, Zt_r = Om, Om_r
                          Inv = S("Inv", [128, TT], BF16); Inv_r = Res()
                          Rm = S("Rm", [128, TT], BF16); Rm_r = Res()
                          qkm = S("qkm", [128, TT], BF16); qkm_r = Res()
                          qd = S("qd", [128, TT], BF16); qd_r = Res()
                          nwt, nwt_r = vT, vT_r
                          oraw = S("oraw", [128, TT]); oraw_r = Res()
                          sqg, sqg_r = Om, Om_r
                          vnew = [S("vnew", [128, 128], BF16)] * 2
                          vnew_r = [Res()] * 2
                          Sst = S("Sst", [128, 128]); Sst_r = Res()
                          Sbf = S("Sbf", [128, 128], BF16); Sbf_r = Res()
                          cols = S("cols", [128, 16]); cols_r = Res()
                          rowA = S("rowA", [1, TT]); rowA_r = Res()
                          rowB = S("rowB", [1, TT]); rowB_r = Res()
                          Grow = [S("Grow", [1, 1 + TT])] * 2
                          Grow_r = [Res()] * 2
                          rGb = S("rGb", [1, TT]); rGb_r = Res()
                          rkb = S("rkb", [1, TT]); rkb_r = Res()
                          rcd = S("rcd", [1, 8]); rcd_r = Res()
                          def run(h, W_, W_r):
                              kb.op("dve", lambda: DVE.memset(Sst[:], 0.0), W=[Sst_r])
                              kb.op("dve", lambda: DVE.memset(Sbf[:], 0.0), W=[Sbf_r])
                              for i in range(3):
                                  kb.op("pool", lambda i=i: POOL.memset(halo[:, i, :], 0.0), W=[halo_r[i]])
                              for tt in range(NT):
                                  tsl = slice(tt * TT, (tt + 1) * TT)
                                  hres = [hr[kc][tt] for kc in range(KC)]
                                  Gc_, Gc_r = Grow[tt % 2], Grow_r[tt % 2]
                                  Gp_, Gp_r = Grow[(tt + 1) % 2], Grow_r[(tt + 1) % 2]
                                  pb, pr = ps()
                                  for kc in range(KC):
                                      mm(pb[0:1, :], wsm[:, kc, h:h + 1], hT[:, kc, tsl], kc == 0, kc == KC - 1, R=[r_wsm, hres[kc]], W=[pr])
                                  act(rowA[:], pb[0:1, :], AF.Exp, R=[pr], W=[rowA_r], scale=-1.0)
                                  act(rowA[:], rowA[:], AF.Ln, R=[rowA_r], W=[rowA_r], bias=1.0)
                                  pb, pr = ps()
                                  for kc in range(KC):
                                      mm(pb[0:1, :], wsm[:, kc, 4 + h:5 + h], hT[:, kc, tsl], kc == 0, kc == KC - 1, R=[r_wsm, hres[kc]], W=[pr])
                                  act(rowB[:], pb[0:1, :], AF.Exp, R=[pr, r_const], W=[rowB_r], bias=hv[0:1, l * 8 + 4 + h:l * 8 + 5 + h])
                                  act(rowB[:], rowB[:], AF.Ln, R=[rowB_r], W=[rowB_r], bias=1.0)
                                  ts_("dve", rowB[:], rowB[:], nA[0:1, h:h + 1], None, ALU.mult, None, R=[rowB_r, nA_r], W=[rowB_r])
                                  if tt == 0:
                                      kb.op("dve", lambda: DVE.memset(Gc_[:, 0:1], 0.0), W=[Gc_r])
                                  else:
                                      kb.op("dve", lambda: DVE.tensor_copy(Gc_[:, 0:1], Gp_[:, TT:TT + 1]), R=[Gp_r], W=[Gc_r])
                                  kb.op("dve", lambda: DVE.tensor_tensor_scan(Gc_[:, 1:1 + TT], onesf[0:1, 0:1].to_broadcast([1, TT]), rowB[:], Gc_[:, 0:1], ALU.mult, ALU.add),
                                        R=[r_const, rowB_r, Gc_r], W=[Gc_r])
                                  Gv = Gc_[:, 1:1 + TT]
                                  Gst4 = Gc_[:, 0:TT:128].unsqueeze(2).to_broadcast([1, 4, 128])
                                  Gla4 = Gc_[:, 128:TT + 1:128].unsqueeze(2).to_broadcast([1, 4, 128])
                                  tt_("dve", rGb[:], Gv, rowA[:], ALU.subtract, R=[Gc_r, rowA_r], W=[rGb_r])
                                  tt_("dve", v4(rkb[:]), v4(rGb[:]), Gst4, ALU.subtract, R=[rGb_r, Gc_r], W=[rkb_r])
                                  act(rkb[:], rkb[:], AF.Exp, R=[rkb_r], W=[rkb_r])
                                  rkd, rkd_r = rowB, rowB_r
                                  rbe, rbe_r = rowA, rowA_r
                                  tt_("dve", v4(rkd[:]), v4(Gv), Gla4, ALU.subtract, R=[Gc_r], W=[rkd_r])
                                  act(rkd[:], rkd[:], AF.Exp, R=[rkd_r], W=[rkd_r], scale=-1.0)
                                  act(rbe[:], rowA[:], AF.Exp, R=[rowA_r], W=[rbe_r], scale=-1.0)
                                  tt_("dve", rcd[:, 0:4], Gc_[:, 128:TT + 1:128], Gc_[:, 0:TT:128], ALU.subtract, R=[Gc_r], W=[rcd_r])
                                  act(rcd[:, 0:4], rcd[:, 0:4], AF.Exp, R=[rcd_r], W=[rcd_r])
                                  pcol, pcol_r = ps()
                                  for cc in range(4):
                                      csl = slice(cc * 128, (cc + 1) * 128)
                                      for qi, (rw, rw_r) in enumerate(((rkb, rkb_r), (rkd, rkd_r), (rbe, rbe_r))):
                                          kb.op("pe", lambda rw=rw, qi=qi: PE.matmul(pcol[:, cc * 4 + qi:cc * 4 + qi + 1], rw[0:1, csl], onesf[0:1, 0:1], start=True, stop=True),
                                                R=[rw_r, r_const], W=[pcol_r], inc=False)
                                      kb.op("pe", lambda: PE.matmul(pcol[:, cc * 4 + 3:cc * 4 + 4], onesf[0:1, 0:128], rcd[0:1, cc:cc + 1], start=True, stop=True),
                                            R=[rcd_r, r_const], W=[pcol_r], inc=(cc == 3))
                                  kb.op("dve", lambda: DVE.tensor_copy(cols[:], pcol[:, 0:16]), R=[pcol_r], W=[cols_r])
                                  colv = cols[:].rearrange("p (c q) -> p c q", q=4)

                                  yield
                                  for xi, (dst, dst_r) in enumerate(((qT, qT_r), (kT, kT_r), (vT, vT_r))):
                                      pb, pr = ps()
                                      for kc in range(KC):
                                          mm(pb[:], W_[:, kc, xi * 128:(xi + 1) * 128], hT[:, kc, tsl], kc == 0, kc == KC - 1, R=[W_r, hres[kc]], W=[pr])
                                      rw, rw_r = raw1, raw1_r
                                      kb.op("pool", lambda xi=xi: POOL.tensor_copy(rw[:, 0:3], halo[:, xi, :]), R=[halo_r[xi]], W=[rw_r])
                                      act(rw[:, 3:3 + TT], pb[:], AF.Copy, R=[pr], W=[rw_r])
                                      cw = lambda tap_, xi=xi: vcol(32 + tap_ * 12 + xi * 4 + h)
                                      ts_("dve", cacc[:], rw[:, 0:TT], cw(0), None, ALU.mult, None, R=[rw_r, r_const], W=[cacc_r])
                                      for tp in (1, 2, 3):
                                          stt_("dve", cacc[:], rw[:, tp:tp + TT], cw(tp), cacc[:], ALU.mult, ALU.add, R=[rw_r, r_const, cacc_r], W=[cacc_r])
                                      kb.op("pool", lambda xi=xi: POOL.tensor_copy(halo[:, xi, :], rw[:, TT:TT + 3]), R=[rw_r], W=[halo_r[xi]])
                                      if xi == 2:
                                          silu_from("dve", vT[:], cacc[:], tmpf[:], R=[cacc_r], W=[vT_r], tmp_res=tmpf_r)
                                      else:
                                          silu_from("dve", tmpg[:], cacc[:], tmpf[:], R=[cacc_r], W=[tmpg_r], tmp_res=tmpf_r)
                                          act(sqg[:], tmpg[:], AF.Square, R=[tmpg_r], W=[sqg_r])
                                          pb2, pr2 = ps()
                                          mm(pb2[:], ones_b, sqg[:], True, True, R=[sqg_r, r_const], W=[pr2])
                                          act(tmpf[:], pb2[:], AF.Ln, R=[pr2], W=[tmpf_r], bias=EPS)
                                          act(tmpf[:], tmpf[:], AF.Exp, R=[tmpf_r], W=[tmpf_r], scale=-0.5)
                                          sc = (128.0 ** -0.5) if xi == 0 else 1.0
                                          stt_("dve", dst[:], tmpg[:], sc, tmpf[:], ALU.mult, ALU.mult, R=[tmpg_r, tmpf_r], W=[dst_r])
                                  yield
                                  pb, pr = ps()
                                  for kc in range(KC):
                                      mm(pb[:], W_[:, kc, 384:512], hT[:, kc, tsl], kc == 0, kc == KC - 1, R=[W_r, hres[kc]], W=[pr])
                                  silu_from("dve", zs[:], pb[:], tmpf[:], R=[pr], W=[zs_r], tmp_res=tmpf_r)

                                  yield
                                  pt, ptr = psb()
                                  for cc in range(4):
                                      csl = slice(cc * 128, (cc + 1) * 128)
                                      kb.op("pe", lambda: PE.transpose(pt[:, csl], vT[:, csl], ident_b), R=[vT_r, r_const], W=[ptr], inc=(cc == 3))
                                  tt_("dve", v4(vb[:]), v4(pt), colv[:, :, 2:3].to_broadcast([128, 4, 128]), ALU.mult, R=[ptr, cols_r], W=[vb_r])
                                  pt, ptr = psb()
                                  for cc in range(4):
                                      csl = slice(cc * 128, (cc + 1) * 128)
                                      kb.op("pe", lambda: PE.transpose(pt[:, csl], kT[:, csl], ident_b), R=[kT_r, r_const], W=[ptr], inc=(cc == 3))
                                  tt_("dve", v4(kbg[:]), v4(pt), colv[:, :, 0:1].to_broadcast([128, 4, 128]), ALU.mult, R=[ptr, cols_r], W=[kbg_r])
                                  tt_("dve", v4(kdc[:]), v4(pt), colv[:, :, 1:2].to_broadcast([128, 4, 128]), ALU.mult, R=[ptr, cols_r], W=[kdc_r])

                                  yield
                                  def expo(lrow, lrow_r, lneg, rrow, rrow_r, rneg, mask_idx):
                                      pb_, pr_ = ps()
                                      for cc in range(4):
                                          csl = slice(cc * 128, (cc + 1) * 128)
                                          kb.op("pe", lambda: PE.matmul(pb_[:, csl], (negf if rneg else onesf)[0:1, 0:128], rrow[0:1, csl], start=True, stop=False),
                                                R=[rrow_r, r_const], W=[pr_], inc=False)
                                          kb.op("pe", lambda: PE.matmul(pb_[:, csl], lrow[0:1, csl], (negf if lneg else onesf)[0:1, 0:128], start=False, stop=False),
                                                R=[lrow_r, r_const], W=[pr_], inc=False)
                                          kb.op("pe", lambda: PE.matmul(pb_[:, csl], ident_b, CB(mask_idx), start=False, stop=True),
                                                R=[r_const], W=[pr_], inc=(cc == 3))
                                      return pb_, pr_
                                  pkk, pkk_r = ps()
                                  for cc in range(4):
                                      csl = slice(cc * 128, (cc + 1) * 128)
                                      mm(pkk[:, csl], kT[:, csl], kT[:, csl], True, True, R=[kT_r], W=[pkk_r])
                                  pe1, pe1_r = expo(Gv, Gc_r, True, rGb, rGb_r, False, C_MN_STR_T)
                                  act(tmpf[:], pe1[:], AF.Exp, R=[pe1_r], W=[tmpf_r])
                                  tt_("dve", AT[:], pkk[:], tmpf[:], ALU.mult, R=[pkk_r, tmpf_r], W=[AT_r])
                                  pe2, pe2_r = expo(rGb, rGb_r, False, Gv, Gc_r, True, C_MN_STR)
                                  act(tmpg[:], pe2[:], AF.Exp, R=[pe2_r], W=[tmpg_r])
                                  tt_("dve", Am[:], pkk[:], tmpg[:], ALU.mult, R=[pkk_r, tmpg_r], W=[Am_r])
                                  pe3, pe3_r = expo(Gv, Gc_r, True, Gv, Gc_r, False, C_MN_INC_T)
                                  act(tmpf[:], pe3[:], AF.Exp, R=[pe3_r], W=[tmpf_r])
                                  pqk, pqk_r = ps()
                                  for cc in range(4):
                                      csl = slice(cc * 128, (cc + 1) * 128)
                                      mm(pqk[:, csl], kT[:, csl], qT[:, csl], True, True, R=[kT_r, qT_r], W=[pqk_r])
                                  tt_("dve", qkm[:], pqk[:], tmpf[:], ALU.mult, R=[pqk_r, tmpf_r], W=[qkm_r])
                                  rGc, rGc_r = rkb, rkb_r
                                  tt_("dve", v4(rGc[:]), v4(Gv), Gst4, ALU.subtract, R=[Gc_r], W=[rGc_r])
                                  pg, pg_r = ps()
                                  for cc in range(4):
                                      csl = slice(cc * 128, (cc + 1) * 128)
                                      kb.op("pe", lambda: PE.matmul(pg[:, csl], onesf[0:1, 0:128], rGc[0:1, csl], start=True, stop=True),
                                            R=[rGc_r, r_const], W=[pg_r], inc=(cc == 3))
                                  act(tmpg[:], pg[:], AF.Exp, R=[pg_r], W=[tmpg_r])
                                  tt_("dve", qd[:], qT[:], tmpg[:], ALU.mult, R=[qT_r, tmpg_r], W=[qd_r])

                                  yield
                                  tt_("dve", v4(Om[:]), v4(Am[:]), b4(CB(C_LV + 0)), ALU.mult, R=[Am_r, r_const], W=[Om_r])
                                  stt_("dve", v4(Inv[:]), v4(Om[:]), -1.0, b4(ident_b), ALU.mult, ALU.add, R=[Om_r, r_const], W=[Inv_r])
                                  tt_("dve", v4(OT[:]), v4(AT[:]), b4(CB(C_LVT + 0)), ALU.mult, R=[AT_r, r_const], W=[OT_r])
                                  stt_("dve", v4(Rm[:]), v4(OT[:]), -1.0, b4(ident_b), ALU.mult, ALU.add, R=[OT_r, r_const], W=[Rm_r])
                                  for li in range(1, 7):
                                      last = (li == 6)
                                      pz, pz_r = ps()
                                      for cc in range(4):
                                          csl = slice(cc * 128, (cc + 1) * 128)
                                          mm(pz[:, csl], Am[:, csl], Rm[:, csl], True, True, R=[Am_r, Rm_r], W=[pz_r])
                                      if not last:
                                          pzp, pzp_r = ps()
                                          for cc in range(4):
                                              csl = slice(cc * 128, (cc + 1) * 128)
                                              mm(pzp[:, csl], AT[:, csl], Inv[:, csl], True, True, R=[AT_r, Inv_r], W=[pzp_r])
                                      tt_("dve", v4(Zt[:]), v4(pz[:]), b4(CB(C_LVT + li)), ALU.mult, R=[pz_r, r_const], W=[Zt_r])
                                      if not last:
                                          tt_("dve", v4(Zp[:]), v4(pzp[:]), b4(CB(C_LV + li)), ALU.mult, R=[pzp_r, r_const], W=[Zp_r])
                                      pr2_, pr2_r = ps()
                                      for cc in range(4):
                                          csl = slice(cc * 128, (cc + 1) * 128)
                                          mm(pr2_[:, csl], Inv[:, csl], Zt[:, csl], True, True, R=[Inv_r, Zt_r], W=[pr2_r])
                                      if not last:
                                          pi2_, pi2_r = ps()
                                          for cc in range(4):
                                              csl = slice(cc * 128, (cc + 1) * 128)
                                              mm(pi2_[:, csl], Rm[:, csl], Zp[:, csl], True, True, R=[Rm_r, Zp_r], W=[pi2_r])
                                      tt_("dve", Rm[:], Rm[:], pr2_[:], ALU.subtract, R=[Rm_r, pr2_r], W=[Rm_r])
                                      if not last:
                                          tt_("dve", Inv[:], Inv[:], pi2_[:], ALU.subtract, R=[Inv_r, pi2_r], W=[Inv_r])
                                      yield
                                  yield
                                  pw, pw_r = ps()
                                  for cc in range(4):
                                      csl = slice(cc * 128, (cc + 1) * 128)
                                      mm(pw[:, csl], kbg[:, csl], Rm[:, csl], True, True, R=[kbg_r, Rm_r], W=[pw_r])
                                  amul(nwt[:], pw[:], -1.0, R=[pw_r], W=[nwt_r])

                                  yield
                                  for cc in range(4):
                                      csl = slice(cc * 128, (cc + 1) * 128)
                                      vn, vn_r = vnew[cc % 2], vnew_r[cc % 2]
                                      pv, pv_r = ps()
                                      mm(pv[:, 0:128], Rm[:, csl], vb[:, csl], True, False, R=[Rm_r, vb_r], W=[pv_r])
                                      mm(pv[:, 0:128], nwt[:, csl], Sbf[:], False, True, R=[nwt_r, Sbf_r], W=[pv_r])
                                      act(vn[:], pv[:, 0:128], AF.Copy, R=[pv_r], W=[vn_r])
                                      po, po_r = ps()
                                      mm(po[:, 0:128], Sbf[:], qd[:, csl], True, False, R=[Sbf_r, qd_r], W=[po_r])
                                      mm(po[:, 0:128], vn[:], qkm[:, csl], False, True, R=[vn_r, qkm_r], W=[po_r])
                                      pd, pd_r = ps()
                                      mm(pd[:, 0:128], kdc[:, csl], vn[:], True, True, R=[kdc_r, vn_r], W=[pd_r])
                                      act(oraw[:, csl], po[:, 0:128], AF.Copy, R=[po_r], W=[oraw_r])
                                      ts_("dve", Sst[:], Sst[:], cols[:, cc * 4 + 3:cc * 4 + 4], None, ALU.mult, None, R=[Sst_r, cols_r], W=[Sst_r])
                                      tt_("dve", Sst[:], pd[:, 0:128], Sst[:], ALU.add, R=[Sst_r, pd_r], W=[Sst_r])
                                      act(Sbf[:], Sst[:], AF.Copy, R=[Sst_r], W=[Sbf_r])
                                      yield

                                  yield
                                  act(sqg[:], oraw[:], AF.Square, R=[oraw_r], W=[sqg_r])
                                  pb2, pr2 = ps()
                                  mm(pb2[:], ones_b, sqg[:], True, True, R=[sqg_r, r_const], W=[pr2])
                                  rmsnorm_rstd(pb2[:], pr2, 128, tmpf[:], tmpf_r)
                                  stt_("dve", tmpg[:], oraw[:], vcol(80), tmpf[:], ALU.mult, ALU.mult, R=[oraw_r, tmpf_r, r_const], W=[tmpg_r])
                                  tt_("pool", oTa[:, h, tsl], tmpg[:], zs[:], ALU.mult, R=[tmpg_r, zs_r], W=[oTa_r[h][tt]])
                          return run
                      runners = [make_head(0), make_head(1)]
                      for pair in (((0, 1), (2, 3)) if "gdn" in phases else ()):
                          ws = [load_head_weights(l, "gdn", h) for h in pair]
                          gens = [runners[i](h, ws[i][0], ws[i][1]) for i, h in enumerate(pair)]
                          while gens:
                              for g in list(gens):
                                  try:
                                      next(g)
                                  except StopIteration:
                                      gens.remove(g)
                  if l == 0:
                      for h in range(4):
                          tap(f"oTa{h}", oTa[:, h, :], [128, T], [oTa_r[h][t_] for t_ in range(NT)])
                  ms2 = ExitStack()
                  ms2.__enter__()
                  kb.barrier()
                  oTb = sb("oTb", [128, 8, T], BF16, ms2)
                  oTb_r = [[Res() for _ in range(NT)] for _ in range(8)]
                  w2b = sb("w2b", [16, 512], BF16, ms2)
                  r_w2b = Res()
                  kb.dma("pool", [(w2b[:], w2_d[:, l * 512:(l + 1) * 512])], k_w2b, W=[r_w2b])
                  with ExitStack() as gs:
                      S = lambda n, shp, d=F32: sb(n, shp, d, gs)
                      hw["slots"] = [S(f"wsl{i}", [128, KC, 768], BF16) for i in range(2)]
                      hw["res"] = [Res(), Res()]
                      nxt = load_head_weights(l, "gla", 0)
                      glrT = S("glrT", [16, TT], BF16); glr_r = Res()
                      qf = S("qf", [128, TT]); qf_r = Res()
                      kf = S("kf", [128, TT]); kf_r = Res()
                      tmpf = S("tmpf2", [128, TT]); tmpf_r = Res()
                      tmpg = S("tmpg2", [128, TT]); tmpg_r = Res()
                      tmph, tmph_r = tmpg, tmpg_r
                      Pp = [S(f"Pp{i}", [128, 1 + TT]) for i in range(2)]; Pp_r = [Res(), Res()]
                      lrow, lrow_r = tmpg, tmpg_r
                      qk2 = S("qk2", [128, 2, TT], BF16)
                      qin = qk2[:, 0, :]; qin_r = Res()
                      kin = qk2[:, 1, :]; kin_r = Res()
                      qdc = S("qdc", [128, TT], BF16); qdc_r = Res()
                      kdcT = S("kdcT", [128, TT], BF16); kdcT_r = Res()
                      kdt = S("kdt", [128, TT], BF16); kdt_r = Res()
                      vtok = S("vtok", [128, 4, 256], BF16); vtok_r = Res()
                      attn = S("attn", [128, TT], BF16); attn_r = Res()
                      rs = S("rs", [128, 2, TT], BF16); rs_r = Res()
                      oraw2 = S("oraw2", [128, 2, TT]); oraw2_r = Res()
                      S2 = S("S2", [128, 256]); S2_r = Res()
                      S2b = S("S2b", [128, 256], BF16); S2b_r = Res()
                      cdc = S("cdc", [128, 4]); cdc_r = Res()
                      ngb = S("ngb", [128, 4]); ngb_r = Res()
                      ts_("dve", ngb[:], vcol(83, 4), -1.0, None, ALU.mult, None, R=[r_const], W=[ngb_r])
                      for h in (range(4) if "gla" in phases else ()):
                          W_, W_r = nxt
                          nxt = load_head_weights(l, "gla", h + 1) if h < 3 else None
                          kb.op("dve", lambda: DVE.memset(S2[:], 0.0), W=[S2_r])
                          kb.op("dve", lambda: DVE.memset(S2b[:], 0.0), W=[S2b_r])
                          for tt in range(NT):
                              tsl = slice(tt * TT, (tt + 1) * TT)
                              hres = [hr[kc][tt] for kc in range(KC)]
                              Pc, Pc_r = Pp[tt % 2], Pp_r[tt % 2]
                              Pv_, Pv_r = Pp[(tt + 1) % 2], Pp_r[(tt + 1) % 2]
                              pb, pr = ps()
                              for kc in range(KC):
                                  mm(pb[:], W_[:, kc, 0:128], hT[:, kc, tsl], kc == 0, kc == KC - 1, R=[W_r, hres[kc]], W=[pr])
                              amul(qf[:], pb[:], 128.0 ** -0.5, R=[pr], W=[qf_r])
                              pb, pr = ps()
                              for kc in range(KC):
                                  mm(pb[:], W_[:, kc, 128:256], hT[:, kc, tsl], kc == 0, kc == KC - 1, R=[W_r, hres[kc]], W=[pr])
                              act(kf[:], pb[:], AF.Copy, R=[pr], W=[kf_r])
                              for half in range(2):
                                  pb, pr = ps()
                                  for c2 in range(2):
                                      cc = half * 2 + c2
                                      for kc in range(KC):
                                          mm(pb[:, c2 * 256:(c2 + 1) * 256], hT[:, kc, tt * TT + cc * 128: tt * TT + (cc + 1) * 128], W_[:, kc, 256:512],
                                             kc == 0, kc == KC - 1, R=[W_r, hres[kc]], W=[pr])
                                  act(vtok[:, half * 2:half * 2 + 2, :], pb[:].rearrange("p (c e) -> p c e", e=256), AF.Copy, R=[pr], W=[vtok_r])
                              for et in range(2):
                                  pb, pr = ps()
                                  for kc in range(KC):
                                      mm(pb[:], W_[:, kc, 512 + et * 128:512 + (et + 1) * 128], hT[:, kc, tsl], kc == 0, kc == KC - 1, R=[W_r, hres[kc]], W=[pr])
                                  silu_from("dve", rs[:, et, :], pb[:], tmpf[:], R=[pr], W=[rs_r], tmp_res=tmpf_r)
                              pb, pr = ps()
                              for kc in range(KC):
                                  mm(pb[0:16, :], wsm[:, kc, 8:24], hT[:, kc, tsl], kc == 0, kc == KC - 1, R=[r_wsm, hres[kc]], W=[pr])
                              act(glrT[:], pb[0:16, :], AF.Copy, R=[pr], W=[glr_r])
                              pb, pr = ps()
                              mm(pb[:], w2b[0:16, h * 128:(h + 1) * 128], glrT[:], True, True, R=[r_w2b, glr_r], W=[pr])
                              act(lrow[:], pb[:], AF.Exp, R=[pr, ngb_r], W=[lrow_r], bias=ngb[:, h:h + 1], scale=-1.0)
                              act(lrow[:], lrow[:], AF.Ln, R=[lrow_r], W=[lrow_r], bias=1.0)
                              if tt == 0:
                                  kb.op("dve", lambda: DVE.memset(Pc[:, 0:1], 0.0), W=[Pc_r])
                              else:
                                  kb.op("dve", lambda: DVE.tensor_copy(Pc[:, 0:1], Pv_[:, TT:TT + 1]), R=[Pv_r], W=[Pc_r])
                              kb.op("dve", lambda: DVE.tensor_tensor_scan(Pc[:, 1:1 + TT], onesf[:, 0:1].to_broadcast([128, TT]), lrow[:], Pc[:, 0:1], ALU.mult, ALU.add),
                                    R=[r_const, lrow_r, Pc_r], W=[Pc_r])
                              Pvw = v4(Pc[:, 1:1 + TT])
                              Pst = Pc[:, 0:TT:128].unsqueeze(2).to_broadcast([128, 4, 128])
                              Pmid = Pc[:, 65:TT + 1:128].unsqueeze(2).to_broadcast([128, 4, 128])
                              Pla = Pc[:, 128:TT + 1:128].unsqueeze(2).to_broadcast([128, 4, 128])
                              isc = 1.0 / 16.0
                              tt_("dve", v4(tmpf[:]), Pvw, Pmid, ALU.subtract, R=[Pc_r], W=[tmpf_r])
                              act(tmpg[:], tmpf[:], AF.Exp, R=[tmpf_r], W=[tmpg_r], scale=-isc)
                              tt_("dve", qin, qf[:], tmpg[:], ALU.mult, R=[qf_r, tmpg_r], W=[qin_r])
                              act(tmph[:], tmpf[:], AF.Exp, R=[tmpf_r], W=[tmph_r], scale=isc)
                              tt_("dve", kin, kf[:], tmph[:], ALU.mult, R=[kf_r, tmph_r], W=[kin_r])
                              tt_("dve", v4(tmpf[:]), Pvw, Pst, ALU.subtract, R=[Pc_r], W=[tmpf_r])
                              act(tmpg[:], tmpf[:], AF.Exp, R=[tmpf_r], W=[tmpg_r], scale=-isc)
                              tt_("dve", qdc[:], qf[:], tmpg[:], ALU.mult, R=[qf_r, tmpg_r], W=[qdc_r])
                              tt_("dve", v4(tmpf[:]), Pvw, Pla, ALU.subtract, R=[Pc_r], W=[tmpf_r])
                              act(tmph[:], tmpf[:], AF.Exp, R=[tmpf_r], W=[tmph_r], scale=isc)
                              tt_("dve", kdcT[:], kf[:], tmph[:], ALU.mult, R=[kf_r, tmph_r], W=[kdcT_r])
                              tt_("dve", cdc[:], Pc[:, 128:TT + 1:128], Pc[:, 0:TT:128], ALU.subtract, R=[Pc_r], W=[cdc_r])
                              act(cdc[:], cdc[:], AF.Exp, R=[cdc_r], W=[cdc_r], scale=-isc)
                              pa, pa_r = ps()
                              for cc in range(4):
                                  csl = slice(cc * 128, (cc + 1) * 128)
                                  mm(pa[:, csl], kin[:, csl], qin[:, csl], True, True, R=[kin_r, qin_r], W=[pa_r])
                              tt_("dve", v4(attn[:]), v4(pa[:]), b4(CB(C_C01_T)), ALU.mult, R=[pa_r, r_const], W=[attn_r])
                              pt, ptr = psb()
                              for cc in range(4):
                                  csl = slice(cc * 128, (cc + 1) * 128)
                                  kb.op("pe", lambda: PE.transpose(pt[:, csl], kdcT[:, csl], ident_b), R=[kdcT_r, r_const], W=[ptr], inc=(cc == 3))
                              act(kdt[:], pt, AF.Copy, R=[ptr], W=[kdt_r])
                              for cc in range(4):
                                  csl = slice(cc * 128, (cc + 1) * 128)
                                  po, po_r = ps()
                                  for et in range(2):
                                      esl = slice(et * 128, (et + 1) * 128)
                                      mm(po[:, esl], vtok[:, cc, esl], attn[:, csl], True, False, R=[vtok_r, attn_r], W=[po_r])
                                      mm(po[:, esl], S2b[:, esl], qdc[:, csl], False, True, R=[S2b_r, qdc_r], W=[po_r])
                                  pd, pd_r = ps()
                                  mm(pd[:, 0:256], kdt[:, csl], vtok[:, cc, :], True, True, R=[kdt_r, vtok_r], W=[pd_r])
                                  act(oraw2[:, :, csl], po[:, 0:256].rearrange("p (e k) -> p e k", k=128), AF.Copy, R=[po_r], W=[oraw2_r])
                                  ts_("dve", S2[:], S2[:], cdc[:, cc:cc + 1], None, ALU.mult, None, R=[S2_r, cdc_r], W=[S2_r])
                                  tt_("dve", S2[:], pd[:, 0:256], S2[:], ALU.add, R=[S2_r, pd_r], W=[S2_r])
                                  act(S2b[:], S2[:], AF.Copy, R=[S2_r], W=[S2b_r])
                              act(qk2[:], oraw2[:], AF.Square, R=[oraw2_r], W=[qin_r, kin_r])
                              pb2, pr2 = ps()
                              for et in range(2):
                                  mm(pb2[:], ones_b, qk2[:, et, :], et == 0, et == 1, R=[qin_r, kin_r, r_const], W=[pr2])
                              rmsnorm_rstd(pb2[:], pr2, 256, tmpf[:], tmpf_r)
                              for et in range(2):
                                  stt_("dve", tmpg[:], oraw2[:, et, :], vcol(81 + et), tmpf[:], ALU.mult, ALU.mult, R=[oraw2_r, tmpf_r, r_const], W=[tmpg_r])
                                  tt_("pool", oTb[:, h * 2 + et, tsl], tmpg[:], rs[:, et, :], ALU.mult, R=[tmpg_r, rs_r], W=[oTb_r[h * 2 + et][tt]])

                  if l == 0:
                      for h in range(8):
                          tap(f"oTb{h}", oTb[:, h, :], [128, T], [oTb_r[h][t_] for t_ in range(NT)])
                  with ExitStack() as gs:
                      kb.barrier()
                      S = lambda n, shp, d=F32: sb(n, shp, d, gs)
                      NW = 2
                      wm = [S(f"wm{i}", [128, 28, 128], BF16) for i in range(NW)]
                      wm_r = [Res() for _ in range(NW)]
                      wo = [S(f"wo{i}", [128, KC, 128], BF16) for i in range(NW)]
                      wo_r = [Res() for _ in range(NW)]
                      stage = S("stage", [128, KC, TT], BF16); stage_r = [Res() for _ in range(KC)]
                      tmpo = S("tmpo", [128, KC, TT]); tmpo_r = [Res() for _ in range(KC)]
                      sa = S("sa", [128, TT]); sa_r = Res()
                      sb_ = S("sb_", [128, TT]); sb_r = Res()
                      m1 = S("m1", [128, TT]); m1_r = Res()
                      sq = [S(f"sqm{i}", [128, TT], BF16) for i in range(2)]; sq_res = [Res(), Res()]
                      rstd = S("rstdm", [128, TT]); rstd_res = Res()
                      t2 = S("t2", [128, TT]); t2_r = Res()
                      cnt = {"m": 0, "o": 0}

                      def load_wm(ct):
                          i = cnt["m"] % NW
                          cnt["m"] += 1
                          csl = slice(ct * 128, (ct + 1) * 128)
                          pairs = [(wm[i][:, 0:4, :], w_oa_d[l, :, csl].rearrange("(k p) c -> p k c", p=128)),
                                   (wm[i][:, 4:12, :], w_ob_d[l, :, csl].rearrange("(k p) c -> p k c", p=128)),
                                   (wm[i][:, 12:20, :], w_in_d[l, :, O_GA + ct * 128:O_GA + (ct + 1) * 128].rearrange("(k p) c -> p k c", p=128)),
                                   (wm[i][:, 20:28, :], w_in_d[l, :, O_GB + ct * 128:O_GB + (ct + 1) * 128].rearrange("(k p) c -> p k c", p=128))]
                          kb.dma("pool", pairs, wm_k[i], W=[wm_r[i]])
                          return wm[i], wm_r[i]

                      def load_wo(ct):
                          i = cnt["o"] % NW
                          cnt["o"] += 1
                          kb.dma("pool", [(wo[i][:], w_o_d[l, :, ct * 128:(ct + 1) * 128].rearrange("(k p) c -> p k c", p=128))], wo_k[i], W=[wo_r[i]])
                          return wo[i], wo_r[i]

                      for tt in (range(NT) if "merge" in phases else ()):
                          tsl = slice(tt * TT, (tt + 1) * TT)
                          nw = load_wm(0)
                          for ct in range(KC):
                              w_, w_r = nw
                              if ct < KC - 1:
                                  nw = load_wm(ct + 1)
                              pya, pya_r = ps()
                              for k in range(4):
                                  mm(pya[:], w_[:, k, :], oTa[:, k, tsl], k == 0, k == 3, R=[w_r, oTa_r[k][tt]], W=[pya_r])
                              pyb, pyb_r = ps()
                              for k in range(8):
                                  mm(pyb[:], w_[:, 4 + k, :], oTb[:, k, tsl], k == 0, k == 7, R=[w_r, oTb_r[k][tt]], W=[pyb_r])
                              pga, pga_r = ps()
                              for k in range(8):
                                  mm(pga[:], w_[:, 12 + k, :], hT[:, k, tsl], k == 0, k == 7, R=[w_r, hr[k][tt]], W=[pga_r])
                              pgb, pgb_r = ps()
                              for k in range(8):
                                  mm(pgb[:], w_[:, 20 + k, :], hT[:, k, tsl], k == 0, k == 7, R=[w_r, hr[k][tt]], W=[pgb_r])
                              act(sa[:], pga[:], AF.Exp, R=[pga_r], W=[sa_r], scale=-1.0)
                              act(sa[:], sa[:], AF.Ln, R=[sa_r], W=[sa_r], bias=1.0)
                              act(sa[:], sa[:], AF.Exp, R=[sa_r], W=[sa_r], scale=-1.0)
                              act(sb_[:], pgb[:], AF.Exp, R=[pgb_r], W=[sb_r], scale=-1.0)
                              act(sb_[:], sb_[:], AF.Ln, R=[sb_r], W=[sb_r], bias=1.0)
                              act(sb_[:], sb_[:], AF.Exp, R=[sb_r], W=[sb_r], scale=-1.0)
                              tt_("dve", m1[:], pya[:], sa[:], ALU.mult, R=[pya_r, sa_r], W=[m1_r])
                              tt_("dve", sb_[:], pyb[:], sb_[:], ALU.mult, R=[pyb_r, sb_r], W=[sb_r])
                              tt_("pool", stage[:, ct, :], m1[:], sb_[:], ALU.add, R=[m1_r, sb_r], W=[stage_r[ct]])
                          pss, pss_r = pacc, pacc_r
                          nw = load_wo(0)
                          for ct in range(KC):
                              w_, w_r = nw
                              if ct < KC - 1:
                                  nw = load_wo(ct + 1)
                              pb, pr = ps()
                              for k in range(KC):
                                  mm(pb[:], w_[:, k, :], stage[:, k, :], k == 0, k == KC - 1, R=[w_r, stage_r[k]], W=[pr])
                              act(tmpo[:, ct, :], pb[:], AF.Copy, R=[pr], W=[tmpo_r[ct]])
                              s = sq[ct % 2]
                              act(s[:], pb[:], AF.Square, R=[pr], W=[sq_res[ct % 2]])
                              mm(pss[:], ones_b, s[:], ct == 0, ct == KC - 1, R=[sq_res[ct % 2], r_const], W=[pss_r])
                          rmsnorm_rstd(pss[:], pss_r, D, rstd[:], rstd_res)
                          for ct in range(KC):
                              stt_("dve", t2[:], tmpo[:, ct, :], vcol(8 + ct), rstd[:], ALU.mult, ALU.mult, R=[tmpo_r[ct], rstd_res, r_const], W=[t2_r])
                              tt_("pool", xT[:, ct, tsl], xT[:, ct, tsl], t2[:], ALU.add, R=[xr[ct][tt], t2_r], W=[xr[ct][tt]])

                  ms2.close()
              if l == 0:
                  for kc in range(KC):
                      tap(f"xmix{kc}", xT[:, kc, :], [128, T], [xr[kc][t_] for t_ in range(NT)])
              with ExitStack() as gs:
                  kb.barrier()
                  S = lambda n, shp, d=F32: sb(n, shp, d, gs)
                  h2 = S("h2", [128, KC, TT], BF16); h2_r = [Res() for _ in range(KC)]
                  uT = S("uT", [128, 32, TT], BF16); uT_r = [Res() for _ in range(32)]
                  NW = 3
                  wu = [S(f"wu{i}", [128, KC, 512], BF16) for i in range(NW)]; wu_r = [Res() for _ in range(NW)]
                  wd = [S(f"wd{i}", [128, 32, 128], BF16) for i in range(NW)]; wd_r = [Res() for _ in range(NW)]
                  tmpo = S("tmpo2", [128, KC, TT]); tmpo_r = [Res() for _ in range(KC)]
                  sq = [S(f"sqn{i}", [128, TT], BF16) for i in range(2)]; sq_res = [Res(), Res()]
                  rstd = S("rstdn", [128, TT]); rstd_res = Res()
                  rl = S("rl", [128, TT]); rl_r = Res()
                  t2 = S("t2n", [128, TT]); t2_r = Res()
                  cnt = {"u": 0, "d": 0}

                  def load_wu(fb):
                      i = cnt["u"] % NW
                      cnt["u"] += 1
                      kb.dma("pool", [(wu[i][:], w_up_d[l, :, fb * 512:(fb + 1) * 512].rearrange("(k p) c -> p k c", p=128))], wu_k[i], W=[wu_r[i]])
                      return wu[i], wu_r[i]

                  def load_wd(ct):
                      i = cnt["d"] % NW
                      cnt["d"] += 1
                      kb.dma("pool", [(wd[i][:], w_dn_d[l, :, ct * 128:(ct + 1) * 128].rearrange("(k p) c -> p k c", p=128))], wd_k[i], W=[wd_r[i]])
                      return wd[i], wd_r[i]

                  for tt in (range(NT) if "mlp" in phases else ()):
                      tsl = slice(tt * TT, (tt + 1) * TT)
                      q_u = [load_wu(0), load_wu(1)]
                      norm_tile(lambda kc: xT[:, kc, tsl], lambda kc: xr[kc][tt], lambda kc: vcol(16 + kc),
                                lambda kc: h2[:, kc, :], lambda kc: h2_r[kc], sq, sq_res, rstd, rstd_res)
                      for fb in range(8):
                          w_, w_r = q_u.pop(0)
                          if fb + 2 < 8:
                              q_u.append(load_wu(fb + 2))
                          for f4 in range(4):
                              ft = fb * 4 + f4
                              pb, pr = ps()
                              for k in range(KC):
                                  mm(pb[:], w_[:, k, f4 * 128:(f4 + 1) * 128], h2[:, k, :], k == 0, k == KC - 1, R=[w_r, h2_r[k]], W=[pr])
                              act(rl[:], pb[:], AF.Relu, R=[pr], W=[rl_r])
                              tt_("pool" if ft % 2 else "dve", uT[:, ft, :], rl[:], rl[:], ALU.mult, R=[rl_r], W=[uT_r[ft]])
                      q_d = [load_wd(0), load_wd(1)]
                      pss, pss_r = pacc, pacc_r
                      for ct in range(KC):
                          w_, w_r = q_d.pop(0)
                          if ct + 2 < KC:
                              q_d.append(load_wd(ct + 2))
                          pb, pr = ps()
                          for k in range(32):
                              mm(pb[:], w_[:, k, :], uT[:, k, :], k == 0, k == 31, R=[w_r, uT_r[k]], W=[pr])
                          act(tmpo[:, ct, :], pb[:], AF.Copy, R=[pr], W=[tmpo_r[ct]])
                          s = sq[ct % 2]
                          act(s[:], pb[:], AF.Square, R=[pr], W=[sq_res[ct % 2]])
                          mm(pss[:], ones_b, s[:], ct == 0, ct == KC - 1, R=[sq_res[ct % 2], r_const], W=[pss_r])
                      rmsnorm_rstd(pss[:], pss_r, D, rstd[:], rstd_res)
                      for ct in range(KC):
                          stt_("dve", t2[:], tmpo[:, ct, :], vcol(24 + ct), rstd[:], ALU.mult, ALU.mult, R=[tmpo_r[ct], rstd_res, r_const], W=[t2_r])
                          tt_("pool", xT[:, ct, tsl], xT[:, ct, tsl], t2[:], ALU.add, R=[xr[ct][tt], t2_r], W=[xr[ct][tt]])

        k_out = kb.dsem("out")
        kb.dma("sp", [(outT_d[kc * 128:(kc + 1) * 128, :], xT[:, kc, :]) for kc in range(KC)], k_out,
               R=[xr[kc][tt] for kc in range(KC) for tt in range(NT)])
        nc.sync.wait_ge(kb.sems[k_out], kb.cnt[k_out])
        for name, key in tap_out.items():
            nc.sync.wait_ge(kb.sems[key], kb.cnt[key])
        print(f"[build] ops={kb.nops} waits={kb.nwaits} counts={ {k: v for k, v in kb.cnt.items() if not k.startswith('d_')} }", flush=True)
    return nc


def pack_small(inputs, l0, nl):
    vecs = np.zeros((128, nl * NVEC), np.float32)
    hv = np.zeros((1, nl * 8), np.float32)
    w2 = np.zeros((16, nl * 512), np.float32)
    for i in range(nl):
        l = l0 + i
        V0 = i * NVEC
        for j, name in enumerate(("norm_mix_pre", "norm_mix_post", "norm_mlp_pre", "norm_mlp_post")):
            vecs[:, V0 + j * 8:V0 + (j + 1) * 8] = np.asarray(inputs[name][l]).reshape(8, 128).T
        cw = np.asarray(inputs["conv_w"][l])
        for tp in range(4):
            vecs[:, V0 + 32 + tp * 12:V0 + 32 + (tp + 1) * 12] = cw[tp].reshape(12, 128).T
        vecs[:, V0 + 80] = np.asarray(inputs["gdn_norm"][l])
        vecs[:, V0 + 81:V0 + 83] = np.asarray(inputs["gla_norm"][l]).reshape(2, 128).T
        vecs[:, V0 + 83:V0 + 87] = np.asarray(inputs["gla_gate_b"][l]).reshape(4, 128).T
        hv[0, i * 8:i * 8 + 4] = np.asarray(inputs["a_log"][l])
        hv[0, i * 8 + 4:i * 8 + 8] = np.asarray(inputs["dt_bias"][l])
        w2[:, i * 512:(i + 1) * 512] = np.asarray(inputs["gla_gate_w2"][l])
    return vecs, hv, w2


_PROG = {}


def run_layers(xT_list, inputs, l0, nl):
    if nl not in _PROG:
        _PROG[nl] = build_program(nl)
    nc = _PROG[nl]
    vecs, hv, w2 = pack_small(inputs, l0, nl)
    consts = make_consts()
    sl = slice(l0, l0 + nl)
    shared = {
        "w_in": np.ascontiguousarray(inputs["w_in"][sl]), "w_out_a": np.ascontiguousarray(inputs["w_out_a"][sl]),
        "w_out_b": np.ascontiguousarray(inputs["w_out_b"][sl]), "w_o": np.ascontiguousarray(inputs["w_o"][sl]),
        "w_mlp_up": np.ascontiguousarray(inputs["w_mlp_up"][sl]), "w_mlp_down": np.ascontiguousarray(inputs["w_mlp_down"][sl]),
        "gate_w2": w2, "vecs": vecs, "hv": hv, "consts": consts,
    }
    in_maps = [dict(shared, xT=xT_list[c]) for c in range(len(xT_list))]
    res = run_bass_kernel_spmd(nc, in_maps, core_ids=list(range(len(xT_list))))
    return [np.asarray(r["outT"]) for r in res.results]


GDN_STOP = 0
N_FUSED = 4


def kernel(**inputs):
    inputs = {k: np.asarray(v) for k, v in inputs.items()}
    x = inputs["x"].astype(np.float32, copy=False)
    xT = [np.ascontiguousarray(x[b].T) for b in range(x.shape[0])]
    for l0 in range(0, L, N_FUSED):
        xT = run_layers(xT, inputs, l0, N_FUSED)
    out = np.stack([t.T for t in xT], axis=0)
    return np.ascontiguousarray(out.astype(np.float32))
```

```python
import types
import numpy as np
from contextlib import ExitStack
import concourse.bass as bass
import concourse.mybir as mybir
from concourse.bass_utils import run_bass_kernel_spmd

F32 = mybir.dt.float32
BF16 = mybir.dt.bfloat16
AF = mybir.ActivationFunctionType
ALU = mybir.AluOpType

D = 1024
T = 2048
L = 4
DFF = 4096
NCOL = 7192
NT = 4
TT = 512
KC = 8
EPS = 1e-6
O_AQ, O_AK, O_AV, O_AZ, O_AB, O_AA = 0, 512, 1024, 1536, 2048, 2052
O_BQ, O_BK, O_BV, O_BR, O_GLR, O_GA, O_GB = 2056, 2568, 3080, 4104, 5128, 5144, 6168
GDN_STOP = 0
GDN_REC = 0
NVEC = 87
C_ID, C_ONES, C_NEG, C_MN_INC_T, C_MN_STR_T, C_MN_STR, C_C01_T = 0, 1, 2, 3, 4, 5, 6
C_LV = 7
C_LVT = 14
NCB = 21


def make_consts():
    c = np.zeros((NCB, 128, 128), np.float32)
    i = np.arange(128)[:, None]
    j = np.arange(128)[None, :]
    c[C_ID] = (i == j)
    c[C_ONES] = 1.0
    c[C_NEG] = -1.0
    c[C_MN_INC_T] = np.where(j >= i, 0.0, -1e30)
    c[C_MN_STR_T] = np.where(j > i, 0.0, -1e30)
    c[C_MN_STR] = np.where(i > j, 0.0, -1e30)
    c[C_C01_T] = (j >= i)
    for li in range(7):
        m = 1 << li
        mm = ((i // (2 * m)) == (j // (2 * m))) & ((i % (2 * m)) >= m) & ((j % (2 * m)) < m)
        c[C_LV + li] = mm
        c[C_LVT + li] = mm.T
    return np.ascontiguousarray(c.transpose(1, 0, 2).reshape(128, NCB * 128))


def _freeze(fn):
    if fn.__closure__ is None:
        return fn
    cells = tuple(types.CellType(c.cell_contents) for c in fn.__closure__)
    return types.FunctionType(fn.__code__, fn.__globals__, fn.__name__, fn.__defaults__, cells)


class Res:
    __slots__ = ("w", "rs", "p")

    def __init__(self):
        self.w = None
        self.rs = {}
        self.p = set()


class KB:
    def __init__(self, nc, es):
        self.nc = nc
        self.es = es
        self.eng = {"pe": nc.tensor, "dve": nc.vector, "act": nc.scalar, "pool": nc.gpsimd, "sp": nc.sync}
        self.sems = {}
        self.cnt = {}
        self.seen = {e: {} for e in self.eng}
        self.pend = {e: [] for e in self.eng}
        for e in ("pe", "dve", "act", "pool"):
            self.sems[e] = es.enter_context(nc.semaphore("s_" + e))
            self.cnt[e] = 0
        self.nops = 0
        self.nwaits = 0
        self.cap = None

    def dsem(self, name):
        key = "d_" + name
        self.sems[key] = self.es.enter_context(self.nc.semaphore(key))
        self.cnt[key] = 0
        return key

    def flush(self, e):
        if not self.pend[e]:
            return
        self.cnt[e] += 1
        self.pend[e][-1][0].then_inc(self.sems[e], 1)
        ev = (e, self.cnt[e])
        for (_, r2, w2) in self.pend[e]:
            for r in list(r2) + list(w2):
                r.p.discard(e)
            self._reg(ev, r2, w2)
        self.pend[e] = []

    def barrier(self):
        for e in self.eng:
            self.flush(e)
        for e in self.eng:
            for k, v in self.cnt.items():
                if v > 0 and self.seen[e].get(k, 0) < v:
                    self.eng[e].wait_ge(self.sems[k], v)
                    self.seen[e][k] = v
                    self.nwaits += 1

    def _deps(self, e, R, W):
        for r in list(R) + list(W):
            for e2 in list(r.p):
                if e2 != e:
                    self.flush(e2)
        deps = {}

        def add(ev):
            k, v = ev
            if deps.get(k, 0) < v:
                deps[k] = v
        for r in R:
            if r.w is not None:
                add(r.w)
        for w in W:
            if w.w is not None:
                add(w.w)
            for k, v in w.rs.items():
                add((k, v))
        for k, v in deps.items():
            if k == e and e == "pe":
                continue
            if self.seen[e].get(k, 0) >= v:
                continue
            self.eng[e].wait_ge(self.sems[k], v)
            self.seen[e][k] = v
            self.nwaits += 1

    def _reg(self, ev, R, W):
        k, v = ev
        for w in W:
            w.w = ev
            w.rs = {}
        for r in R:
            if r.rs.get(k, 0) < v:
                r.rs[k] = v

    def replay(self, lists):
        idx = [0] * len(lists)
        live = True
        while live:
            live = False
            for k, lst in enumerate(lists):
                if idx[k] < len(lst):
                    item = lst[idx[k]]
                    idx[k] += 1
                    live = True
                    if item[0] == "op":
                        self.op(*item[1:])
                    else:
                        self.dma(*item[1:])

    def op(self, e, fn, R=(), W=(), inc=True):
        if self.cap is not None:
            self.cap.append(("op", e, _freeze(fn), R, W, inc))
            return
        self._deps(e, R, W)
        inst = fn()
        self.nops += 1
        if not inc:
            self.pend[e].append((inst, R, W))
            for r in list(R) + list(W):
                r.p.add(e)
            return
        self.cnt[e] += 1
        inst.then_inc(self.sems[e], 1)
        ev = (e, self.cnt[e])
        for (_, r2, w2) in self.pend[e]:
            for r in list(r2) + list(w2):
                r.p.discard(e)
            self._reg(ev, r2, w2)
        self.pend[e] = []
        self._reg(ev, R, W)

    def dma(self, e, pairs, key, R=(), W=()):
        if self.cap is not None:
            self.cap.append(("dma", e, pairs, key, R, W))
            return
        self._deps(e, R, W)
        for (o, i) in pairs:
            inst = self.eng[e].dma_start(out=o, in_=i)
            inst.then_inc(self.sems[key], 16)
            self.cnt[key] += 16
            self.nops += 1
        ev = (key, self.cnt[key])
        self._reg(ev, R, W)


def build_program(n_layers, taps=(), phases=("gdn", "gla", "merge", "mlp")):
    nc = bass.Bass("TRN2", target_bir_lowering=False)
    NL = n_layers
    dt = lambda name, shape, kind="ExternalInput": nc.dram_tensor(name, shape, F32, kind=kind).ap()
    xT_d = dt("xT", [D, T])
    w_in_d = dt("w_in", [NL, D, NCOL])
    w_oa_d = dt("w_out_a", [NL, 512, D])
    w_ob_d = dt("w_out_b", [NL, 1024, D])
    w_o_d = dt("w_o", [NL, D, D])
    w_up_d = dt("w_mlp_up", [NL, D, DFF])
    w_dn_d = dt("w_mlp_down", [NL, DFF, D])
    w2_d = dt("gate_w2", [16, NL * 512])
    vecs_d = dt("vecs", [128, NL * NVEC])
    hv_d = dt("hv", [1, NL * 8])
    consts_d = dt("consts", [128, NCB * 128])
    outT_d = dt("outT", [D, T], kind="ExternalOutput")
    tap_out = {}

    with ExitStack() as es:
        kb = KB(nc, es)
        PE, DVE, ACT, POOL = nc.tensor, nc.vector, nc.scalar, nc.gpsimd

        uid = {"n": 0}

        def sb(name, shape, dtype=F32, stack=es):
            uid["n"] += 1
            return stack.enter_context(nc.sbuf_tensor(f"s{uid['n']}_{name}", shape, dtype))

        NPB = 5
        pbanks = [es.enter_context(nc.psum_tensor(f"pb{i}", [128, 512], F32)) for i in range(NPB)]
        pres = [Res() for _ in range(NPB)]
        pacc = es.enter_context(nc.psum_tensor("pacc", [128, 512], F32))
        pacc_r = Res()
        pbf = [es.enter_context(nc.psum_tensor(f"pbf{i}", [128, 1024], BF16)) for i in range(2)]
        pbf_res = [Res(), Res()]
        st = {"pi": 0, "bi": 0, "chain": None, "ci": [0, 0]}
        allb = pbanks + [pacc]
        allr = pres + [pacc_r]

        def ps():
            c = st["chain"]
            if c is not None:
                i = 3 * c + st["ci"][c]
                st["ci"][c] = (st["ci"][c] + 1) % 3
                return allb[i], allr[i]
            i = st["pi"]
            st["pi"] = (i + 1) % NPB
            return pbanks[i], pres[i]

        def psb():
            c = st["chain"]
            if c is not None:
                return pbf[c][:, 0:512], pbf_res[c]
            i = st["bi"]
            st["bi"] = (i + 1) % 2
            return pbf[i][:, 0:512], pbf_res[i]

        xT = sb("xT", [128, KC, T])
        xr = [[Res() for _ in range(NT)] for _ in range(KC)]
        cb = sb("cb", [128, NCB * 128], BF16)
        cf = sb("cf", [128, 2 * 128])
        hv = sb("hv", [1, NL * 8])
        r_const = Res()

        def CB(i):
            return cb[:, i * 128:(i + 1) * 128]
        onesf = cf[:, 0:128]
        negf = cf[:, 128:256]
        ident_b = CB(C_ID)
        ones_b = CB(C_ONES)

        k_in = kb.dsem("in")
        kb.dma("sp", [(xT[:, kc, :], xT_d[kc * 128:(kc + 1) * 128, :]) for kc in range(KC)], k_in,
               W=[xr[kc][tt] for kc in range(KC) for tt in range(NT)])
        k_c = kb.dsem("c")
        kb.dma("pool", [(cb[:], consts_d)], k_c, W=[r_const])
        k_c2 = kb.dsem("c2")
        kb.dma("sp", [(cf[:], consts_d[:, 128:384]), (hv[:], hv_d)], k_c2, W=[r_const])
        k_vec = kb.dsem("vec")

        def tap(name, ap, shape, R):
            if name not in taps:
                return
            d = nc.dram_tensor("tap_" + name, shape, F32, kind="ExternalOutput").ap()
            key = kb.dsem("t_" + name)
            tap_out[name] = key
            kb.dma("pool", [(d, ap)], key, R=R)

        def mm(out, lhsT, rhs, start, stop, R, W):
            kb.op("pe", lambda: PE.matmul(out, lhsT, rhs, start=start, stop=stop), R=R, W=W, inc=stop)

        def act(out, in_, func, R, W, bias=None, scale=None):
            kw = {}
            if bias is not None:
                kw["bias"] = bias
            if scale is not None:
                kw["scale"] = scale
            kb.op("act", lambda: ACT.activation(out=out, in_=in_, func=func, **kw), R=R, W=W)

        def amul(out, in_, c, R, W):
            kb.op("act", lambda: ACT.mul(out, in_, c), R=R, W=W)

        def tt_(e, out, in0, in1, op, R, W):
            eng = DVE if e == "dve" else POOL
            kb.op(e, lambda: eng.tensor_tensor(out, in0, in1, op), R=R, W=W)

        def ts_(e, out, in0, s1, s2, op0, op1, R, W):
            eng = DVE if e == "dve" else POOL
            if op1 is None and e == "pool" and op0 in (ALU.mult, ALU.add):
                o1, c2 = (ALU.add, 0.0) if op0 == ALU.mult else (ALU.mult, 1.0)
                kb.op(e, lambda: eng.tensor_scalar(out, in0, s1, c2, op0, o1), R=R, W=W)
            elif op1 is None:
                kb.op(e, lambda: eng.tensor_scalar(out, in0, s1, None, op0), R=R, W=W)
            else:
                kb.op(e, lambda: eng.tensor_scalar(out, in0, s1, s2, op0, op1), R=R, W=W)

        def stt_(e, out, in0, s, in1, op0, op1, R, W):
            eng = DVE if e == "dve" else POOL
            kb.op(e, lambda: eng.scalar_tensor_tensor(out=out, in0=in0, scalar=s, in1=in1, op0=op0, op1=op1), R=R, W=W)

        def b4(ap):
            return ap.unsqueeze(1).to_broadcast([ap.shape[0], 4, 128])

        def v4(ap):
            return ap.rearrange("p (c k) -> p c k", k=128)

        def rmsnorm_rstd(ps_ap, ps_res, n, rstd_ap, rstd_res):
            act(rstd_ap, ps_ap, AF.Ln, R=[ps_res], W=[rstd_res], bias=EPS, scale=1.0 / n)
            act(rstd_ap, rstd_ap, AF.Exp, R=[rstd_res], W=[rstd_res], scale=-0.5)

        def norm_tile(src_fn, src_res_fn, wcol_fn, dst_fn, dst_res_fn, sq, sq_res, rstd, rstd_res):
            pb, pr = ps()
            for kc in range(KC):
                s = sq[kc % 2]
                act(s[:], src_fn(kc), AF.Square, R=[src_res_fn(kc)], W=[sq_res[kc % 2]])
                mm(pb[:], ones_b, s[:], kc == 0, kc == KC - 1, R=[sq_res[kc % 2], r_const], W=[pr])
            rmsnorm_rstd(pb[:], pr, D, rstd[:], rstd_res)
            for kc in range(KC):
                stt_("dve", dst_fn(kc), src_fn(kc), wcol_fn(kc), rstd[:], ALU.mult, ALU.mult,
                     R=[src_res_fn(kc), rstd_res, r_const], W=[dst_res_fn(kc)])

        def silu_from(e_eng, out_ap, x_ap, tmp_ap, R, W, tmp_res):
            act(tmp_ap, x_ap, AF.Exp, R=R, W=[tmp_res], scale=-1.0)
            act(tmp_ap, tmp_ap, AF.Ln, R=[tmp_res], W=[tmp_res], bias=1.0)
            act(tmp_ap, tmp_ap, AF.Exp, R=[tmp_res], W=[tmp_res], scale=-1.0)
            tt_(e_eng, out_ap, x_ap, tmp_ap, ALU.mult, R=list(R) + [tmp_res], W=W)

        wslot_key = [kb.dsem("ws0"), kb.dsem("ws1")]
        hw = {"n": 0, "slots": None, "res": None}

        def load_head_weights(l, kind, h):
            i = hw["n"] % 2
            hw["n"] += 1
            w = hw["slots"][i]
            if kind == "gdn":
                cols = [(O_AQ + h * 128, 128, 0), (O_AK + h * 128, 128, 128), (O_AV + h * 128, 128, 256), (O_AZ + h * 128, 128, 384)]
            else:
                cols = [(O_BQ + h * 128, 128, 0), (O_BK + h * 128, 128, 128), (O_BV + h * 256, 256, 256), (O_BR + h * 256, 256, 512)]
            pairs = [(w[:, :, o:o + n], w_in_d[l, :, c0:c0 + n].rearrange("(kc p) c -> p kc c", p=128)) for (c0, n, o) in cols]
            kb.dma("pool", pairs, wslot_key[i], W=[hw["res"][i]])
            return w, hw["res"][i]

        k_wsm = kb.dsem("wsm")
        k_w2b = kb.dsem("w2b")
        wm_k = [kb.dsem(f"wm{i}") for i in range(2)]
        wo_k = [kb.dsem(f"wo{i}") for i in range(2)]
        wu_k = [kb.dsem(f"wu{i}") for i in range(3)]
        wd_k = [kb.dsem(f"wd{i}") for i in range(3)]

        for l in range(NL):
            with ExitStack() as ls:
              vecs = sb("vecs", [128, NVEC], F32, ls)
              kb.barrier()
              kb.dma("sp", [(vecs[:], vecs_d[:, l * NVEC:(l + 1) * NVEC])], k_vec, W=[r_const])

              def vcol(off, n=1, vecs=vecs):
                  return vecs[:, off:off + n]
              with ExitStack() as ms:
                  hT = sb("hT", [128, KC, T], BF16, ms)
                  hr = [[Res() for _ in range(NT)] for _ in range(KC)]
                  oTa = sb("oTa", [128, 4, T], BF16, ms)
                  oTa_r = [[Res() for _ in range(NT)] for _ in range(4)]
                  wsm = sb("wsm", [128, KC, 24], BF16, ms)
                  r_wsm = Res()
                  with nc.allow_non_contiguous_dma(reason="small gate columns"):
                      kb.dma("pool", [(wsm[:, :, 0:8], w_in_d[l, :, O_AB:O_AB + 8].rearrange("(kc p) c -> p kc c", p=128)),
                                      (wsm[:, :, 8:24], w_in_d[l, :, O_GLR:O_GLR + 16].rearrange("(kc p) c -> p kc c", p=128)),
                                      ], k_wsm, W=[r_wsm])
                  with ExitStack() as s1:
                      sq = [sb(f"sq{i}", [128, TT], BF16, s1) for i in range(2)]
                      sq_res = [Res(), Res()]
                      rstd = sb("rstd", [128, TT], F32, s1)
                      rstd_res = Res()
                      for tt in range(NT):
                          tsl = slice(tt * TT, (tt + 1) * TT)
                          norm_tile(lambda kc: xT[:, kc, tsl], lambda kc: xr[kc][tt], lambda kc: vcol(kc),
                                    lambda kc: hT[:, kc, tsl], lambda kc: hr[kc][tt], sq, sq_res, rstd, rstd_res)
                  if l == 0:
                      tap("hT", hT[:, 0, :], [128, T], [hr[0][t_] for t_ in range(NT)])

                  with ExitStack() as gs:
                      kb.barrier()
                      S = lambda n, shp, d=F32: sb(n, shp, d, gs)
                      hw["slots"] = [S(f"wsg{i}", [128, KC, 512], BF16) for i in range(2)]
                      hw["res"] = [Res(), Res()]
                      nA = S("nA", [1, 4]); nA_r = Res()
                      act(nA[:], hv[0:1, l * 8:l * 8 + 4], AF.Exp, R=[r_const], W=[nA_r])
                      ts_("dve", nA[:], nA[:], -1.0, None, ALU.mult, None, R=[nA_r], W=[nA_r])

                      def make_head(i):
                          halo = S("halo", [128, 3, 3]); halo_r = [Res() for _ in range(3)]
                          raw1 = S("raw1", [128, 3 + TT]); raw1_r = Res()
                          cacc = S("cacc", [128, TT]); cacc_r = Res()
                          tmpf = S("tmpf", [128, TT]); tmpf_r = Res()
                          tmpg = S("tmpg", [128, TT]); tmpg_r = Res()
                          qT = S("qT", [128, TT], BF16); qT_r = Res()
                          kT = S("kT", [128, TT], BF16); kT_r = Res()
                          vT = S("vT", [128, TT], BF16); vT_r = Res()
                          zs = S("zs", [128, TT], BF16); zs_r = Res()
                          vb = S("vb", [128, TT], BF16); vb_r = Res()
                          kbg = S("kbg", [128, TT], BF16); kbg_r = Res()
                          kdc = S("kdc", [128, TT], BF16); kdc_r = Res()
                          Am = S("Am", [128, TT], BF16); Am_r = Res()
                          AT = S("AT", [128, TT], BF16); AT_r = Res()
                          Om = S("Om", [128, TT], BF16); Om_r = Res()
                          OT = S("OT", [128, TT], BF16); OT_r = Res()
                          Zp, Zp_r = OT, OT_r
                          Zt, Zt_r = Om, Om_r
                          Inv = S("Inv", [128, TT], BF16); Inv_r = Res()
                          Rm = S("Rm", [128, TT], BF16); Rm_r = Res()
                          qkm = S("qkm", [128, TT], BF16); qkm_r = Res()
                          qd = S("qd", [128, TT], BF16); qd_r = Res()
                          nwt, nwt_r = vT, vT_r
                          oraw, oraw_r = cacc, cacc_r
                          sqg, sqg_r = Om, Om_r
                          vnew = [S("vnew", [128, 128], BF16)] * 2
                          vnew_r = [Res()] * 2
                          Sst = S("Sst", [128, 128]); Sst_r = Res()
                          Sbf = S("Sbf", [128, 128], BF16); Sbf_r = Res()
                          cols = S("cols", [128, 16]); cols_r = Res()
                          rowA = S("rowA", [1, TT]); rowA_r = Res()
                          rowB = S("rowB", [1, TT]); rowB_r = Res()
                          Grow = [S("Grow", [1, 1 + TT])] * 2
                          Grow_r = [Res()] * 2
                          rGb = S("rGb", [1, TT]); rGb_r = Res()
                          rkb = S("rkb", [1, TT]); rkb_r = Res()
                          rcd = S("rcd", [1, 8]); rcd_r = Res()
                          def run(h, W_, W_r):
                              kb.op("dve", lambda: DVE.memset(Sst[:], 0.0), W=[Sst_r])
                              kb.op("dve", lambda: DVE.memset(Sbf[:], 0.0), W=[Sbf_r])
                              for i in range(3):
                                  kb.op("pool", lambda i=i: POOL.memset(halo[:, i, :], 0.0), W=[halo_r[i]])
                              for tt in range(NT):
                                  tsl = slice(tt * TT, (tt + 1) * TT)
                                  hres = [hr[kc][tt] for kc in range(KC)]
                                  Gc_, Gc_r = Grow[tt % 2], Grow_r[tt % 2]
                                  Gp_, Gp_r = Grow[(tt + 1) % 2], Grow_r[(tt + 1) % 2]
                                  pb, pr = ps()
                                  for kc in range(KC):
                                      mm(pb[0:1, :], wsm[:, kc, h:h + 1], hT[:, kc, tsl], kc == 0, kc == KC - 1, R=[r_wsm, hres[kc]], W=[pr])
                                  act(rowA[:], pb[0:1, :], AF.Exp, R=[pr], W=[rowA_r], scale=-1.0)
                                  act(rowA[:], rowA[:], AF.Ln, R=[rowA_r], W=[rowA_r], bias=1.0)
                                  pb, pr = ps()
                                  for kc in range(KC):
                                      mm(pb[0:1, :], wsm[:, kc, 4 + h:5 + h], hT[:, kc, tsl], kc == 0, kc == KC - 1, R=[r_wsm, hres[kc]], W=[pr])
                                  act(rowB[:], pb[0:1, :], AF.Exp, R=[pr, r_const], W=[rowB_r], bias=hv[0:1, l * 8 + 4 + h:l * 8 + 5 + h])
                                  act(rowB[:], rowB[:], AF.Ln, R=[rowB_r], W=[rowB_r], bias=1.0)
                                  ts_("dve", rowB[:], rowB[:], nA[0:1, h:h + 1], None, ALU.mult, None, R=[rowB_r, nA_r], W=[rowB_r])
                                  if tt == 0:
                                      kb.op("dve", lambda: DVE.memset(Gc_[:, 0:1], 0.0), W=[Gc_r])
                                  else:
                                      kb.op("dve", lambda: DVE.tensor_copy(Gc_[:, 0:1], Gp_[:, TT:TT + 1]), R=[Gp_r], W=[Gc_r])
                                  kb.op("dve", lambda: DVE.tensor_tensor_scan(Gc_[:, 1:1 + TT], onesf[0:1, 0:1].to_broadcast([1, TT]), rowB[:], Gc_[:, 0:1], ALU.mult, ALU.add),
                                        R=[r_const, rowB_r, Gc_r], W=[Gc_r])
                                  Gv = Gc_[:, 1:1 + TT]
                                  Gst4 = Gc_[:, 0:TT:128].unsqueeze(2).to_broadcast([1, 4, 128])
                                  Gla4 = Gc_[:, 128:TT + 1:128].unsqueeze(2).to_broadcast([1, 4, 128])
                                  tt_("dve", rGb[:], Gv, rowA[:], ALU.subtract, R=[Gc_r, rowA_r], W=[rGb_r])
                                  tt_("dve", v4(rkb[:]), v4(rGb[:]), Gst4, ALU.subtract, R=[rGb_r, Gc_r], W=[rkb_r])
                                  act(rkb[:], rkb[:], AF.Exp, R=[rkb_r], W=[rkb_r])
                                  rkd, rkd_r = rowB, rowB_r
                                  rbe, rbe_r = rowA, rowA_r
                                  tt_("dve", v4(rkd[:]), v4(Gv), Gla4, ALU.subtract, R=[Gc_r], W=[rkd_r])
                                  act(rkd[:], rkd[:], AF.Exp, R=[rkd_r], W=[rkd_r], scale=-1.0)
                                  act(rbe[:], rowA[:], AF.Exp, R=[rowA_r], W=[rbe_r], scale=-1.0)
                                  tt_("dve", rcd[:, 0:4], Gc_[:, 128:TT + 1:128], Gc_[:, 0:TT:128], ALU.subtract, R=[Gc_r], W=[rcd_r])
                                  act(rcd[:, 0:4], rcd[:, 0:4], AF.Exp, R=[rcd_r], W=[rcd_r])
                                  pcol, pcol_r = ps()
                                  for cc in range(4):
                                      csl = slice(cc * 128, (cc + 1) * 128)
                                      for qi, (rw, rw_r) in enumerate(((rkb, rkb_r), (rkd, rkd_r), (rbe, rbe_r))):
                                          kb.op("pe", lambda rw=rw, qi=qi: PE.matmul(pcol[:, cc * 4 + qi:cc * 4 + qi + 1], rw[0:1, csl], onesf[0:1, 0:1], start=True, stop=True),
                                                R=[rw_r, r_const], W=[pcol_r], inc=False)
                                      kb.op("pe", lambda: PE.matmul(pcol[:, cc * 4 + 3:cc * 4 + 4], onesf[0:1, 0:128], rcd[0:1, cc:cc + 1], start=True, stop=True),
                                            R=[rcd_r, r_const], W=[pcol_r], inc=(cc == 3))
                                  kb.op("dve", lambda: DVE.tensor_copy(cols[:], pcol[:, 0:16]), R=[pcol_r], W=[cols_r])
                                  colv = cols[:].rearrange("p (c q) -> p c q", q=4)

                                  yield
                                  for xi, (dst, dst_r) in enumerate(((qT, qT_r), (kT, kT_r), (vT, vT_r))):
                                      pb, pr = ps()
                                      for kc in range(KC):
                                          mm(pb[:], W_[:, kc, xi * 128:(xi + 1) * 128], hT[:, kc, tsl], kc == 0, kc == KC - 1, R=[W_r, hres[kc]], W=[pr])
                                      rw, rw_r = raw1, raw1_r
                                      kb.op("pool", lambda xi=xi: POOL.tensor_copy(rw[:, 0:3], halo[:, xi, :]), R=[halo_r[xi]], W=[rw_r])
                                      act(rw[:, 3:3 + TT], pb[:], AF.Copy, R=[pr], W=[rw_r])
                                      cw = lambda tap_, xi=xi: vcol(32 + tap_ * 12 + xi * 4 + h)
                                      ts_("dve", cacc[:], rw[:, 0:TT], cw(0), None, ALU.mult, None, R=[rw_r, r_const], W=[cacc_r])
                                      for tp in (1, 2, 3):
                                          stt_("dve", cacc[:], rw[:, tp:tp + TT], cw(tp), cacc[:], ALU.mult, ALU.add, R=[rw_r, r_const, cacc_r], W=[cacc_r])
                                      kb.op("pool", lambda xi=xi: POOL.tensor_copy(halo[:, xi, :], rw[:, TT:TT + 3]), R=[rw_r], W=[halo_r[xi]])
                                      if xi == 2:
                                          silu_from("dve", vT[:], cacc[:], tmpf[:], R=[cacc_r], W=[vT_r], tmp_res=tmpf_r)
                                      else:
                                          silu_from("dve", tmpg[:], cacc[:], tmpf[:], R=[cacc_r], W=[tmpg_r], tmp_res=tmpf_r)
                                          act(sqg[:], tmpg[:], AF.Square, R=[tmpg_r], W=[sqg_r])
                                          pb2, pr2 = ps()
                                          mm(pb2[:], ones_b, sqg[:], True, True, R=[sqg_r, r_const], W=[pr2])
                                          act(tmpf[:], pb2[:], AF.Ln, R=[pr2], W=[tmpf_r], bias=EPS)
                                          act(tmpf[:], tmpf[:], AF.Exp, R=[tmpf_r], W=[tmpf_r], scale=-0.5)
                                          sc = (128.0 ** -0.5) if xi == 0 else 1.0
                                          stt_("dve", dst[:], tmpg[:], sc, tmpf[:], ALU.mult, ALU.mult, R=[tmpg_r, tmpf_r], W=[dst_r])
                                  yield
                                  pb, pr = ps()
                                  for kc in range(KC):
                                      mm(pb[:], W_[:, kc, 384:512], hT[:, kc, tsl], kc == 0, kc == KC - 1, R=[W_r, hres[kc]], W=[pr])
                                  silu_from("dve", zs[:], pb[:], tmpf[:], R=[pr], W=[zs_r], tmp_res=tmpf_r)

                                  yield
                                  pt, ptr = psb()
                                  for cc in range(4):
                                      csl = slice(cc * 128, (cc + 1) * 128)
                                      kb.op("pe", lambda: PE.transpose(pt[:, csl], vT[:, csl], ident_b), R=[vT_r, r_const], W=[ptr], inc=(cc == 3))
                                  tt_("dve", v4(vb[:]), v4(pt), colv[:, :, 2:3].to_broadcast([128, 4, 128]), ALU.mult, R=[ptr, cols_r], W=[vb_r])
                                  pt, ptr = psb()
                                  for cc in range(4):
                                      csl = slice(cc * 128, (cc + 1) * 128)
                                      kb.op("pe", lambda: PE.transpose(pt[:, csl], kT[:, csl], ident_b), R=[kT_r, r_const], W=[ptr], inc=(cc == 3))
                                  tt_("dve", v4(kbg[:]), v4(pt), colv[:, :, 0:1].to_broadcast([128, 4, 128]), ALU.mult, R=[ptr, cols_r], W=[kbg_r])
                                  tt_("dve", v4(kdc[:]), v4(pt), colv[:, :, 1:2].to_broadcast([128, 4, 128]), ALU.mult, R=[ptr, cols_r], W=[kdc_r])

                                  yield
                                  def expo(lrow, lrow_r, lneg, rrow, rrow_r, rneg, mask_idx):
                                      pb_, pr_ = ps()
                                      for cc in range(4):
                                          csl = slice(cc * 128, (cc + 1) * 128)
                                          kb.op("pe", lambda: PE.matmul(pb_[:, csl], (negf if rneg else onesf)[0:1, 0:128], rrow[0:1, csl], start=True, stop=False),
                                                R=[rrow_r, r_const], W=[pr_], inc=False)
                                          kb.op("pe", lambda: PE.matmul(pb_[:, csl], lrow[0:1, csl], (negf if lneg else onesf)[0:1, 0:128], start=False, stop=False),
                                                R=[lrow_r, r_const], W=[pr_], inc=False)
                                          kb.op("pe", lambda: PE.matmul(pb_[:, csl], ident_b, CB(mask_idx), start=False, stop=True),
                                                R=[r_const], W=[pr_], inc=(cc == 3))
                                      return pb_, pr_
                                  pkk, pkk_r = ps()
                                  for cc in range(4):
                                      csl = slice(cc * 128, (cc + 1) * 128)
                                      mm(pkk[:, csl], kT[:, csl], kT[:, csl], True, True, R=[kT_r], W=[pkk_r])
                                  pe1, pe1_r = expo(Gv, Gc_r, True, rGb, rGb_r, False, C_MN_STR_T)
                                  act(tmpf[:], pe1[:], AF.Exp, R=[pe1_r], W=[tmpf_r])
                                  tt_("dve", AT[:], pkk[:], tmpf[:], ALU.mult, R=[pkk_r, tmpf_r], W=[AT_r])
                                  pe2, pe2_r = expo(rGb, rGb_r, False, Gv, Gc_r, True, C_MN_STR)
                                  act(tmpg[:], pe2[:], AF.Exp, R=[pe2_r], W=[tmpg_r])
                                  tt_("dve", Am[:], pkk[:], tmpg[:], ALU.mult, R=[pkk_r, tmpg_r], W=[Am_r])
                                  pe3, pe3_r = expo(Gv, Gc_r, True, Gv, Gc_r, False, C_MN_INC_T)
                                  act(tmpf[:], pe3[:], AF.Exp, R=[pe3_r], W=[tmpf_r])
                                  pqk, pqk_r = ps()
                                  for cc in range(4):
                                      csl = slice(cc * 128, (cc + 1) * 128)
                                      mm(pqk[:, csl], kT[:, csl], qT[:, csl], True, True, R=[kT_r, qT_r], W=[pqk_r])
                                  tt_("dve", qkm[:], pqk[:], tmpf[:], ALU.mult, R=[pqk_r, tmpf_r], W=[qkm_r])
                                  rGc, rGc_r = rkb, rkb_r
                                  tt_("dve", v4(rGc[:]), v4(Gv), Gst4, ALU.subtract, R=[Gc_r], W=[rGc_r])
                                  pg, pg_r = ps()
                                  for cc in range(4):
                                      csl = slice(cc * 128, (cc + 1) * 128)
                                      kb.op("pe", lambda: PE.matmul(pg[:, csl], onesf[0:1, 0:128], rGc[0:1, csl], start=True, stop=True),
                                            R=[rGc_r, r_const], W=[pg_r], inc=(cc == 3))
                                  act(tmpg[:], pg[:], AF.Exp, R=[pg_r], W=[tmpg_r])
                                  tt_("dve", qd[:], qT[:], tmpg[:], ALU.mult, R=[qT_r, tmpg_r], W=[qd_r])

                                  yield
                                  tt_("dve", v4(Om[:]), v4(Am[:]), b4(CB(C_LV + 0)), ALU.mult, R=[Am_r, r_const], W=[Om_r])
                                  stt_("dve", v4(Inv[:]), v4(Om[:]), -1.0, b4(ident_b), ALU.mult, ALU.add, R=[Om_r, r_const], W=[Inv_r])
                                  tt_("dve", v4(OT[:]), v4(AT[:]), b4(CB(C_LVT + 0)), ALU.mult, R=[AT_r, r_const], W=[OT_r])
                                  stt_("dve", v4(Rm[:]), v4(OT[:]), -1.0, b4(ident_b), ALU.mult, ALU.add, R=[OT_r, r_const], W=[Rm_r])
                                  for li in range(1, 7):
                                      last = (li == 6)
                                      pz, pz_r = ps()
                                      for cc in range(4):
                                          csl = slice(cc * 128, (cc + 1) * 128)
                                          mm(pz[:, csl], Am[:, csl], Rm[:, csl], True, True, R=[Am_r, Rm_r], W=[pz_r])
                                      if not last:
                                          pzp, pzp_r = ps()
                                          for cc in range(4):
                                              csl = slice(cc * 128, (cc + 1) * 128)
                                              mm(pzp[:, csl], AT[:, csl], Inv[:, csl], True, True, R=[AT_r, Inv_r], W=[pzp_r])
                                      tt_("dve", v4(Zt[:]), v4(pz[:]), b4(CB(C_LVT + li)), ALU.mult, R=[pz_r, r_const], W=[Zt_r])
                                      if not last:
                                          tt_("dve", v4(Zp[:]), v4(pzp[:]), b4(CB(C_LV + li)), ALU.mult, R=[pzp_r, r_const], W=[Zp_r])
                                      pr2_, pr2_r = ps()
                                      for cc in range(4):
                                          csl = slice(cc * 128, (cc + 1) * 128)
                                          mm(pr2_[:, csl], Inv[:, csl], Zt[:, csl], True, True, R=[Inv_r, Zt_r], W=[pr2_r])
                                      if not last:
                                          pi2_, pi2_r = ps()
                                          for cc in range(4):
                                              csl = slice(cc * 128, (cc + 1) * 128)
                                              mm(pi2_[:, csl], Rm[:, csl], Zp[:, csl], True, True, R=[Rm_r, Zp_r], W=[pi2_r])
                                      tt_("dve", Rm[:], Rm[:], pr2_[:], ALU.subtract, R=[Rm_r, pr2_r], W=[Rm_r])
                                      if not last:
                                          tt_("dve", Inv[:], Inv[:], pi2_[:], ALU.subtract, R=[Inv_r, pi2_r], W=[Inv_r])
                                      yield
                                  yield
                                  pw, pw_r = ps()
                                  for cc in range(4):
                                      csl = slice(cc * 128, (cc + 1) * 128)
                                      mm(pw[:, csl], kbg[:, csl], Rm[:, csl], True, True, R=[kbg_r, Rm_r], W=[pw_r])
                                  amul(nwt[:], pw[:], -1.0, R=[pw_r], W=[nwt_r])

                                  yield
                                  for cc in range(4):
                                      csl = slice(cc * 128, (cc + 1) * 128)
                                      vn, vn_r = vnew[cc % 2], vnew_r[cc % 2]
                                      pv, pv_r = ps()
                                      mm(pv[:, 0:128], Rm[:, csl], vb[:, csl], True, False, R=[Rm_r, vb_r], W=[pv_r])
                                      mm(pv[:, 0:128], nwt[:, csl], Sbf[:], False, True, R=[nwt_r, Sbf_r], W=[pv_r])
                                      act(vn[:], pv[:, 0:128], AF.Copy, R=[pv_r], W=[vn_r])
                                      po, po_r = ps()
                                      mm(po[:, 0:128], Sbf[:], qd[:, csl], True, False, R=[Sbf_r, qd_r], W=[po_r])
                                      mm(po[:, 0:128], vn[:], qkm[:, csl], False, True, R=[vn_r, qkm_r], W=[po_r])
                                      pd, pd_r = ps()
                                      mm(pd[:, 0:128], kdc[:, csl], vn[:], True, True, R=[kdc_r, vn_r], W=[pd_r])
                                      act(oraw[:, csl], po[:, 0:128], AF.Copy, R=[po_r], W=[oraw_r])
                                      ts_("dve", Sst[:], Sst[:], cols[:, cc * 4 + 3:cc * 4 + 4], None, ALU.mult, None, R=[Sst_r, cols_r], W=[Sst_r])
                                      tt_("dve", Sst[:], pd[:, 0:128], Sst[:], ALU.add, R=[Sst_r, pd_r], W=[Sst_r])
                                      act(Sbf[:], Sst[:], AF.Copy, R=[Sst_r], W=[Sbf_r])
                                      yield

                                  yield
                                  act(sqg[:], oraw[:], AF.Square, R=[oraw_r], W=[sqg_r])
                                  pb2, pr2 = ps()
                                  mm(pb2[:], ones_b, sqg[:], True, True, R=[sqg_r, r_const], W=[pr2])
                                  rmsnorm_rstd(pb2[:], pr2, 128, tmpf[:], tmpf_r)
                                  stt_("dve", tmpg[:], oraw[:], vcol(80), tmpf[:], ALU.mult, ALU.mult, R=[oraw_r, tmpf_r, r_const], W=[tmpg_r])
                                  tt_("pool", oTa[:, h, tsl], tmpg[:], zs[:], ALU.mult, R=[tmpg_r, zs_r], W=[oTa_r[h][tt]])
                          return run
                      runners = [make_head(0), make_head(1)]
                      for pair in (((0, 1), (2, 3)) if "gdn" in phases else ()):
                          ws = [load_head_weights(l, "gdn", h) for h in pair]
                          caps = []
                          for i, h in enumerate(pair):
                              st["chain"] = i
                              kb.cap = []
                              for _ in runners[i](h, ws[i][0], ws[i][1]):
                                  pass
                              caps.append(kb.cap)
                              kb.cap = None
                          st["chain"] = None
                          kb.replay(caps)
                  if l == 0:
                      for h in range(4):
                          tap(f"oTa{h}", oTa[:, h, :], [128, T], [oTa_r[h][t_] for t_ in range(NT)])
                  ms2 = ExitStack()
                  ms2.__enter__()
                  kb.barrier()
                  oTb = sb("oTb", [128, 8, T], BF16, ms2)
                  oTb_r = [[Res() for _ in range(NT)] for _ in range(8)]
                  w2b = sb("w2b", [16, 512], BF16, ms2)
                  r_w2b = Res()
                  kb.dma("pool", [(w2b[:], w2_d[:, l * 512:(l + 1) * 512])], k_w2b, W=[r_w2b])
                  with ExitStack() as gs:
                      S = lambda n, shp, d=F32: sb(n, shp, d, gs)
                      hw["slots"] = [S(f"wsl{i}", [128, KC, 768], BF16) for i in range(2)]
                      hw["res"] = [Res(), Res()]
                      nxt = load_head_weights(l, "gla", 0)
                      glrT = S("glrT", [16, TT], BF16); glr_r = Res()
                      qf = S("qf", [128, TT]); qf_r = Res()
                      kf = S("kf", [128, TT]); kf_r = Res()
                      tmpf = S("tmpf2", [128, TT]); tmpf_r = Res()
                      tmpg = S("tmpg2", [128, TT]); tmpg_r = Res()
                      tmph, tmph_r = tmpg, tmpg_r
                      Pp = [S(f"Pp{i}", [128, 1 + TT]) for i in range(2)]; Pp_r = [Res(), Res()]
                      lrow, lrow_r = tmpg, tmpg_r
                      qk2 = S("qk2", [128, 2, TT], BF16)
                      qin = qk2[:, 0, :]; qin_r = Res()
                      kin = qk2[:, 1, :]; kin_r = Res()
                      qdc = S("qdc", [128, TT], BF16); qdc_r = Res()
                      kdcT = S("kdcT", [128, TT], BF16); kdcT_r = Res()
                      kdt = S("kdt", [128, TT], BF16); kdt_r = Res()
                      vtok = S("vtok", [128, 4, 256], BF16); vtok_r = Res()
                      attn = S("attn", [128, TT], BF16); attn_r = Res()
                      rs = S("rs", [128, 2, TT], BF16); rs_r = Res()
                      oraw2 = S("oraw2", [128, 2, TT]); oraw2_r = Res()
                      S2 = S("S2", [128, 256]); S2_r = Res()
                      S2b = S("S2b", [128, 256], BF16); S2b_r = Res()
                      cdc = S("cdc", [128, 4]); cdc_r = Res()
                      ngb = S("ngb", [128, 4]); ngb_r = Res()
                      ts_("dve", ngb[:], vcol(83, 4), -1.0, None, ALU.mult, None, R=[r_const], W=[ngb_r])
                      for h in (range(4) if "gla" in phases else ()):
                          W_, W_r = nxt
                          nxt = load_head_weights(l, "gla", h + 1) if h < 3 else None
                          kb.op("dve", lambda: DVE.memset(S2[:], 0.0), W=[S2_r])
                          kb.op("dve", lambda: DVE.memset(S2b[:], 0.0), W=[S2b_r])
                          for tt in range(NT):
                              tsl = slice(tt * TT, (tt + 1) * TT)
                              hres = [hr[kc][tt] for kc in range(KC)]
                              Pc, Pc_r = Pp[tt % 2], Pp_r[tt % 2]
                              Pv_, Pv_r = Pp[(tt + 1) % 2], Pp_r[(tt + 1) % 2]
                              pb, pr = ps()
                              for kc in range(KC):
                                  mm(pb[:], W_[:, kc, 0:128], hT[:, kc, tsl], kc == 0, kc == KC - 1, R=[W_r, hres[kc]], W=[pr])
                              amul(qf[:], pb[:], 128.0 ** -0.5, R=[pr], W=[qf_r])
                              pb, pr = ps()
                              for kc in range(KC):
                                  mm(pb[:], W_[:, kc, 128:256], hT[:, kc, tsl], kc == 0, kc == KC - 1, R=[W_r, hres[kc]], W=[pr])
                              act(kf[:], pb[:], AF.Copy, R=[pr], W=[kf_r])
                              for half in range(2):
                                  pb, pr = ps()
                                  for c2 in range(2):
                                      cc = half * 2 + c2
                                      for kc in range(KC):
                                          mm(pb[:, c2 * 256:(c2 + 1) * 256], hT[:, kc, tt * TT + cc * 128: tt * TT + (cc + 1) * 128], W_[:, kc, 256:512],
                                             kc == 0, kc == KC - 1, R=[W_r, hres[kc]], W=[pr])
                                  act(vtok[:, half * 2:half * 2 + 2, :], pb[:].rearrange("p (c e) -> p c e", e=256), AF.Copy, R=[pr], W=[vtok_r])
                              for et in range(2):
                                  pb, pr = ps()
                                  for kc in range(KC):
                                      mm(pb[:], W_[:, kc, 512 + et * 128:512 + (et + 1) * 128], hT[:, kc, tsl], kc == 0, kc == KC - 1, R=[W_r, hres[kc]], W=[pr])
                                  silu_from("dve", rs[:, et, :], pb[:], tmpf[:], R=[pr], W=[rs_r], tmp_res=tmpf_r)
                              pb, pr = ps()
                              for kc in range(KC):
                                  mm(pb[0:16, :], wsm[:, kc, 8:24], hT[:, kc, tsl], kc == 0, kc == KC - 1, R=[r_wsm, hres[kc]], W=[pr])
                              act(glrT[:], pb[0:16, :], AF.Copy, R=[pr], W=[glr_r])
                              pb, pr = ps()
                              mm(pb[:], w2b[0:16, h * 128:(h + 1) * 128], glrT[:], True, True, R=[r_w2b, glr_r], W=[pr])
                              act(lrow[:], pb[:], AF.Exp, R=[pr, ngb_r], W=[lrow_r], bias=ngb[:, h:h + 1], scale=-1.0)
                              act(lrow[:], lrow[:], AF.Ln, R=[lrow_r], W=[lrow_r], bias=1.0)
                              if tt == 0:
                                  kb.op("dve", lambda: DVE.memset(Pc[:, 0:1], 0.0), W=[Pc_r])
                              else:
                                  kb.op("dve", lambda: DVE.tensor_copy(Pc[:, 0:1], Pv_[:, TT:TT + 1]), R=[Pv_r], W=[Pc_r])
                              kb.op("dve", lambda: DVE.tensor_tensor_scan(Pc[:, 1:1 + TT], onesf[:, 0:1].to_broadcast([128, TT]), lrow[:], Pc[:, 0:1], ALU.mult, ALU.add),
                                    R=[r_const, lrow_r, Pc_r], W=[Pc_r])
                              Pvw = v4(Pc[:, 1:1 + TT])
                              Pst = Pc[:, 0:TT:128].unsqueeze(2).to_broadcast([128, 4, 128])
                              Pmid = Pc[:, 65:TT + 1:128].unsqueeze(2).to_broadcast([128, 4, 128])
                              Pla = Pc[:, 128:TT + 1:128].unsqueeze(2).to_broadcast([128, 4, 128])
                              isc = 1.0 / 16.0
                              tt_("dve", v4(tmpf[:]), Pvw, Pmid, ALU.subtract, R=[Pc_r], W=[tmpf_r])
                              act(tmpg[:], tmpf[:], AF.Exp, R=[tmpf_r], W=[tmpg_r], scale=-isc)
                              tt_("dve", qin, qf[:], tmpg[:], ALU.mult, R=[qf_r, tmpg_r], W=[qin_r])
                              act(tmph[:], tmpf[:], AF.Exp, R=[tmpf_r], W=[tmph_r], scale=isc)
                              tt_("dve", kin, kf[:], tmph[:], ALU.mult, R=[kf_r, tmph_r], W=[kin_r])
                              tt_("dve", v4(tmpf[:]), Pvw, Pst, ALU.subtract, R=[Pc_r], W=[tmpf_r])
                              act(tmpg[:], tmpf[:], AF.Exp, R=[tmpf_r], W=[tmpg_r], scale=-isc)
                              tt_("dve", qdc[:], qf[:], tmpg[:], ALU.mult, R=[qf_r, tmpg_r], W=[qdc_r])
                              tt_("dve", v4(tmpf[:]), Pvw, Pla, ALU.subtract, R=[Pc_r], W=[tmpf_r])
                              act(tmph[:], tmpf[:], AF.Exp, R=[tmpf_r], W=[tmph_r], scale=isc)
                              tt_("dve", kdcT[:], kf[:], tmph[:], ALU.mult, R=[kf_r, tmph_r], W=[kdcT_r])
                              tt_("dve", cdc[:], Pc[:, 128:TT + 1:128], Pc[:, 0:TT:128], ALU.subtract, R=[Pc_r], W=[cdc_r])
                              act(cdc[:], cdc[:], AF.Exp, R=[cdc_r], W=[cdc_r], scale=-isc)
                              pa, pa_r = ps()
                              for cc in range(4):
                                  csl = slice(cc * 128, (cc + 1) * 128)
                                  mm(pa[:, csl], kin[:, csl], qin[:, csl], True, True, R=[kin_r, qin_r], W=[pa_r])
                              tt_("dve", v4(attn[:]), v4(pa[:]), b4(CB(C_C01_T)), ALU.mult, R=[pa_r, r_const], W=[attn_r])
                              pt, ptr = psb()
                              for cc in range(4):
                                  csl = slice(cc * 128, (cc + 1) * 128)
                                  kb.op("pe", lambda: PE.transpose(pt[:, csl], kdcT[:, csl], ident_b), R=[kdcT_r, r_const], W=[ptr], inc=(cc == 3))
                              act(kdt[:], pt, AF.Copy, R=[ptr], W=[kdt_r])
                              for cc in range(4):
                                  csl = slice(cc * 128, (cc + 1) * 128)
                                  po, po_r = ps()
                                  for et in range(2):
                                      esl = slice(et * 128, (et + 1) * 128)
                                      mm(po[:, esl], vtok[:, cc, esl], attn[:, csl], True, False, R=[vtok_r, attn_r], W=[po_r])
                                      mm(po[:, esl], S2b[:, esl], qdc[:, csl], False, True, R=[S2b_r, qdc_r], W=[po_r])
                                  pd, pd_r = ps()
                                  mm(pd[:, 0:256], kdt[:, csl], vtok[:, cc, :], True, True, R=[kdt_r, vtok_r], W=[pd_r])
                                  act(oraw2[:, :, csl], po[:, 0:256].rearrange("p (e k) -> p e k", k=128), AF.Copy, R=[po_r], W=[oraw2_r])
                                  ts_("dve", S2[:], S2[:], cdc[:, cc:cc + 1], None, ALU.mult, None, R=[S2_r, cdc_r], W=[S2_r])
                                  tt_("dve", S2[:], pd[:, 0:256], S2[:], ALU.add, R=[S2_r, pd_r], W=[S2_r])
                                  act(S2b[:], S2[:], AF.Copy, R=[S2_r], W=[S2b_r])
                              act(qk2[:], oraw2[:], AF.Square, R=[oraw2_r], W=[qin_r, kin_r])
                              pb2, pr2 = ps()
                              for et in range(2):
                                  mm(pb2[:], ones_b, qk2[:, et, :], et == 0, et == 1, R=[qin_r, kin_r, r_const], W=[pr2])
                              rmsnorm_rstd(pb2[:], pr2, 256, tmpf[:], tmpf_r)
                              for et in range(2):
                                  stt_("dve", tmpg[:], oraw2[:, et, :], vcol(81 + et), tmpf[:], ALU.mult, ALU.mult, R=[oraw2_r, tmpf_r, r_const], W=[tmpg_r])
                                  tt_("pool", oTb[:, h * 2 + et, tsl], tmpg[:], rs[:, et, :], ALU.mult, R=[tmpg_r, rs_r], W=[oTb_r[h * 2 + et][tt]])

                  if l == 0:
                      for h in range(8):
                          tap(f"oTb{h}", oTb[:, h, :], [128, T], [oTb_r[h][t_] for t_ in range(NT)])
                  with ExitStack() as gs:
                      kb.barrier()
                      S = lambda n, shp, d=F32: sb(n, shp, d, gs)
                      NW = 2
                      wm = [S(f"wm{i}", [128, 28, 128], BF16) for i in range(NW)]
                      wm_r = [Res() for _ in range(NW)]
                      wo = [S(f"wo{i}", [128, KC, 128], BF16) for i in range(NW)]
                      wo_r = [Res() for _ in range(NW)]
                      stage = S("stage", [128, KC, TT], BF16); stage_r = [Res() for _ in range(KC)]
                      tmpo = S("tmpo", [128, KC, TT]); tmpo_r = [Res() for _ in range(KC)]
                      sa = S("sa", [128, TT]); sa_r = Res()
                      sb_ = S("sb_", [128, TT]); sb_r = Res()
                      m1 = S("m1", [128, TT]); m1_r = Res()
                      sq = [S(f"sqm{i}", [128, TT], BF16) for i in range(2)]; sq_res = [Res(), Res()]
                      rstd = S("rstdm", [128, TT]); rstd_res = Res()
                      t2 = S("t2", [128, TT]); t2_r = Res()
                      cnt = {"m": 0, "o": 0}

                      def load_wm(ct):
                          i = cnt["m"] % NW
                          cnt["m"] += 1
                          csl = slice(ct * 128, (ct + 1) * 128)
                          pairs = [(wm[i][:, 0:4, :], w_oa_d[l, :, csl].rearrange("(k p) c -> p k c", p=128)),
                                   (wm[i][:, 4:12, :], w_ob_d[l, :, csl].rearrange("(k p) c -> p k c", p=128)),
                                   (wm[i][:, 12:20, :], w_in_d[l, :, O_GA + ct * 128:O_GA + (ct + 1) * 128].rearrange("(k p) c -> p k c", p=128)),
                                   (wm[i][:, 20:28, :], w_in_d[l, :, O_GB + ct * 128:O_GB + (ct + 1) * 128].rearrange("(k p) c -> p k c", p=128))]
                          kb.dma("pool", pairs, wm_k[i], W=[wm_r[i]])
                          return wm[i], wm_r[i]

                      def load_wo(ct):
                          i = cnt["o"] % NW
                          cnt["o"] += 1
                          kb.dma("pool", [(wo[i][:], w_o_d[l, :, ct * 128:(ct + 1) * 128].rearrange("(k p) c -> p k c", p=128))], wo_k[i], W=[wo_r[i]])
                          return wo[i], wo_r[i]

                      for tt in (range(NT) if "merge" in phases else ()):
                          tsl = slice(tt * TT, (tt + 1) * TT)
                          nw = load_wm(0)
                          for ct in range(KC):
                              w_, w_r = nw
                              if ct < KC - 1:
                                  nw = load_wm(ct + 1)
                              pya, pya_r = ps()
                              for k in range(4):
                                  mm(pya[:], w_[:, k, :], oTa[:, k, tsl], k == 0, k == 3, R=[w_r, oTa_r[k][tt]], W=[pya_r])
                              pyb, pyb_r = ps()
                              for k in range(8):
                                  mm(pyb[:], w_[:, 4 + k, :], oTb[:, k, tsl], k == 0, k == 7, R=[w_r, oTb_r[k][tt]], W=[pyb_r])
                              pga, pga_r = ps()
                              for k in range(8):
                                  mm(pga[:], w_[:, 12 + k, :], hT[:, k, tsl], k == 0, k == 7, R=[w_r, hr[k][tt]], W=[pga_r])
                              pgb, pgb_r = ps()
                              for k in range(8):
                                  mm(pgb[:], w_[:, 20 + k, :], hT[:, k, tsl], k == 0, k == 7, R=[w_r, hr[k][tt]], W=[pgb_r])
                              act(sa[:], pga[:], AF.Exp, R=[pga_r], W=[sa_r], scale=-1.0)
                              act(sa[:], sa[:], AF.Ln, R=[sa_r], W=[sa_r], bias=1.0)
                              act(sa[:], sa[:], AF.Exp, R=[sa_r], W=[sa_r], scale=-1.0)
                              act(sb_[:], pgb[:], AF.Exp, R=[pgb_r], W=[sb_r], scale=-1.0)
                              act(sb_[:], sb_[:], AF.Ln, R=[sb_r], W=[sb_r], bias=1.0)
                              act(sb_[:], sb_[:], AF.Exp, R=[sb_r], W=[sb_r], scale=-1.0)
                              tt_("dve", m1[:], pya[:], sa[:], ALU.mult, R=[pya_r, sa_r], W=[m1_r])
                              tt_("dve", sb_[:], pyb[:], sb_[:], ALU.mult, R=[pyb_r, sb_r], W=[sb_r])
                              tt_("pool", stage[:, ct, :], m1[:], sb_[:], ALU.add, R=[m1_r, sb_r], W=[stage_r[ct]])
                          pss, pss_r = pacc, pacc_r
                          nw = load_wo(0)
                          for ct in range(KC):
                              w_, w_r = nw
                              if ct < KC - 1:
                                  nw = load_wo(ct + 1)
                              pb, pr = ps()
                              for k in range(KC):
                                  mm(pb[:], w_[:, k, :], stage[:, k, :], k == 0, k == KC - 1, R=[w_r, stage_r[k]], W=[pr])
                              act(tmpo[:, ct, :], pb[:], AF.Copy, R=[pr], W=[tmpo_r[ct]])
                              s = sq[ct % 2]
                              act(s[:], pb[:], AF.Square, R=[pr], W=[sq_res[ct % 2]])
                              mm(pss[:], ones_b, s[:], ct == 0, ct == KC - 1, R=[sq_res[ct % 2], r_const], W=[pss_r])
                          rmsnorm_rstd(pss[:], pss_r, D, rstd[:], rstd_res)
                          for ct in range(KC):
                              stt_("dve", t2[:], tmpo[:, ct, :], vcol(8 + ct), rstd[:], ALU.mult, ALU.mult, R=[tmpo_r[ct], rstd_res, r_const], W=[t2_r])
                              tt_("pool", xT[:, ct, tsl], xT[:, ct, tsl], t2[:], ALU.add, R=[xr[ct][tt], t2_r], W=[xr[ct][tt]])

                  ms2.close()
              if l == 0:
                  for kc in range(KC):
                      tap(f"xmix{kc}", xT[:, kc, :], [128, T], [xr[kc][t_] for t_ in range(NT)])
              with ExitStack() as gs:
                  kb.barrier()
                  S = lambda n, shp, d=F32: sb(n, shp, d, gs)
                  h2 = S("h2", [128, KC, TT], BF16); h2_r = [Res() for _ in range(KC)]
                  uT = S("uT", [128, 32, TT], BF16); uT_r = [Res() for _ in range(32)]
                  NW = 3
                  wu = [S(f"wu{i}", [128, KC, 512], BF16) for i in range(NW)]; wu_r = [Res() for _ in range(NW)]
                  wd = [S(f"wd{i}", [128, 32, 128], BF16) for i in range(NW)]; wd_r = [Res() for _ in range(NW)]
                  tmpo = S("tmpo2", [128, KC, TT]); tmpo_r = [Res() for _ in range(KC)]
                  sq = [S(f"sqn{i}", [128, TT], BF16) for i in range(2)]; sq_res = [Res(), Res()]
                  rstd = S("rstdn", [128, TT]); rstd_res = Res()
                  rl = S("rl", [128, TT]); rl_r = Res()
                  t2 = S("t2n", [128, TT]); t2_r = Res()
                  cnt = {"u": 0, "d": 0}

                  def load_wu(fb):
                      i = cnt["u"] % NW
                      cnt["u"] += 1
                      kb.dma("pool", [(wu[i][:], w_up_d[l, :, fb * 512:(fb + 1) * 512].rearrange("(k p) c -> p k c", p=128))], wu_k[i], W=[wu_r[i]])
                      return wu[i], wu_r[i]

                  def load_wd(ct):
                      i = cnt["d"] % NW
                      cnt["d"] += 1
                      kb.dma("pool", [(wd[i][:], w_dn_d[l, :, ct * 128:(ct + 1) * 128].rearrange("(k p) c -> p k c", p=128))], wd_k[i], W=[wd_r[i]])
                      return wd[i], wd_r[i]

                  for tt in (range(NT) if "mlp" in phases else ()):
                      tsl = slice(tt * TT, (tt + 1) * TT)
                      q_u = [load_wu(0), load_wu(1)]
                      norm_tile(lambda kc: xT[:, kc, tsl], lambda kc: xr[kc][tt], lambda kc: vcol(16 + kc),
                                lambda kc: h2[:, kc, :], lambda kc: h2_r[kc], sq, sq_res, rstd, rstd_res)
                      for fb in range(8):
                          w_, w_r = q_u.pop(0)
                          if fb + 2 < 8:
                              q_u.append(load_wu(fb + 2))
                          for f4 in range(4):
                              ft = fb * 4 + f4
                              pb, pr = ps()
                              for k in range(KC):
                                  mm(pb[:], w_[:, k, f4 * 128:(f4 + 1) * 128], h2[:, k, :], k == 0, k == KC - 1, R=[w_r, h2_r[k]], W=[pr])
                              act(rl[:], pb[:], AF.Relu, R=[pr], W=[rl_r])
                              tt_("pool" if ft % 2 else "dve", uT[:, ft, :], rl[:], rl[:], ALU.mult, R=[rl_r], W=[uT_r[ft]])
                      q_d = [load_wd(0), load_wd(1)]
                      pss, pss_r = pacc, pacc_r
                      for ct in range(KC):
                          w_, w_r = q_d.pop(0)
                          if ct + 2 < KC:
                              q_d.append(load_wd(ct + 2))
                          pb, pr = ps()
                          for k in range(32):
                              mm(pb[:], w_[:, k, :], uT[:, k, :], k == 0, k == 31, R=[w_r, uT_r[k]], W=[pr])
                          act(tmpo[:, ct, :], pb[:], AF.Copy, R=[pr], W=[tmpo_r[ct]])
                          s = sq[ct % 2]
                          act(s[:], pb[:], AF.Square, R=[pr], W=[sq_res[ct % 2]])
                          mm(pss[:], ones_b, s[:], ct == 0, ct == KC - 1, R=[sq_res[ct % 2], r_const], W=[pss_r])
                      rmsnorm_rstd(pss[:], pss_r, D, rstd[:], rstd_res)
                      for ct in range(KC):
                          stt_("dve", t2[:], tmpo[:, ct, :], vcol(24 + ct), rstd[:], ALU.mult, ALU.mult, R=[tmpo_r[ct], rstd_res, r_const], W=[t2_r])
                          tt_("pool", xT[:, ct, tsl], xT[:, ct, tsl], t2[:], ALU.add, R=[xr[ct][tt], t2_r], W=[xr[ct][tt]])

        k_out = kb.dsem("out")
        kb.dma("sp", [(outT_d[kc * 128:(kc + 1) * 128, :], xT[:, kc, :]) for kc in range(KC)], k_out,
               R=[xr[kc][tt] for kc in range(KC) for tt in range(NT)])
        nc.sync.wait_ge(kb.sems[k_out], kb.cnt[k_out])
        for name, key in tap_out.items():
            nc.sync.wait_ge(kb.sems[key], kb.cnt[key])
        print(f"[build] ops={kb.nops} waits={kb.nwaits} counts={ {k: v for k, v in kb.cnt.items() if not k.startswith('d_')} }", flush=True)
    return nc


def pack_small(inputs, l0, nl):
    vecs = np.zeros((128, nl * NVEC), np.float32)
    hv = np.zeros((1, nl * 8), np.float32)
    w2 = np.zeros((16, nl * 512), np.float32)
    for i in range(nl):
        l = l0 + i
        V0 = i * NVEC
        for j, name in enumerate(("norm_mix_pre", "norm_mix_post", "norm_mlp_pre", "norm_mlp_post")):
            vecs[:, V0 + j * 8:V0 + (j + 1) * 8] = np.asarray(inputs[name][l]).reshape(8, 128).T
        cw = np.asarray(inputs["conv_w"][l])
        for tp in range(4):
            vecs[:, V0 + 32 + tp * 12:V0 + 32 + (tp + 1) * 12] = cw[tp].reshape(12, 128).T
        vecs[:, V0 + 80] = np.asarray(inputs["gdn_norm"][l])
        vecs[:, V0 + 81:V0 + 83] = np.asarray(inputs["gla_norm"][l]).reshape(2, 128).T
        vecs[:, V0 + 83:V0 + 87] = np.asarray(inputs["gla_gate_b"][l]).reshape(4, 128).T
        hv[0, i * 8:i * 8 + 4] = np.asarray(inputs["a_log"][l])
        hv[0, i * 8 + 4:i * 8 + 8] = np.asarray(inputs["dt_bias"][l])
        w2[:, i * 512:(i + 1) * 512] = np.asarray(inputs["gla_gate_w2"][l])
    return vecs, hv, w2


_PROG = {}


def run_layers(xT_list, inputs, l0, nl):
    if nl not in _PROG:
        _PROG[nl] = build_program(nl)
    nc = _PROG[nl]
    vecs, hv, w2 = pack_small(inputs, l0, nl)
    consts = make_consts()
    sl = slice(l0, l0 + nl)
    shared = {
        "w_in": np.ascontiguousarray(inputs["w_in"][sl]), "w_out_a": np.ascontiguousarray(inputs["w_out_a"][sl]),
        "w_out_b": np.ascontiguousarray(inputs["w_out_b"][sl]), "w_o": np.ascontiguousarray(inputs["w_o"][sl]),
        "w_mlp_up": np.ascontiguousarray(inputs["w_mlp_up"][sl]), "w_mlp_down": np.ascontiguousarray(inputs["w_mlp_down"][sl]),
        "gate_w2": w2, "vecs": vecs, "hv": hv, "consts": consts,
    }
    in_maps = [dict(shared, xT=xT_list[c]) for c in range(len(xT_list))]
    res = run_bass_kernel_spmd(nc, in_maps, core_ids=list(range(len(xT_list))))
    return [np.asarray(r["outT"]) for r in res.results]


GDN_STOP = 0
N_FUSED = 4


def kernel(**inputs):
    inputs = {k: np.asarray(v) for k, v in inputs.items()}
    x = inputs["x"].astype(np.float32, copy=False)
    xT = [np.ascontiguousarray(x[b].T) for b in range(x.shape[0])]
    for l0 in range(0, L, N_FUSED):
        xT = run_layers(xT, inputs, l0, N_FUSED)
    out = np.stack([t.T for t in xT], axis=0)
    return np.ascontiguousarray(out.astype(np.float32))
```

```python
import types
import numpy as np
from contextlib import ExitStack
import concourse.bass as bass
import concourse.mybir as mybir
from concourse.bass_utils import run_bass_kernel_spmd

F32 = mybir.dt.float32
BF16 = mybir.dt.bfloat16
AF = mybir.ActivationFunctionType
ALU = mybir.AluOpType

D = 1024
T = 2048
L = 4
DFF = 4096
NCOL = 7192
NT = 4
TT = 512
KC = 8
EPS = 1e-6
O_AQ, O_AK, O_AV, O_AZ, O_AB, O_AA = 0, 512, 1024, 1536, 2048, 2052
O_BQ, O_BK, O_BV, O_BR, O_GLR, O_GA, O_GB = 2056, 2568, 3080, 4104, 5128, 5144, 6168
GDN_STOP = 0
GDN_REC = 0
NVEC = 87
C_ID, C_ONES, C_NEG, C_MN_INC_T, C_MN_STR_T, C_MN_STR, C_C01_T = 0, 1, 2, 3, 4, 5, 6
C_LV = 7
C_LVT = 14
NCB = 21


def make_consts():
    c = np.zeros((NCB, 128, 128), np.float32)
    i = np.arange(128)[:, None]
    j = np.arange(128)[None, :]
    c[C_ID] = (i == j)
    c[C_ONES] = 1.0
    c[C_NEG] = -1.0
    c[C_MN_INC_T] = np.where(j >= i, 0.0, -1e30)
    c[C_MN_STR_T] = np.where(j > i, 0.0, -1e30)
    c[C_MN_STR] = np.where(i > j, 0.0, -1e30)
    c[C_C01_T] = (j >= i)
    for li in range(7):
        m = 1 << li
        mm = ((i // (2 * m)) == (j // (2 * m))) & ((i % (2 * m)) >= m) & ((j % (2 * m)) < m)
        c[C_LV + li] = mm
        c[C_LVT + li] = mm.T
    return np.ascontiguousarray(c.transpose(1, 0, 2).reshape(128, NCB * 128))


def _freeze(fn):
    if fn.__closure__ is None:
        return fn
    cells = tuple(types.CellType(c.cell_contents) for c in fn.__closure__)
    return types.FunctionType(fn.__code__, fn.__globals__, fn.__name__, fn.__defaults__, cells)


class Res:
    __slots__ = ("w", "rs", "p")

    def __init__(self):
        self.w = None
        self.rs = {}
        self.p = set()


class KB:
    def __init__(self, nc, es):
        self.nc = nc
        self.es = es
        self.eng = {"pe": nc.tensor, "dve": nc.vector, "act": nc.scalar, "pool": nc.gpsimd, "sp": nc.sync}
        self.sems = {}
        self.cnt = {}
        self.seen = {e: {} for e in self.eng}
        self.pend = {e: [] for e in self.eng}
        for e in ("pe", "dve", "act", "pool"):
            self.sems[e] = es.enter_context(nc.semaphore("s_" + e))
            self.cnt[e] = 0
        self.nops = 0
        self.nwaits = 0
        self.cap = None

    def dsem(self, name):
        key = "d_" + name
        self.sems[key] = self.es.enter_context(self.nc.semaphore(key))
        self.cnt[key] = 0
        return key

    def flush(self, e):
        if not self.pend[e]:
            return
        self.cnt[e] += 1
        self.pend[e][-1][0].then_inc(self.sems[e], 1)
        ev = (e, self.cnt[e])
        for (_, r2, w2) in self.pend[e]:
            for r in list(r2) + list(w2):
                r.p.discard(e)
            self._reg(ev, r2, w2)
        self.pend[e] = []

    def barrier(self):
        for e in self.eng:
            self.flush(e)
        for e in self.eng:
            for k, v in self.cnt.items():
                if v > 0 and self.seen[e].get(k, 0) < v:
                    self.eng[e].wait_ge(self.sems[k], v)
                    self.seen[e][k] = v
                    self.nwaits += 1

    def _deps(self, e, R, W):
        for r in list(R) + list(W):
            for e2 in list(r.p):
                if e2 != e:
                    self.flush(e2)
        deps = {}

        def add(ev):
            k, v = ev
            if deps.get(k, 0) < v:
                deps[k] = v
        for r in R:
            if r.w is not None:
                add(r.w)
        for w in W:
            if w.w is not None:
                add(w.w)
            for k, v in w.rs.items():
                add((k, v))
        for k, v in deps.items():
            if k == e and e == "pe":
                continue
            if self.seen[e].get(k, 0) >= v:
                continue
            self.eng[e].wait_ge(self.sems[k], v)
            self.seen[e][k] = v
            self.nwaits += 1

    def _reg(self, ev, R, W):
        k, v = ev
        for w in W:
            w.w = ev
            w.rs = {}
        for r in R:
            if r.rs.get(k, 0) < v:
                r.rs[k] = v

    def replay(self, lists):
        idx = [0] * len(lists)
        live = True
        while live:
            live = False
            for k, lst in enumerate(lists):
                if idx[k] < len(lst):
                    item = lst[idx[k]]
                    idx[k] += 1
                    live = True
                    if item[0] == "op":
                        self.op(*item[1:])
                    else:
                        self.dma(*item[1:])

    def op(self, e, fn, R=(), W=(), inc=True):
        if self.cap is not None:
            self.cap.append(("op", e, _freeze(fn), R, W, inc))
            return
        self._deps(e, R, W)
        inst = fn()
        self.nops += 1
        if not inc:
            self.pend[e].append((inst, R, W))
            for r in list(R) + list(W):
                r.p.add(e)
            return
        self.cnt[e] += 1
        inst.then_inc(self.sems[e], 1)
        ev = (e, self.cnt[e])
        for (_, r2, w2) in self.pend[e]:
            for r in list(r2) + list(w2):
                r.p.discard(e)
            self._reg(ev, r2, w2)
        self.pend[e] = []
        self._reg(ev, R, W)

    def dma(self, e, pairs, key, R=(), W=()):
        if self.cap is not None:
            self.cap.append(("dma", e, pairs, key, R, W))
            return
        self._deps(e, R, W)
        for (o, i) in pairs:
            inst = self.eng[e].dma_start(out=o, in_=i)
            inst.then_inc(self.sems[key], 16)
            self.cnt[key] += 16
            self.nops += 1
        ev = (key, self.cnt[key])
        self._reg(ev, R, W)


def build_program(n_layers, taps=(), phases=("gdn", "gla", "merge", "mlp")):
    nc = bass.Bass("TRN2", target_bir_lowering=False)
    NL = n_layers
    dt = lambda name, shape, kind="ExternalInput": nc.dram_tensor(name, shape, F32, kind=kind).ap()
    xT_d = dt("xT", [D, T])
    w_in_d = dt("w_in", [NL, D, NCOL])
    w_oa_d = dt("w_out_a", [NL, 512, D])
    w_ob_d = dt("w_out_b", [NL, 1024, D])
    w_o_d = dt("w_o", [NL, D, D])
    w_up_d = dt("w_mlp_up", [NL, D, DFF])
    w_dn_d = dt("w_mlp_down", [NL, DFF, D])
    w2_d = dt("gate_w2", [16, NL * 512])
    vecs_d = dt("vecs", [128, NL * NVEC])
    hv_d = dt("hv", [1, NL * 8])
    consts_d = dt("consts", [128, NCB * 128])
    outT_d = dt("outT", [D, T], kind="ExternalOutput")
    tap_out = {}

    with ExitStack() as es:
        kb = KB(nc, es)
        PE, DVE, ACT, POOL = nc.tensor, nc.vector, nc.scalar, nc.gpsimd

        uid = {"n": 0}

        def sb(name, shape, dtype=F32, stack=es):
            uid["n"] += 1
            return stack.enter_context(nc.sbuf_tensor(f"s{uid['n']}_{name}", shape, dtype))

        NPB = 5
        pbanks = [es.enter_context(nc.psum_tensor(f"pb{i}", [128, 512], F32)) for i in range(NPB)]
        pres = [Res() for _ in range(NPB)]
        pacc = es.enter_context(nc.psum_tensor("pacc", [128, 512], F32))
        pacc_r = Res()
        pbf = [es.enter_context(nc.psum_tensor(f"pbf{i}", [128, 1024], BF16)) for i in range(2)]
        pbf_res = [Res(), Res()]
        st = {"pi": 0, "bi": 0, "chain": None, "ci": [0, 0]}
        allb = pbanks + [pacc]
        allr = pres + [pacc_r]

        def ps():
            c = st["chain"]
            if c is not None:
                i = 3 * c + st["ci"][c]
                st["ci"][c] = (st["ci"][c] + 1) % 3
                return allb[i], allr[i]
            i = st["pi"]
            st["pi"] = (i + 1) % NPB
            return pbanks[i], pres[i]

        def psb():
            c = st["chain"]
            if c is not None:
                return pbf[c][:, 0:512], pbf_res[c]
            i = st["bi"]
            st["bi"] = (i + 1) % 2
            return pbf[i][:, 0:512], pbf_res[i]

        xT = sb("xT", [128, KC, T])
        xr = [[Res() for _ in range(NT)] for _ in range(KC)]
        cb = sb("cb", [128, NCB * 128], BF16)
        cf = sb("cf", [128, 2 * 128])
        hv = sb("hv", [1, NL * 8])
        r_const = Res()

        def CB(i):
            return cb[:, i * 128:(i + 1) * 128]
        onesf = cf[:, 0:128]
        negf = cf[:, 128:256]
        ident_b = CB(C_ID)
        ones_b = CB(C_ONES)

        k_in = kb.dsem("in")
        kb.dma("sp", [(xT[:, kc, :], xT_d[kc * 128:(kc + 1) * 128, :]) for kc in range(KC)], k_in,
               W=[xr[kc][tt] for kc in range(KC) for tt in range(NT)])
        k_c = kb.dsem("c")
        kb.dma("pool", [(cb[:], consts_d)], k_c, W=[r_const])
        k_c2 = kb.dsem("c2")
        kb.dma("sp", [(cf[:], consts_d[:, 128:384]), (hv[:], hv_d)], k_c2, W=[r_const])
        k_vec = kb.dsem("vec")

        def tap(name, ap, shape, R):
            if name not in taps:
                return
            d = nc.dram_tensor("tap_" + name, shape, F32, kind="ExternalOutput").ap()
            key = kb.dsem("t_" + name)
            tap_out[name] = key
            kb.dma("pool", [(d, ap)], key, R=R)

        def mm(out, lhsT, rhs, start, stop, R, W):
            kb.op("pe", lambda: PE.matmul(out, lhsT, rhs, start=start, stop=stop), R=R, W=W, inc=stop)

        def act(out, in_, func, R, W, bias=None, scale=None):
            kw = {}
            if bias is not None:
                kw["bias"] = bias
            if scale is not None:
                kw["scale"] = scale
            kb.op("act", lambda: ACT.activation(out=out, in_=in_, func=func, **kw), R=R, W=W)

        def amul(out, in_, c, R, W):
            kb.op("act", lambda: ACT.mul(out, in_, c), R=R, W=W)

        def tt_(e, out, in0, in1, op, R, W):
            eng = DVE if e == "dve" else POOL
            kb.op(e, lambda: eng.tensor_tensor(out, in0, in1, op), R=R, W=W)

        def ts_(e, out, in0, s1, s2, op0, op1, R, W):
            eng = DVE if e == "dve" else POOL
            if op1 is None and e == "pool" and op0 in (ALU.mult, ALU.add):
                o1, c2 = (ALU.add, 0.0) if op0 == ALU.mult else (ALU.mult, 1.0)
                kb.op(e, lambda: eng.tensor_scalar(out, in0, s1, c2, op0, o1), R=R, W=W)
            elif op1 is None:
                kb.op(e, lambda: eng.tensor_scalar(out, in0, s1, None, op0), R=R, W=W)
            else:
                kb.op(e, lambda: eng.tensor_scalar(out, in0, s1, s2, op0, op1), R=R, W=W)

        def stt_(e, out, in0, s, in1, op0, op1, R, W):
            eng = DVE if e == "dve" else POOL
            kb.op(e, lambda: eng.scalar_tensor_tensor(out=out, in0=in0, scalar=s, in1=in1, op0=op0, op1=op1), R=R, W=W)

        def b4(ap):
            return ap.unsqueeze(1).to_broadcast([ap.shape[0], 4, 128])

        def v4(ap):
            return ap.rearrange("p (c k) -> p c k", k=128)

        def rmsnorm_rstd(ps_ap, ps_res, n, rstd_ap, rstd_res):
            act(rstd_ap, ps_ap, AF.Ln, R=[ps_res], W=[rstd_res], bias=EPS, scale=1.0 / n)
            act(rstd_ap, rstd_ap, AF.Exp, R=[rstd_res], W=[rstd_res], scale=-0.5)

        def norm_tile(src_fn, src_res_fn, wcol_fn, dst_fn, dst_res_fn, sq, sq_res, rstd, rstd_res):
            pb, pr = ps()
            for kc in range(KC):
                s = sq[kc % 2]
                act(s[:], src_fn(kc), AF.Square, R=[src_res_fn(kc)], W=[sq_res[kc % 2]])
                mm(pb[:], ones_b, s[:], kc == 0, kc == KC - 1, R=[sq_res[kc % 2], r_const], W=[pr])
            rmsnorm_rstd(pb[:], pr, D, rstd[:], rstd_res)
            for kc in range(KC):
                stt_("dve", dst_fn(kc), src_fn(kc), wcol_fn(kc), rstd[:], ALU.mult, ALU.mult,
                     R=[src_res_fn(kc), rstd_res, r_const], W=[dst_res_fn(kc)])

        def silu_from(e_eng, out_ap, x_ap, tmp_ap, R, W, tmp_res):
            act(tmp_ap, x_ap, AF.Exp, R=R, W=[tmp_res], scale=-1.0)
            act(tmp_ap, tmp_ap, AF.Ln, R=[tmp_res], W=[tmp_res], bias=1.0)
            act(tmp_ap, tmp_ap, AF.Exp, R=[tmp_res], W=[tmp_res], scale=-1.0)
            tt_(e_eng, out_ap, x_ap, tmp_ap, ALU.mult, R=list(R) + [tmp_res], W=W)

        wslot_key = [kb.dsem("ws0"), kb.dsem("ws1")]
        hw = {"n": 0, "slots": None, "res": None}

        def load_head_weights(l, kind, h):
            i = hw["n"] % 2
            hw["n"] += 1
            w = hw["slots"][i]
            if kind == "gdn":
                cols = [(O_AQ + h * 128, 128, 0), (O_AK + h * 128, 128, 128), (O_AV + h * 128, 128, 256), (O_AZ + h * 128, 128, 384)]
            else:
                cols = [(O_BQ + h * 128, 128, 0), (O_BK + h * 128, 128, 128), (O_BV + h * 256, 256, 256), (O_BR + h * 256, 256, 512)]
            pairs = [(w[:, :, o:o + n], w_in_d[l, :, c0:c0 + n].rearrange("(kc p) c -> p kc c", p=128)) for (c0, n, o) in cols]
            kb.dma("pool", pairs, wslot_key[i], W=[hw["res"][i]])
            return w, hw["res"][i]

        k_wsm = kb.dsem("wsm")
        k_w2b = kb.dsem("w2b")
        wm_k = [kb.dsem(f"wm{i}") for i in range(2)]
        wo_k = [kb.dsem(f"wo{i}") for i in range(2)]
        wu_k = [kb.dsem(f"wu{i}") for i in range(4)]
        wd_k = [kb.dsem(f"wd{i}") for i in range(4)]

        for l in range(NL):
            with ExitStack() as ls:
              vecs = sb("vecs", [128, NVEC], F32, ls)
              kb.barrier()
              kb.dma("sp", [(vecs[:], vecs_d[:, l * NVEC:(l + 1) * NVEC])], k_vec, W=[r_const])

              def vcol(off, n=1, vecs=vecs):
                  return vecs[:, off:off + n]
              with ExitStack() as ms:
                  hT = sb("hT", [128, KC, T], BF16, ms)
                  hr = [[Res() for _ in range(NT)] for _ in range(KC)]
                  oTa = sb("oTa", [128, 4, T], BF16, ms)
                  oTa_r = [[Res() for _ in range(NT)] for _ in range(4)]
                  wsm = sb("wsm", [128, KC, 24], BF16, ms)
                  r_wsm = Res()
                  with nc.allow_non_contiguous_dma(reason="small gate columns"):
                      kb.dma("pool", [(wsm[:, :, 0:8], w_in_d[l, :, O_AB:O_AB + 8].rearrange("(kc p) c -> p kc c", p=128)),
                                      (wsm[:, :, 8:24], w_in_d[l, :, O_GLR:O_GLR + 16].rearrange("(kc p) c -> p kc c", p=128)),
                                      ], k_wsm, W=[r_wsm])
                  with ExitStack() as s1:
                      sq = [sb(f"sq{i}", [128, TT], BF16, s1) for i in range(2)]
                      sq_res = [Res(), Res()]
                      rstd = sb("rstd", [128, TT], F32, s1)
                      rstd_res = Res()
                      for tt in range(NT):
                          tsl = slice(tt * TT, (tt + 1) * TT)
                          norm_tile(lambda kc: xT[:, kc, tsl], lambda kc: xr[kc][tt], lambda kc: vcol(kc),
                                    lambda kc: hT[:, kc, tsl], lambda kc: hr[kc][tt], sq, sq_res, rstd, rstd_res)
                  if l == 0:
                      tap("hT", hT[:, 0, :], [128, T], [hr[0][t_] for t_ in range(NT)])

                  with ExitStack() as gs:
                      kb.barrier()
                      S = lambda n, shp, d=F32: sb(n, shp, d, gs)
                      hw["slots"] = [S(f"wsg{i}", [128, KC, 512], BF16) for i in range(2)]
                      hw["res"] = [Res(), Res()]
                      nA = S("nA", [1, 4]); nA_r = Res()
                      act(nA[:], hv[0:1, l * 8:l * 8 + 4], AF.Exp, R=[r_const], W=[nA_r])
                      ts_("dve", nA[:], nA[:], -1.0, None, ALU.mult, None, R=[nA_r], W=[nA_r])

                      def make_head(i):
                          halo = S("halo", [128, 3, 3]); halo_r = [Res() for _ in range(3)]
                          raw1 = S("raw1", [128, 3 + TT]); raw1_r = Res()
                          cacc = S("cacc", [128, TT]); cacc_r = Res()
                          tmpf = S("tmpf", [128, TT]); tmpf_r = Res()
                          tmpg = S("tmpg", [128, TT]); tmpg_r = Res()
                          qT = S("qT", [128, TT], BF16); qT_r = Res()
                          kT = S("kT", [128, TT], BF16); kT_r = Res()
                          vT = S("vT", [128, TT], BF16); vT_r = Res()
                          zs = S("zs", [128, TT], BF16); zs_r = Res()
                          vb = S("vb", [128, TT], BF16); vb_r = Res()
                          kbg = S("kbg", [128, TT], BF16); kbg_r = Res()
                          kdc = S("kdc", [128, TT], BF16); kdc_r = Res()
                          Am = S("Am", [128, TT], BF16); Am_r = Res()
                          AT = S("AT", [128, TT], BF16); AT_r = Res()
                          Om = S("Om", [128, TT], BF16); Om_r = Res()
                          OT = S("OT", [128, TT], BF16); OT_r = Res()
                          Zp, Zp_r = OT, OT_r
                          Zt, Zt_r = Om, Om_r
                          Inv = S("Inv", [128, TT], BF16); Inv_r = Res()
                          Rm = S("Rm", [128, TT], BF16); Rm_r = Res()
                          qkm = S("qkm", [128, TT], BF16); qkm_r = Res()
                          qd = S("qd", [128, TT], BF16); qd_r = Res()
                          nwt, nwt_r = vT, vT_r
                          oraw, oraw_r = cacc, cacc_r
                          sqg, sqg_r = Om, Om_r
                          vnew = [S("vnew", [128, 128], BF16)] * 2
                          vnew_r = [Res()] * 2
                          Sst = S("Sst", [128, 128]); Sst_r = Res()
                          Sbf = S("Sbf", [128, 128], BF16); Sbf_r = Res()
                          cols = S("cols", [128, 16]); cols_r = Res()
                          rowA = S("rowA", [1, TT]); rowA_r = Res()
                          rowB = S("rowB", [1, TT]); rowB_r = Res()
                          Grow = [S("Grow", [1, 1 + TT])] * 2
                          Grow_r = [Res()] * 2
                          rGb = S("rGb", [1, TT]); rGb_r = Res()
                          rkb = S("rkb", [1, TT]); rkb_r = Res()
                          rcd = S("rcd", [1, 8]); rcd_r = Res()
                          def run(h, W_, W_r):
                              kb.op("dve", lambda: DVE.memset(Sst[:], 0.0), W=[Sst_r])
                              kb.op("dve", lambda: DVE.memset(Sbf[:], 0.0), W=[Sbf_r])
                              for i in range(3):
                                  kb.op("pool", lambda i=i: POOL.memset(halo[:, i, :], 0.0), W=[halo_r[i]])
                              for tt in range(NT):
                                  tsl = slice(tt * TT, (tt + 1) * TT)
                                  hres = [hr[kc][tt] for kc in range(KC)]
                                  Gc_, Gc_r = Grow[tt % 2], Grow_r[tt % 2]
                                  Gp_, Gp_r = Grow[(tt + 1) % 2], Grow_r[(tt + 1) % 2]
                                  pb, pr = ps()
                                  for kc in range(KC):
                                      mm(pb[0:1, :], wsm[:, kc, h:h + 1], hT[:, kc, tsl], kc == 0, kc == KC - 1, R=[r_wsm, hres[kc]], W=[pr])
                                  act(rowA[:], pb[0:1, :], AF.Exp, R=[pr], W=[rowA_r], scale=-1.0)
                                  act(rowA[:], rowA[:], AF.Ln, R=[rowA_r], W=[rowA_r], bias=1.0)
                                  pb, pr = ps()
                                  for kc in range(KC):
                                      mm(pb[0:1, :], wsm[:, kc, 4 + h:5 + h], hT[:, kc, tsl], kc == 0, kc == KC - 1, R=[r_wsm, hres[kc]], W=[pr])
                                  act(rowB[:], pb[0:1, :], AF.Exp, R=[pr, r_const], W=[rowB_r], bias=hv[0:1, l * 8 + 4 + h:l * 8 + 5 + h])
                                  act(rowB[:], rowB[:], AF.Ln, R=[rowB_r], W=[rowB_r], bias=1.0)
                                  ts_("dve", rowB[:], rowB[:], nA[0:1, h:h + 1], None, ALU.mult, None, R=[rowB_r, nA_r], W=[rowB_r])
                                  if tt == 0:
                                      kb.op("dve", lambda: DVE.memset(Gc_[:, 0:1], 0.0), W=[Gc_r])
                                  else:
                                      kb.op("dve", lambda: DVE.tensor_copy(Gc_[:, 0:1], Gp_[:, TT:TT + 1]), R=[Gp_r], W=[Gc_r])
                                  kb.op("dve", lambda: DVE.tensor_tensor_scan(Gc_[:, 1:1 + TT], onesf[0:1, 0:1].to_broadcast([1, TT]), rowB[:], Gc_[:, 0:1], ALU.mult, ALU.add),
                                        R=[r_const, rowB_r, Gc_r], W=[Gc_r])
                                  Gv = Gc_[:, 1:1 + TT]
                                  Gst4 = Gc_[:, 0:TT:128].unsqueeze(2).to_broadcast([1, 4, 128])
                                  Gla4 = Gc_[:, 128:TT + 1:128].unsqueeze(2).to_broadcast([1, 4, 128])
                                  tt_("dve", rGb[:], Gv, rowA[:], ALU.subtract, R=[Gc_r, rowA_r], W=[rGb_r])
                                  tt_("dve", v4(rkb[:]), v4(rGb[:]), Gst4, ALU.subtract, R=[rGb_r, Gc_r], W=[rkb_r])
                                  act(rkb[:], rkb[:], AF.Exp, R=[rkb_r], W=[rkb_r])
                                  rkd, rkd_r = rowB, rowB_r
                                  rbe, rbe_r = rowA, rowA_r
                                  tt_("dve", v4(rkd[:]), v4(Gv), Gla4, ALU.subtract, R=[Gc_r], W=[rkd_r])
                                  act(rkd[:], rkd[:], AF.Exp, R=[rkd_r], W=[rkd_r], scale=-1.0)
                                  act(rbe[:], rowA[:], AF.Exp, R=[rowA_r], W=[rbe_r], scale=-1.0)
                                  tt_("dve", rcd[:, 0:4], Gc_[:, 128:TT + 1:128], Gc_[:, 0:TT:128], ALU.subtract, R=[Gc_r], W=[rcd_r])
                                  act(rcd[:, 0:4], rcd[:, 0:4], AF.Exp, R=[rcd_r], W=[rcd_r])
                                  pcol, pcol_r = ps()
                                  for cc in range(4):
                                      csl = slice(cc * 128, (cc + 1) * 128)
                                      for qi, (rw, rw_r) in enumerate(((rkb, rkb_r), (rkd, rkd_r), (rbe, rbe_r))):
                                          kb.op("pe", lambda rw=rw, qi=qi: PE.matmul(pcol[:, cc * 4 + qi:cc * 4 + qi + 1], rw[0:1, csl], onesf[0:1, 0:1], start=True, stop=True),
                                                R=[rw_r, r_const], W=[pcol_r], inc=False)
                                      kb.op("pe", lambda: PE.matmul(pcol[:, cc * 4 + 3:cc * 4 + 4], onesf[0:1, 0:128], rcd[0:1, cc:cc + 1], start=True, stop=True),
                                            R=[rcd_r, r_const], W=[pcol_r], inc=(cc == 3))
                                  kb.op("dve", lambda: DVE.tensor_copy(cols[:], pcol[:, 0:16]), R=[pcol_r], W=[cols_r])
                                  colv = cols[:].rearrange("p (c q) -> p c q", q=4)

                                  yield
                                  for xi, (dst, dst_r) in enumerate(((qT, qT_r), (kT, kT_r), (vT, vT_r))):
                                      pb, pr = ps()
                                      for kc in range(KC):
                                          mm(pb[:], W_[:, kc, xi * 128:(xi + 1) * 128], hT[:, kc, tsl], kc == 0, kc == KC - 1, R=[W_r, hres[kc]], W=[pr])
                                      rw, rw_r = raw1, raw1_r
                                      kb.op("pool", lambda xi=xi: POOL.tensor_copy(rw[:, 0:3], halo[:, xi, :]), R=[halo_r[xi]], W=[rw_r])
                                      act(rw[:, 3:3 + TT], pb[:], AF.Copy, R=[pr], W=[rw_r])
                                      cw = lambda tap_, xi=xi: vcol(32 + tap_ * 12 + xi * 4 + h)
                                      ts_("dve", cacc[:], rw[:, 0:TT], cw(0), None, ALU.mult, None, R=[rw_r, r_const], W=[cacc_r])
                                      for tp in (1, 2, 3):
                                          stt_("dve", cacc[:], rw[:, tp:tp + TT], cw(tp), cacc[:], ALU.mult, ALU.add, R=[rw_r, r_const, cacc_r], W=[cacc_r])
                                      kb.op("pool", lambda xi=xi: POOL.tensor_copy(halo[:, xi, :], rw[:, TT:TT + 3]), R=[rw_r], W=[halo_r[xi]])
                                      if xi == 2:
                                          silu_from("dve", vT[:], cacc[:], tmpf[:], R=[cacc_r], W=[vT_r], tmp_res=tmpf_r)
                                      else:
                                          silu_from("dve", tmpg[:], cacc[:], tmpf[:], R=[cacc_r], W=[tmpg_r], tmp_res=tmpf_r)
                                          act(sqg[:], tmpg[:], AF.Square, R=[tmpg_r], W=[sqg_r])
                                          pb2, pr2 = ps()
                                          mm(pb2[:], ones_b, sqg[:], True, True, R=[sqg_r, r_const], W=[pr2])
                                          act(tmpf[:], pb2[:], AF.Ln, R=[pr2], W=[tmpf_r], bias=EPS)
                                          act(tmpf[:], tmpf[:], AF.Exp, R=[tmpf_r], W=[tmpf_r], scale=-0.5)
                                          sc = (128.0 ** -0.5) if xi == 0 else 1.0
                                          stt_("dve", dst[:], tmpg[:], sc, tmpf[:], ALU.mult, ALU.mult, R=[tmpg_r, tmpf_r], W=[dst_r])
                                  yield
                                  pb, pr = ps()
                                  for kc in range(KC):
                                      mm(pb[:], W_[:, kc, 384:512], hT[:, kc, tsl], kc == 0, kc == KC - 1, R=[W_r, hres[kc]], W=[pr])
                                  silu_from("dve", zs[:], pb[:], tmpf[:], R=[pr], W=[zs_r], tmp_res=tmpf_r)

                                  yield
                                  pt, ptr = psb()
                                  for cc in range(4):
                                      csl = slice(cc * 128, (cc + 1) * 128)
                                      kb.op("pe", lambda: PE.transpose(pt[:, csl], vT[:, csl], ident_b), R=[vT_r, r_const], W=[ptr], inc=(cc == 3))
                                  tt_("dve", v4(vb[:]), v4(pt), colv[:, :, 2:3].to_broadcast([128, 4, 128]), ALU.mult, R=[ptr, cols_r], W=[vb_r])
                                  pt, ptr = psb()
                                  for cc in range(4):
                                      csl = slice(cc * 128, (cc + 1) * 128)
                                      kb.op("pe", lambda: PE.transpose(pt[:, csl], kT[:, csl], ident_b), R=[kT_r, r_const], W=[ptr], inc=(cc == 3))
                                  tt_("dve", v4(kbg[:]), v4(pt), colv[:, :, 0:1].to_broadcast([128, 4, 128]), ALU.mult, R=[ptr, cols_r], W=[kbg_r])
                                  tt_("dve", v4(kdc[:]), v4(pt), colv[:, :, 1:2].to_broadcast([128, 4, 128]), ALU.mult, R=[ptr, cols_r], W=[kdc_r])

                                  yield
                                  def expo(lrow, lrow_r, lneg, rrow, rrow_r, rneg, mask_idx):
                                      pb_, pr_ = ps()
                                      for cc in range(4):
                                          csl = slice(cc * 128, (cc + 1) * 128)
                                          kb.op("pe", lambda: PE.matmul(pb_[:, csl], (negf if rneg else onesf)[0:1, 0:128], rrow[0:1, csl], start=True, stop=False),
                                                R=[rrow_r, r_const], W=[pr_], inc=False)
                                          kb.op("pe", lambda: PE.matmul(pb_[:, csl], lrow[0:1, csl], (negf if lneg else onesf)[0:1, 0:128], start=False, stop=False),
                                                R=[lrow_r, r_const], W=[pr_], inc=False)
                                          kb.op("pe", lambda: PE.matmul(pb_[:, csl], ident_b, CB(mask_idx), start=False, stop=True),
                                                R=[r_const], W=[pr_], inc=(cc == 3))
                                      return pb_, pr_
                                  pkk, pkk_r = ps()
                                  for cc in range(4):
                                      csl = slice(cc * 128, (cc + 1) * 128)
                                      mm(pkk[:, csl], kT[:, csl], kT[:, csl], True, True, R=[kT_r], W=[pkk_r])
                                  pe1, pe1_r = expo(Gv, Gc_r, True, rGb, rGb_r, False, C_MN_STR_T)
                                  act(tmpf[:], pe1[:], AF.Exp, R=[pe1_r], W=[tmpf_r])
                                  tt_("dve", AT[:], pkk[:], tmpf[:], ALU.mult, R=[pkk_r, tmpf_r], W=[AT_r])
                                  pe2, pe2_r = expo(rGb, rGb_r, False, Gv, Gc_r, True, C_MN_STR)
                                  act(tmpg[:], pe2[:], AF.Exp, R=[pe2_r], W=[tmpg_r])
                                  tt_("dve", Am[:], pkk[:], tmpg[:], ALU.mult, R=[pkk_r, tmpg_r], W=[Am_r])
                                  pe3, pe3_r = expo(Gv, Gc_r, True, Gv, Gc_r, False, C_MN_INC_T)
                                  act(tmpf[:], pe3[:], AF.Exp, R=[pe3_r], W=[tmpf_r])
                                  pqk, pqk_r = ps()
                                  for cc in range(4):
                                      csl = slice(cc * 128, (cc + 1) * 128)
                                      mm(pqk[:, csl], kT[:, csl], qT[:, csl], True, True, R=[kT_r, qT_r], W=[pqk_r])
                                  tt_("dve", qkm[:], pqk[:], tmpf[:], ALU.mult, R=[pqk_r, tmpf_r], W=[qkm_r])
                                  rGc, rGc_r = rkb, rkb_r
                                  tt_("dve", v4(rGc[:]), v4(Gv), Gst4, ALU.subtract, R=[Gc_r], W=[rGc_r])
                                  pg, pg_r = ps()
                                  for cc in range(4):
                                      csl = slice(cc * 128, (cc + 1) * 128)
                                      kb.op("pe", lambda: PE.matmul(pg[:, csl], onesf[0:1, 0:128], rGc[0:1, csl], start=True, stop=True),
                                            R=[rGc_r, r_const], W=[pg_r], inc=(cc == 3))
                                  act(tmpg[:], pg[:], AF.Exp, R=[pg_r], W=[tmpg_r])
                                  tt_("dve", qd[:], qT[:], tmpg[:], ALU.mult, R=[qT_r, tmpg_r], W=[qd_r])

                                  yield
                                  tt_("dve", v4(Om[:]), v4(Am[:]), b4(CB(C_LV + 0)), ALU.mult, R=[Am_r, r_const], W=[Om_r])
                                  stt_("dve", v4(Inv[:]), v4(Om[:]), -1.0, b4(ident_b), ALU.mult, ALU.add, R=[Om_r, r_const], W=[Inv_r])
                                  tt_("dve", v4(OT[:]), v4(AT[:]), b4(CB(C_LVT + 0)), ALU.mult, R=[AT_r, r_const], W=[OT_r])
                                  stt_("dve", v4(Rm[:]), v4(OT[:]), -1.0, b4(ident_b), ALU.mult, ALU.add, R=[OT_r, r_const], W=[Rm_r])
                                  for li in range(1, 7):
                                      last = (li == 6)
                                      pz, pz_r = ps()
                                      for cc in range(4):
                                          csl = slice(cc * 128, (cc + 1) * 128)
                                          mm(pz[:, csl], Am[:, csl], Rm[:, csl], True, True, R=[Am_r, Rm_r], W=[pz_r])
                                      if not last:
                                          pzp, pzp_r = ps()
                                          for cc in range(4):
                                              csl = slice(cc * 128, (cc + 1) * 128)
                                              mm(pzp[:, csl], AT[:, csl], Inv[:, csl], True, True, R=[AT_r, Inv_r], W=[pzp_r])
                                      tt_("dve", v4(Zt[:]), v4(pz[:]), b4(CB(C_LVT + li)), ALU.mult, R=[pz_r, r_const], W=[Zt_r])
                                      if not last:
                                          tt_("dve", v4(Zp[:]), v4(pzp[:]), b4(CB(C_LV + li)), ALU.mult, R=[pzp_r, r_const], W=[Zp_r])
                                      pr2_, pr2_r = ps()
                                      for cc in range(4):
                                          csl = slice(cc * 128, (cc + 1) * 128)
                                          mm(pr2_[:, csl], Inv[:, csl], Zt[:, csl], True, True, R=[Inv_r, Zt_r], W=[pr2_r])
                                      if not last:
                                          pi2_, pi2_r = ps()
                                          for cc in range(4):
                                              csl = slice(cc * 128, (cc + 1) * 128)
                                              mm(pi2_[:, csl], Rm[:, csl], Zp[:, csl], True, True, R=[Rm_r, Zp_r], W=[pi2_r])
                                      tt_("dve", Rm[:], Rm[:], pr2_[:], ALU.subtract, R=[Rm_r, pr2_r], W=[Rm_r])
                                      if not last:
                                          tt_("dve", Inv[:], Inv[:], pi2_[:], ALU.subtract, R=[Inv_r, pi2_r], W=[Inv_r])
                                      yield
                                  yield
                                  pw, pw_r = ps()
                                  for cc in range(4):
                                      csl = slice(cc * 128, (cc + 1) * 128)
                                      mm(pw[:, csl], kbg[:, csl], Rm[:, csl], True, True, R=[kbg_r, Rm_r], W=[pw_r])
                                  amul(nwt[:], pw[:], -1.0, R=[pw_r], W=[nwt_r])

                                  yield
                                  for cc in range(4):
                                      csl = slice(cc * 128, (cc + 1) * 128)
                                      vn, vn_r = vnew[cc % 2], vnew_r[cc % 2]
                                      pv, pv_r = ps()
                                      mm(pv[:, 0:128], Rm[:, csl], vb[:, csl], True, False, R=[Rm_r, vb_r], W=[pv_r])
                                      mm(pv[:, 0:128], nwt[:, csl], Sbf[:], False, True, R=[nwt_r, Sbf_r], W=[pv_r])
                                      act(vn[:], pv[:, 0:128], AF.Copy, R=[pv_r], W=[vn_r])
                                      po, po_r = ps()
                                      mm(po[:, 0:128], Sbf[:], qd[:, csl], True, False, R=[Sbf_r, qd_r], W=[po_r])
                                      mm(po[:, 0:128], vn[:], qkm[:, csl], False, True, R=[vn_r, qkm_r], W=[po_r])
                                      pd, pd_r = ps()
                                      mm(pd[:, 0:128], kdc[:, csl], vn[:], True, True, R=[kdc_r, vn_r], W=[pd_r])
                                      act(oraw[:, csl], po[:, 0:128], AF.Copy, R=[po_r], W=[oraw_r])
                                      ts_("dve", Sst[:], Sst[:], cols[:, cc * 4 + 3:cc * 4 + 4], None, ALU.mult, None, R=[Sst_r, cols_r], W=[Sst_r])
                                      tt_("dve", Sst[:], pd[:, 0:128], Sst[:], ALU.add, R=[Sst_r, pd_r], W=[Sst_r])
                                      act(Sbf[:], Sst[:], AF.Copy, R=[Sst_r], W=[Sbf_r])
                                      yield

                                  yield
                                  act(sqg[:], oraw[:], AF.Square, R=[oraw_r], W=[sqg_r])
                                  pb2, pr2 = ps()
                                  mm(pb2[:], ones_b, sqg[:], True, True, R=[sqg_r, r_const], W=[pr2])
                                  rmsnorm_rstd(pb2[:], pr2, 128, tmpf[:], tmpf_r)
                                  stt_("dve", tmpg[:], oraw[:], vcol(80), tmpf[:], ALU.mult, ALU.mult, R=[oraw_r, tmpf_r, r_const], W=[tmpg_r])
                                  tt_("pool", oTa[:, h, tsl], tmpg[:], zs[:], ALU.mult, R=[tmpg_r, zs_r], W=[oTa_r[h][tt]])
                          return run
                      runners = [make_head(0), make_head(1)]
                      for pair in (((0, 1), (2, 3)) if "gdn" in phases else ()):
                          ws = [load_head_weights(l, "gdn", h) for h in pair]
                          caps = []
                          for i, h in enumerate(pair):
                              st["chain"] = i
                              kb.cap = []
                              for _ in runners[i](h, ws[i][0], ws[i][1]):
                                  pass
                              caps.append(kb.cap)
                              kb.cap = None
                          st["chain"] = None
                          kb.replay(caps)
                  if l == 0:
                      for h in range(4):
                          tap(f"oTa{h}", oTa[:, h, :], [128, T], [oTa_r[h][t_] for t_ in range(NT)])
                  ms2 = ExitStack()
                  ms2.__enter__()
                  kb.barrier()
                  oTb = sb("oTb", [128, 8, T], BF16, ms2)
                  oTb_r = [[Res() for _ in range(NT)] for _ in range(8)]
                  w2b = sb("w2b", [16, 512], BF16, ms2)
                  r_w2b = Res()
                  kb.dma("pool", [(w2b[:], w2_d[:, l * 512:(l + 1) * 512])], k_w2b, W=[r_w2b])
                  with ExitStack() as gs:
                      S = lambda n, shp, d=F32: sb(n, shp, d, gs)
                      hw["slots"] = [S(f"wsl{i}", [128, KC, 768], BF16) for i in range(2)]
                      hw["res"] = [Res(), Res()]
                      nxt = load_head_weights(l, "gla", 0)
                      glrT = S("glrT", [16, TT], BF16); glr_r = Res()
                      qf = S("qf", [128, TT]); qf_r = Res()
                      kf = S("kf", [128, TT]); kf_r = Res()
                      tmpf = S("tmpf2", [128, TT]); tmpf_r = Res()
                      tmpg = S("tmpg2", [128, TT]); tmpg_r = Res()
                      tmph, tmph_r = tmpg, tmpg_r
                      Pp = [S(f"Pp{i}", [128, 1 + TT]) for i in range(2)]; Pp_r = [Res(), Res()]
                      lrow, lrow_r = tmpg, tmpg_r
                      qk2 = S("qk2", [128, 2, TT], BF16)
                      qin = qk2[:, 0, :]; qin_r = Res()
                      kin = qk2[:, 1, :]; kin_r = Res()
                      qdc = S("qdc", [128, TT], BF16); qdc_r = Res()
                      kdcT = S("kdcT", [128, TT], BF16); kdcT_r = Res()
                      kdt = S("kdt", [128, TT], BF16); kdt_r = Res()
                      vtok = S("vtok", [128, 4, 256], BF16); vtok_r = Res()
                      attn = S("attn", [128, TT], BF16); attn_r = Res()
                      rs = S("rs", [128, 2, TT], BF16); rs_r = Res()
                      oraw2 = S("oraw2", [128, 2, TT]); oraw2_r = Res()
                      S2 = S("S2", [128, 256]); S2_r = Res()
                      S2b = S("S2b", [128, 256], BF16); S2b_r = Res()
                      cdc = S("cdc", [128, 4]); cdc_r = Res()
                      ngb = S("ngb", [128, 4]); ngb_r = Res()
                      ts_("dve", ngb[:], vcol(83, 4), -1.0, None, ALU.mult, None, R=[r_const], W=[ngb_r])
                      for h in (range(4) if "gla" in phases else ()):
                          W_, W_r = nxt
                          nxt = load_head_weights(l, "gla", h + 1) if h < 3 else None
                          kb.op("dve", lambda: DVE.memset(S2[:], 0.0), W=[S2_r])
                          kb.op("dve", lambda: DVE.memset(S2b[:], 0.0), W=[S2b_r])
                          for tt in range(NT):
                              tsl = slice(tt * TT, (tt + 1) * TT)
                              hres = [hr[kc][tt] for kc in range(KC)]
                              Pc, Pc_r = Pp[tt % 2], Pp_r[tt % 2]
                              Pv_, Pv_r = Pp[(tt + 1) % 2], Pp_r[(tt + 1) % 2]
                              pb, pr = ps()
                              for kc in range(KC):
                                  mm(pb[:], W_[:, kc, 0:128], hT[:, kc, tsl], kc == 0, kc == KC - 1, R=[W_r, hres[kc]], W=[pr])
                              amul(qf[:], pb[:], 128.0 ** -0.5, R=[pr], W=[qf_r])
                              pb, pr = ps()
                              for kc in range(KC):
                                  mm(pb[:], W_[:, kc, 128:256], hT[:, kc, tsl], kc == 0, kc == KC - 1, R=[W_r, hres[kc]], W=[pr])
                              act(kf[:], pb[:], AF.Copy, R=[pr], W=[kf_r])
                              for half in range(2):
                                  pb, pr = ps()
                                  for c2 in range(2):
                                      cc = half * 2 + c2
                                      for kc in range(KC):
                                          mm(pb[:, c2 * 256:(c2 + 1) * 256], hT[:, kc, tt * TT + cc * 128: tt * TT + (cc + 1) * 128], W_[:, kc, 256:512],
                                             kc == 0, kc == KC - 1, R=[W_r, hres[kc]], W=[pr])
                                  act(vtok[:, half * 2:half * 2 + 2, :], pb[:].rearrange("p (c e) -> p c e", e=256), AF.Copy, R=[pr], W=[vtok_r])
                              pb, pr = ps()
                              for kc in range(KC):
                                  mm(pb[0:16, :], wsm[:, kc, 8:24], hT[:, kc, tsl], kc == 0, kc == KC - 1, R=[r_wsm, hres[kc]], W=[pr])
                              act(glrT[:], pb[0:16, :], AF.Copy, R=[pr], W=[glr_r])
                              pb, pr = ps()
                              mm(pb[:], w2b[0:16, h * 128:(h + 1) * 128], glrT[:], True, True, R=[r_w2b, glr_r], W=[pr])
                              act(lrow[:], pb[:], AF.Exp, R=[pr, ngb_r], W=[lrow_r], bias=ngb[:, h:h + 1], scale=-1.0)
                              act(lrow[:], lrow[:], AF.Ln, R=[lrow_r], W=[lrow_r], bias=1.0)
                              if tt == 0:
                                  kb.op("dve", lambda: DVE.memset(Pc[:, 0:1], 0.0), W=[Pc_r])
                              else:
                                  kb.op("dve", lambda: DVE.tensor_copy(Pc[:, 0:1], Pv_[:, TT:TT + 1]), R=[Pv_r], W=[Pc_r])
                              kb.op("dve", lambda: DVE.tensor_tensor_scan(Pc[:, 1:1 + TT], onesf[:, 0:1].to_broadcast([128, TT]), lrow[:], Pc[:, 0:1], ALU.mult, ALU.add),
                                    R=[r_const, lrow_r, Pc_r], W=[Pc_r])
                              Pvw = v4(Pc[:, 1:1 + TT])
                              Pst = Pc[:, 0:TT:128].unsqueeze(2).to_broadcast([128, 4, 128])
                              Pmid = Pc[:, 65:TT + 1:128].unsqueeze(2).to_broadcast([128, 4, 128])
                              Pla = Pc[:, 128:TT + 1:128].unsqueeze(2).to_broadcast([128, 4, 128])
                              isc = 1.0 / 16.0
                              tt_("dve", v4(tmpf[:]), Pvw, Pmid, ALU.subtract, R=[Pc_r], W=[tmpf_r])
                              act(tmpg[:], tmpf[:], AF.Exp, R=[tmpf_r], W=[tmpg_r], scale=-isc)
                              tt_("dve", qin, qf[:], tmpg[:], ALU.mult, R=[qf_r, tmpg_r], W=[qin_r])
                              act(tmph[:], tmpf[:], AF.Exp, R=[tmpf_r], W=[tmph_r], scale=isc)
                              tt_("dve", kin, kf[:], tmph[:], ALU.mult, R=[kf_r, tmph_r], W=[kin_r])
                              tt_("dve", v4(tmpf[:]), Pvw, Pst, ALU.subtract, R=[Pc_r], W=[tmpf_r])
                              act(tmpg[:], tmpf[:], AF.Exp, R=[tmpf_r], W=[tmpg_r], scale=-isc)
                              tt_("dve", qdc[:], qf[:], tmpg[:], ALU.mult, R=[qf_r, tmpg_r], W=[qdc_r])
                              tt_("dve", v4(tmpf[:]), Pvw, Pla, ALU.subtract, R=[Pc_r], W=[tmpf_r])
                              act(tmph[:], tmpf[:], AF.Exp, R=[tmpf_r], W=[tmph_r], scale=isc)
                              tt_("dve", kdcT[:], kf[:], tmph[:], ALU.mult, R=[kf_r, tmph_r], W=[kdcT_r])
                              tt_("dve", cdc[:], Pc[:, 128:TT + 1:128], Pc[:, 0:TT:128], ALU.subtract, R=[Pc_r], W=[cdc_r])
                              act(cdc[:], cdc[:], AF.Exp, R=[cdc_r], W=[cdc_r], scale=-isc)
                              pa, pa_r = ps()
                              for cc in range(4):
                                  csl = slice(cc * 128, (cc + 1) * 128)
                                  mm(pa[:, csl], kin[:, csl], qin[:, csl], True, True, R=[kin_r, qin_r], W=[pa_r])
                              tt_("dve", v4(attn[:]), v4(pa[:]), b4(CB(C_C01_T)), ALU.mult, R=[pa_r, r_const], W=[attn_r])
                              pt, ptr = psb()
                              for cc in range(4):
                                  csl = slice(cc * 128, (cc + 1) * 128)
                                  kb.op("pe", lambda: PE.transpose(pt[:, csl], kdcT[:, csl], ident_b), R=[kdcT_r, r_const], W=[ptr], inc=(cc == 3))
                              act(kdt[:], pt, AF.Copy, R=[ptr], W=[kdt_r])
                              for cc in range(4):
                                  csl = slice(cc * 128, (cc + 1) * 128)
                                  po, po_r = ps()
                                  for et in range(2):
                                      esl = slice(et * 128, (et + 1) * 128)
                                      mm(po[:, esl], vtok[:, cc, esl], attn[:, csl], True, False, R=[vtok_r, attn_r], W=[po_r])
                                      mm(po[:, esl], S2b[:, esl], qdc[:, csl], False, True, R=[S2b_r, qdc_r], W=[po_r])
                                  pd, pd_r = ps()
                                  mm(pd[:, 0:256], kdt[:, csl], vtok[:, cc, :], True, True, R=[kdt_r, vtok_r], W=[pd_r])
                                  act(oraw2[:, :, csl], po[:, 0:256].rearrange("p (e k) -> p e k", k=128), AF.Copy, R=[po_r], W=[oraw2_r])
                                  ts_("dve", S2[:], S2[:], cdc[:, cc:cc + 1], None, ALU.mult, None, R=[S2_r, cdc_r], W=[S2_r])
                                  tt_("dve", S2[:], pd[:, 0:256], S2[:], ALU.add, R=[S2_r, pd_r], W=[S2_r])
                                  act(S2b[:], S2[:], AF.Copy, R=[S2_r], W=[S2b_r])
                                  if cc < 2:
                                      et = cc
                                      pb, pr = ps()
                                      for kc in range(KC):
                                          mm(pb[:], W_[:, kc, 512 + et * 128:512 + (et + 1) * 128], hT[:, kc, tsl], kc == 0, kc == KC - 1, R=[W_r, hres[kc]], W=[pr])
                                      silu_from("dve", rs[:, et, :], pb[:], tmpf[:], R=[pr], W=[rs_r], tmp_res=tmpf_r)
                              act(qk2[:], oraw2[:], AF.Square, R=[oraw2_r], W=[qin_r, kin_r])
                              pb2, pr2 = ps()
                              for et in range(2):
                                  mm(pb2[:], ones_b, qk2[:, et, :], et == 0, et == 1, R=[qin_r, kin_r, r_const], W=[pr2])
                              rmsnorm_rstd(pb2[:], pr2, 256, tmpf[:], tmpf_r)
                              for et in range(2):
                                  stt_("dve", tmpg[:], oraw2[:, et, :], vcol(81 + et), tmpf[:], ALU.mult, ALU.mult, R=[oraw2_r, tmpf_r, r_const], W=[tmpg_r])
                                  tt_("pool", oTb[:, h * 2 + et, tsl], tmpg[:], rs[:, et, :], ALU.mult, R=[tmpg_r, rs_r], W=[oTb_r[h * 2 + et][tt]])

                  if l == 0:
                      for h in range(8):
                          tap(f"oTb{h}", oTb[:, h, :], [128, T], [oTb_r[h][t_] for t_ in range(NT)])
                  with ExitStack() as gs:
                      kb.barrier()
                      S = lambda n, shp, d=F32: sb(n, shp, d, gs)
                      NW = 2
                      wm = [S(f"wm{i}", [128, 28, 128], BF16) for i in range(NW)]
                      wm_r = [Res() for _ in range(NW)]
                      wo = [S(f"wo{i}", [128, KC, 128], BF16) for i in range(NW)]
                      wo_r = [Res() for _ in range(NW)]
                      stage = S("stage", [128, KC, TT], BF16); stage_r = [Res() for _ in range(KC)]
                      tmpo = S("tmpo", [128, KC, TT]); tmpo_r = [Res() for _ in range(KC)]
                      sa = S("sa", [128, TT]); sa_r = Res()
                      sb_ = S("sb_", [128, TT]); sb_r = Res()
                      m1 = S("m1", [128, TT]); m1_r = Res()
                      sq = [S(f"sqm{i}", [128, TT], BF16) for i in range(2)]; sq_res = [Res(), Res()]
                      rstd = S("rstdm", [128, TT]); rstd_res = Res()
                      t2 = S("t2", [128, TT]); t2_r = Res()
                      cnt = {"m": 0, "o": 0}

                      def load_wm(ct):
                          i = cnt["m"] % NW
                          cnt["m"] += 1
                          csl = slice(ct * 128, (ct + 1) * 128)
                          pairs = [(wm[i][:, 0:4, :], w_oa_d[l, :, csl].rearrange("(k p) c -> p k c", p=128)),
                                   (wm[i][:, 4:12, :], w_ob_d[l, :, csl].rearrange("(k p) c -> p k c", p=128)),
                                   (wm[i][:, 12:20, :], w_in_d[l, :, O_GA + ct * 128:O_GA + (ct + 1) * 128].rearrange("(k p) c -> p k c", p=128)),
                                   (wm[i][:, 20:28, :], w_in_d[l, :, O_GB + ct * 128:O_GB + (ct + 1) * 128].rearrange("(k p) c -> p k c", p=128))]
                          kb.dma("pool", pairs, wm_k[i], W=[wm_r[i]])
                          return wm[i], wm_r[i]

                      def load_wo(ct):
                          i = cnt["o"] % NW
                          cnt["o"] += 1
                          kb.dma("pool", [(wo[i][:], w_o_d[l, :, ct * 128:(ct + 1) * 128].rearrange("(k p) c -> p k c", p=128))], wo_k[i], W=[wo_r[i]])
                          return wo[i], wo_r[i]

                      for tt in (range(NT) if "merge" in phases else ()):
                          tsl = slice(tt * TT, (tt + 1) * TT)
                          nw = load_wm(0)
                          for ct in range(KC):
                              w_, w_r = nw
                              if ct < KC - 1:
                                  nw = load_wm(ct + 1)
                              pya, pya_r = ps()
                              for k in range(4):
                                  mm(pya[:], w_[:, k, :], oTa[:, k, tsl], k == 0, k == 3, R=[w_r, oTa_r[k][tt]], W=[pya_r])
                              pyb, pyb_r = ps()
                              for k in range(8):
                                  mm(pyb[:], w_[:, 4 + k, :], oTb[:, k, tsl], k == 0, k == 7, R=[w_r, oTb_r[k][tt]], W=[pyb_r])
                              pga, pga_r = ps()
                              for k in range(8):
                                  mm(pga[:], w_[:, 12 + k, :], hT[:, k, tsl], k == 0, k == 7, R=[w_r, hr[k][tt]], W=[pga_r])
                              pgb, pgb_r = ps()
                              for k in range(8):
                                  mm(pgb[:], w_[:, 20 + k, :], hT[:, k, tsl], k == 0, k == 7, R=[w_r, hr[k][tt]], W=[pgb_r])
                              act(sa[:], pga[:], AF.Exp, R=[pga_r], W=[sa_r], scale=-1.0)
                              act(sa[:], sa[:], AF.Ln, R=[sa_r], W=[sa_r], bias=1.0)
                              act(sa[:], sa[:], AF.Exp, R=[sa_r], W=[sa_r], scale=-1.0)
                              act(sb_[:], pgb[:], AF.Exp, R=[pgb_r], W=[sb_r], scale=-1.0)
                              act(sb_[:], sb_[:], AF.Ln, R=[sb_r], W=[sb_r], bias=1.0)
                              act(sb_[:], sb_[:], AF.Exp, R=[sb_r], W=[sb_r], scale=-1.0)
                              tt_("dve", m1[:], pya[:], sa[:], ALU.mult, R=[pya_r, sa_r], W=[m1_r])
                              tt_("dve", sb_[:], pyb[:], sb_[:], ALU.mult, R=[pyb_r, sb_r], W=[sb_r])
                              tt_("dve", stage[:, ct, :], m1[:], sb_[:], ALU.add, R=[m1_r, sb_r], W=[stage_r[ct]])
                          pss, pss_r = pacc, pacc_r
                          nw = load_wo(0)
                          for ct in range(KC):
                              w_, w_r = nw
                              if ct < KC - 1:
                                  nw = load_wo(ct + 1)
                              pb, pr = ps()
                              for k in range(KC):
                                  mm(pb[:], w_[:, k, :], stage[:, k, :], k == 0, k == KC - 1, R=[w_r, stage_r[k]], W=[pr])
                              act(tmpo[:, ct, :], pb[:], AF.Copy, R=[pr], W=[tmpo_r[ct]])
                              s = sq[ct % 2]
                              act(s[:], pb[:], AF.Square, R=[pr], W=[sq_res[ct % 2]])
                              mm(pss[:], ones_b, s[:], ct == 0, ct == KC - 1, R=[sq_res[ct % 2], r_const], W=[pss_r])
                          rmsnorm_rstd(pss[:], pss_r, D, rstd[:], rstd_res)
                          for ct in range(KC):
                              stt_("dve", t2[:], tmpo[:, ct, :], vcol(8 + ct), rstd[:], ALU.mult, ALU.mult, R=[tmpo_r[ct], rstd_res, r_const], W=[t2_r])
                              tt_("pool", xT[:, ct, tsl], xT[:, ct, tsl], t2[:], ALU.add, R=[xr[ct][tt], t2_r], W=[xr[ct][tt]])

                  ms2.close()
              if l == 0:
                  for kc in range(KC):
                      tap(f"xmix{kc}", xT[:, kc, :], [128, T], [xr[kc][t_] for t_ in range(NT)])
              with ExitStack() as gs:
                  kb.barrier()
                  S = lambda n, shp, d=F32: sb(n, shp, d, gs)
                  h2 = S("h2", [128, KC, TT], BF16); h2_r = [Res() for _ in range(KC)]
                  uT = S("uT", [128, 32, TT], BF16); uT_r = [Res() for _ in range(32)]
                  NW = 4
                  wu = [S(f"wu{i}", [128, KC, 512], BF16) for i in range(NW)]; wu_r = [Res() for _ in range(NW)]
                  wd = [S(f"wd{i}", [128, 32, 128], BF16) for i in range(NW)]; wd_r = [Res() for _ in range(NW)]
                  tmpo = S("tmpo2", [128, KC, TT]); tmpo_r = [Res() for _ in range(KC)]
                  sq = [S(f"sqn{i}", [128, TT], BF16) for i in range(2)]; sq_res = [Res(), Res()]
                  rstd = S("rstdn", [128, TT]); rstd_res = Res()
                  rl = [S(f"rl{i}", [128, TT]) for i in range(2)]; rl_r = [Res(), Res()]
                  t2 = S("t2n", [128, TT]); t2_r = Res()
                  cnt = {"u": 0, "d": 0}

                  def load_wu(fb):
                      i = cnt["u"] % NW
                      cnt["u"] += 1
                      kb.dma("pool", [(wu[i][:], w_up_d[l, :, fb * 512:(fb + 1) * 512].rearrange("(k p) c -> p k c", p=128))], wu_k[i], W=[wu_r[i]])
                      return wu[i], wu_r[i]

                  def load_wd(ct):
                      i = cnt["d"] % NW
                      cnt["d"] += 1
                      kb.dma("pool", [(wd[i][:], w_dn_d[l, :, ct * 128:(ct + 1) * 128].rearrange("(k p) c -> p k c", p=128))], wd_k[i], W=[wd_r[i]])
                      return wd[i], wd_r[i]

                  for tt in (range(NT) if "mlp" in phases else ()):
                      tsl = slice(tt * TT, (tt + 1) * TT)
                      q_u = [load_wu(0), load_wu(1), load_wu(2)]
                      norm_tile(lambda kc: xT[:, kc, tsl], lambda kc: xr[kc][tt], lambda kc: vcol(16 + kc),
                                lambda kc: h2[:, kc, :], lambda kc: h2_r[kc], sq, sq_res, rstd, rstd_res)
                      for fb in range(8):
                          w_, w_r = q_u.pop(0)
                          if fb + 3 < 8:
                              q_u.append(load_wu(fb + 3))
                          for f4 in range(4):
                              ft = fb * 4 + f4
                              pb, pr = ps()
                              for k in range(KC):
                                  mm(pb[:], w_[:, k, f4 * 128:(f4 + 1) * 128], h2[:, k, :], k == 0, k == KC - 1, R=[w_r, h2_r[k]], W=[pr])
                              act(rl[ft % 2][:], pb[:], AF.Relu, R=[pr], W=[rl_r[ft % 2]])
                              tt_("pool" if ft % 2 else "dve", uT[:, ft, :], rl[ft % 2][:], rl[ft % 2][:], ALU.mult, R=[rl_r[ft % 2]], W=[uT_r[ft]])
                      q_d = [load_wd(0), load_wd(1), load_wd(2)]
                      pss, pss_r = pacc, pacc_r
                      for ct in range(KC):
                          w_, w_r = q_d.pop(0)
                          if ct + 3 < KC:
                              q_d.append(load_wd(ct + 3))
                          pb, pr = ps()
                          for k in range(32):
                              mm(pb[:], w_[:, k, :], uT[:, k, :], k == 0, k == 31, R=[w_r, uT_r[k]], W=[pr])
                          act(tmpo[:, ct, :], pb[:], AF.Copy, R=[pr], W=[tmpo_r[ct]])
                          s = sq[ct % 2]
                          act(s[:], pb[:], AF.Square, R=[pr], W=[sq_res[ct % 2]])
                          mm(pss[:], ones_b, s[:], ct == 0, ct == KC - 1, R=[sq_res[ct % 2], r_const], W=[pss_r])
                      rmsnorm_rstd(pss[:], pss_r, D, rstd[:], rstd_res)
                      for ct in range(KC):
                          stt_("dve", t2[:], tmpo[:, ct, :], vcol(24 + ct), rstd[:], ALU.mult, ALU.mult, R=[tmpo_r[ct], rstd_res, r_const], W=[t2_r])
                          tt_("pool", xT[:, ct, tsl], xT[:, ct, tsl], t2[:], ALU.add, R=[xr[ct][tt], t2_r], W=[xr[ct][tt]])

        k_out = kb.dsem("out")
        kb.dma("sp", [(outT_d[kc * 128:(kc + 1) * 128, :], xT[:, kc, :]) for kc in range(KC)], k_out,
               R=[xr[kc][tt] for kc in range(KC) for tt in range(NT)])
        nc.sync.wait_ge(kb.sems[k_out], kb.cnt[k_out])
        for name, key in tap_out.items():
            nc.sync.wait_ge(kb.sems[key], kb.cnt[key])
        print(f"[build] ops={kb.nops} waits={kb.nwaits} counts={ {k: v for k, v in kb.cnt.items() if not k.startswith('d_')} }", flush=True)
    return nc


def pack_small(inputs, l0, nl):
    vecs = np.zeros((128, nl * NVEC), np.float32)
    hv = np.zeros((1, nl * 8), np.float32)
    w2 = np.zeros((16, nl * 512), np.float32)
    for i in range(nl):
        l = l0 + i
        V0 = i * NVEC
        for j, name in enumerate(("norm_mix_pre", "norm_mix_post", "norm_mlp_pre", "norm_mlp_post")):
            vecs[:, V0 + j * 8:V0 + (j + 1) * 8] = np.asarray(inputs[name][l]).reshape(8, 128).T
        cw = np.asarray(inputs["conv_w"][l])
        for tp in range(4):
            vecs[:, V0 + 32 + tp * 12:V0 + 32 + (tp + 1) * 12] = cw[tp].reshape(12, 128).T
        vecs[:, V0 + 80] = np.asarray(inputs["gdn_norm"][l])
        vecs[:, V0 + 81:V0 + 83] = np.asarray(inputs["gla_norm"][l]).reshape(2, 128).T
        vecs[:, V0 + 83:V0 + 87] = np.asarray(inputs["gla_gate_b"][l]).reshape(4, 128).T
        hv[0, i * 8:i * 8 + 4] = np.asarray(inputs["a_log"][l])
        hv[0, i * 8 + 4:i * 8 + 8] = np.asarray(inputs["dt_bias"][l])
        w2[:, i * 512:(i + 1) * 512] = np.asarray(inputs["gla_gate_w2"][l])
    return vecs, hv, w2


_PROG = {}


def run_layers(xT_list, inputs, l0, nl):
    if nl not in _PROG:
        _PROG[nl] = build_program(nl)
    nc = _PROG[nl]
    vecs, hv, w2 = pack_small(inputs, l0, nl)
    consts = make_consts()
    sl = slice(l0, l0 + nl)
    shared = {
        "w_in": np.ascontiguousarray(inputs["w_in"][sl]), "w_out_a": np.ascontiguousarray(inputs["w_out_a"][sl]),
        "w_out_b": np.ascontiguousarray(inputs["w_out_b"][sl]), "w_o": np.ascontiguousarray(inputs["w_o"][sl]),
        "w_mlp_up": np.ascontiguousarray(inputs["w_mlp_up"][sl]), "w_mlp_down": np.ascontiguousarray(inputs["w_mlp_down"][sl]),
        "gate_w2": w2, "vecs": vecs, "hv": hv, "consts": consts,
    }
    in_maps = [dict(shared, xT=xT_list[c]) for c in range(len(xT_list))]
    res = run_bass_kernel_spmd(nc, in_maps, core_ids=list(range(len(xT_list))))
    return [np.asarray(r["outT"]) for r in res.results]


GDN_STOP = 0
N_FUSED = 4


def kernel(**inputs):
    inputs = {k: np.asarray(v) for k, v in inputs.items()}
    x = inputs["x"].astype(np.float32, copy=False)
    xT = [np.ascontiguousarray(x[b].T) for b in range(x.shape[0])]
    for l0 in range(0, L, N_FUSED):
        xT = run_layers(xT, inputs, l0, N_FUSED)
    out = np.stack([t.T for t in xT], axis=0)
    return np.ascontiguousarray(out.astype(np.float32))
```

```python
import types
import numpy as np
from contextlib import ExitStack
import concourse.bass as bass
import concourse.mybir as mybir
from concourse.bass_utils import run_bass_kernel_spmd

F32 = mybir.dt.float32
BF16 = mybir.dt.bfloat16
AF = mybir.ActivationFunctionType
ALU = mybir.AluOpType

D = 1024
T = 2048
L = 4
DFF = 4096
NCOL = 7192
NT = 4
TT = 512
KC = 8
EPS = 1e-6
O_AQ, O_AK, O_AV, O_AZ, O_AB, O_AA = 0, 512, 1024, 1536, 2048, 2052
O_BQ, O_BK, O_BV, O_BR, O_GLR, O_GA, O_GB = 2056, 2568, 3080, 4104, 5128, 5144, 6168
GDN_STOP = 0
GDN_REC = 0
NVEC = 87
C_ID, C_ONES, C_NEG, C_MN_INC_T, C_MN_STR_T, C_MN_STR, C_C01_T = 0, 1, 2, 3, 4, 5, 6
C_LV = 7
C_LVT = 14
NCB = 21


def make_consts():
    c = np.zeros((NCB, 128, 128), np.float32)
    i = np.arange(128)[:, None]
    j = np.arange(128)[None, :]
    c[C_ID] = (i == j)
    c[C_ONES] = 1.0
    c[C_NEG] = -1.0
    c[C_MN_INC_T] = np.where(j >= i, 0.0, -1e30)
    c[C_MN_STR_T] = np.where(j > i, 0.0, -1e30)
    c[C_MN_STR] = np.where(i > j, 0.0, -1e30)
    c[C_C01_T] = (j >= i)
    for li in range(7):
        m = 1 << li
        mm = ((i // (2 * m)) == (j // (2 * m))) & ((i % (2 * m)) >= m) & ((j % (2 * m)) < m)
        c[C_LV + li] = mm
        c[C_LVT + li] = mm.T
    return np.ascontiguousarray(c.transpose(1, 0, 2).reshape(128, NCB * 128))


def _freeze(fn):
    if fn.__closure__ is None:
        return fn
    cells = tuple(types.CellType(c.cell_contents) for c in fn.__closure__)
    return types.FunctionType(fn.__code__, fn.__globals__, fn.__name__, fn.__defaults__, cells)


class Res:
    __slots__ = ("w", "rs", "p")

    def __init__(self):
        self.w = None
        self.rs = {}
        self.p = set()


class KB:
    def __init__(self, nc, es):
        self.nc = nc
        self.es = es
        self.eng = {"pe": nc.tensor, "dve": nc.vector, "act": nc.scalar, "pool": nc.gpsimd, "sp": nc.sync}
        self.sems = {}
        self.cnt = {}
        self.seen = {e: {} for e in self.eng}
        self.pend = {e: [] for e in self.eng}
        for e in ("pe", "dve", "act", "pool"):
            self.sems[e] = es.enter_context(nc.semaphore("s_" + e))
            self.cnt[e] = 0
        self.nops = 0
        self.nwaits = 0
        self.cap = None

    def dsem(self, name):
        key = "d_" + name
        self.sems[key] = self.es.enter_context(self.nc.semaphore(key))
        self.cnt[key] = 0
        return key

    def flush(self, e):
        if not self.pend[e]:
            return
        self.cnt[e] += 1
        self.pend[e][-1][0].then_inc(self.sems[e], 1)
        ev = (e, self.cnt[e])
        for (_, r2, w2) in self.pend[e]:
            for r in list(r2) + list(w2):
                r.p.discard(e)
            self._reg(ev, r2, w2)
        self.pend[e] = []

    def barrier(self):
        for e in self.eng:
            self.flush(e)
        for e in self.eng:
            for k, v in self.cnt.items():
                if v > 0 and self.seen[e].get(k, 0) < v:
                    self.eng[e].wait_ge(self.sems[k], v)
                    self.seen[e][k] = v
                    self.nwaits += 1

    def _deps(self, e, R, W):
        for r in list(R) + list(W):
            for e2 in list(r.p):
                if e2 != e:
                    self.flush(e2)
        deps = {}

        def add(ev):
            k, v = ev
            if deps.get(k, 0) < v:
                deps[k] = v
        for r in R:
            if r.w is not None:
                add(r.w)
        for w in W:
            if w.w is not None:
                add(w.w)
            for k, v in w.rs.items():
                add((k, v))
        for k, v in deps.items():
            if k == e and e == "pe":
                continue
            if self.seen[e].get(k, 0) >= v:
                continue
            self.eng[e].wait_ge(self.sems[k], v)
            self.seen[e][k] = v
            self.nwaits += 1

    def _reg(self, ev, R, W):
        k, v = ev
        for w in W:
            w.w = ev
            w.rs = {}
        for r in R:
            if r.rs.get(k, 0) < v:
                r.rs[k] = v

    def replay(self, lists):
        idx = [0] * len(lists)
        live = True
        while live:
            live = False
            for k, lst in enumerate(lists):
                if idx[k] < len(lst):
                    item = lst[idx[k]]
                    idx[k] += 1
                    live = True
                    if item[0] == "op":
                        self.op(*item[1:])
                    else:
                        self.dma(*item[1:])

    def op(self, e, fn, R=(), W=(), inc=True):
        if self.cap is not None:
            self.cap.append(("op", e, _freeze(fn), R, W, inc))
            return
        self._deps(e, R, W)
        inst = fn()
        self.nops += 1
        if not inc:
            self.pend[e].append((inst, R, W))
            for r in list(R) + list(W):
                r.p.add(e)
            return
        self.cnt[e] += 1
        inst.then_inc(self.sems[e], 1)
        ev = (e, self.cnt[e])
        for (_, r2, w2) in self.pend[e]:
            for r in list(r2) + list(w2):
                r.p.discard(e)
            self._reg(ev, r2, w2)
        self.pend[e] = []
        self._reg(ev, R, W)

    def dma(self, e, pairs, key, R=(), W=()):
        if self.cap is not None:
            self.cap.append(("dma", e, pairs, key, R, W))
            return
        self._deps(e, R, W)
        for (o, i) in pairs:
            inst = self.eng[e].dma_start(out=o, in_=i)
            inst.then_inc(self.sems[key], 16)
            self.cnt[key] += 16
            self.nops += 1
        ev = (key, self.cnt[key])
        self._reg(ev, R, W)


def build_program(n_layers, taps=(), phases=("gdn", "gla", "merge", "mlp")):
    nc = bass.Bass("TRN2", target_bir_lowering=False)
    NL = n_layers
    dt = lambda name, shape, kind="ExternalInput": nc.dram_tensor(name, shape, F32, kind=kind).ap()
    xT_d = dt("xT", [D, T])
    w_in_d = dt("w_in", [NL, D, NCOL])
    w_oa_d = dt("w_out_a", [NL, 512, D])
    w_ob_d = dt("w_out_b", [NL, 1024, D])
    w_o_d = dt("w_o", [NL, D, D])
    w_up_d = dt("w_mlp_up", [NL, D, DFF])
    w_dn_d = dt("w_mlp_down", [NL, DFF, D])
    w2_d = dt("gate_w2", [16, NL * 512])
    vecs_d = dt("vecs", [128, NL * NVEC])
    hv_d = dt("hv", [1, NL * 8])
    consts_d = dt("consts", [128, NCB * 128])
    outT_d = dt("outT", [D, T], kind="ExternalOutput")
    tap_out = {}

    with ExitStack() as es:
        kb = KB(nc, es)
        PE, DVE, ACT, POOL = nc.tensor, nc.vector, nc.scalar, nc.gpsimd

        uid = {"n": 0}

        def sb(name, shape, dtype=F32, stack=es):
            uid["n"] += 1
            return stack.enter_context(nc.sbuf_tensor(f"s{uid['n']}_{name}", shape, dtype))

        NPB = 5
        pbanks = [es.enter_context(nc.psum_tensor(f"pb{i}", [128, 512], F32)) for i in range(NPB)]
        pres = [Res() for _ in range(NPB)]
        pacc = es.enter_context(nc.psum_tensor("pacc", [128, 512], F32))
        pacc_r = Res()
        pbf = [es.enter_context(nc.psum_tensor(f"pbf{i}", [128, 1024], BF16)) for i in range(2)]
        pbf_res = [Res(), Res()]
        st = {"pi": 0, "bi": 0, "chain": None, "ci": [0, 0]}
        allb = pbanks + [pacc]
        allr = pres + [pacc_r]

        def ps():
            c = st["chain"]
            if c is not None:
                i = 3 * c + st["ci"][c]
                st["ci"][c] = (st["ci"][c] + 1) % 3
                return allb[i], allr[i]
            i = st["pi"]
            st["pi"] = (i + 1) % NPB
            return pbanks[i], pres[i]

        def psb():
            c = st["chain"]
            if c is not None:
                return pbf[c][:, 0:512], pbf_res[c]
            i = st["bi"]
            st["bi"] = (i + 1) % 2
            return pbf[i][:, 0:512], pbf_res[i]

        xT = sb("xT", [128, KC, T])
        xr = [[Res() for _ in range(NT)] for _ in range(KC)]
        cb = sb("cb", [128, NCB * 128], BF16)
        cf = sb("cf", [128, 2 * 128])
        hv = sb("hv", [1, NL * 8])
        r_const = Res()

        def CB(i):
            return cb[:, i * 128:(i + 1) * 128]
        onesf = cf[:, 0:128]
        negf = cf[:, 128:256]
        ident_b = CB(C_ID)
        ones_b = CB(C_ONES)

        k_in = kb.dsem("in")
        kb.dma("sp", [(xT[:, kc, :], xT_d[kc * 128:(kc + 1) * 128, :]) for kc in range(KC)], k_in,
               W=[xr[kc][tt] for kc in range(KC) for tt in range(NT)])
        k_c = kb.dsem("c")
        kb.dma("pool", [(cb[:], consts_d)], k_c, W=[r_const])
        k_c2 = kb.dsem("c2")
        kb.dma("sp", [(cf[:], consts_d[:, 128:384]), (hv[:], hv_d)], k_c2, W=[r_const])
        k_vec = kb.dsem("vec")

        def tap(name, ap, shape, R):
            if name not in taps:
                return
            d = nc.dram_tensor("tap_" + name, shape, F32, kind="ExternalOutput").ap()
            key = kb.dsem("t_" + name)
            tap_out[name] = key
            kb.dma("pool", [(d, ap)], key, R=R)

        def mm(out, lhsT, rhs, start, stop, R, W):
            kb.op("pe", lambda: PE.matmul(out, lhsT, rhs, start=start, stop=stop), R=R, W=W, inc=stop)

        def act(out, in_, func, R, W, bias=None, scale=None):
            kw = {}
            if bias is not None:
                kw["bias"] = bias
            if scale is not None:
                kw["scale"] = scale
            kb.op("act", lambda: ACT.activation(out=out, in_=in_, func=func, **kw), R=R, W=W)

        def amul(out, in_, c, R, W):
            kb.op("act", lambda: ACT.mul(out, in_, c), R=R, W=W)

        def tt_(e, out, in0, in1, op, R, W):
            eng = DVE if e == "dve" else POOL
            kb.op(e, lambda: eng.tensor_tensor(out, in0, in1, op), R=R, W=W)

        def ts_(e, out, in0, s1, s2, op0, op1, R, W):
            eng = DVE if e == "dve" else POOL
            if op1 is None and e == "pool" and op0 in (ALU.mult, ALU.add):
                o1, c2 = (ALU.add, 0.0) if op0 == ALU.mult else (ALU.mult, 1.0)
                kb.op(e, lambda: eng.tensor_scalar(out, in0, s1, c2, op0, o1), R=R, W=W)
            elif op1 is None:
                kb.op(e, lambda: eng.tensor_scalar(out, in0, s1, None, op0), R=R, W=W)
            else:
                kb.op(e, lambda: eng.tensor_scalar(out, in0, s1, s2, op0, op1), R=R, W=W)

        def stt_(e, out, in0, s, in1, op0, op1, R, W):
            eng = DVE if e == "dve" else POOL
            kb.op(e, lambda: eng.scalar_tensor_tensor(out=out, in0=in0, scalar=s, in1=in1, op0=op0, op1=op1), R=R, W=W)

        def b4(ap):
            return ap.unsqueeze(1).to_broadcast([ap.shape[0], 4, 128])

        def v4(ap):
            return ap.rearrange("p (c k) -> p c k", k=128)

        def rmsnorm_rstd(ps_ap, ps_res, n, rstd_ap, rstd_res):
            act(rstd_ap, ps_ap, AF.Ln, R=[ps_res], W=[rstd_res], bias=EPS, scale=1.0 / n)
            act(rstd_ap, rstd_ap, AF.Exp, R=[rstd_res], W=[rstd_res], scale=-0.5)

        def norm_tile(src_fn, src_res_fn, wcol_fn, dst_fn, dst_res_fn, sq, sq_res, rstd, rstd_res):
            pb, pr = ps()
            for kc in range(KC):
                s = sq[kc % 2]
                act(s[:], src_fn(kc), AF.Square, R=[src_res_fn(kc)], W=[sq_res[kc % 2]])
                mm(pb[:], ones_b, s[:], kc == 0, kc == KC - 1, R=[sq_res[kc % 2], r_const], W=[pr])
            rmsnorm_rstd(pb[:], pr, D, rstd[:], rstd_res)
            for kc in range(KC):
                stt_("dve", dst_fn(kc), src_fn(kc), wcol_fn(kc), rstd[:], ALU.mult, ALU.mult,
                     R=[src_res_fn(kc), rstd_res, r_const], W=[dst_res_fn(kc)])

        def silu_from(e_eng, out_ap, x_ap, tmp_ap, R, W, tmp_res):
            act(tmp_ap, x_ap, AF.Exp, R=R, W=[tmp_res], scale=-1.0)
            act(tmp_ap, tmp_ap, AF.Ln, R=[tmp_res], W=[tmp_res], bias=1.0)
            act(tmp_ap, tmp_ap, AF.Exp, R=[tmp_res], W=[tmp_res], scale=-1.0)
            tt_(e_eng, out_ap, x_ap, tmp_ap, ALU.mult, R=list(R) + [tmp_res], W=W)

        wslot_key = [kb.dsem("ws0"), kb.dsem("ws1")]
        hw = {"n": 0, "slots": None, "res": None}

        def load_head_weights(l, kind, h):
            i = hw["n"] % 2
            hw["n"] += 1
            w = hw["slots"][i]
            if kind == "gdn":
                cols = [(O_AQ + h * 128, 128, 0), (O_AK + h * 128, 128, 128), (O_AV + h * 128, 128, 256), (O_AZ + h * 128, 128, 384)]
            else:
                cols = [(O_BQ + h * 128, 128, 0), (O_BK + h * 128, 128, 128), (O_BV + h * 256, 256, 256), (O_BR + h * 256, 256, 512)]
            pairs = [(w[:, :, o:o + n], w_in_d[l, :, c0:c0 + n].rearrange("(kc p) c -> p kc c", p=128)) for (c0, n, o) in cols]
            kb.dma("pool", pairs, wslot_key[i], W=[hw["res"][i]])
            return w, hw["res"][i]

        k_wsm = kb.dsem("wsm")
        k_w2b = kb.dsem("w2b")
        wm_k = [kb.dsem(f"wm{i}") for i in range(2)]
        wo_k = [kb.dsem(f"wo{i}") for i in range(2)]
        wu_k = [kb.dsem(f"wu{i}") for i in range(4)]
        wd_k = [kb.dsem(f"wd{i}") for i in range(4)]

        for l in range(NL):
            with ExitStack() as ls:
              vecs = sb("vecs", [128, NVEC], F32, ls)
              kb.barrier()
              kb.dma("sp", [(vecs[:], vecs_d[:, l * NVEC:(l + 1) * NVEC])], k_vec, W=[r_const])

              def vcol(off, n=1, vecs=vecs):
                  return vecs[:, off:off + n]
              with ExitStack() as ms:
                  hT = sb("hT", [128, KC, T], BF16, ms)
                  hr = [[Res() for _ in range(NT)] for _ in range(KC)]
                  oTa = sb("oTa", [128, 4, T], BF16, ms)
                  oTa_r = [[Res() for _ in range(NT)] for _ in range(4)]
                  wsm = sb("wsm", [128, KC, 24], BF16, ms)
                  r_wsm = Res()
                  with nc.allow_non_contiguous_dma(reason="small gate columns"):
                      kb.dma("pool", [(wsm[:, :, 0:8], w_in_d[l, :, O_AB:O_AB + 8].rearrange("(kc p) c -> p kc c", p=128)),
                                      (wsm[:, :, 8:24], w_in_d[l, :, O_GLR:O_GLR + 16].rearrange("(kc p) c -> p kc c", p=128)),
                                      ], k_wsm, W=[r_wsm])
                  with ExitStack() as s1:
                      sq = [sb(f"sq{i}", [128, TT], BF16, s1) for i in range(2)]
                      sq_res = [Res(), Res()]
                      rstd = sb("rstd", [128, TT], F32, s1)
                      rstd_res = Res()
                      for tt in range(NT):
                          tsl = slice(tt * TT, (tt + 1) * TT)
                          norm_tile(lambda kc: xT[:, kc, tsl], lambda kc: xr[kc][tt], lambda kc: vcol(kc),
                                    lambda kc: hT[:, kc, tsl], lambda kc: hr[kc][tt], sq, sq_res, rstd, rstd_res)
                  if l == 0:
                      tap("hT", hT[:, 0, :], [128, T], [hr[0][t_] for t_ in range(NT)])

                  with ExitStack() as gs:
                      kb.barrier()
                      S = lambda n, shp, d=F32: sb(n, shp, d, gs)
                      hw["slots"] = [S(f"wsg{i}", [128, KC, 512], BF16) for i in range(2)]
                      hw["res"] = [Res(), Res()]
                      nA = S("nA", [1, 4]); nA_r = Res()
                      act(nA[:], hv[0:1, l * 8:l * 8 + 4], AF.Exp, R=[r_const], W=[nA_r])
                      ts_("dve", nA[:], nA[:], -1.0, None, ALU.mult, None, R=[nA_r], W=[nA_r])

                      def make_head(i):
                          halo = S("halo", [128, 3, 3]); halo_r = [Res() for _ in range(3)]
                          raw1 = S("raw1", [128, 3 + TT]); raw1_r = Res()
                          cacc = S("cacc", [128, TT]); cacc_r = Res()
                          tmpf = S("tmpf", [128, TT]); tmpf_r = Res()
                          tmpg = S("tmpg", [128, TT]); tmpg_r = Res()
                          qT = S("qT", [128, TT], BF16); qT_r = Res()
                          kT = S("kT", [128, TT], BF16); kT_r = Res()
                          vT = S("vT", [128, TT], BF16); vT_r = Res()
                          zs = S("zs", [128, TT], BF16); zs_r = Res()
                          vb = S("vb", [128, TT], BF16); vb_r = Res()
                          kbg = S("kbg", [128, TT], BF16); kbg_r = Res()
                          kdc = S("kdc", [128, TT], BF16); kdc_r = Res()
                          Am = S("Am", [128, TT], BF16); Am_r = Res()
                          AT = S("AT", [128, TT], BF16); AT_r = Res()
                          Om = S("Om", [128, TT], BF16); Om_r = Res()
                          OT = S("OT", [128, TT], BF16); OT_r = Res()
                          Zp, Zp_r = OT, OT_r
                          Zt, Zt_r = Om, Om_r
                          Inv = S("Inv", [128, TT], BF16); Inv_r = Res()
                          Rm = S("Rm", [128, TT], BF16); Rm_r = Res()
                          qkm = S("qkm", [128, TT], BF16); qkm_r = Res()
                          qd = S("qd", [128, TT], BF16); qd_r = Res()
                          nwt, nwt_r = vT, vT_r
                          oraw, oraw_r = cacc, cacc_r
                          sqg, sqg_r = Om, Om_r
                          vnew = [S("vnew", [128, 128], BF16)] * 2
                          vnew_r = [Res()] * 2
                          Sst = S("Sst", [128, 128]); Sst_r = Res()
                          Sbf = S("Sbf", [128, 128], BF16); Sbf_r = Res()
                          cols = S("cols", [128, 16]); cols_r = Res()
                          rowA = S("rowA", [1, TT]); rowA_r = Res()
                          rowB = S("rowB", [1, TT]); rowB_r = Res()
                          Grow = [S("Grow", [1, 1 + TT])] * 2
                          Grow_r = [Res()] * 2
                          rGb = S("rGb", [1, TT]); rGb_r = Res()
                          rkb = S("rkb", [1, TT]); rkb_r = Res()
                          rcd = S("rcd", [1, 8]); rcd_r = Res()
                          def run(h, W_, W_r):
                              kb.op("dve", lambda: DVE.memset(Sst[:], 0.0), W=[Sst_r])
                              kb.op("dve", lambda: DVE.memset(Sbf[:], 0.0), W=[Sbf_r])
                              for i in range(3):
                                  kb.op("pool", lambda i=i: POOL.memset(halo[:, i, :], 0.0), W=[halo_r[i]])
                              for tt in range(NT):
                                  tsl = slice(tt * TT, (tt + 1) * TT)
                                  hres = [hr[kc][tt] for kc in range(KC)]
                                  Gc_, Gc_r = Grow[tt % 2], Grow_r[tt % 2]
                                  Gp_, Gp_r = Grow[(tt + 1) % 2], Grow_r[(tt + 1) % 2]
                                  pb, pr = ps()
                                  for kc in range(KC):
                                      mm(pb[0:1, :], wsm[:, kc, h:h + 1], hT[:, kc, tsl], kc == 0, kc == KC - 1, R=[r_wsm, hres[kc]], W=[pr])
                                  act(rowA[:], pb[0:1, :], AF.Exp, R=[pr], W=[rowA_r], scale=-1.0)
                                  act(rowA[:], rowA[:], AF.Ln, R=[rowA_r], W=[rowA_r], bias=1.0)
                                  pb, pr = ps()
                                  for kc in range(KC):
                                      mm(pb[0:1, :], wsm[:, kc, 4 + h:5 + h], hT[:, kc, tsl], kc == 0, kc == KC - 1, R=[r_wsm, hres[kc]], W=[pr])
                                  act(rowB[:], pb[0:1, :], AF.Exp, R=[pr, r_const], W=[rowB_r], bias=hv[0:1, l * 8 + 4 + h:l * 8 + 5 + h])
                                  act(rowB[:], rowB[:], AF.Ln, R=[rowB_r], W=[rowB_r], bias=1.0)
                                  ts_("dve", rowB[:], rowB[:], nA[0:1, h:h + 1], None, ALU.mult, None, R=[rowB_r, nA_r], W=[rowB_r])
                                  if tt == 0:
                                      kb.op("dve", lambda: DVE.memset(Gc_[:, 0:1], 0.0), W=[Gc_r])
                                  else:
                                      kb.op("dve", lambda: DVE.tensor_copy(Gc_[:, 0:1], Gp_[:, TT:TT + 1]), R=[Gp_r], W=[Gc_r])
                                  kb.op("dve", lambda: DVE.tensor_tensor_scan(Gc_[:, 1:1 + TT], onesf[0:1, 0:1].to_broadcast([1, TT]), rowB[:], Gc_[:, 0:1], ALU.mult, ALU.add),
                                        R=[r_const, rowB_r, Gc_r], W=[Gc_r])
                                  Gv = Gc_[:, 1:1 + TT]
                                  Gst4 = Gc_[:, 0:TT:128].unsqueeze(2).to_broadcast([1, 4, 128])
                                  Gla4 = Gc_[:, 128:TT + 1:128].unsqueeze(2).to_broadcast([1, 4, 128])
                                  tt_("dve", rGb[:], Gv, rowA[:], ALU.subtract, R=[Gc_r, rowA_r], W=[rGb_r])
                                  tt_("dve", v4(rkb[:]), v4(rGb[:]), Gst4, ALU.subtract, R=[rGb_r, Gc_r], W=[rkb_r])
                                  act(rkb[:], rkb[:], AF.Exp, R=[rkb_r], W=[rkb_r])
                                  rkd, rkd_r = rowB, rowB_r
                                  rbe, rbe_r = rowA, rowA_r
                                  tt_("dve", v4(rkd[:]), v4(Gv), Gla4, ALU.subtract, R=[Gc_r], W=[rkd_r])
                                  act(rkd[:], rkd[:], AF.Exp, R=[rkd_r], W=[rkd_r], scale=-1.0)
                                  act(rbe[:], rowA[:], AF.Exp, R=[rowA_r], W=[rbe_r], scale=-1.0)
                                  tt_("dve", rcd[:, 0:4], Gc_[:, 128:TT + 1:128], Gc_[:, 0:TT:128], ALU.subtract, R=[Gc_r], W=[rcd_r])
                                  act(rcd[:, 0:4], rcd[:, 0:4], AF.Exp, R=[rcd_r], W=[rcd_r])
                                  pcol, pcol_r = ps()
                                  for cc in range(4):
                                      csl = slice(cc * 128, (cc + 1) * 128)
                                      for qi, (rw, rw_r) in enumerate(((rkb, rkb_r), (rkd, rkd_r), (rbe, rbe_r))):
                                          kb.op("pe", lambda rw=rw, qi=qi: PE.matmul(pcol[:, cc * 4 + qi:cc * 4 + qi + 1], rw[0:1, csl], onesf[0:1, 0:1], start=True, stop=True),
                                                R=[rw_r, r_const], W=[pcol_r], inc=False)
                                      kb.op("pe", lambda: PE.matmul(pcol[:, cc * 4 + 3:cc * 4 + 4], onesf[0:1, 0:128], rcd[0:1, cc:cc + 1], start=True, stop=True),
                                            R=[rcd_r, r_const], W=[pcol_r], inc=(cc == 3))
                                  kb.op("dve", lambda: DVE.tensor_copy(cols[:], pcol[:, 0:16]), R=[pcol_r], W=[cols_r])
                                  colv = cols[:].rearrange("p (c q) -> p c q", q=4)

                                  yield
                                  for xi, (dst, dst_r) in enumerate(((qT, qT_r), (kT, kT_r), (vT, vT_r))):
                                      pb, pr = ps()
                                      for kc in range(KC):
                                          mm(pb[:], W_[:, kc, xi * 128:(xi + 1) * 128], hT[:, kc, tsl], kc == 0, kc == KC - 1, R=[W_r, hres[kc]], W=[pr])
                                      rw, rw_r = raw1, raw1_r
                                      kb.op("pool", lambda xi=xi: POOL.tensor_copy(rw[:, 0:3], halo[:, xi, :]), R=[halo_r[xi]], W=[rw_r])
                                      act(rw[:, 3:3 + TT], pb[:], AF.Copy, R=[pr], W=[rw_r])
                                      cw = lambda tap_, xi=xi: vcol(32 + tap_ * 12 + xi * 4 + h)
                                      ts_("dve", cacc[:], rw[:, 0:TT], cw(0), None, ALU.mult, None, R=[rw_r, r_const], W=[cacc_r])
                                      for tp in (1, 2, 3):
                                          stt_("dve", cacc[:], rw[:, tp:tp + TT], cw(tp), cacc[:], ALU.mult, ALU.add, R=[rw_r, r_const, cacc_r], W=[cacc_r])
                                      kb.op("pool", lambda xi=xi: POOL.tensor_copy(halo[:, xi, :], rw[:, TT:TT + 3]), R=[rw_r], W=[halo_r[xi]])
                                      if xi == 2:
                                          silu_from("dve", vT[:], cacc[:], tmpf[:], R=[cacc_r], W=[vT_r], tmp_res=tmpf_r)
                                      else:
                                          silu_from("dve", tmpg[:], cacc[:], tmpf[:], R=[cacc_r], W=[tmpg_r], tmp_res=tmpf_r)
                                          act(sqg[:], tmpg[:], AF.Square, R=[tmpg_r], W=[sqg_r])
                                          pb2, pr2 = ps()
                                          mm(pb2[:], ones_b, sqg[:], True, True, R=[sqg_r, r_const], W=[pr2])
                                          act(tmpf[:], pb2[:], AF.Ln, R=[pr2], W=[tmpf_r], bias=EPS)
                                          act(tmpf[:], tmpf[:], AF.Exp, R=[tmpf_r], W=[tmpf_r], scale=-0.5)
                                          sc = (128.0 ** -0.5) if xi == 0 else 1.0
                                          stt_("dve", dst[:], tmpg[:], sc, tmpf[:], ALU.mult, ALU.mult, R=[tmpg_r, tmpf_r], W=[dst_r])
                                  yield
                                  pb, pr = ps()
                                  for kc in range(KC):
                                      mm(pb[:], W_[:, kc, 384:512], hT[:, kc, tsl], kc == 0, kc == KC - 1, R=[W_r, hres[kc]], W=[pr])
                                  silu_from("dve", zs[:], pb[:], tmpf[:], R=[pr], W=[zs_r], tmp_res=tmpf_r)

                                  yield
                                  pt, ptr = psb()
                                  for cc in range(4):
                                      csl = slice(cc * 128, (cc + 1) * 128)
                                      kb.op("pe", lambda: PE.transpose(pt[:, csl], vT[:, csl], ident_b), R=[vT_r, r_const], W=[ptr], inc=(cc == 3))
                                  tt_("dve", v4(vb[:]), v4(pt), colv[:, :, 2:3].to_broadcast([128, 4, 128]), ALU.mult, R=[ptr, cols_r], W=[vb_r])
                                  pt, ptr = psb()
                                  for cc in range(4):
                                      csl = slice(cc * 128, (cc + 1) * 128)
                                      kb.op("pe", lambda: PE.transpose(pt[:, csl], kT[:, csl], ident_b), R=[kT_r, r_const], W=[ptr], inc=(cc == 3))
                                  tt_("dve", v4(kbg[:]), v4(pt), colv[:, :, 0:1].to_broadcast([128, 4, 128]), ALU.mult, R=[ptr, cols_r], W=[kbg_r])
                                  tt_("dve", v4(kdc[:]), v4(pt), colv[:, :, 1:2].to_broadcast([128, 4, 128]), ALU.mult, R=[ptr, cols_r], W=[kdc_r])

                                  yield
                                  def expo(lrow, lrow_r, lneg, rrow, rrow_r, rneg, mask_idx):
                                      pb_, pr_ = ps()
                                      for cc in range(4):
                                          csl = slice(cc * 128, (cc + 1) * 128)
                                          kb.op("pe", lambda: PE.matmul(pb_[:, csl], (negf if rneg else onesf)[0:1, 0:128], rrow[0:1, csl], start=True, stop=False),
                                                R=[rrow_r, r_const], W=[pr_], inc=False)
                                          kb.op("pe", lambda: PE.matmul(pb_[:, csl], lrow[0:1, csl], (negf if lneg else onesf)[0:1, 0:128], start=False, stop=False),
                                                R=[lrow_r, r_const], W=[pr_], inc=False)
                                          kb.op("pe", lambda: PE.matmul(pb_[:, csl], ident_b, CB(mask_idx), start=False, stop=True),
                                                R=[r_const], W=[pr_], inc=(cc == 3))
                                      return pb_, pr_
                                  pkk, pkk_r = ps()
                                  for cc in range(4):
                                      csl = slice(cc * 128, (cc + 1) * 128)
                                      mm(pkk[:, csl], kT[:, csl], kT[:, csl], True, True, R=[kT_r], W=[pkk_r])
                                  pe1, pe1_r = expo(Gv, Gc_r, True, rGb, rGb_r, False, C_MN_STR_T)
                                  act(tmpf[:], pe1[:], AF.Exp, R=[pe1_r], W=[tmpf_r])
                                  tt_("dve", AT[:], pkk[:], tmpf[:], ALU.mult, R=[pkk_r, tmpf_r], W=[AT_r])
                                  pe2, pe2_r = expo(rGb, rGb_r, False, Gv, Gc_r, True, C_MN_STR)
                                  act(tmpg[:], pe2[:], AF.Exp, R=[pe2_r], W=[tmpg_r])
                                  tt_("dve", Am[:], pkk[:], tmpg[:], ALU.mult, R=[pkk_r, tmpg_r], W=[Am_r])
                                  pe3, pe3_r = expo(Gv, Gc_r, True, Gv, Gc_r, False, C_MN_INC_T)
                                  act(tmpf[:], pe3[:], AF.Exp, R=[pe3_r], W=[tmpf_r])
                                  pqk, pqk_r = ps()
                                  for cc in range(4):
                                      csl = slice(cc * 128, (cc + 1) * 128)
                                      mm(pqk[:, csl], kT[:, csl], qT[:, csl], True, True, R=[kT_r, qT_r], W=[pqk_r])
                                  tt_("dve", qkm[:], pqk[:], tmpf[:], ALU.mult, R=[pqk_r, tmpf_r], W=[qkm_r])
                                  rGc, rGc_r = rkb, rkb_r
                                  tt_("dve", v4(rGc[:]), v4(Gv), Gst4, ALU.subtract, R=[Gc_r], W=[rGc_r])
                                  pg, pg_r = ps()
                                  for cc in range(4):
                                      csl = slice(cc * 128, (cc + 1) * 128)
                                      kb.op("pe", lambda: PE.matmul(pg[:, csl], onesf[0:1, 0:128], rGc[0:1, csl], start=True, stop=True),
                                            R=[rGc_r, r_const], W=[pg_r], inc=(cc == 3))
                                  act(tmpg[:], pg[:], AF.Exp, R=[pg_r], W=[tmpg_r])
                                  tt_("dve", qd[:], qT[:], tmpg[:], ALU.mult, R=[qT_r, tmpg_r], W=[qd_r])

                                  yield
                                  tt_("dve", v4(Om[:]), v4(Am[:]), b4(CB(C_LV + 0)), ALU.mult, R=[Am_r, r_const], W=[Om_r])
                                  stt_("dve", v4(Inv[:]), v4(Om[:]), -1.0, b4(ident_b), ALU.mult, ALU.add, R=[Om_r, r_const], W=[Inv_r])
                                  tt_("dve", v4(OT[:]), v4(AT[:]), b4(CB(C_LVT + 0)), ALU.mult, R=[AT_r, r_const], W=[OT_r])
                                  stt_("dve", v4(Rm[:]), v4(OT[:]), -1.0, b4(ident_b), ALU.mult, ALU.add, R=[OT_r, r_const], W=[Rm_r])
                                  for li in range(1, 7):
                                      last = (li == 6)
                                      pz, pz_r = ps()
                                      for cc in range(4):
                                          csl = slice(cc * 128, (cc + 1) * 128)
                                          mm(pz[:, csl], Am[:, csl], Rm[:, csl], True, True, R=[Am_r, Rm_r], W=[pz_r])
                                      if not last:
                                          pzp, pzp_r = ps()
                                          for cc in range(4):
                                              csl = slice(cc * 128, (cc + 1) * 128)
                                              mm(pzp[:, csl], AT[:, csl], Inv[:, csl], True, True, R=[AT_r, Inv_r], W=[pzp_r])
                                      tt_("dve", v4(Zt[:]), v4(pz[:]), b4(CB(C_LVT + li)), ALU.mult, R=[pz_r, r_const], W=[Zt_r])
                                      if not last:
                                          tt_("dve", v4(Zp[:]), v4(pzp[:]), b4(CB(C_LV + li)), ALU.mult, R=[pzp_r, r_const], W=[Zp_r])
                                      pr2_, pr2_r = ps()
                                      for cc in range(4):
                                          csl = slice(cc * 128, (cc + 1) * 128)
                                          mm(pr2_[:, csl], Inv[:, csl], Zt[:, csl], True, True, R=[Inv_r, Zt_r], W=[pr2_r])
                                      if not last:
                                          pi2_, pi2_r = ps()
                                          for cc in range(4):
                                              csl = slice(cc * 128, (cc + 1) * 128)
                                              mm(pi2_[:, csl], Rm[:, csl], Zp[:, csl], True, True, R=[Rm_r, Zp_r], W=[pi2_r])
                                      tt_("dve", Rm[:], Rm[:], pr2_[:], ALU.subtract, R=[Rm_r, pr2_r], W=[Rm_r])
                                      if not last:
                                          tt_("dve", Inv[:], Inv[:], pi2_[:], ALU.subtract, R=[Inv_r, pi2_r], W=[Inv_r])
                                      yield
                                  yield
                                  pw, pw_r = ps()
                                  for cc in range(4):
                                      csl = slice(cc * 128, (cc + 1) * 128)
                                      mm(pw[:, csl], kbg[:, csl], Rm[:, csl], True, True, R=[kbg_r, Rm_r], W=[pw_r])
                                  amul(nwt[:], pw[:], -1.0, R=[pw_r], W=[nwt_r])

                                  yield
                                  for cc in range(4):
                                      csl = slice(cc * 128, (cc + 1) * 128)
                                      vn, vn_r = vnew[cc % 2], vnew_r[cc % 2]
                                      pv, pv_r = ps()
                                      mm(pv[:, 0:128], Rm[:, csl], vb[:, csl], True, False, R=[Rm_r, vb_r], W=[pv_r])
                                      mm(pv[:, 0:128], nwt[:, csl], Sbf[:], False, True, R=[nwt_r, Sbf_r], W=[pv_r])
                                      act(vn[:], pv[:, 0:128], AF.Copy, R=[pv_r], W=[vn_r])
                                      po, po_r = ps()
                                      mm(po[:, 0:128], Sbf[:], qd[:, csl], True, False, R=[Sbf_r, qd_r], W=[po_r])
                                      mm(po[:, 0:128], vn[:], qkm[:, csl], False, True, R=[vn_r, qkm_r], W=[po_r])
                                      pd, pd_r = ps()
                                      mm(pd[:, 0:128], kdc[:, csl], vn[:], True, True, R=[kdc_r, vn_r], W=[pd_r])
                                      act(oraw[:, csl], po[:, 0:128], AF.Copy, R=[po_r], W=[oraw_r])
                                      ts_("dve", Sst[:], Sst[:], cols[:, cc * 4 + 3:cc * 4 + 4], None, ALU.mult, None, R=[Sst_r, cols_r], W=[Sst_r])
                                      tt_("dve", Sst[:], pd[:, 0:128], Sst[:], ALU.add, R=[Sst_r, pd_r], W=[Sst_r])
                                      act(Sbf[:], Sst[:], AF.Copy, R=[Sst_r], W=[Sbf_r])
                                      yield

                                  yield
                                  act(sqg[:], oraw[:], AF.Square, R=[oraw_r], W=[sqg_r])
                                  pb2, pr2 = ps()
                                  mm(pb2[:], ones_b, sqg[:], True, True, R=[sqg_r, r_const], W=[pr2])
                                  rmsnorm_rstd(pb2[:], pr2, 128, tmpf[:], tmpf_r)
                                  stt_("dve", tmpg[:], oraw[:], vcol(80), tmpf[:], ALU.mult, ALU.mult, R=[oraw_r, tmpf_r, r_const], W=[tmpg_r])
                                  tt_("pool", oTa[:, h, tsl], tmpg[:], zs[:], ALU.mult, R=[tmpg_r, zs_r], W=[oTa_r[h][tt]])
                          return run
                      runners = [make_head(0), make_head(1)]
                      for pair in (((0, 1), (2, 3)) if "gdn" in phases else ()):
                          ws = [load_head_weights(l, "gdn", h) for h in pair]
                          caps = []
                          for i, h in enumerate(pair):
                              st["chain"] = i
                              kb.cap = []
                              for _ in runners[i](h, ws[i][0], ws[i][1]):
                                  pass
                              caps.append(kb.cap)
                              kb.cap = None
                          st["chain"] = None
                          kb.replay(caps)
                  if l == 0:
                      for h in range(4):
                          tap(f"oTa{h}", oTa[:, h, :], [128, T], [oTa_r[h][t_] for t_ in range(NT)])
                  ms2 = ExitStack()
                  ms2.__enter__()
                  kb.barrier()
                  oTb = sb("oTb", [128, 8, T], BF16, ms2)
                  oTb_r = [[Res() for _ in range(NT)] for _ in range(8)]
                  w2b = sb("w2b", [16, 512], BF16, ms2)
                  r_w2b = Res()
                  kb.dma("pool", [(w2b[:], w2_d[:, l * 512:(l + 1) * 512])], k_w2b, W=[r_w2b])
                  with ExitStack() as gs:
                      S = lambda n, shp, d=F32: sb(n, shp, d, gs)
                      hw["slots"] = [S(f"wsl{i}", [128, KC, 768], BF16) for i in range(2)]
                      hw["res"] = [Res(), Res()]
                      nxt = load_head_weights(l, "gla", 0)
                      glrT = S("glrT", [16, TT], BF16); glr_r = Res()
                      qf = S("qf", [128, TT]); qf_r = Res()
                      kf = S("kf", [128, TT]); kf_r = Res()
                      tmpf = S("tmpf2", [128, TT]); tmpf_r = Res()
                      tmpg = S("tmpg2", [128, TT]); tmpg_r = Res()
                      tmph, tmph_r = tmpg, tmpg_r
                      Pp = [S(f"Pp{i}", [128, 1 + TT]) for i in range(2)]; Pp_r = [Res(), Res()]
                      lrow, lrow_r = tmpg, tmpg_r
                      qk2 = S("qk2", [128, 2, TT], BF16)
                      qin = qk2[:, 0, :]; qin_r = Res()
                      kin = qk2[:, 1, :]; kin_r = Res()
                      qdc = S("qdc", [128, TT], BF16); qdc_r = Res()
                      kdcT = S("kdcT", [128, TT], BF16); kdcT_r = Res()
                      kdt = S("kdt", [128, TT], BF16); kdt_r = Res()
                      vtok = S("vtok", [128, 4, 256], BF16); vtok_r = Res()
                      attn = S("attn", [128, TT], BF16); attn_r = Res()
                      rs = S("rs", [128, 2, TT], BF16); rs_r = Res()
                      oraw2 = S("oraw2", [128, 2, TT]); oraw2_r = Res()
                      S2 = S("S2", [128, 256]); S2_r = Res()
                      S2b = S("S2b", [128, 256], BF16); S2b_r = Res()
                      cdc = S("cdc", [128, 4]); cdc_r = Res()
                      ngb = S("ngb", [128, 4]); ngb_r = Res()
                      ts_("dve", ngb[:], vcol(83, 4), -1.0, None, ALU.mult, None, R=[r_const], W=[ngb_r])
                      for h in (range(4) if "gla" in phases else ()):
                          W_, W_r = nxt
                          nxt = load_head_weights(l, "gla", h + 1) if h < 3 else None
                          kb.op("dve", lambda: DVE.memset(S2[:], 0.0), W=[S2_r])
                          kb.op("dve", lambda: DVE.memset(S2b[:], 0.0), W=[S2b_r])
                          for tt in range(NT):
                              tsl = slice(tt * TT, (tt + 1) * TT)
                              hres = [hr[kc][tt] for kc in range(KC)]
                              Pc, Pc_r = Pp[tt % 2], Pp_r[tt % 2]
                              Pv_, Pv_r = Pp[(tt + 1) % 2], Pp_r[(tt + 1) % 2]
                              pb, pr = ps()
                              for kc in range(KC):
                                  mm(pb[:], W_[:, kc, 0:128], hT[:, kc, tsl], kc == 0, kc == KC - 1, R=[W_r, hres[kc]], W=[pr])
                              amul(qf[:], pb[:], 128.0 ** -0.5, R=[pr], W=[qf_r])
                              pb, pr = ps()
                              for kc in range(KC):
                                  mm(pb[:], W_[:, kc, 128:256], hT[:, kc, tsl], kc == 0, kc == KC - 1, R=[W_r, hres[kc]], W=[pr])
                              act(kf[:], pb[:], AF.Copy, R=[pr], W=[kf_r])
                              for half in range(2):
                                  pb, pr = ps()
                                  for c2 in range(2):
                                      cc = half * 2 + c2
                                      for kc in range(KC):
                                          mm(pb[:, c2 * 256:(c2 + 1) * 256], hT[:, kc, tt * TT + cc * 128: tt * TT + (cc + 1) * 128], W_[:, kc, 256:512],
                                             kc == 0, kc == KC - 1, R=[W_r, hres[kc]], W=[pr])
                                  act(vtok[:, half * 2:half * 2 + 2, :], pb[:].rearrange("p (c e) -> p c e", e=256), AF.Copy, R=[pr], W=[vtok_r])
                              pb, pr = ps()
                              for kc in range(KC):
                                  mm(pb[0:16, :], wsm[:, kc, 8:24], hT[:, kc, tsl], kc == 0, kc == KC - 1, R=[r_wsm, hres[kc]], W=[pr])
                              act(glrT[:], pb[0:16, :], AF.Copy, R=[pr], W=[glr_r])
                              pb, pr = ps()
                              mm(pb[:], w2b[0:16, h * 128:(h + 1) * 128], glrT[:], True, True, R=[r_w2b, glr_r], W=[pr])
                              act(lrow[:], pb[:], AF.Exp, R=[pr, ngb_r], W=[lrow_r], bias=ngb[:, h:h + 1], scale=-1.0)
                              act(lrow[:], lrow[:], AF.Ln, R=[lrow_r], W=[lrow_r], bias=1.0)
                              if tt == 0:
                                  kb.op("dve", lambda: DVE.memset(Pc[:, 0:1], 0.0), W=[Pc_r])
                              else:
                                  kb.op("dve", lambda: DVE.tensor_copy(Pc[:, 0:1], Pv_[:, TT:TT + 1]), R=[Pv_r], W=[Pc_r])
                              kb.op("dve", lambda: DVE.tensor_tensor_scan(Pc[:, 1:1 + TT], onesf[:, 0:1].to_broadcast([128, TT]), lrow[:], Pc[:, 0:1], ALU.mult, ALU.add),
                                    R=[r_const, lrow_r, Pc_r], W=[Pc_r])
                              Pvw = v4(Pc[:, 1:1 + TT])
                              Pst = Pc[:, 0:TT:128].unsqueeze(2).to_broadcast([128, 4, 128])
                              Pmid = Pc[:, 65:TT + 1:128].unsqueeze(2).to_broadcast([128, 4, 128])
                              Pla = Pc[:, 128:TT + 1:128].unsqueeze(2).to_broadcast([128, 4, 128])
                              isc = 1.0 / 16.0
                              tt_("dve", v4(tmpf[:]), Pvw, Pmid, ALU.subtract, R=[Pc_r], W=[tmpf_r])
                              act(tmpg[:], tmpf[:], AF.Exp, R=[tmpf_r], W=[tmpg_r], scale=-isc)
                              tt_("dve", qin, qf[:], tmpg[:], ALU.mult, R=[qf_r, tmpg_r], W=[qin_r])
                              act(tmph[:], tmpf[:], AF.Exp, R=[tmpf_r], W=[tmph_r], scale=isc)
                              tt_("dve", kin, kf[:], tmph[:], ALU.mult, R=[kf_r, tmph_r], W=[kin_r])
                              tt_("dve", v4(tmpf[:]), Pvw, Pst, ALU.subtract, R=[Pc_r], W=[tmpf_r])
                              act(tmpg[:], tmpf[:], AF.Exp, R=[tmpf_r], W=[tmpg_r], scale=-isc)
                              tt_("dve", qdc[:], qf[:], tmpg[:], ALU.mult, R=[qf_r, tmpg_r], W=[qdc_r])
                              tt_("dve", v4(tmpf[:]), Pvw, Pla, ALU.subtract, R=[Pc_r], W=[tmpf_r])
                              act(tmph[:], tmpf[:], AF.Exp, R=[tmpf_r], W=[tmph_r], scale=isc)
                              tt_("dve", kdcT[:], kf[:], tmph[:], ALU.mult, R=[kf_r, tmph_r], W=[kdcT_r])
                              tt_("dve", cdc[:], Pc[:, 128:TT + 1:128], Pc[:, 0:TT:128], ALU.subtract, R=[Pc_r], W=[cdc_r])
                              act(cdc[:], cdc[:], AF.Exp, R=[cdc_r], W=[cdc_r], scale=-isc)
                              pa, pa_r = ps()
                              for cc in range(4):
                                  csl = slice(cc * 128, (cc + 1) * 128)
                                  mm(pa[:, csl], kin[:, csl], qin[:, csl], True, True, R=[kin_r, qin_r], W=[pa_r])
                              tt_("dve", v4(attn[:]), v4(pa[:]), b4(CB(C_C01_T)), ALU.mult, R=[pa_r, r_const], W=[attn_r])
                              pt, ptr = psb()
                              for cc in range(4):
                                  csl = slice(cc * 128, (cc + 1) * 128)
                                  kb.op("pe", lambda: PE.transpose(pt[:, csl], kdcT[:, csl], ident_b), R=[kdcT_r, r_const], W=[ptr], inc=(cc == 3))
                              act(kdt[:], pt, AF.Copy, R=[ptr], W=[kdt_r])
                              for cc in range(4):
                                  csl = slice(cc * 128, (cc + 1) * 128)
                                  po, po_r = ps()
                                  for et in range(2):
                                      esl = slice(et * 128, (et + 1) * 128)
                                      mm(po[:, esl], vtok[:, cc, esl], attn[:, csl], True, False, R=[vtok_r, attn_r], W=[po_r])
                                      mm(po[:, esl], S2b[:, esl], qdc[:, csl], False, True, R=[S2b_r, qdc_r], W=[po_r])
                                  pd, pd_r = ps()
                                  mm(pd[:, 0:256], kdt[:, csl], vtok[:, cc, :], True, True, R=[kdt_r, vtok_r], W=[pd_r])
                                  act(oraw2[:, :, csl], po[:, 0:256].rearrange("p (e k) -> p e k", k=128), AF.Copy, R=[po_r], W=[oraw2_r])
                                  ts_("dve", S2[:], S2[:], cdc[:, cc:cc + 1], None, ALU.mult, None, R=[S2_r, cdc_r], W=[S2_r])
                                  tt_("dve", S2[:], pd[:, 0:256], S2[:], ALU.add, R=[S2_r, pd_r], W=[S2_r])
                                  act(S2b[:], S2[:], AF.Copy, R=[S2_r], W=[S2b_r])
                                  if cc < 2:
                                      et = cc
                                      pb, pr = ps()
                                      for kc in range(KC):
                                          mm(pb[:], W_[:, kc, 512 + et * 128:512 + (et + 1) * 128], hT[:, kc, tsl], kc == 0, kc == KC - 1, R=[W_r, hres[kc]], W=[pr])
                                      silu_from("dve", rs[:, et, :], pb[:], tmpf[:], R=[pr], W=[rs_r], tmp_res=tmpf_r)
                              act(qk2[:], oraw2[:], AF.Square, R=[oraw2_r], W=[qin_r, kin_r])
                              pb2, pr2 = ps()
                              for et in range(2):
                                  mm(pb2[:], ones_b, qk2[:, et, :], et == 0, et == 1, R=[qin_r, kin_r, r_const], W=[pr2])
                              rmsnorm_rstd(pb2[:], pr2, 256, tmpf[:], tmpf_r)
                              for et in range(2):
                                  stt_("dve", tmpg[:], oraw2[:, et, :], vcol(81 + et), tmpf[:], ALU.mult, ALU.mult, R=[oraw2_r, tmpf_r, r_const], W=[tmpg_r])
                                  tt_("pool", oTb[:, h * 2 + et, tsl], tmpg[:], rs[:, et, :], ALU.mult, R=[tmpg_r, rs_r], W=[oTb_r[h * 2 + et][tt]])

                  if l == 0:
                      for h in range(8):
                          tap(f"oTb{h}", oTb[:, h, :], [128, T], [oTb_r[h][t_] for t_ in range(NT)])
                  with ExitStack() as gs:
                      kb.barrier()
                      S = lambda n, shp, d=F32: sb(n, shp, d, gs)
                      NW = 2
                      wm = [S(f"wm{i}", [128, 28, 128], BF16) for i in range(NW)]
                      wm_r = [Res() for _ in range(NW)]
                      wo = [S(f"wo{i}", [128, KC, 128], BF16) for i in range(NW)]
                      wo_r = [Res() for _ in range(NW)]
                      stage = S("stage", [128, KC, TT], BF16); stage_r = [Res() for _ in range(KC)]
                      tmpo = S("tmpo", [128, KC, TT]); tmpo_r = [Res() for _ in range(KC)]
                      sa = S("sa", [128, TT]); sa_r = Res()
                      sb_ = S("sb_", [128, TT]); sb_r = Res()
                      m1 = S("m1", [128, TT]); m1_r = Res()
                      sq = [S(f"sqm{i}", [128, TT], BF16) for i in range(2)]; sq_res = [Res(), Res()]
                      rstd = S("rstdm", [128, TT]); rstd_res = Res()
                      t2 = S("t2", [128, TT]); t2_r = Res()
                      cnt = {"m": 0, "o": 0}

                      def load_wm(ct):
                          i = cnt["m"] % NW
                          cnt["m"] += 1
                          csl = slice(ct * 128, (ct + 1) * 128)
                          pairs = [(wm[i][:, 0:4, :], w_oa_d[l, :, csl].rearrange("(k p) c -> p k c", p=128)),
                                   (wm[i][:, 4:12, :], w_ob_d[l, :, csl].rearrange("(k p) c -> p k c", p=128)),
                                   (wm[i][:, 12:20, :], w_in_d[l, :, O_GA + ct * 128:O_GA + (ct + 1) * 128].rearrange("(k p) c -> p k c", p=128)),
                                   (wm[i][:, 20:28, :], w_in_d[l, :, O_GB + ct * 128:O_GB + (ct + 1) * 128].rearrange("(k p) c -> p k c", p=128))]
                          kb.dma("pool", pairs, wm_k[i], W=[wm_r[i]])
                          return wm[i], wm_r[i]

                      def load_wo(ct):
                          i = cnt["o"] % NW
                          cnt["o"] += 1
                          kb.dma("pool", [(wo[i][:], w_o_d[l, :, ct * 128:(ct + 1) * 128].rearrange("(k p) c -> p k c", p=128))], wo_k[i], W=[wo_r[i]])
                          return wo[i], wo_r[i]

                      for tt in (range(NT) if "merge" in phases else ()):
                          tsl = slice(tt * TT, (tt + 1) * TT)
                          nw = load_wm(0)
                          for ct in range(KC):
                              w_, w_r = nw
                              if ct < KC - 1:
                                  nw = load_wm(ct + 1)
                              pya, pya_r = ps()
                              for k in range(4):
                                  mm(pya[:], w_[:, k, :], oTa[:, k, tsl], k == 0, k == 3, R=[w_r, oTa_r[k][tt]], W=[pya_r])
                              pyb, pyb_r = ps()
                              for k in range(8):
                                  mm(pyb[:], w_[:, 4 + k, :], oTb[:, k, tsl], k == 0, k == 7, R=[w_r, oTb_r[k][tt]], W=[pyb_r])
                              pga, pga_r = ps()
                              for k in range(8):
                                  mm(pga[:], w_[:, 12 + k, :], hT[:, k, tsl], k == 0, k == 7, R=[w_r, hr[k][tt]], W=[pga_r])
                              pgb, pgb_r = ps()
                              for k in range(8):
                                  mm(pgb[:], w_[:, 20 + k, :], hT[:, k, tsl], k == 0, k == 7, R=[w_r, hr[k][tt]], W=[pgb_r])
                              act(sa[:], pga[:], AF.Exp, R=[pga_r], W=[sa_r], scale=-1.0)
                              act(sa[:], sa[:], AF.Ln, R=[sa_r], W=[sa_r], bias=1.0)
                              act(sa[:], sa[:], AF.Exp, R=[sa_r], W=[sa_r], scale=-1.0)
                              act(sb_[:], pgb[:], AF.Exp, R=[pgb_r], W=[sb_r], scale=-1.0)
                              act(sb_[:], sb_[:], AF.Ln, R=[sb_r], W=[sb_r], bias=1.0)
                              act(sb_[:], sb_[:], AF.Exp, R=[sb_r], W=[sb_r], scale=-1.0)
                              tt_("dve", m1[:], pya[:], sa[:], ALU.mult, R=[pya_r, sa_r], W=[m1_r])
                              tt_("dve", sb_[:], pyb[:], sb_[:], ALU.mult, R=[pyb_r, sb_r], W=[sb_r])
                              tt_("dve", stage[:, ct, :], m1[:], sb_[:], ALU.add, R=[m1_r, sb_r], W=[stage_r[ct]])
                          pss, pss_r = pacc, pacc_r
                          nw = load_wo(0)
                          for ct in range(KC):
                              w_, w_r = nw
                              if ct < KC - 1:
                                  nw = load_wo(ct + 1)
                              pb, pr = ps()
                              for k in range(KC):
                                  mm(pb[:], w_[:, k, :], stage[:, k, :], k == 0, k == KC - 1, R=[w_r, stage_r[k]], W=[pr])
                              act(tmpo[:, ct, :], pb[:], AF.Copy, R=[pr], W=[tmpo_r[ct]])
                              s = sq[ct % 2]
                              act(s[:], pb[:], AF.Square, R=[pr], W=[sq_res[ct % 2]])
                              mm(pss[:], ones_b, s[:], ct == 0, ct == KC - 1, R=[sq_res[ct % 2], r_const], W=[pss_r])
                          rmsnorm_rstd(pss[:], pss_r, D, rstd[:], rstd_res)
                          for ct in range(KC):
                              stt_("dve", t2[:], tmpo[:, ct, :], vcol(8 + ct), rstd[:], ALU.mult, ALU.mult, R=[tmpo_r[ct], rstd_res, r_const], W=[t2_r])
                              tt_("dve", xT[:, ct, tsl], xT[:, ct, tsl], t2[:], ALU.add, R=[xr[ct][tt], t2_r], W=[xr[ct][tt]])

                  ms2.close()
              if l == 0:
                  for kc in range(KC):
                      tap(f"xmix{kc}", xT[:, kc, :], [128, T], [xr[kc][t_] for t_ in range(NT)])
              with ExitStack() as gs:
                  kb.barrier()
                  S = lambda n, shp, d=F32: sb(n, shp, d, gs)
                  h2 = S("h2", [128, KC, TT], BF16); h2_r = [Res() for _ in range(KC)]
                  uT = S("uT", [128, 32, TT], BF16); uT_r = [Res() for _ in range(32)]
                  NW = 4
                  wu = [S(f"wu{i}", [128, KC, 512], BF16) for i in range(NW)]; wu_r = [Res() for _ in range(NW)]
                  wd = [S(f"wd{i}", [128, 32, 128], BF16) for i in range(NW)]; wd_r = [Res() for _ in range(NW)]
                  tmpo = S("tmpo2", [128, KC, TT]); tmpo_r = [Res() for _ in range(KC)]
                  sq = [S(f"sqn{i}", [128, TT], BF16) for i in range(2)]; sq_res = [Res(), Res()]
                  rstd = S("rstdn", [128, TT]); rstd_res = Res()
                  rl = [S(f"rl{i}", [128, TT]) for i in range(2)]; rl_r = [Res(), Res()]
                  t2 = S("t2n", [128, TT]); t2_r = Res()
                  cnt = {"u": 0, "d": 0}

                  def load_wu(fb):
                      i = cnt["u"] % NW
                      cnt["u"] += 1
                      kb.dma("pool", [(wu[i][:], w_up_d[l, :, fb * 512:(fb + 1) * 512].rearrange("(k p) c -> p k c", p=128))], wu_k[i], W=[wu_r[i]])
                      return wu[i], wu_r[i]

                  def load_wd(ct):
                      i = cnt["d"] % NW
                      cnt["d"] += 1
                      kb.dma("pool", [(wd[i][:], w_dn_d[l, :, ct * 128:(ct + 1) * 128].rearrange("(k p) c -> p k c", p=128))], wd_k[i], W=[wd_r[i]])
                      return wd[i], wd_r[i]

                  for tt in (range(NT) if "mlp" in phases else ()):
                      tsl = slice(tt * TT, (tt + 1) * TT)
                      q_u = [load_wu(0), load_wu(1), load_wu(2)]
                      norm_tile(lambda kc: xT[:, kc, tsl], lambda kc: xr[kc][tt], lambda kc: vcol(16 + kc),
                                lambda kc: h2[:, kc, :], lambda kc: h2_r[kc], sq, sq_res, rstd, rstd_res)
                      for fb in range(8):
                          w_, w_r = q_u.pop(0)
                          if fb + 3 < 8:
                              q_u.append(load_wu(fb + 3))
                          for f4 in range(4):
                              ft = fb * 4 + f4
                              pb, pr = ps()
                              for k in range(KC):
                                  mm(pb[:], w_[:, k, f4 * 128:(f4 + 1) * 128], h2[:, k, :], k == 0, k == KC - 1, R=[w_r, h2_r[k]], W=[pr])
                              act(rl[ft % 2][:], pb[:], AF.Relu, R=[pr], W=[rl_r[ft % 2]])
                              tt_("dve", uT[:, ft, :], rl[ft % 2][:], rl[ft % 2][:], ALU.mult, R=[rl_r[ft % 2]], W=[uT_r[ft]])
                      q_d = [load_wd(0), load_wd(1), load_wd(2)]
                      pss, pss_r = pacc, pacc_r
                      for ct in range(KC):
                          w_, w_r = q_d.pop(0)
                          if ct + 3 < KC:
                              q_d.append(load_wd(ct + 3))
                          pb, pr = ps()
                          for k in range(32):
                              mm(pb[:], w_[:, k, :], uT[:, k, :], k == 0, k == 31, R=[w_r, uT_r[k]], W=[pr])
                          act(tmpo[:, ct, :], pb[:], AF.Copy, R=[pr], W=[tmpo_r[ct]])
                          s = sq[ct % 2]
                          act(s[:], pb[:], AF.Square, R=[pr], W=[sq_res[ct % 2]])
                          mm(pss[:], ones_b, s[:], ct == 0, ct == KC - 1, R=[sq_res[ct % 2], r_const], W=[pss_r])
                      rmsnorm_rstd(pss[:], pss_r, D, rstd[:], rstd_res)
                      for ct in range(KC):
                          stt_("dve", t2[:], tmpo[:, ct, :], vcol(24 + ct), rstd[:], ALU.mult, ALU.mult, R=[tmpo_r[ct], rstd_res, r_const], W=[t2_r])
                          tt_("dve", xT[:, ct, tsl], xT[:, ct, tsl], t2[:], ALU.add, R=[xr[ct][tt], t2_r], W=[xr[ct][tt]])

        k_out = kb.dsem("out")
        kb.dma("sp", [(outT_d[kc * 128:(kc + 1) * 128, :], xT[:, kc, :]) for kc in range(KC)], k_out,
               R=[xr[kc][tt] for kc in range(KC) for tt in range(NT)])
        nc.sync.wait_ge(kb.sems[k_out], kb.cnt[k_out])
        for name, key in tap_out.items():
            nc.sync.wait_ge(kb.sems[key], kb.cnt[key])
        print(f"[build] ops={kb.nops} waits={kb.nwaits} counts={ {k: v for k, v in kb.cnt.items() if not k.startswith('d_')} }", flush=True)
    return nc


def pack_small(inputs, l0, nl):
    vecs = np.zeros((128, nl * NVEC), np.float32)
    hv = np.zeros((1, nl * 8), np.float32)
    w2 = np.zeros((16, nl * 512), np.float32)
    for i in range(nl):
        l = l0 + i
        V0 = i * NVEC
        for j, name in enumerate(("norm_mix_pre", "norm_mix_post", "norm_mlp_pre", "norm_mlp_post")):
            vecs[:, V0 + j * 8:V0 + (j + 1) * 8] = np.asarray(inputs[name][l]).reshape(8, 128).T
        cw = np.asarray(inputs["conv_w"][l])
        for tp in range(4):
            vecs[:, V0 + 32 + tp * 12:V0 + 32 + (tp + 1) * 12] = cw[tp].reshape(12, 128).T
        vecs[:, V0 + 80] = np.asarray(inputs["gdn_norm"][l])
        vecs[:, V0 + 81:V0 + 83] = np.asarray(inputs["gla_norm"][l]).reshape(2, 128).T
        vecs[:, V0 + 83:V0 + 87] = np.asarray(inputs["gla_gate_b"][l]).reshape(4, 128).T
        hv[0, i * 8:i * 8 + 4] = np.asarray(inputs["a_log"][l])
        hv[0, i * 8 + 4:i * 8 + 8] = np.asarray(inputs["dt_bias"][l])
        w2[:, i * 512:(i + 1) * 512] = np.asarray(inputs["gla_gate_w2"][l])
    return vecs, hv, w2


_PROG = {}


def run_layers(xT_list, inputs, l0, nl):
    if nl not in _PROG:
        _PROG[nl] = build_program(nl)
    nc = _PROG[nl]
    vecs, hv, w2 = pack_small(inputs, l0, nl)
    consts = make_consts()
    sl = slice(l0, l0 + nl)
    shared = {
        "w_in": np.ascontiguousarray(inputs["w_in"][sl]), "w_out_a": np.ascontiguousarray(inputs["w_out_a"][sl]),
        "w_out_b": np.ascontiguousarray(inputs["w_out_b"][sl]), "w_o": np.ascontiguousarray(inputs["w_o"][sl]),
        "w_mlp_up": np.ascontiguousarray(inputs["w_mlp_up"][sl]), "w_mlp_down": np.ascontiguousarray(inputs["w_mlp_down"][sl]),
        "gate_w2": w2, "vecs": vecs, "hv": hv, "consts": consts,
    }
    in_maps = [dict(shared, xT=xT_list[c]) for c in range(len(xT_list))]
    res = run_bass_kernel_spmd(nc, in_maps, core_ids=list(range(len(xT_list))))
    return [np.asarray(r["outT"]) for r in res.results]


GDN_STOP = 0
N_FUSED = 4


def kernel(**inputs):
    inputs = {k: np.asarray(v) for k, v in inputs.items()}
    x = inputs["x"].astype(np.float32, copy=False)
    xT = [np.ascontiguousarray(x[b].T) for b in range(x.shape[0])]
    for l0 in range(0, L, N_FUSED):
        xT = run_layers(xT, inputs, l0, N_FUSED)
    out = np.stack([t.T for t in xT], axis=0)
    return np.ascontiguousarray(out.astype(np.float32))
```

```python
import types
import numpy as np
from contextlib import ExitStack
import concourse.bass as bass
import concourse.mybir as mybir
from concourse.bass_utils import run_bass_kernel_spmd

F32 = mybir.dt.float32
BF16 = mybir.dt.bfloat16
AF = mybir.ActivationFunctionType
ALU = mybir.AluOpType

D = 1024
T = 2048
L = 4
DFF = 4096
NCOL = 7192
NT = 4
TT = 512
KC = 8
EPS = 1e-6
O_AQ, O_AK, O_AV, O_AZ, O_AB, O_AA = 0, 512, 1024, 1536, 2048, 2052
O_BQ, O_BK, O_BV, O_BR, O_GLR, O_GA, O_GB = 2056, 2568, 3080, 4104, 5128, 5144, 6168
GDN_STOP = 0
GDN_REC = 0
NVEC = 87
C_ID, C_ONES, C_NEG, C_MN_INC_T, C_MN_STR_T, C_MN_STR, C_C01_T = 0, 1, 2, 3, 4, 5, 6
C_LV = 7
C_LVT = 14
NCB = 21


def make_consts():
    c = np.zeros((NCB, 128, 128), np.float32)
    i = np.arange(128)[:, None]
    j = np.arange(128)[None, :]
    c[C_ID] = (i == j)
    c[C_ONES] = 1.0
    c[C_NEG] = -1.0
    c[C_MN_INC_T] = np.where(j >= i, 0.0, -1e30)
    c[C_MN_STR_T] = np.where(j > i, 0.0, -1e30)
    c[C_MN_STR] = np.where(i > j, 0.0, -1e30)
    c[C_C01_T] = (j >= i)
    for li in range(7):
        m = 1 << li
        mm = ((i // (2 * m)) == (j // (2 * m))) & ((i % (2 * m)) >= m) & ((j % (2 * m)) < m)
        c[C_LV + li] = mm
        c[C_LVT + li] = mm.T
    return np.ascontiguousarray(c.transpose(1, 0, 2).reshape(128, NCB * 128))


def _freeze(fn):
    if fn.__closure__ is None:
        return fn
    cells = tuple(types.CellType(c.cell_contents) for c in fn.__closure__)
    return types.FunctionType(fn.__code__, fn.__globals__, fn.__name__, fn.__defaults__, cells)


class Res:
    __slots__ = ("w", "rs", "p")

    def __init__(self):
        self.w = None
        self.rs = {}
        self.p = set()


class KB:
    def __init__(self, nc, es):
        self.nc = nc
        self.es = es
        self.eng = {"pe": nc.tensor, "dve": nc.vector, "act": nc.scalar, "pool": nc.gpsimd, "sp": nc.sync}
        self.sems = {}
        self.cnt = {}
        self.seen = {e: {} for e in self.eng}
        self.pend = {e: [] for e in self.eng}
        for e in ("pe", "dve", "act", "pool"):
            self.sems[e] = es.enter_context(nc.semaphore("s_" + e))
            self.cnt[e] = 0
        self.nops = 0
        self.nwaits = 0
        self.cap = None

    def dsem(self, name):
        key = "d_" + name
        self.sems[key] = self.es.enter_context(self.nc.semaphore(key))
        self.cnt[key] = 0
        return key

    def flush(self, e):
        if not self.pend[e]:
            return
        self.cnt[e] += 1
        self.pend[e][-1][0].then_inc(self.sems[e], 1)
        ev = (e, self.cnt[e])
        for (_, r2, w2) in self.pend[e]:
            for r in list(r2) + list(w2):
                r.p.discard(e)
            self._reg(ev, r2, w2)
        self.pend[e] = []

    def barrier(self):
        for e in self.eng:
            self.flush(e)
        for e in self.eng:
            for k, v in self.cnt.items():
                if v > 0 and self.seen[e].get(k, 0) < v:
                    self.eng[e].wait_ge(self.sems[k], v)
                    self.seen[e][k] = v
                    self.nwaits += 1

    def _deps(self, e, R, W):
        for r in list(R) + list(W):
            for e2 in list(r.p):
                if e2 != e:
                    self.flush(e2)
        deps = {}

        def add(ev):
            k, v = ev
            if deps.get(k, 0) < v:
                deps[k] = v
        for r in R:
            if r.w is not None:
                add(r.w)
        for w in W:
            if w.w is not None:
                add(w.w)
            for k, v in w.rs.items():
                add((k, v))
        for k, v in deps.items():
            if k == e and e == "pe":
                continue
            if self.seen[e].get(k, 0) >= v:
                continue
            self.eng[e].wait_ge(self.sems[k], v)
            self.seen[e][k] = v
            self.nwaits += 1

    def _reg(self, ev, R, W):
        k, v = ev
        for w in W:
            w.w = ev
            w.rs = {}
        for r in R:
            if r.rs.get(k, 0) < v:
                r.rs[k] = v

    def replay(self, lists):
        idx = [0] * len(lists)
        live = True
        while live:
            live = False
            for k, lst in enumerate(lists):
                if idx[k] < len(lst):
                    item = lst[idx[k]]
                    idx[k] += 1
                    live = True
                    if item[0] == "op":
                        self.op(*item[1:])
                    else:
                        self.dma(*item[1:])

    def op(self, e, fn, R=(), W=(), inc=True):
        if self.cap is not None:
            self.cap.append(("op", e, _freeze(fn), R, W, inc))
            return
        self._deps(e, R, W)
        inst = fn()
        self.nops += 1
        if not inc:
            self.pend[e].append((inst, R, W))
            for r in list(R) + list(W):
                r.p.add(e)
            return
        self.cnt[e] += 1
        inst.then_inc(self.sems[e], 1)
        ev = (e, self.cnt[e])
        for (_, r2, w2) in self.pend[e]:
            for r in list(r2) + list(w2):
                r.p.discard(e)
            self._reg(ev, r2, w2)
        self.pend[e] = []
        self._reg(ev, R, W)

    def dma(self, e, pairs, key, R=(), W=()):
        if self.cap is not None:
            self.cap.append(("dma", e, pairs, key, R, W))
            return
        self._deps(e, R, W)
        for (o, i) in pairs:
            inst = self.eng[e].dma_start(out=o, in_=i)
            inst.then_inc(self.sems[key], 16)
            self.cnt[key] += 16
            self.nops += 1
        ev = (key, self.cnt[key])
        self._reg(ev, R, W)


def build_program(n_layers, taps=(), phases=("gdn", "gla", "merge", "mlp")):
    nc = bass.Bass("TRN2", target_bir_lowering=False)
    NL = n_layers
    dt = lambda name, shape, kind="ExternalInput": nc.dram_tensor(name, shape, F32, kind=kind).ap()
    xT_d = dt("xT", [D, T])
    w_in_d = dt("w_in", [NL, D, NCOL])
    w_oa_d = dt("w_out_a", [NL, 512, D])
    w_ob_d = dt("w_out_b", [NL, 1024, D])
    w_o_d = dt("w_o", [NL, D, D])
    w_up_d = dt("w_mlp_up", [NL, D, DFF])
    w_dn_d = dt("w_mlp_down", [NL, DFF, D])
    w2_d = dt("gate_w2", [16, NL * 512])
    vecs_d = dt("vecs", [128, NL * NVEC])
    hv_d = dt("hv", [1, NL * 8])
    consts_d = dt("consts", [128, NCB * 128])
    outT_d = dt("outT", [D, T], kind="ExternalOutput")
    tap_out = {}

    with ExitStack() as es:
        kb = KB(nc, es)
        PE, DVE, ACT, POOL = nc.tensor, nc.vector, nc.scalar, nc.gpsimd

        uid = {"n": 0}

        def sb(name, shape, dtype=F32, stack=es):
            uid["n"] += 1
            return stack.enter_context(nc.sbuf_tensor(f"s{uid['n']}_{name}", shape, dtype))

        NPB = 5
        pbanks = [es.enter_context(nc.psum_tensor(f"pb{i}", [128, 512], F32)) for i in range(NPB)]
        pres = [Res() for _ in range(NPB)]
        pacc = es.enter_context(nc.psum_tensor("pacc", [128, 512], F32))
        pacc_r = Res()
        pbf = [es.enter_context(nc.psum_tensor(f"pbf{i}", [128, 1024], BF16)) for i in range(2)]
        pbf_res = [Res(), Res()]
        st = {"pi": 0, "bi": 0, "chain": None, "ci": [0, 0]}
        allb = pbanks + [pacc]
        allr = pres + [pacc_r]

        def ps():
            c = st["chain"]
            if c is not None:
                i = 3 * c + st["ci"][c]
                st["ci"][c] = (st["ci"][c] + 1) % 3
                return allb[i], allr[i]
            i = st["pi"]
            st["pi"] = (i + 1) % NPB
            return pbanks[i], pres[i]

        def psb():
            c = st["chain"]
            if c is not None:
                return pbf[c][:, 0:512], pbf_res[c]
            i = st["bi"]
            st["bi"] = (i + 1) % 2
            return pbf[i][:, 0:512], pbf_res[i]

        xT = sb("xT", [128, KC, T])
        xr = [[Res() for _ in range(NT)] for _ in range(KC)]
        cb = sb("cb", [128, NCB * 128], BF16)
        cf = sb("cf", [128, 2 * 128])
        hv = sb("hv", [1, NL * 8])
        r_const = Res()

        def CB(i):
            return cb[:, i * 128:(i + 1) * 128]
        onesf = cf[:, 0:128]
        negf = cf[:, 128:256]
        ident_b = CB(C_ID)
        ones_b = CB(C_ONES)

        k_in = kb.dsem("in")
        kb.dma("sp", [(xT[:, kc, :], xT_d[kc * 128:(kc + 1) * 128, :]) for kc in range(KC)], k_in,
               W=[xr[kc][tt] for kc in range(KC) for tt in range(NT)])
        k_c = kb.dsem("c")
        kb.dma("pool", [(cb[:], consts_d)], k_c, W=[r_const])
        k_c2 = kb.dsem("c2")
        kb.dma("sp", [(cf[:], consts_d[:, 128:384]), (hv[:], hv_d)], k_c2, W=[r_const])
        k_vec = kb.dsem("vec")

        def tap(name, ap, shape, R):
            if name not in taps:
                return
            d = nc.dram_tensor("tap_" + name, shape, F32, kind="ExternalOutput").ap()
            key = kb.dsem("t_" + name)
            tap_out[name] = key
            kb.dma("pool", [(d, ap)], key, R=R)

        def mm(out, lhsT, rhs, start, stop, R, W):
            kb.op("pe", lambda: PE.matmul(out, lhsT, rhs, start=start, stop=stop), R=R, W=W, inc=stop)

        def act(out, in_, func, R, W, bias=None, scale=None):
            kw = {}
            if bias is not None:
                kw["bias"] = bias
            if scale is not None:
                kw["scale"] = scale
            kb.op("act", lambda: ACT.activation(out=out, in_=in_, func=func, **kw), R=R, W=W)

        def amul(out, in_, c, R, W):
            kb.op("act", lambda: ACT.mul(out, in_, c), R=R, W=W)

        def tt_(e, out, in0, in1, op, R, W):
            eng = DVE if e == "dve" else POOL
            kb.op(e, lambda: eng.tensor_tensor(out, in0, in1, op), R=R, W=W)

        def ts_(e, out, in0, s1, s2, op0, op1, R, W):
            eng = DVE if e == "dve" else POOL
            if op1 is None and e == "pool" and op0 in (ALU.mult, ALU.add):
                o1, c2 = (ALU.add, 0.0) if op0 == ALU.mult else (ALU.mult, 1.0)
                kb.op(e, lambda: eng.tensor_scalar(out, in0, s1, c2, op0, o1), R=R, W=W)
            elif op1 is None:
                kb.op(e, lambda: eng.tensor_scalar(out, in0, s1, None, op0), R=R, W=W)
            else:
                kb.op(e, lambda: eng.tensor_scalar(out, in0, s1, s2, op0, op1), R=R, W=W)

        def stt_(e, out, in0, s, in1, op0, op1, R, W):
            eng = DVE if e == "dve" else POOL
            kb.op(e, lambda: eng.scalar_tensor_tensor(out=out, in0=in0, scalar=s, in1=in1, op0=op0, op1=op1), R=R, W=W)

        def b4(ap):
            return ap.unsqueeze(1).to_broadcast([ap.shape[0], 4, 128])

        def v4(ap):
            return ap.rearrange("p (c k) -> p c k", k=128)

        def rmsnorm_rstd(ps_ap, ps_res, n, rstd_ap, rstd_res):
            act(rstd_ap, ps_ap, AF.Ln, R=[ps_res], W=[rstd_res], bias=EPS, scale=1.0 / n)
            act(rstd_ap, rstd_ap, AF.Exp, R=[rstd_res], W=[rstd_res], scale=-0.5)

        def norm_tile(src_fn, src_res_fn, wcol_fn, dst_fn, dst_res_fn, sq, sq_res, rstd, rstd_res):
            pb, pr = ps()
            for kc in range(KC):
                s = sq[kc % 2]
                act(s[:], src_fn(kc), AF.Square, R=[src_res_fn(kc)], W=[sq_res[kc % 2]])
                mm(pb[:], ones_b, s[:], kc == 0, kc == KC - 1, R=[sq_res[kc % 2], r_const], W=[pr])
            rmsnorm_rstd(pb[:], pr, D, rstd[:], rstd_res)
            for kc in range(KC):
                stt_("dve", dst_fn(kc), src_fn(kc), wcol_fn(kc), rstd[:], ALU.mult, ALU.mult,
                     R=[src_res_fn(kc), rstd_res, r_const], W=[dst_res_fn(kc)])

        def silu_from(e_eng, out_ap, x_ap, tmp_ap, R, W, tmp_res):
            act(tmp_ap, x_ap, AF.Exp, R=R, W=[tmp_res], scale=-1.0)
            act(tmp_ap, tmp_ap, AF.Ln, R=[tmp_res], W=[tmp_res], bias=1.0)
            act(tmp_ap, tmp_ap, AF.Exp, R=[tmp_res], W=[tmp_res], scale=-1.0)
            tt_(e_eng, out_ap, x_ap, tmp_ap, ALU.mult, R=list(R) + [tmp_res], W=W)

        wslot_key = [kb.dsem("ws0"), kb.dsem("ws1")]
        hw = {"n": 0, "slots": None, "res": None}

        def load_head_weights(l, kind, h):
            i = hw["n"] % 2
            hw["n"] += 1
            w = hw["slots"][i]
            if kind == "gdn":
                cols = [(O_AQ + h * 128, 128, 0), (O_AK + h * 128, 128, 128), (O_AV + h * 128, 128, 256), (O_AZ + h * 128, 128, 384)]
            else:
                cols = [(O_BQ + h * 128, 128, 0), (O_BK + h * 128, 128, 128), (O_BV + h * 256, 256, 256), (O_BR + h * 256, 256, 512)]
            pairs = [(w[:, :, o:o + n], w_in_d[l, :, c0:c0 + n].rearrange("(kc p) c -> p kc c", p=128)) for (c0, n, o) in cols]
            kb.dma("pool", pairs, wslot_key[i], W=[hw["res"][i]])
            return w, hw["res"][i]

        k_wsm = kb.dsem("wsm")
        k_w2b = kb.dsem("w2b")
        wm_k = [kb.dsem(f"wm{i}") for i in range(3)]
        wo_k = [kb.dsem(f"wo{i}") for i in range(2)]
        wu_k = [kb.dsem(f"wu{i}") for i in range(4)]
        wd_k = [kb.dsem(f"wd{i}") for i in range(4)]

        for l in range(NL):
            with ExitStack() as ls:
              vecs = sb("vecs", [128, NVEC], F32, ls)
              kb.barrier()
              kb.dma("sp", [(vecs[:], vecs_d[:, l * NVEC:(l + 1) * NVEC])], k_vec, W=[r_const])

              def vcol(off, n=1, vecs=vecs):
                  return vecs[:, off:off + n]
              with ExitStack() as ms:
                  hT = sb("hT", [128, KC, T], BF16, ms)
                  hr = [[Res() for _ in range(NT)] for _ in range(KC)]
                  oTa = sb("oTa", [128, 4, T], BF16, ms)
                  oTa_r = [[Res() for _ in range(NT)] for _ in range(4)]
                  wsm = sb("wsm", [128, KC, 24], BF16, ms)
                  r_wsm = Res()
                  with nc.allow_non_contiguous_dma(reason="small gate columns"):
                      kb.dma("pool", [(wsm[:, :, 0:8], w_in_d[l, :, O_AB:O_AB + 8].rearrange("(kc p) c -> p kc c", p=128)),
                                      (wsm[:, :, 8:24], w_in_d[l, :, O_GLR:O_GLR + 16].rearrange("(kc p) c -> p kc c", p=128)),
                                      ], k_wsm, W=[r_wsm])
                  with ExitStack() as s1:
                      sq = [sb(f"sq{i}", [128, TT], BF16, s1) for i in range(2)]
                      sq_res = [Res(), Res()]
                      rstd = sb("rstd", [128, TT], F32, s1)
                      rstd_res = Res()
                      for tt in range(NT):
                          tsl = slice(tt * TT, (tt + 1) * TT)
                          norm_tile(lambda kc: xT[:, kc, tsl], lambda kc: xr[kc][tt], lambda kc: vcol(kc),
                                    lambda kc: hT[:, kc, tsl], lambda kc: hr[kc][tt], sq, sq_res, rstd, rstd_res)
                  if l == 0:
                      tap("hT", hT[:, 0, :], [128, T], [hr[0][t_] for t_ in range(NT)])

                  with ExitStack() as gs:
                      kb.barrier()
                      S = lambda n, shp, d=F32: sb(n, shp, d, gs)
                      hw["slots"] = [S(f"wsg{i}", [128, KC, 512], BF16) for i in range(2)]
                      hw["res"] = [Res(), Res()]
                      nA = S("nA", [1, 4]); nA_r = Res()
                      act(nA[:], hv[0:1, l * 8:l * 8 + 4], AF.Exp, R=[r_const], W=[nA_r])
                      ts_("dve", nA[:], nA[:], -1.0, None, ALU.mult, None, R=[nA_r], W=[nA_r])

                      def make_head(i):
                          halo = S("halo", [128, 3, 3]); halo_r = [Res() for _ in range(3)]
                          raw1 = S("raw1", [128, 3 + TT]); raw1_r = Res()
                          cacc = S("cacc", [128, TT]); cacc_r = Res()
                          tmpf = S("tmpf", [128, TT]); tmpf_r = Res()
                          tmpg = S("tmpg", [128, TT]); tmpg_r = Res()
                          qT = S("qT", [128, TT], BF16); qT_r = Res()
                          kT = S("kT", [128, TT], BF16); kT_r = Res()
                          vT = S("vT", [128, TT], BF16); vT_r = Res()
                          zs = S("zs", [128, TT], BF16); zs_r = Res()
                          vb = S("vb", [128, TT], BF16); vb_r = Res()
                          kbg = S("kbg", [128, TT], BF16); kbg_r = Res()
                          kdc = S("kdc", [128, TT], BF16); kdc_r = Res()
                          Am = S("Am", [128, TT], BF16); Am_r = Res()
                          AT = S("AT", [128, TT], BF16); AT_r = Res()
                          Om = S("Om", [128, TT], BF16); Om_r = Res()
                          OT = S("OT", [128, TT], BF16); OT_r = Res()
                          Zp, Zp_r = OT, OT_r
                          Zt, Zt_r = Om, Om_r
                          Inv = S("Inv", [128, TT], BF16); Inv_r = Res()
                          Rm = S("Rm", [128, TT], BF16); Rm_r = Res()
                          qkm = S("qkm", [128, TT], BF16); qkm_r = Res()
                          qd = S("qd", [128, TT], BF16); qd_r = Res()
                          nwt, nwt_r = vT, vT_r
                          oraw, oraw_r = cacc, cacc_r
                          sqg, sqg_r = Om, Om_r
                          vnew = [S("vnew", [128, 128], BF16)] * 2
                          vnew_r = [Res()] * 2
                          Sst = S("Sst", [128, 128]); Sst_r = Res()
                          Sbf = S("Sbf", [128, 128], BF16); Sbf_r = Res()
                          cols = S("cols", [128, 16]); cols_r = Res()
                          rowA = S("rowA", [1, TT]); rowA_r = Res()
                          rowB = S("rowB", [1, TT]); rowB_r = Res()
                          Grow = [S("Grow", [1, 1 + TT])] * 2
                          Grow_r = [Res()] * 2
                          rGb = S("rGb", [1, TT]); rGb_r = Res()
                          rkb = S("rkb", [1, TT]); rkb_r = Res()
                          rcd = S("rcd", [1, 8]); rcd_r = Res()
                          def run(h, W_, W_r):
                              kb.op("dve", lambda: DVE.memset(Sst[:], 0.0), W=[Sst_r])
                              kb.op("dve", lambda: DVE.memset(Sbf[:], 0.0), W=[Sbf_r])
                              for i in range(3):
                                  kb.op("pool", lambda i=i: POOL.memset(halo[:, i, :], 0.0), W=[halo_r[i]])
                              for tt in range(NT):
                                  tsl = slice(tt * TT, (tt + 1) * TT)
                                  hres = [hr[kc][tt] for kc in range(KC)]
                                  Gc_, Gc_r = Grow[tt % 2], Grow_r[tt % 2]
                                  Gp_, Gp_r = Grow[(tt + 1) % 2], Grow_r[(tt + 1) % 2]
                                  pb, pr = ps()
                                  for kc in range(KC):
                                      mm(pb[0:1, :], wsm[:, kc, h:h + 1], hT[:, kc, tsl], kc == 0, kc == KC - 1, R=[r_wsm, hres[kc]], W=[pr])
                                  act(rowA[:], pb[0:1, :], AF.Exp, R=[pr], W=[rowA_r], scale=-1.0)
                                  act(rowA[:], rowA[:], AF.Ln, R=[rowA_r], W=[rowA_r], bias=1.0)
                                  pb, pr = ps()
                                  for kc in range(KC):
                                      mm(pb[0:1, :], wsm[:, kc, 4 + h:5 + h], hT[:, kc, tsl], kc == 0, kc == KC - 1, R=[r_wsm, hres[kc]], W=[pr])
                                  act(rowB[:], pb[0:1, :], AF.Exp, R=[pr, r_const], W=[rowB_r], bias=hv[0:1, l * 8 + 4 + h:l * 8 + 5 + h])
                                  act(rowB[:], rowB[:], AF.Ln, R=[rowB_r], W=[rowB_r], bias=1.0)
                                  ts_("dve", rowB[:], rowB[:], nA[0:1, h:h + 1], None, ALU.mult, None, R=[rowB_r, nA_r], W=[rowB_r])
                                  if tt == 0:
                                      kb.op("dve", lambda: DVE.memset(Gc_[:, 0:1], 0.0), W=[Gc_r])
                                  else:
                                      kb.op("dve", lambda: DVE.tensor_copy(Gc_[:, 0:1], Gp_[:, TT:TT + 1]), R=[Gp_r], W=[Gc_r])
                                  kb.op("dve", lambda: DVE.tensor_tensor_scan(Gc_[:, 1:1 + TT], onesf[0:1, 0:1].to_broadcast([1, TT]), rowB[:], Gc_[:, 0:1], ALU.mult, ALU.add),
                                        R=[r_const, rowB_r, Gc_r], W=[Gc_r])
                                  Gv = Gc_[:, 1:1 + TT]
                                  Gst4 = Gc_[:, 0:TT:128].unsqueeze(2).to_broadcast([1, 4, 128])
                                  Gla4 = Gc_[:, 128:TT + 1:128].unsqueeze(2).to_broadcast([1, 4, 128])
                                  tt_("dve", rGb[:], Gv, rowA[:], ALU.subtract, R=[Gc_r, rowA_r], W=[rGb_r])
                                  tt_("dve", v4(rkb[:]), v4(rGb[:]), Gst4, ALU.subtract, R=[rGb_r, Gc_r], W=[rkb_r])
                                  act(rkb[:], rkb[:], AF.Exp, R=[rkb_r], W=[rkb_r])
                                  rkd, rkd_r = rowB, rowB_r
                                  rbe, rbe_r = rowA, rowA_r
                                  tt_("dve", v4(rkd[:]), v4(Gv), Gla4, ALU.subtract, R=[Gc_r], W=[rkd_r])
                                  act(rkd[:], rkd[:], AF.Exp, R=[rkd_r], W=[rkd_r], scale=-1.0)
                                  act(rbe[:], rowA[:], AF.Exp, R=[rowA_r], W=[rbe_r], scale=-1.0)
                                  tt_("dve", rcd[:, 0:4], Gc_[:, 128:TT + 1:128], Gc_[:, 0:TT:128], ALU.subtract, R=[Gc_r], W=[rcd_r])
                                  act(rcd[:, 0:4], rcd[:, 0:4], AF.Exp, R=[rcd_r], W=[rcd_r])
                                  pcol, pcol_r = ps()
                                  for cc in range(4):
                                      csl = slice(cc * 128, (cc + 1) * 128)
                                      for qi, (rw, rw_r) in enumerate(((rkb, rkb_r), (rkd, rkd_r), (rbe, rbe_r))):
                                          kb.op("pe", lambda rw=rw, qi=qi: PE.matmul(pcol[:, cc * 4 + qi:cc * 4 + qi + 1], rw[0:1, csl], onesf[0:1, 0:1], start=True, stop=True),
                                                R=[rw_r, r_const], W=[pcol_r], inc=False)
                                      kb.op("pe", lambda: PE.matmul(pcol[:, cc * 4 + 3:cc * 4 + 4], onesf[0:1, 0:128], rcd[0:1, cc:cc + 1], start=True, stop=True),
                                            R=[rcd_r, r_const], W=[pcol_r], inc=(cc == 3))
                                  kb.op("dve", lambda: DVE.tensor_copy(cols[:], pcol[:, 0:16]), R=[pcol_r], W=[cols_r])
                                  colv = cols[:].rearrange("p (c q) -> p c q", q=4)

                                  yield
                                  for xi, (dst, dst_r) in enumerate(((qT, qT_r), (kT, kT_r), (vT, vT_r))):
                                      pb, pr = ps()
                                      for kc in range(KC):
                                          mm(pb[:], W_[:, kc, xi * 128:(xi + 1) * 128], hT[:, kc, tsl], kc == 0, kc == KC - 1, R=[W_r, hres[kc]], W=[pr])
                                      rw, rw_r = raw1, raw1_r
                                      kb.op("pool", lambda xi=xi: POOL.tensor_copy(rw[:, 0:3], halo[:, xi, :]), R=[halo_r[xi]], W=[rw_r])
                                      act(rw[:, 3:3 + TT], pb[:], AF.Copy, R=[pr], W=[rw_r])
                                      cw = lambda tap_, xi=xi: vcol(32 + tap_ * 12 + xi * 4 + h)
                                      ts_("dve", cacc[:], rw[:, 0:TT], cw(0), None, ALU.mult, None, R=[rw_r, r_const], W=[cacc_r])
                                      for tp in (1, 2, 3):
                                          stt_("dve", cacc[:], rw[:, tp:tp + TT], cw(tp), cacc[:], ALU.mult, ALU.add, R=[rw_r, r_const, cacc_r], W=[cacc_r])
                                      kb.op("pool", lambda xi=xi: POOL.tensor_copy(halo[:, xi, :], rw[:, TT:TT + 3]), R=[rw_r], W=[halo_r[xi]])
                                      if xi == 2:
                                          silu_from("dve", vT[:], cacc[:], tmpf[:], R=[cacc_r], W=[vT_r], tmp_res=tmpf_r)
                                      else:
                                          silu_from("dve", tmpg[:], cacc[:], tmpf[:], R=[cacc_r], W=[tmpg_r], tmp_res=tmpf_r)
                                          act(sqg[:], tmpg[:], AF.Square, R=[tmpg_r], W=[sqg_r])
                                          pb2, pr2 = ps()
                                          mm(pb2[:], ones_b, sqg[:], True, True, R=[sqg_r, r_const], W=[pr2])
                                          act(tmpf[:], pb2[:], AF.Ln, R=[pr2], W=[tmpf_r], bias=EPS)
                                          act(tmpf[:], tmpf[:], AF.Exp, R=[tmpf_r], W=[tmpf_r], scale=-0.5)
                                          sc = (128.0 ** -0.5) if xi == 0 else 1.0
                                          stt_("dve", dst[:], tmpg[:], sc, tmpf[:], ALU.mult, ALU.mult, R=[tmpg_r, tmpf_r], W=[dst_r])
                                  yield
                                  pb, pr = ps()
                                  for kc in range(KC):
                                      mm(pb[:], W_[:, kc, 384:512], hT[:, kc, tsl], kc == 0, kc == KC - 1, R=[W_r, hres[kc]], W=[pr])
                                  silu_from("dve", zs[:], pb[:], tmpf[:], R=[pr], W=[zs_r], tmp_res=tmpf_r)

                                  yield
                                  pt, ptr = psb()
                                  for cc in range(4):
                                      csl = slice(cc * 128, (cc + 1) * 128)
                                      kb.op("pe", lambda: PE.transpose(pt[:, csl], vT[:, csl], ident_b), R=[vT_r, r_const], W=[ptr], inc=(cc == 3))
                                  tt_("dve", v4(vb[:]), v4(pt), colv[:, :, 2:3].to_broadcast([128, 4, 128]), ALU.mult, R=[ptr, cols_r], W=[vb_r])
                                  pt, ptr = psb()
                                  for cc in range(4):
                                      csl = slice(cc * 128, (cc + 1) * 128)
                                      kb.op("pe", lambda: PE.transpose(pt[:, csl], kT[:, csl], ident_b), R=[kT_r, r_const], W=[ptr], inc=(cc == 3))
                                  tt_("dve", v4(kbg[:]), v4(pt), colv[:, :, 0:1].to_broadcast([128, 4, 128]), ALU.mult, R=[ptr, cols_r], W=[kbg_r])
                                  tt_("dve", v4(kdc[:]), v4(pt), colv[:, :, 1:2].to_broadcast([128, 4, 128]), ALU.mult, R=[ptr, cols_r], W=[kdc_r])

                                  yield
                                  def expo(lrow, lrow_r, lneg, rrow, rrow_r, rneg, mask_idx):
                                      pb_, pr_ = ps()
                                      for cc in range(4):
                                          csl = slice(cc * 128, (cc + 1) * 128)
                                          kb.op("pe", lambda: PE.matmul(pb_[:, csl], (negf if rneg else onesf)[0:1, 0:128], rrow[0:1, csl], start=True, stop=False),
                                                R=[rrow_r, r_const], W=[pr_], inc=False)
                                          kb.op("pe", lambda: PE.matmul(pb_[:, csl], lrow[0:1, csl], (negf if lneg else onesf)[0:1, 0:128], start=False, stop=False),
                                                R=[lrow_r, r_const], W=[pr_], inc=False)
                                          kb.op("pe", lambda: PE.matmul(pb_[:, csl], ident_b, CB(mask_idx), start=False, stop=True),
                                                R=[r_const], W=[pr_], inc=(cc == 3))
                                      return pb_, pr_
                                  pkk, pkk_r = ps()
                                  for cc in range(4):
                                      csl = slice(cc * 128, (cc + 1) * 128)
                                      mm(pkk[:, csl], kT[:, csl], kT[:, csl], True, True, R=[kT_r], W=[pkk_r])
                                  pe1, pe1_r = expo(Gv, Gc_r, True, rGb, rGb_r, False, C_MN_STR_T)
                                  act(tmpf[:], pe1[:], AF.Exp, R=[pe1_r], W=[tmpf_r])
                                  tt_("dve", AT[:], pkk[:], tmpf[:], ALU.mult, R=[pkk_r, tmpf_r], W=[AT_r])
                                  pe2, pe2_r = expo(rGb, rGb_r, False, Gv, Gc_r, True, C_MN_STR)
                                  act(tmpg[:], pe2[:], AF.Exp, R=[pe2_r], W=[tmpg_r])
                                  tt_("dve", Am[:], pkk[:], tmpg[:], ALU.mult, R=[pkk_r, tmpg_r], W=[Am_r])
                                  pe3, pe3_r = expo(Gv, Gc_r, True, Gv, Gc_r, False, C_MN_INC_T)
                                  act(tmpf[:], pe3[:], AF.Exp, R=[pe3_r], W=[tmpf_r])
                                  pqk, pqk_r = ps()
                                  for cc in range(4):
                                      csl = slice(cc * 128, (cc + 1) * 128)
                                      mm(pqk[:, csl], kT[:, csl], qT[:, csl], True, True, R=[kT_r, qT_r], W=[pqk_r])
                                  tt_("dve", qkm[:], pqk[:], tmpf[:], ALU.mult, R=[pqk_r, tmpf_r], W=[qkm_r])
                                  rGc, rGc_r = rkb, rkb_r
                                  tt_("dve", v4(rGc[:]), v4(Gv), Gst4, ALU.subtract, R=[Gc_r], W=[rGc_r])
                                  pg, pg_r = ps()
                                  for cc in range(4):
                                      csl = slice(cc * 128, (cc + 1) * 128)
                                      kb.op("pe", lambda: PE.matmul(pg[:, csl], onesf[0:1, 0:128], rGc[0:1, csl], start=True, stop=True),
                                            R=[rGc_r, r_const], W=[pg_r], inc=(cc == 3))
                                  act(tmpg[:], pg[:], AF.Exp, R=[pg_r], W=[tmpg_r])
                                  tt_("dve", qd[:], qT[:], tmpg[:], ALU.mult, R=[qT_r, tmpg_r], W=[qd_r])

                                  yield
                                  tt_("dve", v4(Om[:]), v4(Am[:]), b4(CB(C_LV + 0)), ALU.mult, R=[Am_r, r_const], W=[Om_r])
                                  stt_("dve", v4(Inv[:]), v4(Om[:]), -1.0, b4(ident_b), ALU.mult, ALU.add, R=[Om_r, r_const], W=[Inv_r])
                                  tt_("dve", v4(OT[:]), v4(AT[:]), b4(CB(C_LVT + 0)), ALU.mult, R=[AT_r, r_const], W=[OT_r])
                                  stt_("dve", v4(Rm[:]), v4(OT[:]), -1.0, b4(ident_b), ALU.mult, ALU.add, R=[OT_r, r_const], W=[Rm_r])
                                  for li in range(1, 7):
                                      last = (li == 6)
                                      pz, pz_r = ps()
                                      for cc in range(4):
                                          csl = slice(cc * 128, (cc + 1) * 128)
                                          mm(pz[:, csl], Am[:, csl], Rm[:, csl], True, True, R=[Am_r, Rm_r], W=[pz_r])
                                      if not last:
                                          pzp, pzp_r = ps()
                                          for cc in range(4):
                                              csl = slice(cc * 128, (cc + 1) * 128)
                                              mm(pzp[:, csl], AT[:, csl], Inv[:, csl], True, True, R=[AT_r, Inv_r], W=[pzp_r])
                                      tt_("dve", v4(Zt[:]), v4(pz[:]), b4(CB(C_LVT + li)), ALU.mult, R=[pz_r, r_const], W=[Zt_r])
                                      if not last:
                                          tt_("dve", v4(Zp[:]), v4(pzp[:]), b4(CB(C_LV + li)), ALU.mult, R=[pzp_r, r_const], W=[Zp_r])
                                      pr2_, pr2_r = ps()
                                      for cc in range(4):
                                          csl = slice(cc * 128, (cc + 1) * 128)
                                          mm(pr2_[:, csl], Inv[:, csl], Zt[:, csl], True, True, R=[Inv_r, Zt_r], W=[pr2_r])
                                      if not last:
                                          pi2_, pi2_r = ps()
                                          for cc in range(4):
                                              csl = slice(cc * 128, (cc + 1) * 128)
                                              mm(pi2_[:, csl], Rm[:, csl], Zp[:, csl], True, True, R=[Rm_r, Zp_r], W=[pi2_r])
                                      tt_("dve", Rm[:], Rm[:], pr2_[:], ALU.subtract, R=[Rm_r, pr2_r], W=[Rm_r])
                                      if not last:
                                          tt_("dve", Inv[:], Inv[:], pi2_[:], ALU.subtract, R=[Inv_r, pi2_r], W=[Inv_r])
                                      yield
                                  yield
                                  pw, pw_r = ps()
                                  for cc in range(4):
                                      csl = slice(cc * 128, (cc + 1) * 128)
                                      mm(pw[:, csl], kbg[:, csl], Rm[:, csl], True, True, R=[kbg_r, Rm_r], W=[pw_r])
                                  amul(nwt[:], pw[:], -1.0, R=[pw_r], W=[nwt_r])

                                  yield
                                  for cc in range(4):
                                      csl = slice(cc * 128, (cc + 1) * 128)
                                      vn, vn_r = vnew[cc % 2], vnew_r[cc % 2]
                                      pv, pv_r = ps()
                                      mm(pv[:, 0:128], Rm[:, csl], vb[:, csl], True, False, R=[Rm_r, vb_r], W=[pv_r])
                                      mm(pv[:, 0:128], nwt[:, csl], Sbf[:], False, True, R=[nwt_r, Sbf_r], W=[pv_r])
                                      act(vn[:], pv[:, 0:128], AF.Copy, R=[pv_r], W=[vn_r])
                                      po, po_r = ps()
                                      mm(po[:, 0:128], Sbf[:], qd[:, csl], True, False, R=[Sbf_r, qd_r], W=[po_r])
                                      mm(po[:, 0:128], vn[:], qkm[:, csl], False, True, R=[vn_r, qkm_r], W=[po_r])
                                      pd, pd_r = ps()
                                      mm(pd[:, 0:128], kdc[:, csl], vn[:], True, True, R=[kdc_r, vn_r], W=[pd_r])
                                      act(oraw[:, csl], po[:, 0:128], AF.Copy, R=[po_r], W=[oraw_r])
                                      ts_("dve", Sst[:], Sst[:], cols[:, cc * 4 + 3:cc * 4 + 4], None, ALU.mult, None, R=[Sst_r, cols_r], W=[Sst_r])
                                      tt_("dve", Sst[:], pd[:, 0:128], Sst[:], ALU.add, R=[Sst_r, pd_r], W=[Sst_r])
                                      act(Sbf[:], Sst[:], AF.Copy, R=[Sst_r], W=[Sbf_r])
                                      yield

                                  yield
                                  act(sqg[:], oraw[:], AF.Square, R=[oraw_r], W=[sqg_r])
                                  pb2, pr2 = ps()
                                  mm(pb2[:], ones_b, sqg[:], True, True, R=[sqg_r, r_const], W=[pr2])
                                  rmsnorm_rstd(pb2[:], pr2, 128, tmpf[:], tmpf_r)
                                  stt_("dve", tmpg[:], oraw[:], vcol(80), tmpf[:], ALU.mult, ALU.mult, R=[oraw_r, tmpf_r, r_const], W=[tmpg_r])
                                  tt_("pool", oTa[:, h, tsl], tmpg[:], zs[:], ALU.mult, R=[tmpg_r, zs_r], W=[oTa_r[h][tt]])
                          return run
                      runners = [make_head(0), make_head(1)]
                      for pair in (((0, 1), (2, 3)) if "gdn" in phases else ()):
                          ws = [load_head_weights(l, "gdn", h) for h in pair]
                          caps = []
                          for i, h in enumerate(pair):
                              st["chain"] = i
                              kb.cap = []
                              for _ in runners[i](h, ws[i][0], ws[i][1]):
                                  pass
                              caps.append(kb.cap)
                              kb.cap = None
                          st["chain"] = None
                          kb.replay(caps)
                  if l == 0:
                      for h in range(4):
                          tap(f"oTa{h}", oTa[:, h, :], [128, T], [oTa_r[h][t_] for t_ in range(NT)])
                  ms2 = ExitStack()
                  ms2.__enter__()
                  kb.barrier()
                  oTb = sb("oTb", [128, 8, T], BF16, ms2)
                  oTb_r = [[Res() for _ in range(NT)] for _ in range(8)]
                  w2b = sb("w2b", [16, 512], BF16, ms2)
                  r_w2b = Res()
                  kb.dma("pool", [(w2b[:], w2_d[:, l * 512:(l + 1) * 512])], k_w2b, W=[r_w2b])
                  with ExitStack() as gs:
                      S = lambda n, shp, d=F32: sb(n, shp, d, gs)
                      hw["slots"] = [S(f"wsl{i}", [128, KC, 768], BF16) for i in range(2)]
                      hw["res"] = [Res(), Res()]
                      nxt = load_head_weights(l, "gla", 0)
                      glrT = S("glrT", [16, TT], BF16); glr_r = Res()
                      qf = S("qf", [128, TT]); qf_r = Res()
                      kf = S("kf", [128, TT]); kf_r = Res()
                      tmpf = S("tmpf2", [128, TT]); tmpf_r = Res()
                      tmpg = S("tmpg2", [128, TT]); tmpg_r = Res()
                      tmph, tmph_r = tmpg, tmpg_r
                      Pp = [S(f"Pp{i}", [128, 1 + TT]) for i in range(2)]; Pp_r = [Res(), Res()]
                      lrow, lrow_r = tmpg, tmpg_r
                      qk2 = S("qk2", [128, 2, TT], BF16)
                      qin = qk2[:, 0, :]; qin_r = Res()
                      kin = qk2[:, 1, :]; kin_r = Res()
                      qdc = S("qdc", [128, TT], BF16); qdc_r = Res()
                      kdcT = S("kdcT", [128, TT], BF16); kdcT_r = Res()
                      kdt = S("kdt", [128, TT], BF16); kdt_r = Res()
                      vtok = S("vtok", [128, 4, 256], BF16); vtok_r = Res()
                      attn = S("attn", [128, TT], BF16); attn_r = Res()
                      rs = S("rs", [128, 2, TT], BF16); rs_r = Res()
                      oraw2 = S("oraw2", [128, 2, TT]); oraw2_r = Res()
                      S2 = S("S2", [128, 256]); S2_r = Res()
                      S2b = S("S2b", [128, 256], BF16); S2b_r = Res()
                      cdc = S("cdc", [128, 4]); cdc_r = Res()
                      ngb = S("ngb", [128, 4]); ngb_r = Res()
                      ts_("dve", ngb[:], vcol(83, 4), -1.0, None, ALU.mult, None, R=[r_const], W=[ngb_r])
                      for h in (range(4) if "gla" in phases else ()):
                          W_, W_r = nxt
                          nxt = load_head_weights(l, "gla", h + 1) if h < 3 else None
                          kb.op("dve", lambda: DVE.memset(S2[:], 0.0), W=[S2_r])
                          kb.op("dve", lambda: DVE.memset(S2b[:], 0.0), W=[S2b_r])
                          for tt in range(NT):
                              tsl = slice(tt * TT, (tt + 1) * TT)
                              hres = [hr[kc][tt] for kc in range(KC)]
                              Pc, Pc_r = Pp[tt % 2], Pp_r[tt % 2]
                              Pv_, Pv_r = Pp[(tt + 1) % 2], Pp_r[(tt + 1) % 2]
                              pb, pr = ps()
                              for kc in range(KC):
                                  mm(pb[:], W_[:, kc, 0:128], hT[:, kc, tsl], kc == 0, kc == KC - 1, R=[W_r, hres[kc]], W=[pr])
                              amul(qf[:], pb[:], 128.0 ** -0.5, R=[pr], W=[qf_r])
                              pb, pr = ps()
                              for kc in range(KC):
                                  mm(pb[:], W_[:, kc, 128:256], hT[:, kc, tsl], kc == 0, kc == KC - 1, R=[W_r, hres[kc]], W=[pr])
                              act(kf[:], pb[:], AF.Copy, R=[pr], W=[kf_r])
                              for half in range(2):
                                  pb, pr = ps()
                                  for c2 in range(2):
                                      cc = half * 2 + c2
                                      for kc in range(KC):
                                          mm(pb[:, c2 * 256:(c2 + 1) * 256], hT[:, kc, tt * TT + cc * 128: tt * TT + (cc + 1) * 128], W_[:, kc, 256:512],
                                             kc == 0, kc == KC - 1, R=[W_r, hres[kc]], W=[pr])
                                  act(vtok[:, half * 2:half * 2 + 2, :], pb[:].rearrange("p (c e) -> p c e", e=256), AF.Copy, R=[pr], W=[vtok_r])
                              pb, pr = ps()
                              for kc in range(KC):
                                  mm(pb[0:16, :], wsm[:, kc, 8:24], hT[:, kc, tsl], kc == 0, kc == KC - 1, R=[r_wsm, hres[kc]], W=[pr])
                              act(glrT[:], pb[0:16, :], AF.Copy, R=[pr], W=[glr_r])
                              pb, pr = ps()
                              mm(pb[:], w2b[0:16, h * 128:(h + 1) * 128], glrT[:], True, True, R=[r_w2b, glr_r], W=[pr])
                              act(lrow[:], pb[:], AF.Exp, R=[pr, ngb_r], W=[lrow_r], bias=ngb[:, h:h + 1], scale=-1.0)
                              act(lrow[:], lrow[:], AF.Ln, R=[lrow_r], W=[lrow_r], bias=1.0)
                              if tt == 0:
                                  kb.op("dve", lambda: DVE.memset(Pc[:, 0:1], 0.0), W=[Pc_r])
                              else:
                                  kb.op("dve", lambda: DVE.tensor_copy(Pc[:, 0:1], Pv_[:, TT:TT + 1]), R=[Pv_r], W=[Pc_r])
                              kb.op("dve", lambda: DVE.tensor_tensor_scan(Pc[:, 1:1 + TT], onesf[:, 0:1].to_broadcast([128, TT]), lrow[:], Pc[:, 0:1], ALU.mult, ALU.add),
                                    R=[r_const, lrow_r, Pc_r], W=[Pc_r])
                              Pvw = v4(Pc[:, 1:1 + TT])
                              Pst = Pc[:, 0:TT:128].unsqueeze(2).to_broadcast([128, 4, 128])
                              Pmid = Pc[:, 65:TT + 1:128].unsqueeze(2).to_broadcast([128, 4, 128])
                              Pla = Pc[:, 128:TT + 1:128].unsqueeze(2).to_broadcast([128, 4, 128])
                              isc = 1.0 / 16.0
                              tt_("dve", v4(tmpf[:]), Pvw, Pmid, ALU.subtract, R=[Pc_r], W=[tmpf_r])
                              act(tmpg[:], tmpf[:], AF.Exp, R=[tmpf_r], W=[tmpg_r], scale=-isc)
                              tt_("dve", qin, qf[:], tmpg[:], ALU.mult, R=[qf_r, tmpg_r], W=[qin_r])
                              act(tmph[:], tmpf[:], AF.Exp, R=[tmpf_r], W=[tmph_r], scale=isc)
                              tt_("dve", kin, kf[:], tmph[:], ALU.mult, R=[kf_r, tmph_r], W=[kin_r])
                              tt_("dve", v4(tmpf[:]), Pvw, Pst, ALU.subtract, R=[Pc_r], W=[tmpf_r])
                              act(tmpg[:], tmpf[:], AF.Exp, R=[tmpf_r], W=[tmpg_r], scale=-isc)
                              tt_("dve", qdc[:], qf[:], tmpg[:], ALU.mult, R=[qf_r, tmpg_r], W=[qdc_r])
                              tt_("dve", v4(tmpf[:]), Pvw, Pla, ALU.subtract, R=[Pc_r], W=[tmpf_r])
                              act(tmph[:], tmpf[:], AF.Exp, R=[tmpf_r], W=[tmph_r], scale=isc)
                              tt_("dve", kdcT[:], kf[:], tmph[:], ALU.mult, R=[kf_r, tmph_r], W=[kdcT_r])
                              tt_("dve", cdc[:], Pc[:, 128:TT + 1:128], Pc[:, 0:TT:128], ALU.subtract, R=[Pc_r], W=[cdc_r])
                              act(cdc[:], cdc[:], AF.Exp, R=[cdc_r], W=[cdc_r], scale=-isc)
                              pa, pa_r = ps()
                              for cc in range(4):
                                  csl = slice(cc * 128, (cc + 1) * 128)
                                  mm(pa[:, csl], kin[:, csl], qin[:, csl], True, True, R=[kin_r, qin_r], W=[pa_r])
                              tt_("dve", v4(attn[:]), v4(pa[:]), b4(CB(C_C01_T)), ALU.mult, R=[pa_r, r_const], W=[attn_r])
                              pt, ptr = psb()
                              for cc in range(4):
                                  csl = slice(cc * 128, (cc + 1) * 128)
                                  kb.op("pe", lambda: PE.transpose(pt[:, csl], kdcT[:, csl], ident_b), R=[kdcT_r, r_const], W=[ptr], inc=(cc == 3))
                              act(kdt[:], pt, AF.Copy, R=[ptr], W=[kdt_r])
                              for cc in range(4):
                                  csl = slice(cc * 128, (cc + 1) * 128)
                                  po, po_r = ps()
                                  for et in range(2):
                                      esl = slice(et * 128, (et + 1) * 128)
                                      mm(po[:, esl], vtok[:, cc, esl], attn[:, csl], True, False, R=[vtok_r, attn_r], W=[po_r])
                                      mm(po[:, esl], S2b[:, esl], qdc[:, csl], False, True, R=[S2b_r, qdc_r], W=[po_r])
                                  pd, pd_r = ps()
                                  mm(pd[:, 0:256], kdt[:, csl], vtok[:, cc, :], True, True, R=[kdt_r, vtok_r], W=[pd_r])
                                  act(oraw2[:, :, csl], po[:, 0:256].rearrange("p (e k) -> p e k", k=128), AF.Copy, R=[po_r], W=[oraw2_r])
                                  ts_("dve", S2[:], S2[:], cdc[:, cc:cc + 1], None, ALU.mult, None, R=[S2_r, cdc_r], W=[S2_r])
                                  tt_("dve", S2[:], pd[:, 0:256], S2[:], ALU.add, R=[S2_r, pd_r], W=[S2_r])
                                  act(S2b[:], S2[:], AF.Copy, R=[S2_r], W=[S2b_r])
                                  if cc < 2:
                                      et = cc
                                      pb, pr = ps()
                                      for kc in range(KC):
                                          mm(pb[:], W_[:, kc, 512 + et * 128:512 + (et + 1) * 128], hT[:, kc, tsl], kc == 0, kc == KC - 1, R=[W_r, hres[kc]], W=[pr])
                                      silu_from("dve", rs[:, et, :], pb[:], tmpf[:], R=[pr], W=[rs_r], tmp_res=tmpf_r)
                              act(qk2[:], oraw2[:], AF.Square, R=[oraw2_r], W=[qin_r, kin_r])
                              pb2, pr2 = ps()
                              for et in range(2):
                                  mm(pb2[:], ones_b, qk2[:, et, :], et == 0, et == 1, R=[qin_r, kin_r, r_const], W=[pr2])
                              rmsnorm_rstd(pb2[:], pr2, 256, tmpf[:], tmpf_r)
                              for et in range(2):
                                  stt_("dve", tmpg[:], oraw2[:, et, :], vcol(81 + et), tmpf[:], ALU.mult, ALU.mult, R=[oraw2_r, tmpf_r, r_const], W=[tmpg_r])
                                  tt_("pool", oTb[:, h * 2 + et, tsl], tmpg[:], rs[:, et, :], ALU.mult, R=[tmpg_r, rs_r], W=[oTb_r[h * 2 + et][tt]])

                  if l == 0:
                      for h in range(8):
                          tap(f"oTb{h}", oTb[:, h, :], [128, T], [oTb_r[h][t_] for t_ in range(NT)])
                  with ExitStack() as gs:
                      kb.barrier()
                      S = lambda n, shp, d=F32: sb(n, shp, d, gs)
                      NW = 2
                      NWM = 3
                      wm = [S(f"wm{i}", [128, 28, 128], BF16) for i in range(NWM)]
                      wm_r = [Res() for _ in range(NWM)]
                      wo = [S(f"wo{i}", [128, KC, 128], BF16) for i in range(NW)]
                      wo_r = [Res() for _ in range(NW)]
                      stage = S("stage", [128, KC, TT], BF16); stage_r = [Res() for _ in range(KC)]
                      tmpo = S("tmpo", [128, KC, TT]); tmpo_r = [Res() for _ in range(KC)]
                      sa = S("sa", [128, TT]); sa_r = Res()
                      sb_ = S("sb_", [128, TT]); sb_r = Res()
                      sq = [S(f"sqm{i}", [128, TT], BF16) for i in range(2)]; sq_res = [Res(), Res()]
                      rstd, rstd_res = sa, sa_r
                      t2, t2_r = sb_, sb_r
                      cnt = {"m": 0, "o": 0}

                      def load_wm(ct):
                          i = cnt["m"] % NWM
                          cnt["m"] += 1
                          csl = slice(ct * 128, (ct + 1) * 128)
                          pairs = [(wm[i][:, 0:4, :], w_oa_d[l, :, csl].rearrange("(k p) c -> p k c", p=128)),
                                   (wm[i][:, 4:12, :], w_ob_d[l, :, csl].rearrange("(k p) c -> p k c", p=128)),
                                   (wm[i][:, 12:20, :], w_in_d[l, :, O_GA + ct * 128:O_GA + (ct + 1) * 128].rearrange("(k p) c -> p k c", p=128)),
                                   (wm[i][:, 20:28, :], w_in_d[l, :, O_GB + ct * 128:O_GB + (ct + 1) * 128].rearrange("(k p) c -> p k c", p=128))]
                          kb.dma("pool", pairs, wm_k[i], W=[wm_r[i]])
                          return wm[i], wm_r[i]

                      def load_wo(ct):
                          i = cnt["o"] % NW
                          cnt["o"] += 1
                          kb.dma("pool", [(wo[i][:], w_o_d[l, :, ct * 128:(ct + 1) * 128].rearrange("(k p) c -> p k c", p=128))], wo_k[i], W=[wo_r[i]])
                          return wo[i], wo_r[i]

                      for tt in (range(NT) if "merge" in phases else ()):
                          tsl = slice(tt * TT, (tt + 1) * TT)
                          q_m = [load_wm(0), load_wm(1)]
                          for ct in range(KC):
                              w_, w_r = q_m.pop(0)
                              if ct + 2 < KC:
                                  q_m.append(load_wm(ct + 2))
                              pya, pya_r = ps()
                              for k in range(4):
                                  mm(pya[:], w_[:, k, :], oTa[:, k, tsl], k == 0, k == 3, R=[w_r, oTa_r[k][tt]], W=[pya_r])
                              pyb, pyb_r = ps()
                              for k in range(8):
                                  mm(pyb[:], w_[:, 4 + k, :], oTb[:, k, tsl], k == 0, k == 7, R=[w_r, oTb_r[k][tt]], W=[pyb_r])
                              pga, pga_r = ps()
                              for k in range(8):
                                  mm(pga[:], w_[:, 12 + k, :], hT[:, k, tsl], k == 0, k == 7, R=[w_r, hr[k][tt]], W=[pga_r])
                              pgb, pgb_r = ps()
                              for k in range(8):
                                  mm(pgb[:], w_[:, 20 + k, :], hT[:, k, tsl], k == 0, k == 7, R=[w_r, hr[k][tt]], W=[pgb_r])
                              act(sa[:], pga[:], AF.Exp, R=[pga_r], W=[sa_r], scale=-1.0)
                              act(sa[:], sa[:], AF.Ln, R=[sa_r], W=[sa_r], bias=1.0)
                              act(sa[:], sa[:], AF.Exp, R=[sa_r], W=[sa_r], scale=-1.0)
                              act(sb_[:], pgb[:], AF.Exp, R=[pgb_r], W=[sb_r], scale=-1.0)
                              act(sb_[:], sb_[:], AF.Ln, R=[sb_r], W=[sb_r], bias=1.0)
                              act(sb_[:], sb_[:], AF.Exp, R=[sb_r], W=[sb_r], scale=-1.0)
                              tt_("dve", sa[:], pya[:], sa[:], ALU.mult, R=[pya_r, sa_r], W=[sa_r])
                              tt_("dve", sb_[:], pyb[:], sb_[:], ALU.mult, R=[pyb_r, sb_r], W=[sb_r])
                              tt_("dve", stage[:, ct, :], sa[:], sb_[:], ALU.add, R=[sa_r, sb_r], W=[stage_r[ct]])
                          pss, pss_r = pacc, pacc_r
                          nw = load_wo(0)
                          for ct in range(KC):
                              w_, w_r = nw
                              if ct < KC - 1:
                                  nw = load_wo(ct + 1)
                              pb, pr = ps()
                              for k in range(KC):
                                  mm(pb[:], w_[:, k, :], stage[:, k, :], k == 0, k == KC - 1, R=[w_r, stage_r[k]], W=[pr])
                              act(tmpo[:, ct, :], pb[:], AF.Copy, R=[pr], W=[tmpo_r[ct]])
                              s = sq[ct % 2]
                              act(s[:], pb[:], AF.Square, R=[pr], W=[sq_res[ct % 2]])
                              mm(pss[:], ones_b, s[:], ct == 0, ct == KC - 1, R=[sq_res[ct % 2], r_const], W=[pss_r])
                          rmsnorm_rstd(pss[:], pss_r, D, rstd[:], rstd_res)
                          for ct in range(KC):
                              stt_("dve", t2[:], tmpo[:, ct, :], vcol(8 + ct), rstd[:], ALU.mult, ALU.mult, R=[tmpo_r[ct], rstd_res, r_const], W=[t2_r])
                              tt_("dve", xT[:, ct, tsl], xT[:, ct, tsl], t2[:], ALU.add, R=[xr[ct][tt], t2_r], W=[xr[ct][tt]])

                  ms2.close()
              if l == 0:
                  for kc in range(KC):
                      tap(f"xmix{kc}", xT[:, kc, :], [128, T], [xr[kc][t_] for t_ in range(NT)])
              with ExitStack() as gs:
                  kb.barrier()
                  S = lambda n, shp, d=F32: sb(n, shp, d, gs)
                  h2 = S("h2", [128, KC, TT], BF16); h2_r = [Res() for _ in range(KC)]
                  uT = S("uT", [128, 32, TT], BF16); uT_r = [Res() for _ in range(32)]
                  NW = 4
                  wu = [S(f"wu{i}", [128, KC, 512], BF16) for i in range(NW)]; wu_r = [Res() for _ in range(NW)]
                  wd = [S(f"wd{i}", [128, 32, 128], BF16) for i in range(NW)]; wd_r = [Res() for _ in range(NW)]
                  tmpo = S("tmpo2", [128, KC, TT]); tmpo_r = [Res() for _ in range(KC)]
                  sq = [S(f"sqn{i}", [128, TT], BF16) for i in range(2)]; sq_res = [Res(), Res()]
                  rstd = S("rstdn", [128, TT]); rstd_res = Res()
                  rl = [S(f"rl{i}", [128, TT]) for i in range(2)]; rl_r = [Res(), Res()]
                  t2 = S("t2n", [128, TT]); t2_r = Res()
                  cnt = {"u": 0, "d": 0}

                  def load_wu(fb):
                      i = cnt["u"] % NW
                      cnt["u"] += 1
                      kb.dma("pool", [(wu[i][:], w_up_d[l, :, fb * 512:(fb + 1) * 512].rearrange("(k p) c -> p k c", p=128))], wu_k[i], W=[wu_r[i]])
                      return wu[i], wu_r[i]

                  def load_wd(ct):
                      i = cnt["d"] % NW
                      cnt["d"] += 1
                      kb.dma("pool", [(wd[i][:], w_dn_d[l, :, ct * 128:(ct + 1) * 128].rearrange("(k p) c -> p k c", p=128))], wd_k[i], W=[wd_r[i]])
                      return wd[i], wd_r[i]

                  for tt in (range(NT) if "mlp" in phases else ()):
                      tsl = slice(tt * TT, (tt + 1) * TT)
                      q_u = [load_wu(0), load_wu(1), load_wu(2)]
                      norm_tile(lambda kc: xT[:, kc, tsl], lambda kc: xr[kc][tt], lambda kc: vcol(16 + kc),
                                lambda kc: h2[:, kc, :], lambda kc: h2_r[kc], sq, sq_res, rstd, rstd_res)
                      for fb in range(8):
                          w_, w_r = q_u.pop(0)
                          if fb + 3 < 8:
                              q_u.append(load_wu(fb + 3))
                          for f4 in range(4):
                              ft = fb * 4 + f4
                              pb, pr = ps()
                              for k in range(KC):
                                  mm(pb[:], w_[:, k, f4 * 128:(f4 + 1) * 128], h2[:, k, :], k == 0, k == KC - 1, R=[w_r, h2_r[k]], W=[pr])
                              act(rl[ft % 2][:], pb[:], AF.Relu, R=[pr], W=[rl_r[ft % 2]])
                              tt_("dve", uT[:, ft, :], rl[ft % 2][:], rl[ft % 2][:], ALU.mult, R=[rl_r[ft % 2]], W=[uT_r[ft]])
                      q_d = [load_wd(0), load_wd(1), load_wd(2)]
                      pss, pss_r = pacc, pacc_r
                      for ct in range(KC):
                          w_, w_r = q_d.pop(0)
                          if ct + 3 < KC:
                              q_d.append(load_wd(ct + 3))
                          pb, pr = ps()
                          for k in range(32):
                              mm(pb[:], w_[:, k, :], uT[:, k, :], k == 0, k == 31, R=[w_r, uT_r[k]], W=[pr])
                          act(tmpo[:, ct, :], pb[:], AF.Copy, R=[pr], W=[tmpo_r[ct]])
                          s = sq[ct % 2]
                          act(s[:], pb[:], AF.Square, R=[pr], W=[sq_res[ct % 2]])
                          mm(pss[:], ones_b, s[:], ct == 0, ct == KC - 1, R=[sq_res[ct % 2], r_const], W=[pss_r])
                      rmsnorm_rstd(pss[:], pss_r, D, rstd[:], rstd_res)
                      for ct in range(KC):
                          stt_("dve", t2[:], tmpo[:, ct, :], vcol(24 + ct), rstd[:], ALU.mult, ALU.mult, R=[tmpo_r[ct], rstd_res, r_const], W=[t2_r])
                          tt_("dve", xT[:, ct, tsl], xT[:, ct, tsl], t2[:], ALU.add, R=[xr[ct][tt], t2_r], W=[xr[ct][tt]])

        k_out = kb.dsem("out")
        kb.dma("sp", [(outT_d[kc * 128:(kc + 1) * 128, :], xT[:, kc, :]) for kc in range(KC)], k_out,
               R=[xr[kc][tt] for kc in range(KC) for tt in range(NT)])
        nc.sync.wait_ge(kb.sems[k_out], kb.cnt[k_out])
        for name, key in tap_out.items():
            nc.sync.wait_ge(kb.sems[key], kb.cnt[key])
        print(f"[build] ops={kb.nops} waits={kb.nwaits} counts={ {k: v for k, v in kb.cnt.items() if not k.startswith('d_')} }", flush=True)
    return nc


def pack_small(inputs, l0, nl):
    vecs = np.zeros((128, nl * NVEC), np.float32)
    hv = np.zeros((1, nl * 8), np.float32)
    w2 = np.zeros((16, nl * 512), np.float32)
    for i in range(nl):
        l = l0 + i
        V0 = i * NVEC
        for j, name in enumerate(("norm_mix_pre", "norm_mix_post", "norm_mlp_pre", "norm_mlp_post")):
            vecs[:, V0 + j * 8:V0 + (j + 1) * 8] = np.asarray(inputs[name][l]).reshape(8, 128).T
        cw = np.asarray(inputs["conv_w"][l])
        for tp in range(4):
            vecs[:, V0 + 32 + tp * 12:V0 + 32 + (tp + 1) * 12] = cw[tp].reshape(12, 128).T
        vecs[:, V0 + 80] = np.asarray(inputs["gdn_norm"][l])
        vecs[:, V0 + 81:V0 + 83] = np.asarray(inputs["gla_norm"][l]).reshape(2, 128).T
        vecs[:, V0 + 83:V0 + 87] = np.asarray(inputs["gla_gate_b"][l]).reshape(4, 128).T
        hv[0, i * 8:i * 8 + 4] = np.asarray(inputs["a_log"][l])
        hv[0, i * 8 + 4:i * 8 + 8] = np.asarray(inputs["dt_bias"][l])
        w2[:, i * 512:(i + 1) * 512] = np.asarray(inputs["gla_gate_w2"][l])
    return vecs, hv, w2


_PROG = {}


def run_layers(xT_list, inputs, l0, nl):
    if nl not in _PROG:
        _PROG[nl] = build_program(nl)
    nc = _PROG[nl]
    vecs, hv, w2 = pack_small(inputs, l0, nl)
    consts = make_consts()
    sl = slice(l0, l0 + nl)
    shared = {
        "w_in": np.ascontiguousarray(inputs["w_in"][sl]), "w_out_a": np.ascontiguousarray(inputs["w_out_a"][sl]),
        "w_out_b": np.ascontiguousarray(inputs["w_out_b"][sl]), "w_o": np.ascontiguousarray(inputs["w_o"][sl]),
        "w_mlp_up": np.ascontiguousarray(inputs["w_mlp_up"][sl]), "w_mlp_down": np.ascontiguousarray(inputs["w_mlp_down"][sl]),
        "gate_w2": w2, "vecs": vecs, "hv": hv, "consts": consts,
    }
    in_maps = [dict(shared, xT=xT_list[c]) for c in range(len(xT_list))]
    res = run_bass_kernel_spmd(nc, in_maps, core_ids=list(range(len(xT_list))))
    return [np.asarray(r["outT"]) for r in res.results]


GDN_STOP = 0
N_FUSED = 4


def kernel(**inputs):
    inputs = {k: np.asarray(v) for k, v in inputs.items()}
    x = inputs["x"].astype(np.float32, copy=False)
    xT = [np.ascontiguousarray(x[b].T) for b in range(x.shape[0])]
    for l0 in range(0, L, N_FUSED):
        xT = run_layers(xT, inputs, l0, N_FUSED)
    out = np.stack([t.T for t in xT], axis=0)
    return np.ascontiguousarray(out.astype(np.float32))
```
